# Optimizing a Trainium2 kernel written in Bass

```python
import math
import jax
import jax.numpy as jnp
from jax import lax
import numpy as np

D_MODEL = 1024
BATCH = 16
SEQ = 2048
DEPTH = 4

N_MIXERS = 4
EXPAND = 2
D_INNER = EXPAND * D_MODEL
EPS = 1e-6

SSD_HEAD_DIM = 64
SSD_HEADS = D_INNER // SSD_HEAD_DIM
SSD_GROUPS = 8
SSD_HPG = SSD_HEADS // SSD_GROUPS
SSD_STATE = 128
SSD_CONV = 5
SSD_CHUNK = 128
SSD_CONV_DIM = D_INNER + 2 * SSD_GROUPS * SSD_STATE
SSD_PROJ = D_INNER + SSD_CONV_DIM + 2 * SSD_HEADS

GLA_HEADS = 4
GLA_DK = D_MODEL // 2 // GLA_HEADS
GLA_DV = D_INNER // GLA_HEADS
GLA_RANK = 16
GLA_NORMALIZER = 16.0
GLA_CHUNK = 64
GLA_QK = GLA_HEADS * GLA_DK
GLA_PROJ = 2 * GLA_QK + 2 * D_INNER + 2 * GLA_RANK

HY_ORDER = 2
HY_SHORT = 3
HY_EMB = 33
HY_BANDS = (HY_EMB - 1) // 2
HY_FFN = 64
HY_INNER = 2
HY_FAST_PCT = 0.3
HY_SLOW_PCT = 1.5
HY_TARGET = 1e-2
HY_PROJ = (HY_ORDER + 2) * D_INNER
HY_FILT = 2 * HY_ORDER * D_INNER

ML_HEADS = 4
ML_DH = D_INNER // ML_HEADS
ML_DK = ML_DH // 2
ML_CONV = 5
ML_CHUNK = 64
ML_PROJ = 3 * D_INNER + 4 * ML_HEADS

kernel_name = "bidir_hybrid_ssd_gla_hyena_mlstm"


def rmsnorm(x, g):
    xf = x.astype(jnp.float32)
    y = xf * lax.rsqrt(jnp.mean(xf * xf, axis=-1, keepdims=True) + EPS)
    return (y * g.astype(jnp.float32)).astype(x.dtype)


def dwconv(x, w, b):
    k = w.shape[0]
    y = lax.conv_general_dilated(x, w[:, None, :].astype(x.dtype), (1,), [((k - 1) // 2, (k - 1) // 2)],
                                 dimension_numbers=('NWC', 'WIO', 'NWC'), feature_group_count=x.shape[-1])
    return y + b.astype(x.dtype)


def rev(t):
    return jnp.flip(t, axis=1)


def to_chunks(t, q):
    b, l = t.shape[0], t.shape[1]
    return jnp.moveaxis(t.reshape((b, l // q, q) + t.shape[2:]), 1, 0)


def from_chunks(t):
    t = jnp.moveaxis(t, 0, 1)
    return t.reshape((t.shape[0], t.shape[1] * t.shape[2]) + t.shape[3:])


def ssd_scan(xh, dt, a, bm, cm):
    bsz, seq = xh.shape[:2]
    grp = (SSD_GROUPS, SSD_HPG)
    xs = to_chunks(xh.reshape((bsz, seq) + grp + (SSD_HEAD_DIM,)), SSD_CHUNK)
    dts = to_chunks(dt.reshape((bsz, seq) + grp), SSD_CHUNK)
    las = to_chunks((dt * a).reshape((bsz, seq) + grp), SSD_CHUNK)
    bs = to_chunks(bm, SSD_CHUNK)
    cs = to_chunks(cm, SSD_CHUNK)
    causal = jnp.tril(jnp.ones((SSD_CHUNK, SSD_CHUNK), bool))[None, :, :, None, None]

    def step(state, inp):
        xc, dtc, lac, bc, cc = inp
        cum = jnp.cumsum(lac, axis=1)
        decay = jnp.exp(jnp.where(causal, cum[:, :, None] - cum[:, None], -jnp.inf))
        xdt = xc * dtc[..., None]
        cb = jnp.einsum('blgn,bsgn->blsg', cc, bc)
        y = jnp.einsum('blsgr,bsgrp->blgrp', cb[..., None] * decay, xdt)
        y = y + jnp.einsum('blgn,bgrpn->blgrp', cc, state) * jnp.exp(cum)[..., None]
        tail = jnp.exp(cum[:, -1:] - cum)
        state = (state * jnp.exp(cum[:, -1])[..., None, None]
                 + jnp.einsum('bsgn,bsgrp->bgrpn', bc, xdt * tail[..., None]))
        return state, y

    state0 = jnp.zeros((bsz,) + grp + (SSD_HEAD_DIM, SSD_STATE), jnp.float32)
    _, ys = lax.scan(step, state0, (xs, dts, las, bs, cs))
    return from_chunks(ys).reshape(xh.shape)


def ssd_mixer(u, w_in, conv_w, conv_b, dt_bias, a_log, d_skip, gnorm, w_out):
    f32 = jnp.float32
    bsz, seq = u.shape[:2]
    z, xbc, dt = jnp.split(u @ w_in, [D_INNER, D_INNER + SSD_CONV_DIM], axis=-1)
    xbc = jax.nn.silu(dwconv(xbc, conv_w, conv_b)).astype(f32)
    xh, bm, cm = jnp.split(xbc, [D_INNER, D_INNER + SSD_GROUPS * SSD_STATE], axis=-1)
    xh = xh.reshape(bsz, seq, SSD_HEADS, SSD_HEAD_DIM)
    bm = bm.reshape(bsz, seq, SSD_GROUPS, SSD_STATE)
    cm = cm.reshape(bsz, seq, SSD_GROUPS, SSD_STATE)
    dt = jax.nn.softplus(dt.astype(f32).reshape(bsz, seq, 2, SSD_HEADS) + dt_bias.astype(f32))
    a = -jnp.exp(a_log.astype(f32))
    y = (ssd_scan(xh, dt[:, :, 0], a[0], bm, cm)
         + rev(ssd_scan(rev(xh), rev(dt[:, :, 1]), a[1], rev(bm), rev(cm)))
         + xh * d_skip.astype(f32)[:, None])
    y = y.reshape(bsz, seq, D_INNER) * jax.nn.silu(z.astype(f32))
    y = rmsnorm(y.reshape(bsz, seq, SSD_GROUPS, D_INNER // SSD_GROUPS), gnorm.reshape(SSD_GROUPS, -1))
    return y.reshape(bsz, seq, D_INNER).astype(u.dtype) @ w_out


def gla_scan(q, k, v, lg):
    bsz = q.shape[0]
    causal = jnp.tril(jnp.ones((GLA_CHUNK, GLA_CHUNK), bool))[None, None]

    def step(s, inp):
        qc, kc, vc, gc = inp
        cum = jnp.cumsum(gc, axis=1)
        qg = qc * jnp.exp(cum)
        kg = kc * jnp.exp(-cum)
        att = jnp.where(causal, jnp.einsum('blhk,bshk->bhls', qg, kg), 0.0)
        o = jnp.einsum('bhls,bshv->blhv', att, vc) + jnp.einsum('blhk,bhkv->blhv', qg, s)
        kd = kc * jnp.exp(cum[:, -1:] - cum)
        s = s * jnp.exp(cum[:, -1])[..., None] + jnp.einsum('bshk,bshv->bhkv', kd, vc)
        return s, o

    s0 = jnp.zeros((bsz, GLA_HEADS, GLA_DK, GLA_DV), jnp.float32)
    _, os_ = lax.scan(step, s0, tuple(to_chunks(t, GLA_CHUNK) for t in (q, k, v, lg)))
    return from_chunks(os_)


def gla_mixer(u, w_in, w_gate, b_gate, onorm, w_out):
    f32 = jnp.float32
    bsz, seq = u.shape[:2]
    q, k, v, z, gl = jnp.split(u @ w_in, [GLA_QK, 2 * GLA_QK, 2 * GLA_QK + D_INNER, 2 * GLA_QK + 2 * D_INNER], axis=-1)
    q = q.astype(f32).reshape(bsz, seq, GLA_HEADS, GLA_DK) * (GLA_DK ** -0.5)
    k = k.astype(f32).reshape(bsz, seq, GLA_HEADS, GLA_DK)
    v = v.astype(f32).reshape(bsz, seq, GLA_HEADS, GLA_DV)
    gl = gl.astype(f32).reshape(bsz, seq, 2, GLA_RANK)
    lg = jax.nn.log_sigmoid(jnp.einsum('bldr,drk->bldk', gl, w_gate.astype(f32)) + b_gate.astype(f32)) / GLA_NORMALIZER
    lg = lg.reshape(bsz, seq, 2, GLA_HEADS, GLA_DK)
    o = gla_scan(q, k, v, lg[:, :, 0]) + rev(gla_scan(rev(q), rev(k), rev(v), rev(lg[:, :, 1])))
    o = rmsnorm(o, onorm).reshape(bsz, seq, D_INNER) * jax.nn.silu(z.astype(f32))
    return o.astype(u.dtype) @ w_out


def hyena_filters(seq_len, w_in, b_in, w_hid, b_hid, freq, w_out):
    f32 = jnp.float32
    t = jnp.linspace(0.0, 1.0, seq_len, dtype=f32)[:, None]
    pos = jnp.arange(seq_len, dtype=f32)[:, None]
    bands = jnp.linspace(1e-4, HY_BANDS - 1, HY_BANDS, dtype=f32)[None]
    ang = (2.0 * math.pi / seq_len) * pos * bands
    feats = jnp.concatenate([t, jnp.cos(ang), -jnp.sin(ang)], axis=-1)
    freq = freq.astype(f32)
    h = jnp.sin(freq[0] * (feats @ w_in.astype(f32) + b_in.astype(f32)))
    for j in range(HY_INNER):
        h = jnp.sin(freq[j + 1] * (h @ w_hid[j].astype(f32) + b_hid[j].astype(f32)))
    h = (h @ w_out.astype(f32)).reshape(seq_len, 2, HY_ORDER, D_INNER)
    max_decay = math.log(HY_TARGET) / HY_FAST_PCT
    min_decay = math.log(HY_TARGET) / HY_SLOW_PCT
    deltas = jnp.abs(jnp.linspace(min_decay, max_decay, D_INNER, dtype=f32))
    return h * jnp.exp(-t[:, :, None, None] * deltas)


def long_conv(y, h_f, h_b):
    seq = y.shape[1]
    k = jnp.concatenate([h_f, jnp.zeros_like(h_f[:1]), jnp.flip(h_b[1:], axis=0)], axis=0)
    yf = jnp.fft.rfft(y, n=2 * seq, axis=1)
    kf = jnp.fft.rfft(k, axis=0)
    return jnp.fft.irfft(yf * kf[None], n=2 * seq, axis=1)[:, :seq]


def hyena_mixer(u, w_in, conv_w, conv_b, ffn_w_in, ffn_b_in, ffn_w_hid, ffn_b_hid, ffn_freq, ffn_w_out, d_bias, w_out):
    f32 = jnp.float32
    seq = u.shape[1]
    proj = u @ w_in
    sig = dwconv(proj[..., :3 * D_INNER], conv_w, conv_b).astype(f32)
    z = proj[..., 3 * D_INNER:]
    v, x1, x2 = jnp.split(sig, 3, axis=-1)
    filt = hyena_filters(seq, ffn_w_in, ffn_b_in, ffn_w_hid, ffn_b_hid, ffn_freq, ffn_w_out)
    d_bias = d_bias.astype(f32)
    y = v
    for o, gate in enumerate((x1, x2)):
        y = gate * (long_conv(y, filt[:, 0, o], filt[:, 1, o]) + y * d_bias[o])
    y = y * jax.nn.silu(z.astype(f32))
    return y.astype(u.dtype) @ w_out


def mlstm_scan(q, k, v, li, lf):
    bsz = q.shape[0]
    causal = jnp.tril(jnp.ones((ML_CHUNK, ML_CHUNK), bool))[None, :, :, None]

    def step(carry, inp):
        c, n, m = carry
        qc, kc, vc, ic, fc = inp
        b = jnp.cumsum(fc, axis=1)
        logd = jnp.where(causal, b[:, :, None] - b[:, None] + ic[:, None], -jnp.inf)
        inter = b + m[:, None]
        m_row = jnp.maximum(jnp.max(logd, axis=2), inter)
        s = jnp.einsum('blhk,bshk->blsh', qc, kc) * jnp.exp(logd - m_row[:, :, None])
        scale = jnp.exp(inter - m_row)
        num = jnp.einsum('blsh,bshv->blhv', s, vc) + jnp.einsum('blhk,bhkv->blhv', qc, c) * scale[..., None]
        den = jnp.sum(s, axis=2) + jnp.einsum('blhk,bhk->blh', qc, n) * scale
        hc = num / jnp.maximum(jnp.abs(den), jnp.exp(-m_row))[..., None]
        tot = b[:, -1]
        lw = tot[:, None] - b + ic
        m_new = jnp.maximum(tot + m, jnp.max(lw, axis=1))
        w = jnp.exp(lw - m_new[:, None])
        dec = jnp.exp(tot + m - m_new)
        c = c * dec[..., None, None] + jnp.einsum('bsh,bshk,bshv->bhkv', w, kc, vc)
        n = n * dec[..., None] + jnp.einsum('bsh,bshk->bhk', w, kc)
        return (c, n, m_new), hc

    carry0 = (jnp.zeros((bsz, ML_HEADS, ML_DK, ML_DH), jnp.float32),
              jnp.zeros((bsz, ML_HEADS, ML_DK), jnp.float32),
              jnp.full((bsz, ML_HEADS), -jnp.inf, jnp.float32))
    _, hs = lax.scan(step, carry0, tuple(to_chunks(t, ML_CHUNK) for t in (q, k, v, li, lf)))
    return from_chunks(hs)


def mlstm_mixer(u, w_in, conv_w, conv_b, w_q, w_k, w_v, gate_b, skip, onorm, w_out):
    f32 = jnp.float32
    bsz, seq = u.shape[:2]
    xm, z, og, gates = jnp.split(u @ w_in, [D_INNER, 2 * D_INNER, 3 * D_INNER], axis=-1)
    ch = jax.nn.silu(dwconv(xm, conv_w, conv_b)).astype(f32).reshape(bsz, seq, ML_HEADS, ML_DH)
    xmh = xm.astype(f32).reshape(bsz, seq, ML_HEADS, ML_DH)
    q = jnp.einsum('blhd,hdk->blhk', ch, w_q.astype(f32))
    k = jnp.einsum('blhd,hdk->blhk', ch, w_k.astype(f32)) * (ML_DK ** -0.5)
    v = jnp.einsum('blhd,hde->blhe', xmh, w_v.astype(f32))
    g = gates.astype(f32).reshape(bsz, seq, 2, 2, ML_HEADS) + gate_b.astype(f32)
    li = g[:, :, :, 0]
    lf = jax.nn.log_sigmoid(g[:, :, :, 1])
    h = (mlstm_scan(q, k, v, li[:, :, 0], lf[:, :, 0])
         + rev(mlstm_scan(rev(q), rev(k), rev(v), rev(li[:, :, 1]), rev(lf[:, :, 1]))))
    h = jax.nn.sigmoid(og.astype(f32)).reshape(bsz, seq, ML_HEADS, ML_DH) * h
    h = rmsnorm(h, onorm) + skip.astype(f32) * ch
    h = h.reshape(bsz, seq, D_INNER) * jax.nn.silu(z.astype(f32))
    return h.astype(u.dtype) @ w_out


def setup_inputs(seed: int = 0) -> dict:
    key = jax.random.key(seed)
    keys = iter(jax.random.split(key, 64))
    f32 = jnp.float32

    def nrm(shape, scale):
        return scale * jax.random.normal(next(keys), shape, f32)

    def gain(shape):
        return 1.0 + nrm(shape, 0.02)

    na, nb, nh, nm = (len(range(t, DEPTH, N_MIXERS)) for t in range(N_MIXERS))
    w_in_s = D_MODEL ** -0.5
    w_out_s = D_INNER ** -0.5
    x = nrm((BATCH, SEQ, D_MODEL), 1.0)
    dt0 = jnp.exp(jax.random.uniform(next(keys), (na, 2, SSD_HEADS), f32, math.log(1e-3), math.log(1e-1)))
    ssd_dt_bias = dt0 + jnp.log(-jnp.expm1(-dt0))
    ssd_a_log = jnp.log(jax.random.uniform(next(keys), (na, 2, SSD_HEADS), f32, 1.0, 16.0))
    ml_gate_b = jnp.concatenate([nrm((nm, 2, 1, ML_HEADS), 0.1),
                                 jnp.linspace(3.0, 6.0, ML_HEADS, dtype=f32) + nrm((nm, 2, 1, ML_HEADS), 0.1)], axis=2)
    return {
        "x": x,
        "ssd_norm": gain((na, D_MODEL)),
        "ssd_w_in": nrm((na, D_MODEL, SSD_PROJ), w_in_s),
        "ssd_conv_w": nrm((na, SSD_CONV, SSD_CONV_DIM), SSD_CONV ** -0.5),
        "ssd_conv_b": nrm((na, SSD_CONV_DIM), 0.02),
        "ssd_dt_bias": ssd_dt_bias,
        "ssd_a_log": ssd_a_log,
        "ssd_d": gain((na, SSD_HEADS)),
        "ssd_gnorm": gain((na, D_INNER)),
        "ssd_w_out": nrm((na, D_INNER, D_MODEL), w_out_s),
        "gla_norm": gain((nb, D_MODEL)),
        "gla_w_in": nrm((nb, D_MODEL, GLA_PROJ), w_in_s),
        "gla_w_gate": nrm((nb, 2, GLA_RANK, GLA_QK), GLA_RANK ** -0.5),
        "gla_b_gate": nrm((nb, 2, GLA_QK), 0.02),
        "gla_onorm": gain((nb, GLA_DV)),
        "gla_w_out": nrm((nb, D_INNER, D_MODEL), w_out_s),
        "hy_norm": gain((nh, D_MODEL)),
        "hy_w_in": nrm((nh, D_MODEL, HY_PROJ), w_in_s),
        "hy_conv_w": nrm((nh, HY_SHORT, 3 * D_INNER), HY_SHORT ** -0.5),
        "hy_conv_b": nrm((nh, 3 * D_INNER), 0.02),
        "hy_ffn_w_in": nrm((nh, HY_EMB, HY_FFN), HY_EMB ** -0.5),
        "hy_ffn_b_in": nrm((nh, HY_FFN), 0.1),
        "hy_ffn_w_hid": nrm((nh, HY_INNER, HY_FFN, HY_FFN), HY_FFN ** -0.5),
        "hy_ffn_b_hid": nrm((nh, HY_INNER, HY_FFN), 0.1),
        "hy_ffn_freq": gain((nh, HY_INNER + 1, HY_FFN)),
        "hy_ffn_w_out": nrm((nh, HY_FFN, HY_FILT), 0.05 * HY_FFN ** -0.5),
        "hy_d": nrm((nh, HY_ORDER, D_INNER), 1.0),
        "hy_w_out": nrm((nh, D_INNER, D_MODEL), w_out_s),
        "ml_norm": gain((nm, D_MODEL)),
        "ml_w_in": nrm((nm, D_MODEL, ML_PROJ), w_in_s),
        "ml_conv_w": nrm((nm, ML_CONV, D_INNER), ML_CONV ** -0.5),
        "ml_conv_b": nrm((nm, D_INNER), 0.02),
        "ml_w_q": nrm((nm, ML_HEADS, ML_DH, ML_DK), ML_DH ** -0.5),
        "ml_w_k": nrm((nm, ML_HEADS, ML_DH, ML_DK), ML_DH ** -0.5),
        "ml_w_v": nrm((nm, ML_HEADS, ML_DH, ML_DH), ML_DH ** -0.5),
        "ml_gate_b": ml_gate_b,
        "ml_skip": gain((nm, ML_HEADS, ML_DH)),
        "ml_onorm": gain((nm, ML_DH)),
        "ml_w_out": nrm((nm, D_INNER, D_MODEL), w_out_s),
        "final_norm": gain((D_MODEL,)),
    }


def reference(x,
              ssd_norm, ssd_w_in, ssd_conv_w, ssd_conv_b, ssd_dt_bias, ssd_a_log, ssd_d, ssd_gnorm, ssd_w_out,
              gla_norm, gla_w_in, gla_w_gate, gla_b_gate, gla_onorm, gla_w_out,
              hy_norm, hy_w_in, hy_conv_w, hy_conv_b, hy_ffn_w_in, hy_ffn_b_in, hy_ffn_w_hid, hy_ffn_b_hid,
              hy_ffn_freq, hy_ffn_w_out, hy_d, hy_w_out,
              ml_norm, ml_w_in, ml_conv_w, ml_conv_b, ml_w_q, ml_w_k, ml_w_v, ml_gate_b, ml_skip, ml_onorm, ml_w_out,
              final_norm):
    h = x
    for i in range(DEPTH):
        kind, j = i % N_MIXERS, i // N_MIXERS
        if kind == 0:
            h = h + ssd_mixer(rmsnorm(h, ssd_norm[j]), ssd_w_in[j], ssd_conv_w[j], ssd_conv_b[j], ssd_dt_bias[j],
                              ssd_a_log[j], ssd_d[j], ssd_gnorm[j], ssd_w_out[j])
        elif kind == 1:
            h = h + gla_mixer(rmsnorm(h, gla_norm[j]), gla_w_in[j], gla_w_gate[j], gla_b_gate[j], gla_onorm[j],
                              gla_w_out[j])
        elif kind == 2:
            h = h + hyena_mixer(rmsnorm(h, hy_norm[j]), hy_w_in[j], hy_conv_w[j], hy_conv_b[j], hy_ffn_w_in[j],
                                hy_ffn_b_in[j], hy_ffn_w_hid[j], hy_ffn_b_hid[j], hy_ffn_freq[j], hy_ffn_w_out[j],
                                hy_d[j], hy_w_out[j])
        else:
            h = h + mlstm_mixer(rmsnorm(h, ml_norm[j]), ml_w_in[j], ml_conv_w[j], ml_conv_b[j], ml_w_q[j], ml_w_k[j],
                                ml_w_v[j], ml_gate_b[j], ml_skip[j], ml_onorm[j], ml_w_out[j])
    return rmsnorm(h, final_norm)
```

```python
import math
from contextlib import ExitStack
import numpy as np
import ml_dtypes
import concourse.bass as bass
import concourse.mybir as mybir
from concourse.bass_utils import run_bass_kernel_spmd

F32 = mybir.dt.float32
BF16 = mybir.dt.bfloat16
AF = mybir.ActivationFunctionType
ALU = mybir.AluOpType
AX = mybir.AxisListType

NCORES = 8
BL = 2
L = 2048
T = BL * L
D = 1024
DI = 2048
Q = 128
NQ = L // Q
NT = T // 128
EPS = 1e-6
EPOCH = 30000
PI = math.pi


def _kt(k):
    if isinstance(k, str):
        return (k,)
    return tuple(k)


class V:
    __slots__ = ("ap", "k")

    def __init__(self, ap, k):
        self.ap = ap
        self.k = _kt(k)


class Buf:
    def __init__(self, t, key):
        self.t = t
        self.key = key

    def __getitem__(self, idx):
        return V(self.t[idx], self.key)

    def sub(self, sub):
        return Buf(self.t, f"{self.key}.{sub}")

    def subs(self, subs):
        return Buf(self.t, tuple(f"{self.key}.{x}" for x in subs))


class Sched:
    def __init__(self, nc):
        self.nc = nc
        self.eng = {"pe": nc.tensor, "dve": nc.vector, "act": nc.scalar,
                    "pool": nc.gpsimd, "sp": nc.sync}
        self.nsem = 0
        self.esem, self.ecnt = {}, {}
        for e in self.eng:
            self._new_epoch(e)
        self.seen = {e: {} for e in self.eng}
        self.res = {}
        self.dsem = {}
        self.dfree = []
        self.ninst = 0

    def _alloc(self):
        self.nsem += 1
        return self.nc.alloc_semaphore(name=f"s{self.nsem}")

    def _new_epoch(self, e):
        self.esem[e] = self._alloc()
        self.ecnt[e] = 0

    def _wait(self, e, tok):
        sem, val = tok
        sid = id(sem)
        if self.seen[e].get(sid, 0) >= val:
            return
        self.eng[e].wait_ge(sem, val)
        self.seen[e][sid] = val

    def _deps(self, e, reads, writes, pe_accum=False, dsem=None):
        for k in reads:
            r = self.res.get(k)
            if r and r["w"] is not None:
                self._wait(e, r["w"])
        for k in writes:
            r = self.res.get(k)
            if r:
                w = r["w"]
                if w is not None:
                    skip = (pe_accum and r["we"] == "pe") or (dsem is not None and w[0] is dsem)
                    if not skip:
                        self._wait(e, w)
                for t in r["r"]:
                    self._wait(e, t)

    def _record(self, e, tok, reads, writes):
        for k in reads:
            r = self.res.setdefault(k, {"w": None, "r": [], "we": None})
            r["r"] = [t for t in r["r"] if t[0] is not tok[0]] + [tok]
        for k in writes:
            self.res[k] = {"w": tok, "r": [], "we": e}

    def op(self, e, fn, reads=(), writes=(), pe_accum=False):
        reads = [k for ks in reads if ks is not None for k in _kt(ks)]
        writes = [k for ks in writes for k in _kt(ks)]
        self._deps(e, reads, writes, pe_accum)
        if self.ecnt[e] >= EPOCH:
            self._new_epoch(e)
        inst = fn()
        self.ecnt[e] += 1
        inst.then_inc(self.esem[e], 1)
        self._record(e, (self.esem[e], self.ecnt[e]), reads, writes)
        self.ninst += 1
        return inst

    def dma(self, e, out, in_, **kw):
        reads, writes = list(in_.k), list(out.k)
        sk = out.k[0]
        if sk not in self.dsem:
            if self.dfree:
                self.dsem[sk] = self.dfree.pop()
            else:
                self.dsem[sk] = [self._alloc(), 0]
        ds = self.dsem[sk]
        self._deps(e, reads, writes, dsem=ds[0])
        inst = self.eng[e].dma_start(out=out.ap, in_=in_.ap, **kw)
        ds[1] += 16
        inst.then_inc(ds[0], 16)
        self._record(e, (ds[0], ds[1]), reads, writes)
        self.ninst += 1
        return inst

    def barrier(self):
        toks = [(self.esem[e], self.ecnt[e]) for e in self.eng if self.ecnt[e] > 0]
        toks += [(d[0], d[1]) for d in self.dsem.values() if d[1] > 0]
        for e in self.eng:
            for t in toks:
                if t[0] is not self.esem[e]:
                    self._wait(e, t)
        for d in self.dsem.values():
            if d[1] < 40000:
                self.dfree.append(d)
        self.dsem = {}
        self.res = {}

    def mm(self, out, lhsT, rhs, start=True, stop=True):
        nc = self.nc
        return self.op("pe", lambda: nc.tensor.matmul(out.ap, lhsT=lhsT.ap, rhs=rhs.ap, start=start, stop=stop),
                       reads=[lhsT.k, rhs.k], writes=[out.k], pe_accum=True)

    def tr(self, out, in_, ident):
        nc = self.nc
        return self.op("pe", lambda: nc.tensor.transpose(out.ap, in_.ap, ident.ap),
                       reads=[in_.k, ident.k], writes=[out.k], pe_accum=True)

    def act(self, out, in_, func, bias=None, scale=None, accum=None):
        nc = self.nc
        kw = {}
        rd = [in_.k]
        wr = [out.k]
        if bias is not None:
            if isinstance(bias, V):
                kw["bias"] = bias.ap
                rd.append(bias.k)
            else:
                kw["bias"] = bias
        if scale is not None:
            if isinstance(scale, V):
                kw["scale"] = scale.ap
                rd.append(scale.k)
            else:
                kw["scale"] = scale
        if accum is not None:
            kw["accum_out"] = accum.ap
            wr.append(accum.k)
        return self.op("act", lambda: nc.scalar.activation(out=out.ap, in_=in_.ap, func=func, **kw),
                       reads=rd, writes=wr)

    def _e(self, e):
        return self.eng[e]

    def tt(self, e, out, in0, in1, op):
        return self.op(e, lambda: self._e(e).tensor_tensor(out=out.ap, in0=in0.ap, in1=in1.ap, op=op),
                       reads=[in0.k, in1.k], writes=[out.k])

    def ts(self, e, out, in0, s1, s2, op0, op1=None):
        rd = [in0.k]
        a1 = s1.ap if isinstance(s1, V) else s1
        a2 = s2.ap if isinstance(s2, V) else s2
        if isinstance(s1, V):
            rd.append(s1.k)
        if isinstance(s2, V):
            rd.append(s2.k)
        if op1 is None:
            return self.op(e, lambda: self._e(e).tensor_scalar(out=out.ap, in0=in0.ap, scalar1=a1, scalar2=None, op0=op0),
                           reads=rd, writes=[out.k])
        return self.op(e, lambda: self._e(e).tensor_scalar(out=out.ap, in0=in0.ap, scalar1=a1, scalar2=a2, op0=op0, op1=op1),
                       reads=rd, writes=[out.k])

    def stt(self, e, out, in0, scalar, in1, op0, op1):
        rd = [in0.k, in1.k]
        a = scalar.ap if isinstance(scalar, V) else scalar
        if isinstance(scalar, V):
            rd.append(scalar.k)
        return self.op(e, lambda: self._e(e).scalar_tensor_tensor(out=out.ap, in0=in0.ap, scalar=a, in1=in1.ap, op0=op0, op1=op1),
                       reads=rd, writes=[out.k])

    def cp(self, e, out, in_):
        if e == "act":
            return self.op(e, lambda: self.nc.scalar.copy(out=out.ap, in_=in_.ap), reads=[in_.k], writes=[out.k])
        return self.op(e, lambda: self._e(e).tensor_copy(out=out.ap, in_=in_.ap), reads=[in_.k], writes=[out.k])

    def memset(self, e, out, val):
        return self.op(e, lambda: self._e(e).memset(out.ap, val), reads=[], writes=[out.k])

    def red(self, e, out, in_, op, axis=AX.X):
        return self.op(e, lambda: self._e(e).tensor_reduce(out=out.ap, in_=in_.ap, axis=axis, op=op),
                       reads=[in_.k], writes=[out.k])

    def recip(self, e, out, in_):
        return self.op(e, lambda: self._e(e).reciprocal(out=out.ap, in_=in_.ap), reads=[in_.k], writes=[out.k])


class Ctx:
    pass


class Ring:
    def __init__(self, bufs):
        self.bufs = bufs
        self.i = 0

    def next(self):
        b = self.bufs[self.i % len(self.bufs)]
        self.i += 1
        return b


_UID = [0]


def sb(es, nc, name, shape, dt):
    _UID[0] += 1
    name = f"{name}_{_UID[0]}"
    t = es.enter_context(nc.sbuf_tensor(name, shape, dt))
    return Buf(t, name)


def sbring(es, nc, name, shape, dt, n):
    return Ring([sb(es, nc, f"{name}{i}", shape, dt) for i in range(n)])


def bc(v, shape):
    return V(v.ap.to_broadcast(shape), v.k)


def phase_norm(c, hin, gvec, uT):
    nc, s = c.nc, c.s
    with ExitStack() as es:
        gt = sb(es, nc, "n_g", [128, D], F32)
        xr = sbring(es, nc, "n_x", [128, D], F32, 2)
        sq = sb(es, nc, "n_sq", [128, D], F32)
        ur = sbring(es, nc, "n_u", [128, D], BF16, 2)
        st = sb(es, nc, "n_st", [128, NT, 4], F32)
        s.dma("sp", gt[:], V(gvec.partition_broadcast(128), "w_const"))
        for i in range(NT):
            xt = xr.next()
            ub = ur.next()
            stv = st.sub(str(i))
            s.dma("sp", xt[:], hin[i * 128:(i + 1) * 128, :])
            s.act(sq[:], xt[:], AF.Square, accum=stv[:, i, 0:1])
            s.ts("dve", stv[:, i, 1:2], stv[:, i, 0:1], 1.0 / D, EPS, ALU.mult, ALU.add)
            s.act(stv[:, i, 2:3], stv[:, i, 1:2], AF.Sqrt)
            s.recip("dve", stv[:, i, 3:4], stv[:, i, 2:3])
            s.stt("dve", ub[:], xt[:], stv[:, i, 3:4], gt[:], ALU.mult, ALU.mult)
            pt = c.ptr.next()
            for k in range(8):
                s.tr(pt[:, k, :], ub[:, k * 128:(k + 1) * 128], c.identb[:])
            s.cp("act", uT[:, :, i * 128:(i + 1) * 128], pt[:, 0:8, :])
    s.barrier()


def phase_project(c, uT, w_ap, segs):
    nc, s = c.nc, c.s
    with ExitStack() as es:
        wfr = sbring(es, nc, "p_wf", [128, 8, 512], F32, 2)
        wbr = sbring(es, nc, "p_wb", [128, 8, 512], BF16, 2)
        ofr = sbring(es, nc, "p_of", [128, 512], F32, 3)
        obr = sbring(es, nc, "p_ob", [128, 512], BF16, 3)
        wv = w_ap.rearrange("(ko p) n -> p ko n", p=128)
        ev = 0
        for (col0, ncols, mode, dst, dt, scale) in segs:
            for cb in range(0, ncols, 512):
                nb = min(512, ncols - cb)
                wf = wfr.next()
                wb = wbr.next()
                s.dma("sp", wf[:, :, 0:nb], V(wv[:, :, col0 + cb:col0 + cb + nb], "w_const"))
                s.cp("pool", wb[:, :, 0:nb], wf[:, :, 0:nb])
                if mode == "tm":
                    for i in range(NT):
                        ps = c.pr.next()
                        for ko in range(8):
                            s.mm(ps[:, 0:nb], uT[:, ko, i * 128:(i + 1) * 128], wb[:, ko, 0:nb],
                                 start=(ko == 0), stop=(ko == 7))
                        ot = (ofr if dt == F32 else obr).next()
                        if ev % 2 == 0:
                            s.act(ot[:, 0:nb], ps[:, 0:nb], AF.Copy, scale=scale)
                        else:
                            s.ts("dve", ot[:, 0:nb], ps[:, 0:nb], scale, None, ALU.mult)
                        ev += 1
                        s.dma("pool", dst[i * 128:(i + 1) * 128, cb:cb + nb], ot[:, 0:nb])
                else:
                    for fb in range(0, nb, 128):
                        fn = min(128, nb - fb)
                        for tb in range(T // 512):
                            ps = c.pr.next()
                            for ko in range(8):
                                s.mm(ps[0:fn, :], wb[:, ko, fb:fb + fn], uT[:, ko, tb * 512:(tb + 1) * 512],
                                     start=(ko == 0), stop=(ko == 7))
                            ot = (ofr if dt == F32 else obr).next()
                            if ev % 2 == 0:
                                s.act(ot[0:fn, :], ps[0:fn, :], AF.Copy, scale=scale)
                            else:
                                s.ts("dve", ot[0:fn, :], ps[0:fn, :], scale, None, ALU.mult)
                            ev += 1
                            s.dma("pool", dst[cb + fb:cb + fb + fn, tb * 512:(tb + 1) * 512], ot[0:fn, :])
    s.barrier()


class Finalizer:
    def __init__(self, c, es, w_out_ap, hin, hout):
        nc = c.nc
        self.c = c
        self.wo = sb(es, nc, "f_wo", [128, 16, D], BF16)
        self.yT = sbring(es, nc, "f_yT", [128, 16, 128], BF16, 2)
        self.hr = sbring(es, nc, "f_h", [128, D], F32, 2)
        self.hin, self.hout = hin, hout
        with ExitStack() as es2:
            stg = sbring(es2, nc, "f_stg", [128, 2, D], F32, 2)
            wv = w_out_ap.rearrange("(ko p) n -> p ko n", p=128)
            for k0 in range(0, 16, 2):
                st = stg.next()
                c.s.dma("sp", st[:], V(wv[:, k0:k0 + 2, :], "w_const"))
                c.s.cp("pool", self.wo[:, k0:k0 + 2, :], st[:])
            c.s.barrier()

    def run(self, y, i):
        c, s = self.c, self.c.s
        yT = self.yT.next()
        for half in range(2):
            pt = c.ptr.next()
            for k in range(8):
                kk = half * 8 + k
                s.tr(pt[:, k, :], V(y.t[:, kk * 128:(kk + 1) * 128], y.key), c.identb[:])
            s.cp("act" if half == 0 else "dve", yT[:, half * 8:half * 8 + 8, :], pt[:, 0:8, :])
        ht = self.hr.next()
        s.dma("sp", ht[:], self.hin[i * 128:(i + 1) * 128, :])
        for n in range(2):
            ps = c.pr.next()
            for kc in range(16):
                s.mm(ps[:, :], yT[:, kc, :], self.wo[:, kc, n * 512:(n + 1) * 512],
                     start=(kc == 0), stop=(kc == 15))
            s.tt("dve", ht[:, n * 512:(n + 1) * 512], ps[:, :], ht[:, n * 512:(n + 1) * 512], ALU.add)
        s.dma("pool", self.hout[i * 128:(i + 1) * 128, :], ht[:])


def phase_final_norm(c, hin, gvec, out):
    nc, s = c.nc, c.s
    with ExitStack() as es:
        gt = sb(es, nc, "fn_g", [128, D], F32)
        xr = sbring(es, nc, "fn_x", [128, D], F32, 2)
        sq = sb(es, nc, "fn_sq", [128, D], F32)
        orr = sbring(es, nc, "fn_o", [128, D], F32, 2)
        st = sb(es, nc, "fn_st", [128, NT, 4], F32)
        s.dma("sp", gt[:], V(gvec.partition_broadcast(128), "w_const"))
        for i in range(NT):
            xt = xr.next()
            ot = orr.next()
            stv = st.sub(str(i))
            s.dma("sp", xt[:], hin[i * 128:(i + 1) * 128, :])
            s.act(sq[:], xt[:], AF.Square, accum=stv[:, i, 0:1])
            s.ts("dve", stv[:, i, 1:2], stv[:, i, 0:1], 1.0 / D, EPS, ALU.mult, ALU.add)
            s.act(stv[:, i, 2:3], stv[:, i, 1:2], AF.Sqrt)
            s.recip("dve", stv[:, i, 3:4], stv[:, i, 2:3])
            s.stt("dve", ot[:], xt[:], stv[:, i, 3:4], gt[:], ALU.mult, ALU.mult)
            s.dma("pool", out[i * 128:(i + 1) * 128, :], ot[:])
    s.barrier()


def conv_fm(c, srcT, nch_tiles, K, cw_ap, cb_ap, silu, emit):
    nc, s = c.nc, c.s
    pad = (K - 1) // 2
    with ExitStack() as es:
        cw = sb(es, nc, "cv_w", [128, nch_tiles, K], F32)
        cbias = sb(es, nc, "cv_b", [128, nch_tiles], F32)
        xr = sbring(es, nc, "cv_x", [128, L + 2 * pad], BF16, 2)
        dg = sbring(es, nc, "cv_dg", [128, K, 128], BF16, 2)
        cvr = sbring(es, nc, "cv_o", [128, 512], BF16, 3)
        s.dma("sp", cw[:], V(cw_ap.rearrange("(ct p) k -> p ct k", p=128), "w_const"))
        s.dma("sp", cbias[:], V(cb_ap.rearrange("(ct p) -> p ct", p=128), "w_const"), allow_slow_non_contiguous=True)
        for xb in xr.bufs:
            s.memset("pool", xb[:, 0:pad], 0.0)
            s.memset("pool", xb[:, L + pad:L + 2 * pad], 0.0)
        for ct in range(nch_tiles):
            d = dg.next()
            for k in range(K):
                s.ts("dve", d[:, k, :], c.identf[:], cw[:, ct, k:k + 1], None, ALU.mult)
            for b in range(BL):
                xt = xr.next()
                s.dma("sp", V(xt.t[:, pad:L + pad], xt.key + ".d"), srcT[ct * 128:(ct + 1) * 128, b * L:(b + 1) * L])
                for tb in range(L // 512):
                    ps = c.pr.next()
                    for k in range(K):
                        s.mm(ps[:, :], d[:, k, :],
                             V(xt.t[:, tb * 512 + k:tb * 512 + k + 512], (xt.key, xt.key + ".d")),
                             start=(k == 0), stop=(k == K - 1))
                    cv = cvr.next()
                    s.act(cv[:, :], ps[:, :], AF.Silu if silu else AF.Identity, bias=cbias[:, ct:ct + 1])
                    emit(ct, b, tb, cv[:, :])


def to_tm_store(c, cv, dst, row0, col0, stg_ring):
    s = c.s
    pt = c.ptr.next()
    for j in range(4):
        s.tr(pt[:, j, :], V(cv.ap[:, j * 128:(j + 1) * 128], cv.k), c.identb[:])
    st = stg_ring.next()
    s.cp("dve", st[:, 0:4, :], pt[:, 0:4, :])
    s.dma("pool", V(dst.t[row0:row0 + 512, col0:col0 + 128].rearrange("(j p) c -> p j c", p=128), dst.key),
          st[:, 0:4, :])


def layer_ssd(c, W, hin, hout):
    nc, s = c.nc, c.s
    H, P, G, N = 32, 64, 8, 128
    dr = c.dram
    z_tm = dr("ssd_z", [T, DI], BF16)
    xbcT = dr("ssd_xbcT", [4096, T], BF16)
    dt_tm = dr("ssd_dt", [T, 64], F32)
    x_tm = dr("ssd_x", [T, DI], BF16)
    B_tm = dr("ssd_B", [T, 1024], BF16)
    BT = dr("ssd_BT", [1024, T], BF16)
    CT = dr("ssd_CT", [1024, T], BF16)
    yf = dr("ssd_yf", [T, DI], F32)

    with ExitStack() as es:
        uT = sb(es, nc, "uT", [128, 8, T], BF16)
        phase_norm(c, hin, W["ssd_norm"], uT)
        phase_project(c, uT, W["ssd_w_in"], [
            (0, DI, "tm", z_tm, BF16, 1.0),
            (DI, 4096, "fm", xbcT, BF16, 1.0),
            (DI + 4096, 64, "tm", dt_tm, F32, 1.0),
        ])

    with ExitStack() as es:
        stg = sbring(es, nc, "sc_stg", [128, 4, 128], BF16, 3)

        def emit(ct, b, tb, cv):
            col = b * L + tb * 512
            if ct < 16:
                to_tm_store(c, cv, x_tm, col, ct * 128, stg)
            elif ct < 24:
                to_tm_store(c, cv, B_tm, col, (ct - 16) * 128, stg)
                s.dma("pool", BT[(ct - 16) * 128:(ct - 15) * 128, col:col + 512], cv)
            else:
                s.dma("pool", CT[(ct - 24) * 128:(ct - 23) * 128, col:col + 512], cv)

        conv_fm(c, xbcT, 32, 5, W["ssd_conv_wT"], W["ssd_conv_b"], True, emit)
    s.barrier()

    with ExitStack() as es:
        c.pr = Ring(c.pbanks[0:4])
        fin = Finalizer(c, es, W["ssd_w_out"], hin, hout)
        dtb = sb(es, nc, "ss_dtb", [128, 64], F32)
        aneg = sb(es, nc, "ss_a", [128, 64], F32)
        dsk = sb(es, nc, "ss_dsk", [128, 32], F32)
        gn = sb(es, nc, "ss_gn", [128, DI], F32)
        s.dma("sp", dtb[:], V(W["ssd_dt_bias"].rearrange("d h -> (d h)").partition_broadcast(128), "w_const"))
        s.dma("sp", aneg[:], V(W["ssd_a_log"].rearrange("d h -> (d h)").partition_broadcast(128), "w_const"))
        s.dma("sp", dsk[:], V(W["ssd_d"].partition_broadcast(128), "w_const"))
        s.dma("sp", gn[:], V(W["ssd_gnorm"].partition_broadcast(128), "w_const"))
        s.act(aneg[:], aneg[:], AF.Exp)
        s.ts("dve", aneg[:], aneg[:], -1.0, None, ALU.mult)
        xr = sbring(es, nc, "ss_x", [128, DI], BF16, 2)
        Br = sbring(es, nc, "ss_B", [128, 1024], BF16, 2)
        BTr = sbring(es, nc, "ss_BT", [128, G, 128], BF16, 2)
        CTr = sbring(es, nc, "ss_CT", [128, G, 128], BF16, 2)
        dtr = sbring(es, nc, "ss_dt", [128, 64], F32, 2)
        sm = sbring(es, nc, "ss_sm", [128, 8, 32], F32, 2)
        labc = sb(es, nc, "ss_labc", [128, H, 128], F32)
        latri = sb(es, nc, "ss_latri", [128, H, 128], F32)
        xdt = sbring(es, nc, "ss_xdt", [128, DI], BF16, 1)
        xw = sbring(es, nc, "ss_xw", [128, DI], BF16, 1)
        Er = sbring(es, nc, "ss_E", [128, 512], F32, 2)
        PTr = sbring(es, nc, "ss_PT", [128, 4, 128], BF16, 2)
        t1r = sbring(es, nc, "ss_t1", [128, 256], F32, 2)
        yacc = sbring(es, nc, "ss_yacc", [128, DI], F32, 1)
        st32 = sb(es, nc, "ss_st32", [128, G, 256], F32)
        stbf = sb(es, nc, "ss_stbf", [128, G, 256], BF16)
        zr = sbring(es, nc, "ss_z", [128, DI], BF16, 1)
        yfr = sbring(es, nc, "ss_yf", [128, DI], F32, 1)
        tmp = sb(es, nc, "ss_tmp", [128, DI], F32)
        gst = sbring(es, nc, "ss_gst", [128, 4, 8], F32, 2)
        yb = sbring(es, nc, "ss_yb", [128, DI], BF16, 1)
        allg = [str(g) for g in range(G)]

        def r3(buf, q):
            return V(buf.t[:].rearrange("p (h q) -> p h q", q=q), buf.key)

        for b in range(BL):
            for d in range(2):
                s.memset("dve", st32.subs(allg)[:], 0.0)
                s.memset("pool", stbf.subs(allg)[:], 0.0)
                tri = c.tri[:, d, :]
                order = range(NQ) if d == 0 else range(NQ - 1, -1, -1)
                for ci in order:
                    i = b * NQ + ci
                    r0 = i * 128
                    xt, Bt, BTt, CTt, dtt = xr.next(), Br.next(), BTr.next(), CTr.next(), dtr.next()
                    s.dma("sp", xt[:], x_tm[r0:r0 + 128, :])
                    s.dma("sp", Bt[:], B_tm[r0:r0 + 128, :])
                    s.dma("sp", BTt[:], V(BT.t[:, r0:r0 + 128].rearrange("(g n) t -> n g t", n=128), BT.key))
                    s.dma("sp", CTt[:], V(CT.t[:, r0:r0 + 128].rearrange("(g n) t -> n g t", n=128), CT.key))
                    s.dma("sp", dtt[:], dt_tm[r0:r0 + 128, :])
                    m = sm.next()
                    s.tt("dve", m[:, 0:2, :], V(dtt.t[:].rearrange("p (a h) -> p a h", a=2), dtt.key),
                         V(dtb.t[:].rearrange("p (a h) -> p a h", a=2), dtb.key), ALU.add)
                    s.act(m[:, 0:2, :], m[:, 0:2, :], AF.Exp)
                    s.act(m[:, 0:2, :], m[:, 0:2, :], AF.Ln, bias=c.one[:, 0:1])
                    dtd = m[:, d, :]
                    s.tt("dve", m[:, 2, :], dtd, aneg[:, d * 32:(d + 1) * 32], ALU.mult)
                    la = m[:, 2, :]
                    pc = c.pr.next()
                    s.mm(pc[:, 0:32], tri, la)
                    s.mm(pc[:, 32:64], c.onesf[:], la)
                    s.act(m[:, 3, :], pc[:, 0:32], AF.Exp)
                    s.cp("dve", m[:, 5, :], pc[:, 0:32])
                    s.tt("dve", m[:, 4, :], pc[:, 32:64], m[:, 5, :], ALU.subtract)
                    s.act(m[:, 4, :], m[:, 4, :], AF.Exp)
                    s.act(m[:, 6, :], pc[:, 32:64], AF.Exp)
                    s.tt("dve", m[:, 7, :], m[:, 4, :], dtd, ALU.mult)
                    xd, xwt = xdt.next(), xw.next()
                    x3 = r3(xt, P)
                    s.tt("dve", r3(xd, P), x3, bc(V(dtd.ap.unsqueeze(2), dtd.k), [128, H, P]), ALU.mult)
                    s.tt("pool", r3(xwt, P), x3, bc(V(m.t[:, 7, :].unsqueeze(2), m.key), [128, H, P]), ALU.mult)
                    s.cp("dve", labc[:], bc(V(la.ap.unsqueeze(2), la.k), [128, H, 128]))
                    s.tt("pool", latri[:], bc(V(la.ap.unsqueeze(2), la.k), [128, H, 128]),
                         bc(V(tri.ap.unsqueeze(1), tri.k), [128, H, 128]), ALU.mult)
                    pcb = c.pbanks[4:6]
                    for g in range(G):
                        s.mm(V(pcb[g // 4].t[:, (g % 4) * 128:(g % 4 + 1) * 128], pcb[g // 4].key),
                             BTt[:, g, :], CTt[:, g, :])
                    ya = yacc.next()
                    for g in range(G):
                        pa = c.pr.next()
                        for hh in range(4):
                            h = g * 4 + hh
                            o = V(pa.t[:, hh * 128:(hh + 1) * 128], pa.key)
                            s.mm(o, labc[:, h, :], tri, start=True, stop=False)
                            s.mm(o, latri[:, h, :], c.nonesf[:], start=False, stop=False)
                            s.mm(o, c.identf[:], c.negm[:, d, :], start=False, stop=True)
                        E = Er.next()
                        s.act(E[:], pa[:], AF.Exp)
                        PT = PTr.next()
                        cbv = V(pcb[g // 4].t[:, (g % 4) * 128:(g % 4 + 1) * 128].unsqueeze(1), pcb[g // 4].key)
                        s.tt("dve", PT[:], V(E.t[:].rearrange("p (a l) -> p a l", a=4), E.key),
                             bc(cbv, [128, 4, 128]), ALU.mult)
                        py = c.pr.next()
                        for hh in range(4):
                            h = g * 4 + hh
                            s.mm(V(py.t[:, hh * 64:(hh + 1) * 64], py.key), PT[:, hh, :], xd[:, h * 64:(h + 1) * 64])
                        s.mm(V(py.t[:, 256:512], py.key), CTt[:, g, :], stbf.sub(str(g))[:, g, :])
                        t1 = t1r.next()
                        s.tt("dve", V(t1.t[:].rearrange("p (a q) -> p a q", a=4), t1.key),
                             V(py.t[:, 256:512].rearrange("p (a q) -> p a q", a=4), py.key),
                             bc(V(m.t[:, 3, g * 4:(g + 1) * 4].unsqueeze(2), m.key), [128, 4, P]), ALU.mult)
                        s.tt("dve", ya[:, g * 256:(g + 1) * 256], py[:, 0:256], t1[:], ALU.add)
                        pst = c.pr.next()
                        s.mm(pst[:, 0:256], Bt[:, g * 128:(g + 1) * 128], xwt[:, g * 256:(g + 1) * 256])
                        sg = st32.sub(str(g))
                        sg3 = V(sg.t[:, g, :].rearrange("p (a q) -> p a q", a=4), sg.key)
                        s.tt("pool", sg3, sg3,
                             bc(V(m.t[:, 6, g * 4:(g + 1) * 4].unsqueeze(2), m.key), [128, 4, P]), ALU.mult)
                        s.tt("dve", sg[:, g, :], sg[:, g, :], pst[:, 0:256], ALU.add)
                        s.cp("act", stbf.sub(str(g))[:, g, :], sg[:, g, :])
                    if d == 0:
                        s.dma("pool", yf[r0:r0 + 128, :], ya[:])
                    else:
                        yft, zt = yfr.next(), zr.next()
                        s.dma("sp", yft[:], yf[r0:r0 + 128, :])
                        s.dma("sp", zt[:], z_tm[r0:r0 + 128, :])
                        s.tt("pool", yft[:], yft[:], ya[:], ALU.add)
                        s.tt("dve", r3(tmp, P), x3, bc(V(dsk.t[:].unsqueeze(2), dsk.key), [128, H, P]), ALU.mult)
                        s.tt("dve", yft[:], yft[:], tmp[:], ALU.add)
                        s.act(tmp[:], zt[:], AF.Silu)
                        s.tt("dve", yft[:], yft[:], tmp[:], ALU.mult)
                        gs = gst.next()
                        s.tt("pool", tmp[:], yft[:], yft[:], ALU.mult)
                        s.red("dve", gs[:, 0, :], r3(tmp, 256), ALU.add)
                        s.ts("dve", gs[:, 1, :], gs[:, 0, :], 1.0 / 256, EPS, ALU.mult, ALU.add)
                        s.act(gs[:, 2, :], gs[:, 1, :], AF.Sqrt)
                        s.recip("dve", gs[:, 3, :], gs[:, 2, :])
                        s.tt("dve", r3(yft, 256), r3(yft, 256),
                             bc(V(gs.t[:, 3, :].unsqueeze(2), gs.key), [128, 8, 256]), ALU.mult)
                        ybt = yb.next()
                        s.tt("dve", ybt[:], yft[:], gn[:], ALU.mult)
                        fin.run(ybt, i)
    s.barrier()
    c.pr = Ring(c.pbanks)


def layer_gla(c, W, hin, hout):
    nc, s = c.nc, c.s
    H, DK, DV = 4, 128, 512
    dr = c.dram
    q_tm = dr("gla_q", [T, 512], BF16)
    k_tm = dr("gla_k", [T, 512], BF16)
    v_tm = dr("gla_v", [T, DI], BF16)
    z_tm = dr("gla_z", [T, DI], BF16)
    glT = dr("gla_glT", [32, T], F32)
    of = dr("gla_of", [T, DI], F32)

    with ExitStack() as es:
        uT = sb(es, nc, "uT", [128, 8, T], BF16)
        phase_norm(c, hin, W["gla_norm"], uT)
        phase_project(c, uT, W["gla_w_in"], [
            (0, 512, "tm", q_tm, BF16, DK ** -0.5),
            (512, 512, "tm", k_tm, BF16, 1.0),
            (1024, DI, "tm", v_tm, BF16, 1.0),
            (3072, DI, "tm", z_tm, BF16, 1.0),
            (5120, 32, "fm", glT, F32, 1.0),
        ])

    with ExitStack() as es:
        fin = Finalizer(c, es, W["gla_w_out"], hin, hout)
        wg = sb(es, nc, "g_wg", [16, 2, 512], F32)
        bg = sb(es, nc, "g_bg", [128, 2, 512], F32)
        on = sb(es, nc, "g_on", [128, 512], F32)
        s.dma("sp", wg[:], V(W["gla_w_gate"].rearrange("d r k -> r d k"), "w_const"))
        s.dma("sp", V(bg.t[:].rearrange("p d k -> p (d k)"), bg.key),
              V(W["gla_b_gate"].rearrange("d k -> (d k)").partition_broadcast(128), "w_const"))
        s.dma("sp", on[:], V(W["gla_onorm"].partition_broadcast(128), "w_const"))
        qr = sbring(es, nc, "g_q", [128, 512], BF16, 2)
        kr = sbring(es, nc, "g_k", [128, 512], BF16, 2)
        vr = sbring(es, nc, "g_v", [128, DI], BF16, 2)
        glr = sbring(es, nc, "g_gl", [16, 128], F32, 2)
        Lp = sb(es, nc, "g_Lp", [128, 512], F32)
        Eq = sb(es, nc, "g_Eq", [128, 512], F32)
        Ek = sb(es, nc, "g_Ek", [128, 512], F32)
        Ee = sbring(es, nc, "g_Ee", [128, 4], F32, 2)
        qs = sb(es, nc, "g_qs", [128, 512], BF16)
        ks = sb(es, nc, "g_ks", [128, 512], BF16)
        qkT = sb(es, nc, "g_qkT", [128, 8, 128], BF16)
        PT = sb(es, nc, "g_PT", [128, 4, 128], BF16)
        oacc = sb(es, nc, "g_oacc", [128, DI], F32)
        S32 = sb(es, nc, "g_S32", [128, H, DV], F32)
        Sbf = sb(es, nc, "g_Sbf", [128, H, DV], BF16)
        oft = sb(es, nc, "g_of", [128, DI], F32)
        zt = sb(es, nc, "g_z", [128, DI], BF16)
        tmp = sb(es, nc, "g_tmp", [128, DI], F32)
        gst = sbring(es, nc, "g_st", [128, 4, 4], F32, 2)
        yb = sb(es, nc, "g_yb", [128, DI], BF16)
        allh = [str(h) for h in range(H)]

        def r3(buf, q):
            return V(buf.t[:].rearrange("p (h q) -> p h q", q=q), buf.key)

        for b in range(BL):
            for d in range(2):
                s.memset("dve", S32.subs(allh)[:], 0.0)
                s.memset("pool", Sbf.subs(allh)[:], 0.0)
                tri = c.tri[:, d, :]
                order = range(NQ) if d == 0 else range(NQ - 1, -1, -1)
                for ci in order:
                    i = b * NQ + ci
                    r0 = i * 128
                    qt, kt, vt, gt = qr.next(), kr.next(), vr.next(), glr.next()
                    s.dma("sp", qt[:], q_tm[r0:r0 + 128, :])
                    s.dma("sp", kt[:], k_tm[r0:r0 + 128, :])
                    s.dma("sp", vt[:], v_tm[r0:r0 + 128, :])
                    s.dma("sp", gt[:], glT[d * 16:(d + 1) * 16, r0:r0 + 128])
                    pg = c.pr.next()
                    s.mm(pg[:, :], gt[:, :], wg[:, d, :])
                    s.tt("dve", Lp[:], pg[:, :], bg[:, d, :], ALU.add)
                    s.act(Lp[:], Lp[:], AF.Exp, scale=-1.0)
                    s.act(Lp[:], Lp[:], AF.Ln, bias=c.one[:, 0:1])
                    pcum = c.pr.next()
                    s.mm(pcum[:, :], tri, Lp[:])
                    s.act(Eq[:], pcum[:, :], AF.Exp, scale=-1.0 / 16)
                    s.act(Ek[:], pcum[:, :], AF.Exp, scale=1.0 / 16)
                    ptot = c.pr.next()
                    for h in range(H):
                        s.mm(ptot[:, h:h + 1], Lp[:, h * 128:(h + 1) * 128], c.onesf[:, 0:1])
                    ee = Ee.next()
                    s.act(ee[:], ptot[:, 0:4], AF.Exp, scale=-1.0 / 16)
                    s.tt("dve", qs[:], qt[:], Eq[:], ALU.mult)
                    s.tt("pool", ks[:], kt[:], Ek[:], ALU.mult)
                    pt = c.ptr.next()
                    for h in range(H):
                        s.tr(pt[:, h, :], qs[:, h * 128:(h + 1) * 128], c.identb[:])
                        s.tr(pt[:, 4 + h, :], ks[:, h * 128:(h + 1) * 128], c.identb[:])
                    s.cp("act", qkT[:], pt[:, :, :])
                    pS = c.pr.next()
                    for h in range(H):
                        s.mm(pS[:, h * 128:(h + 1) * 128], qkT[:, 4 + h, :], qkT[:, h, :])
                    s.tt("dve", PT[:], V(pS.t[:, :].rearrange("p (a l) -> p a l", a=4), pS.key),
                         bc(V(tri.ap.unsqueeze(1), tri.k), [128, 4, 128]), ALU.mult)
                    for h in range(H):
                        po = c.pr.next()
                        s.mm(po[:, :], PT[:, h, :], vt[:, h * DV:(h + 1) * DV], start=True, stop=False)
                        s.mm(po[:, :], qkT[:, h, :], Sbf.sub(str(h))[:, h, :], start=False, stop=True)
                        s.cp("act", oacc[:, h * DV:(h + 1) * DV], po[:, :])
                        pu = c.pr.next()
                        s.mm(pu[:, :], ks[:, h * 128:(h + 1) * 128], vt[:, h * DV:(h + 1) * DV])
                        sh = S32.sub(str(h))
                        s.tt("dve", sh[:, h, :], sh[:, h, :], pu[:, :], ALU.add)
                        s.ts("pool", sh[:, h, :], sh[:, h, :], ee[:, h:h + 1], None, ALU.mult)
                        s.cp("act", Sbf.sub(str(h))[:, h, :], sh[:, h, :])
                    if d == 0:
                        s.dma("pool", of[r0:r0 + 128, :], oacc[:])
                    else:
                        s.dma("sp", oft[:], of[r0:r0 + 128, :])
                        s.dma("sp", zt[:], z_tm[r0:r0 + 128, :])
                        s.tt("pool", oft[:], oft[:], oacc[:], ALU.add)
                        gs = gst.next()
                        s.tt("pool", tmp[:], oft[:], oft[:], ALU.mult)
                        s.red("dve", gs[:, 0, :], r3(tmp, DV), ALU.add)
                        s.ts("dve", gs[:, 1, :], gs[:, 0, :], 1.0 / DV, EPS, ALU.mult, ALU.add)
                        s.act(gs[:, 2, :], gs[:, 1, :], AF.Sqrt)
                        s.recip("dve", gs[:, 3, :], gs[:, 2, :])
                        s.tt("dve", r3(oft, DV), r3(oft, DV), bc(V(gs.t[:, 3, :].unsqueeze(2), gs.key), [128, H, DV]), ALU.mult)
                        s.tt("pool", r3(oft, DV), r3(oft, DV), bc(V(on.t[:].unsqueeze(1), on.key), [128, H, DV]), ALU.mult)
                        s.act(tmp[:], zt[:], AF.Silu)
                        s.tt("dve", yb[:], oft[:], tmp[:], ALU.mult)
                        fin.run(yb, i)
    s.barrier()


def layer_mlstm(c, W, hin, hout):
    nc, s = c.nc, c.s
    H, DK, DV = 4, 256, 512
    dr = c.dram
    xmT = dr("ml_xmT", [DI, T], BF16)
    z_tm = dr("ml_z", [T, DI], BF16)
    og_tm = dr("ml_og", [T, DI], BF16)
    gt_tm = dr("ml_gates", [T, 16], F32)
    chT = dr("ml_chT", [DI, T], BF16)
    ch_tm = dr("ml_ch", [T, DI], BF16)
    qk_tm = dr("ml_qk", [T, H * 512], BF16)
    v_tm = dr("ml_v", [T, DI], BF16)
    hf = dr("ml_hf", [T, DI], F32)

    with ExitStack() as es:
        uT = sb(es, nc, "uT", [128, 8, T], BF16)
        phase_norm(c, hin, W["ml_norm"], uT)
        phase_project(c, uT, W["ml_w_in"], [
            (0, DI, "fm", xmT, BF16, 1.0),
            (DI, DI, "tm", z_tm, BF16, 1.0),
            (2 * DI, DI, "tm", og_tm, BF16, 1.0),
            (3 * DI, 16, "tm", gt_tm, F32, 1.0),
        ])

    with ExitStack() as es:
        stg = sbring(es, nc, "mc_stg", [128, 4, 128], BF16, 3)

        def emit(ct, b, tb, cv):
            col = b * L + tb * 512
            to_tm_store(c, cv, ch_tm, col, ct * 128, stg)
            s.dma("pool", chT[ct * 128:(ct + 1) * 128, col:col + 512], cv)

        conv_fm(c, xmT, 16, 5, W["ml_conv_wT"], W["ml_conv_b"], True, emit)
    s.barrier()

    with ExitStack() as es:
        wq = sb(es, nc, "mq_wq", [128, H, 4, 256], BF16)
        wk = sb(es, nc, "mq_wk", [128, H, 4, 256], BF16)
        wv = sb(es, nc, "mq_wv", [128, H, 4, 512], BF16)
        with ExitStack() as es2:
            stf = sbring(es2, nc, "mq_stg", [128, 4096], F32, 2)
            st = stf.next()
            sv = V(st.t[:].rearrange("p (h k n) -> p h k n", h=4, k=4), st.key)
            s.dma("sp", sv, V(W["ml_w_q"].rearrange("h (k p) n -> p h k n", p=128), "w_const"))
            s.cp("pool", wq[:], sv)
            st = stf.next()
            sv = V(st.t[:].rearrange("p (h k n) -> p h k n", h=4, k=4), st.key)
            s.dma("sp", sv, V(W["ml_w_k"].rearrange("h (k p) n -> p h k n", p=128), "w_const"))
            s.cp("pool", wk[:], sv)
            for hh in range(2):
                st = stf.next()
                sv = V(st.t[:].rearrange("p (h k n) -> p h k n", h=2, k=4), st.key)
                s.dma("sp", sv, V(W["ml_w_v"][hh * 2:hh * 2 + 2].rearrange("h (k p) n -> p h k n", p=128), "w_const"))
                s.cp("pool", wv[:, hh * 2:hh * 2 + 2, :, :], sv)
            s.barrier()
        chr_ = sbring(es, nc, "mq_ch", [128, 16, 128], BF16, 2)
        xmr = sbring(es, nc, "mq_xm", [128, 16, 128], BF16, 2)
        qko = sbring(es, nc, "mq_qko", [128, H * 512], BF16, 2)
        vo = sbring(es, nc, "mq_vo", [128, DI], BF16, 2)
        for i in range(NT):
            cht, xmt, qo, vot = chr_.next(), xmr.next(), qko.next(), vo.next()
            s.dma("sp", cht[:], V(chT.t[:, i * 128:(i + 1) * 128].rearrange("(j p) t -> p j t", p=128), chT.key))
            s.dma("sp", xmt[:], V(xmT.t[:, i * 128:(i + 1) * 128].rearrange("(j p) t -> p j t", p=128), xmT.key))
            for h in range(H):
                pq = c.pr.next()
                for kc in range(4):
                    s.mm(pq[:, 0:256], cht[:, h * 4 + kc, :], wq[:, h, kc, :], start=(kc == 0), stop=(kc == 3))
                for kc in range(4):
                    s.mm(pq[:, 256:512], cht[:, h * 4 + kc, :], wk[:, h, kc, :], start=(kc == 0), stop=(kc == 3))
                s.cp("act", qo[:, h * 512:(h + 1) * 512], pq[:, :])
                pv = c.pr.next()
                for kc in range(4):
                    s.mm(pv[:, :], xmt[:, h * 4 + kc, :], wv[:, h, kc, :], start=(kc == 0), stop=(kc == 3))
                s.cp("dve", vot[:, h * 512:(h + 1) * 512], pv[:, :])
            s.dma("pool", qk_tm[i * 128:(i + 1) * 128, :], qo[:])
            s.dma("pool", v_tm[i * 128:(i + 1) * 128, :], vot[:])
    s.barrier()

    with ExitStack() as es:
        c.pr = Ring(c.pbanks[0:5])
        psm = c.pbanks[5]
        fin = Finalizer(c, es, W["ml_w_out"], hin, hout)
        gb = sb(es, nc, "m_gb", [128, 16], F32)
        on = sb(es, nc, "m_on", [128, 512], F32)
        skp = sb(es, nc, "m_skip", [128, DI], F32)
        s.dma("sp", gb[:], V(W["ml_gate_b"].rearrange("a b c -> (a b c)").partition_broadcast(128), "w_const"))
        s.dma("sp", on[:], V(W["ml_onorm"].partition_broadcast(128), "w_const"))
        s.dma("sp", skp[:], V(W["ml_skip"].rearrange("h d -> (h d)").partition_broadcast(128), "w_const"))
        onesb = sb(es, nc, "m_onesb", [128, 2], BF16)
        s.memset("dve", onesb[:], 1.0)
        qkr = sbring(es, nc, "m_qk", [128, H, 512], BF16, 2)
        vr = sbring(es, nc, "m_v", [128, DI], BF16, 2)
        gr = sbring(es, nc, "m_g", [128, 16], F32, 2)
        sm = sbring(es, nc, "m_sm", [128, 8, 4], F32, 2)
        qs = sb(es, nc, "m_qs", [128, H, 256], BF16)
        ks = sb(es, nc, "m_ks", [128, H, 256], BF16)
        qT = sb(es, nc, "m_qT", [128, 8, 128], BF16)
        kT = sb(es, nc, "m_kT", [128, 8, 128], BF16)
        PT = sb(es, nc, "m_PT", [128, 4, 128], BF16)
        hacc = sb(es, nc, "m_hacc", [128, DI], F32)
        C32 = sb(es, nc, "m_C32", [128, 8, DV], F32)
        Cbf = sb(es, nc, "m_Cbf", [128, 8, DV], BF16)
        n32 = sb(es, nc, "m_n32", [128, 8], F32)
        nbf = sb(es, nc, "m_nbf", [128, 8], BF16)
        hft = sb(es, nc, "m_hf", [128, DI], F32)
        ogt = sb(es, nc, "m_og", [128, DI], BF16)
        zt = sb(es, nc, "m_z", [128, DI], BF16)
        cht = sb(es, nc, "m_ch", [128, DI], BF16)
        tmp = sb(es, nc, "m_tmp", [128, DI], F32)
        gst = sbring(es, nc, "m_st", [128, 4, 4], F32, 2)
        yb = sb(es, nc, "m_yb", [128, DI], BF16)
        allj = [str(j) for j in range(8)]

        def r3(buf, q):
            return V(buf.t[:].rearrange("p (h q) -> p h q", q=q), buf.key)

        for b in range(BL):
            for d in range(2):
                s.memset("dve", C32.subs(allj)[:], 0.0)
                s.memset("pool", Cbf.subs(allj)[:], 0.0)
                s.memset("dve", n32[:], 0.0)
                s.memset("pool", nbf[:], 0.0)
                tri = c.tri[:, d, :]
                order = range(NQ) if d == 0 else range(NQ - 1, -1, -1)
                for ci in order:
                    i = b * NQ + ci
                    r0 = i * 128
                    qkt, vt, gt = qkr.next(), vr.next(), gr.next()
                    s.dma("sp", V(qkt.t[:].rearrange("p h n -> p (h n)"), qkt.key), qk_tm[r0:r0 + 128, :])
                    s.dma("sp", vt[:], v_tm[r0:r0 + 128, :])
                    s.dma("sp", gt[:], gt_tm[r0:r0 + 128, :])
                    m = sm.next()
                    s.tt("dve", gt[:], gt[:], gb[:], ALU.add)
                    ig = gt[:, d * 8:d * 8 + 4]
                    fr = gt[:, d * 8 + 4:d * 8 + 8]
                    s.act(m[:, 0, :], fr, AF.Exp, scale=-1.0)
                    s.act(m[:, 0, :], m[:, 0, :], AF.Ln, bias=c.one[:, 0:1])
                    s.mm(psm[:, 0:4], tri, m[:, 0, :])
                    s.mm(psm[:, 4:8], c.onesf[:], m[:, 0, :])
                    s.act(m[:, 1, :], psm[:, 0:4], AF.Exp, scale=-1.0)
                    s.tt("dve", m[:, 2, :], psm[:, 0:4], ig, ALU.add)
                    s.act(m[:, 2, :], m[:, 2, :], AF.Exp, bias=c.one[:, 3:4])
                    s.act(m[:, 3, :], psm[:, 4:8], AF.Exp, scale=-1.0)
                    s.tt("dve", qs[:], V(qkt.t[:, :, 0:256], qkt.key),
                         bc(V(m.t[:, 1, :].unsqueeze(2), m.key), [128, H, 256]), ALU.mult)
                    s.tt("pool", ks[:], V(qkt.t[:, :, 256:512], qkt.key),
                         bc(V(m.t[:, 2, :].unsqueeze(2), m.key), [128, H, 256]), ALU.mult)
                    pt = c.ptr.next()
                    for j in range(8):
                        s.tr(pt[:, j, :], qs[:, j // 2, (j % 2) * 128:(j % 2 + 1) * 128], c.identb[:])
                    s.cp("act", qT[:], pt[:, :, :])
                    pt = c.ptr.next()
                    for j in range(8):
                        s.tr(pt[:, j, :], ks[:, j // 2, (j % 2) * 128:(j % 2 + 1) * 128], c.identb[:])
                    s.cp("dve", kT[:], pt[:, :, :])
                    pS = c.pr.next()
                    for h in range(H):
                        for kc in range(2):
                            s.mm(pS[:, h * 128:(h + 1) * 128], kT[:, h * 2 + kc, :], qT[:, h * 2 + kc, :],
                                 start=(kc == 0), stop=(kc == 1))
                    s.tt("dve", PT[:], V(pS.t[:, :].rearrange("p (a l) -> p a l", a=4), pS.key),
                         bc(V(tri.ap.unsqueeze(1), tri.k), [128, 4, 128]), ALU.mult)
                    for h in range(H):
                        s.mm(psm[:, 8 + h:9 + h], PT[:, h, :], onesb[:, 0:1], start=True, stop=False)
                        for kc in range(2):
                            s.mm(psm[:, 8 + h:9 + h], qT[:, h * 2 + kc, :], nbf[:, h * 2 + kc:h * 2 + kc + 1],
                                 start=False, stop=(kc == 1))
                    s.act(m[:, 4, :], psm[:, 8:12], AF.Abs)
                    s.ts("dve", m[:, 4, :], m[:, 4, :], 1.0, None, ALU.max)
                    s.recip("dve", m[:, 5, :], m[:, 4, :])
                    for h in range(H):
                        po = c.pr.next()
                        s.mm(po[:, :], PT[:, h, :], vt[:, h * DV:(h + 1) * DV], start=True, stop=False)
                        for kc in range(2):
                            s.mm(po[:, :], qT[:, h * 2 + kc, :], Cbf.sub(str(h * 2 + kc))[:, h * 2 + kc, :],
                                 start=False, stop=(kc == 1))
                        s.act(hacc[:, h * DV:(h + 1) * DV], po[:, :], AF.Copy, scale=m[:, 5, h:h + 1])
                        for kc in range(2):
                            j = h * 2 + kc
                            pu = c.pr.next()
                            s.mm(pu[:, :], ks[:, h, kc * 128:(kc + 1) * 128], vt[:, h * DV:(h + 1) * DV])
                            s.mm(psm[:, 16 + j:17 + j], ks[:, h, kc * 128:(kc + 1) * 128], onesb[:, 0:1])
                            cj = C32.sub(str(j))
                            s.tt("dve", cj[:, j, :], cj[:, j, :], pu[:, :], ALU.add)
                            s.ts("pool", cj[:, j, :], cj[:, j, :], m[:, 3, h:h + 1], None, ALU.mult)
                            s.cp("act", Cbf.sub(str(j))[:, j, :], cj[:, j, :])
                    s.tt("dve", n32[:], n32[:], psm[:, 16:24], ALU.add)
                    s.tt("dve", V(n32.t[:].rearrange("p (h k) -> p h k", k=2), n32.key),
                         V(n32.t[:].rearrange("p (h k) -> p h k", k=2), n32.key),
                         bc(V(m.t[:, 3, :].unsqueeze(2), m.key), [128, H, 2]), ALU.mult)
                    s.cp("dve", nbf[:], n32[:])
                    if d == 0:
                        s.dma("pool", hf[r0:r0 + 128, :], hacc[:])
                    else:
                        s.dma("sp", hft[:], hf[r0:r0 + 128, :])
                        s.dma("sp", ogt[:], og_tm[r0:r0 + 128, :])
                        s.dma("sp", zt[:], z_tm[r0:r0 + 128, :])
                        s.dma("sp", cht[:], ch_tm[r0:r0 + 128, :])
                        s.tt("pool", hft[:], hft[:], hacc[:], ALU.add)
                        s.act(tmp[:], ogt[:], AF.Sigmoid)
                        s.tt("dve", hft[:], hft[:], tmp[:], ALU.mult)
                        gs = gst.next()
                        s.tt("pool", tmp[:], hft[:], hft[:], ALU.mult)
                        s.red("dve", gs[:, 0, :], r3(tmp, DV), ALU.add)
                        s.ts("dve", gs[:, 1, :], gs[:, 0, :], 1.0 / DV, EPS, ALU.mult, ALU.add)
                        s.act(gs[:, 2, :], gs[:, 1, :], AF.Sqrt)
                        s.recip("dve", gs[:, 3, :], gs[:, 2, :])
                        s.tt("dve", r3(hft, DV), r3(hft, DV), bc(V(gs.t[:, 3, :].unsqueeze(2), gs.key), [128, H, DV]), ALU.mult)
                        s.tt("pool", r3(hft, DV), r3(hft, DV), bc(V(on.t[:].unsqueeze(1), on.key), [128, H, DV]), ALU.mult)
                        s.tt("dve", tmp[:], cht[:], skp[:], ALU.mult)
                        s.tt("dve", hft[:], hft[:], tmp[:], ALU.add)
                        s.act(tmp[:], zt[:], AF.Silu)
                        s.tt("dve", yb[:], hft[:], tmp[:], ALU.mult)
                        fin.run(yb, i)
    s.barrier()
    c.pr = Ring(c.pbanks)


def load_mat(c, dst, src_ap):
    v = src_ap.rearrange("(st p) f -> p st f", p=128)
    for q in range(4):
        c.s.dma("sp", dst[:, q * 4:(q + 1) * 4, :], V(v[:, q * 4:(q + 1) * 4, :], "w_const"))


def layer_hyena(c, W, hin, hout):
    nc, s = c.nc, c.s
    C = c.C
    dr = c.dram
    vxT = dr("hy_vxT", [3 * DI, T], BF16)
    z_tm = dr("hy_z", [T, DI], BF16)
    sig = [dr("hy_v", [T, DI], BF16), dr("hy_x1", [T, DI], BF16), dr("hy_x2", [T, DI], BF16)]
    y1_tm = dr("hy_y1", [T, DI], BF16)
    y2_tm = dr("hy_y2", [T, DI], BF16)
    Kf = dr("hy_Kf", [2, 2, L, DI], BF16)
    Zd = dr("hy_Z", [BL, 2, L, DI], BF16)

    with ExitStack() as es:
        uT = sb(es, nc, "uT", [128, 8, T], BF16)
        phase_norm(c, hin, W["hy_norm"], uT)
        phase_project(c, uT, W["hy_w_in"], [
            (0, 3 * DI, "fm", vxT, BF16, 1.0),
            (3 * DI, DI, "tm", z_tm, BF16, 1.0),
        ])

    with ExitStack() as es:
        stg = sbring(es, nc, "hc_stg", [128, 4, 128], BF16, 3)

        def emit(ct, b, tb, cv):
            to_tm_store(c, cv, sig[ct // 16], b * L + tb * 512, (ct % 16) * 128, stg)

        conv_fm(c, vxT, 48, 3, W["hy_conv_wT"], W["hy_conv_b"], False, emit)
    s.barrier()

    def fwd_transform(Cm, Sn, o, ysrc):
        with ExitStack() as es:
            yr = sbring(es, nc, "hf_y", [128, 16, 512], BF16, 2)
            kr = sbring(es, nc, "hf_k", [128, 2, 512], BF16, 2)
            yre = sb(es, nc, "hf_yre", [128, 512], F32)
            yim = sb(es, nc, "hf_yim", [128, 512], F32)
            t1 = sb(es, nc, "hf_t1", [128, 512], F32)
            t2 = sb(es, nc, "hf_t2", [128, 512], F32)
            t3 = sb(es, nc, "hf_t3", [128, 512], F32)
            t4 = sb(es, nc, "hf_t4", [128, 512], F32)
            zr = sbring(es, nc, "hf_z", [128, 2, 512], BF16, 2)
            for b in range(BL):
                for cb in range(4):
                    yt = yr.next()
                    cs = slice(cb * 512, (cb + 1) * 512)
                    s.dma("sp", yt[:], V(ysrc.t[b * L:(b + 1) * L, cs].rearrange("(st p) c -> p st c", p=128), ysrc.key))
                    for ft in range(16):
                        fs = slice(ft * 128, (ft + 1) * 128)
                        kt = kr.next()
                        s.dma("sp", kt[:], V(Kf.t[o, :, fs, cs].rearrange("a p c -> p a c"), Kf.key))
                        pre, pim = c.pr.next(), c.pr.next()
                        for st in range(16):
                            s.mm(pre[:, :], Cm[:, st, fs], yt[:, st, :], start=(st == 0), stop=(st == 15))
                        for st in range(16):
                            s.mm(pim[:, :], Sn[:, st, fs], yt[:, st, :], start=(st == 0), stop=(st == 15))
                        s.cp("act", yre[:], pre[:, :])
                        s.cp("act", yim[:], pim[:, :])
                        s.tt("dve", t1[:], yre[:], kt[:, 0, :], ALU.mult)
                        s.tt("dve", t2[:], yim[:], kt[:, 1, :], ALU.mult)
                        s.tt("pool", t3[:], yre[:], kt[:, 1, :], ALU.mult)
                        s.tt("pool", t4[:], yim[:], kt[:, 0, :], ALU.mult)
                        zt = zr.next()
                        s.tt("dve", zt[:, 0, :], t1[:], t2[:], ALU.subtract)
                        s.tt("pool", zt[:, 1, :], t3[:], t4[:], ALU.add)
                        s.dma("pool", V(Zd.t[b, :, fs, cs].rearrange("a p c -> p a c"), Zd.key), zt[:])
        s.barrier()

    def inv_transform(o, ysrc, xg_src, ydst):
        with ExitStack() as es:
            CI = sb(es, nc, "hi_CI", [128, 16, L], BF16)
            SI = sb(es, nc, "hi_SI", [128, 16, L], BF16)
            load_mat(c, CI, C["c_CI"])
            load_mat(c, SI, C["c_SI"])
            dbc = sb(es, nc, "hi_d", [128, DI], F32)
            s.dma("sp", dbc[:], V(W["hy_d"][o].partition_broadcast(128), "w_const"))
            zb = sb(es, nc, "hi_zb", [128, 2, 16, 512], BF16)
            ypr = sbring(es, nc, "hi_yp", [128, 512], BF16, 2)
            xgr = sbring(es, nc, "hi_xg", [128, 512], BF16, 2)
            zzr = sbring(es, nc, "hi_zz", [128, 512], BF16, 2)
            ta = sbring(es, nc, "hi_ta", [128, 512], F32, 2)
            tb_ = sbring(es, nc, "hi_tb", [128, 512], F32, 2)
            yo = sbring(es, nc, "hi_yo", [128, 512], BF16, 2)
            for b in range(BL):
                for cb in range(4):
                    cs = slice(cb * 512, (cb + 1) * 512)
                    for a in range(2):
                        for q in range(2):
                            s.dma("sp", zb[:, a, q * 8:(q + 1) * 8, :],
                                  V(Zd.t[b, a, q * 1024:(q + 1) * 1024, cs].rearrange("(ft p) c -> p ft c", p=128), Zd.key))
                    for tt in range(16):
                        ts_ = slice(tt * 128, (tt + 1) * 128)
                        rows = slice(b * L + tt * 128, b * L + (tt + 1) * 128)
                        po = c.pr.next()
                        for ft in range(16):
                            s.mm(po[:, :], CI[:, ft, ts_], zb[:, 0, ft, :], start=(ft == 0), stop=False)
                        for ft in range(16):
                            s.mm(po[:, :], SI[:, ft, ts_], zb[:, 1, ft, :], start=False, stop=(ft == 15))
                        yp, xg = ypr.next(), xgr.next()
                        s.dma("sp", yp[:], ysrc[rows, cs])
                        s.dma("sp", xg[:], xg_src[rows, cs])
                        t_a = ta.next()
                        s.tt("dve", t_a[:], yp[:], dbc[:, cs], ALU.mult)
                        s.tt("dve", t_a[:], t_a[:], po[:, :], ALU.add)
                        yot = yo.next()
                        if o == 0:
                            s.tt("pool", yot[:], t_a[:], xg[:], ALU.mult)
                        else:
                            zz, t_b = zzr.next(), tb_.next()
                            s.dma("sp", zz[:], z_tm[rows, cs])
                            s.act(t_b[:], zz[:], AF.Silu)
                            s.tt("pool", t_a[:], t_a[:], xg[:], ALU.mult)
                            s.tt("dve", yot[:], t_a[:], t_b[:], ALU.mult)
                        s.dma("pool", ydst[rows, cs], yot[:])
        s.barrier()

    with ExitStack() as es:
        Cm = sb(es, nc, "hy_Cm", [128, 16, L], BF16)
        Sn = sb(es, nc, "hy_Sn", [128, 16, L], BF16)
        load_mat(c, Cm, C["c_Cm"])
        load_mat(c, Sn, C["c_Sn"])
        with ExitStack() as es2:
            hA = sb(es2, nc, "hm_hA", [64, L], F32)
            hB = sb(es2, nc, "hm_hB", [64, L], F32)
            with ExitStack() as es3:
                feats = sb(es3, nc, "hm_feats", [33, L], F32)
                w1 = sb(es3, nc, "hm_w1", [33, 64], F32)
                wh = sb(es3, nc, "hm_wh", [64, 2, 64], F32)
                prm = sb(es3, nc, "hm_prm", [64, 8], F32)
                tr_ = sb(es3, nc, "hm_t", [64, 512], F32)
                tki = sb(es3, nc, "hm_ki", [64, 512], mybir.dt.int32)
                tkf = sb(es3, nc, "hm_kf", [64, 512], F32)
                s.dma("sp", feats[:], V(C["c_featsT"], "w_const"))
                s.dma("sp", w1[:], V(W["hy_ffn_w_in"], "w_const"))
                s.dma("sp", wh[:], V(W["hy_ffn_w_hid"].rearrange("j a b -> a j b"), "w_const"))
                s.dma("sp", prm[:, 0:1], V(W["hy_ffn_b_in"].rearrange("(p o) -> p o", o=1), "w_const"))
                s.dma("sp", prm[:, 1:3], V(W["hy_ffn_b_hidT"], "w_const"))
                s.dma("sp", prm[:, 3:6], V(W["hy_ffn_freqT"], "w_const"))
                cur, nxt = hA, hB
                for layer in range(3):
                    for blk in range(4):
                        bs = slice(blk * 512, (blk + 1) * 512)
                        ps = c.pr.next()
                        if layer == 0:
                            s.mm(ps[0:64, :], w1[:, :], feats[:, bs])
                            dst = cur
                        else:
                            s.mm(ps[0:64, :], wh[:, layer - 1, :], cur[:, bs])
                            dst = nxt
                        s.ts("dve", tr_[:], ps[0:64, :], prm[:, layer:layer + 1], prm[:, 3 + layer:4 + layer], ALU.add, ALU.mult)
                        s.ts("dve", tr_[:], tr_[:], 1.0 / (2.0 * PI), 8.5, ALU.mult, ALU.add)
                        s.cp("dve", tki[:], tr_[:])
                        s.cp("dve", tkf[:], tki[:])
                        s.tt("dve", tr_[:], tr_[:], tkf[:], ALU.subtract)
                        s.ts("dve", tkf[:], tr_[:], 0.0, None, ALU.is_lt)
                        s.tt("dve", tr_[:], tr_[:], tkf[:], ALU.add)
                        s.act(dst[:, bs], tr_[:], AF.Sin, bias=c.one[0:64, 1:2], scale=2.0 * PI)
                    if layer > 0:
                        cur, nxt = nxt, cur
                h3 = cur
                s.barrier()
            dlt = sb(es2, nc, "hm_dlt", [128, DI], F32)
            ngt = sb(es2, nc, "hm_negt", [128, 16], F32)
            s.dma("sp", dlt[:], V(C["c_deltas"].partition_broadcast(128), "w_const"))
            s.dma("sp", ngt[:], V(C["c_negt"], "w_const"))
            wor = sbring(es2, nc, "hm_wo", [64, 2, 256], F32, 2)
            Ar = sb(es2, nc, "hm_A", [128, 16, 256], BF16)
            Br_ = sb(es2, nc, "hm_B", [128, 16, 256], BF16)
            dec = sbring(es2, nc, "hm_dec", [128, 256], F32, 2)
            hbs = sbring(es2, nc, "hm_hb", [128, 256], F32, 2)
            sa = sbring(es2, nc, "hm_sa", [128, 256], F32, 2)
            sbm = sbring(es2, nc, "hm_sb", [128, 256], F32, 2)
            ko = sbring(es2, nc, "hm_ko", [128, 512], BF16, 2)
            wov = W["hy_ffn_w_out"]
            for o in range(2):
                for cb in range(8):
                    cs = slice(cb * 256, (cb + 1) * 256)
                    wo = wor.next()
                    for dd in range(2):
                        c0 = dd * 2 * DI + o * DI + cb * 256
                        s.dma("sp", wo[:, dd, :], V(wov[:, c0:c0 + 256], "w_const"))
                    for tt in range(16):
                        ps = c.pr.next()
                        s.mm(ps[:, 0:256], h3[:, tt * 128:(tt + 1) * 128], wo[:, 0, :])
                        s.mm(ps[:, 256:512], h3[:, tt * 128:(tt + 1) * 128], wo[:, 1, :])
                        dc, hb_, a_, b_ = dec.next(), hbs.next(), sa.next(), sbm.next()
                        s.act(dc[:], dlt[:, cs], AF.Exp, scale=ngt[:, tt:tt + 1])
                        s.cp("dve", hb_[:], ps[:, 256:512])
                        if tt == 0:
                            s.memset("dve", hb_[0:1, :], 0.0)
                        s.tt("dve", a_[:], ps[:, 0:256], hb_[:], ALU.add)
                        s.tt("dve", b_[:], ps[:, 0:256], hb_[:], ALU.subtract)
                        s.tt("pool", Ar[:, tt, :], a_[:], dc[:], ALU.mult)
                        s.tt("pool", Br_[:, tt, :], b_[:], dc[:], ALU.mult)
                    for ft in range(16):
                        fs = slice(ft * 128, (ft + 1) * 128)
                        pk = c.pr.next()
                        for tt in range(16):
                            s.mm(pk[:, 0:256], Cm[:, tt, fs], Ar[:, tt, :], start=(tt == 0), stop=(tt == 15))
                        for tt in range(16):
                            s.mm(pk[:, 256:512], Sn[:, tt, fs], Br_[:, tt, :], start=(tt == 0), stop=(tt == 15))
                        kt = ko.next()
                        s.cp("act", kt[:], pk[:, :])
                        s.dma("pool", V(Kf.t[o, :, fs, cs].rearrange("a p c -> p a c"), Kf.key),
                              V(kt.t[:].rearrange("p (a c) -> p a c", a=2), kt.key))
            s.barrier()
        fwd_transform(Cm, Sn, 0, sig[0])
    inv_transform(0, sig[0], sig[1], y1_tm)
    with ExitStack() as es:
        Cm = sb(es, nc, "hy_Cm2", [128, 16, L], BF16)
        Sn = sb(es, nc, "hy_Sn2", [128, 16, L], BF16)
        load_mat(c, Cm, C["c_Cm"])
        load_mat(c, Sn, C["c_Sn"])
        fwd_transform(Cm, Sn, 1, y1_tm)
    inv_transform(1, y1_tm, sig[2], y2_tm)

    with ExitStack() as es:
        fin = Finalizer(c, es, W["hy_w_out"], hin, hout)
        yr = sbring(es, nc, "ho_y", [128, DI], BF16, 2)
        for i in range(NT):
            yt = yr.next()
            s.dma("sp", yt[:], y2_tm[i * 128:(i + 1) * 128, :])
            fin.run(yt, i)
    s.barrier()


N_IMPL = 4


def host_constants():
    cst = {}
    cst["c_identb"] = np.eye(128, dtype=np.float32).astype(ml_dtypes.bfloat16)
    cst["c_identf"] = np.eye(128, dtype=np.float32)
    sidx = np.arange(128)[:, None]
    lidx = np.arange(128)[None, :]
    tri = np.stack([(sidx <= lidx), (sidx >= lidx)], axis=1).astype(np.float32)
    cst["c_tri"] = tri
    cst["c_negm"] = ((1.0 - tri) * -1.0e5).astype(np.float32)
    sI = np.arange(L, dtype=np.int64)[:, None]
    fI = np.arange(L, dtype=np.int64)[None, :]
    ph = ((2 * fI + 1) * sI) % (4 * L)
    th = (2.0 * np.pi / (4 * L)) * ph.astype(np.float64)
    cm = np.cos(th)
    sn = np.sin(th)
    bf = ml_dtypes.bfloat16
    cst["c_Cm"] = cm.astype(np.float32).astype(bf)
    cst["c_Sn"] = (-sn).astype(np.float32).astype(bf)
    cst["c_CI"] = np.ascontiguousarray((cm.T / L)).astype(np.float32).astype(bf)
    cst["c_SI"] = np.ascontiguousarray((-sn.T / L)).astype(np.float32).astype(bf)
    t = np.linspace(0.0, 1.0, L, dtype=np.float32)[:, None]
    pos = np.arange(L, dtype=np.float32)[:, None]
    bands = np.linspace(1e-4, 15.0, 16, dtype=np.float32)[None]
    ang = (np.float32(2.0 * math.pi / L) * pos * bands).astype(np.float32)
    feats = np.concatenate([t, np.cos(ang), -np.sin(ang)], axis=-1).astype(np.float32)
    cst["c_featsT"] = np.ascontiguousarray(feats.T)
    max_decay = math.log(1e-2) / 0.3
    min_decay = math.log(1e-2) / 1.5
    cst["c_deltas"] = np.abs(np.linspace(min_decay, max_decay, DI, dtype=np.float32)).astype(np.float32)
    cst["c_negt"] = np.ascontiguousarray(-(t[:, 0].reshape(16, 128).T)).astype(np.float32)
    return cst


def host_prepare(inputs):
    W = {}
    for k, v in inputs.items():
        if k in ("x", "final_norm"):
            continue
        W[k] = np.ascontiguousarray(v[0])
    W["final_norm"] = np.ascontiguousarray(inputs["final_norm"])
    W["ssd_conv_wT"] = np.ascontiguousarray(W.pop("ssd_conv_w").T)
    W["ml_conv_wT"] = np.ascontiguousarray(W.pop("ml_conv_w").T)
    W["hy_conv_wT"] = np.ascontiguousarray(W.pop("hy_conv_w").T)
    W["hy_ffn_b_hidT"] = np.ascontiguousarray(W.pop("hy_ffn_b_hid").T)
    W["hy_ffn_freqT"] = np.ascontiguousarray(W.pop("hy_ffn_freq").T)
    return W


def build_program(wshapes, cshapes, n_layers=4, final_norm=True):
    n_layers = min(n_layers, N_IMPL)
    nc = bass.Bass("TRN2", target_bir_lowering=False)
    c = Ctx()
    c.nc = nc
    c.s = Sched(nc)
    s = c.s
    x_in = Buf(nc.dram_tensor("x", [T, D], F32, kind="ExternalInput").ap(), "x_in")
    out = Buf(nc.dram_tensor("out", [T, D], F32, kind="ExternalOutput").ap(), "out")
    W = {k: nc.dram_tensor(k, list(shp), F32 if dt == np.float32 else BF16, kind="ExternalInput").ap()
         for k, (shp, dt) in wshapes.items()}
    C = {k: nc.dram_tensor(k, list(shp), F32 if dt == np.float32 else BF16, kind="ExternalInput").ap()
         for k, (shp, dt) in cshapes.items()}

    def dram(name, shape, dt):
        return Buf(nc.dram_tensor(name, shape, dt, kind="Internal").ap(), name)

    c.dram = dram
    c.C = C
    hA = dram("hA", [T, D], F32)
    hB = dram("hB", [T, D], F32)

    with ExitStack() as es:
        c.identb = sb(es, nc, "k_identb", [128, 128], BF16)
        c.identf = sb(es, nc, "k_identf", [128, 128], F32)
        c.tri = sb(es, nc, "k_tri", [128, 2, 128], F32)
        c.negm = sb(es, nc, "k_negm", [128, 2, 128], F32)
        c.onesf = sb(es, nc, "k_onesf", [128, 128], F32)
        c.nonesf = sb(es, nc, "k_nonesf", [128, 128], F32)
        c.one = sb(es, nc, "k_one", [128, 4], F32)
        s.dma("sp", c.identb[:], V(C["c_identb"], "w_const"))
        s.dma("sp", c.identf[:], V(C["c_identf"], "w_const"))
        s.dma("sp", c.tri[:], V(C["c_tri"], "w_const"))
        s.dma("sp", c.negm[:], V(C["c_negm"], "w_const"))
        s.memset("dve", c.onesf[:], 1.0)
        s.memset("dve", c.nonesf[:], -1.0)
        s.memset("dve", c.one[:, 0:1], 1.0)
        s.memset("dve", c.one[:, 1:2], -PI)
        s.memset("dve", c.one[:, 2:3], 0.0)
        s.memset("dve", c.one[:, 3:4], math.log(1.0 / 16.0))
        pbanks = [Buf(es.enter_context(nc.psum_tensor(f"ps{i}", [128, 512], F32)), f"ps{i}") for i in range(6)]
        tbanks = [Buf(es.enter_context(nc.psum_tensor(f"pt{i}", [128, 8, 128], BF16)), f"pt{i}") for i in range(2)]
        c.pbanks = pbanks
        c.pr = Ring(pbanks)
        c.ptr = Ring(tbanks)
        s.barrier()
        c.identb.key = c.identf.key = c.tri.key = c.negm.key = "konst"
        c.onesf.key = c.nonesf.key = c.one.key = "konst"

        layers = [layer_ssd, layer_gla, layer_hyena, layer_mlstm]
        hs = [x_in, hA, hB, hA, hB]
        hcur = x_in
        for li in range(n_layers):
            hnext = hA if (li % 2 == 0) else hB
            layers[li](c, W, hcur, hnext)
            hcur = hnext
        if final_norm:
            phase_final_norm(c, hcur, W["final_norm"], out)
        else:
            with ExitStack() as es2:
                cr = sbring(es2, nc, "cp_x", [128, D], F32, 2)
                for i in range(NT):
                    t = cr.next()
                    s.dma("sp", t[:], hcur[i * 128:(i + 1) * 128, :])
                    s.dma("pool", out[i * 128:(i + 1) * 128, :], t[:])
            s.barrier()
    return nc


_CACHE = {}


def kernel(**inputs):
    x = np.ascontiguousarray(inputs["x"], dtype=np.float32)
    W = host_prepare(inputs)
    Cst = host_constants()
    wshapes = {k: (v.shape, v.dtype.type if v.dtype != ml_dtypes.bfloat16 else "bf16") for k, v in W.items()}
    cshapes = {k: (v.shape, v.dtype.type if v.dtype != ml_dtypes.bfloat16 else "bf16") for k, v in Cst.items()}
    nc = build_program(wshapes, cshapes)
    in_maps = []
    for i in range(NCORES):
        m = {"x": x[i * BL:(i + 1) * BL].reshape(T, D)}
        m.update(W)
        m.update(Cst)
        in_maps.append(m)
    res = run_bass_kernel_spmd(nc, in_maps, core_ids=list(range(NCORES)))
    outs = [r["out"].reshape(BL, L, D) for r in res.results]
    return np.concatenate(outs, axis=0).astype(np.float32)
```

```python
import math
from contextlib import ExitStack
import numpy as np
import ml_dtypes
import concourse.bass as bass
import concourse.mybir as mybir
from concourse.bass_utils import run_bass_kernel_spmd

F32 = mybir.dt.float32
BF16 = mybir.dt.bfloat16
AF = mybir.ActivationFunctionType
ALU = mybir.AluOpType
AX = mybir.AxisListType

NCORES = 8
BL = 2
L = 2048
T = BL * L
D = 1024
DI = 2048
Q = 128
NQ = L // Q
NT = T // 128
EPS = 1e-6
EPOCH = 30000
PI = math.pi


def _kt(k):
    if isinstance(k, str):
        return (k,)
    return tuple(k)


class V:
    __slots__ = ("ap", "k")

    def __init__(self, ap, k):
        self.ap = ap
        self.k = _kt(k)


class Buf:
    def __init__(self, t, key):
        self.t = t
        self.key = key

    def __getitem__(self, idx):
        return V(self.t[idx], self.key)

    def sub(self, sub):
        return Buf(self.t, f"{self.key}.{sub}")

    def subs(self, subs):
        return Buf(self.t, tuple(f"{self.key}.{x}" for x in subs))


class Sched:
    def __init__(self, nc):
        self.nc = nc
        self.eng = {"pe": nc.tensor, "dve": nc.vector, "act": nc.scalar,
                    "pool": nc.gpsimd, "sp": nc.sync}
        self.nsem = 0
        self.esem, self.ecnt = {}, {}
        for e in self.eng:
            self._new_epoch(e)
        self.seen = {e: {} for e in self.eng}
        self.res = {}
        self.dsem = {}
        self.dfree = []
        self.ninst = 0

    def _alloc(self):
        self.nsem += 1
        return self.nc.alloc_semaphore(name=f"s{self.nsem}")

    def _new_epoch(self, e):
        self.esem[e] = self._alloc()
        self.ecnt[e] = 0

    def _wait(self, e, tok):
        sem, val = tok
        sid = id(sem)
        if self.seen[e].get(sid, 0) >= val:
            return
        self.eng[e].wait_ge(sem, val)
        self.seen[e][sid] = val

    def _deps(self, e, reads, writes, pe_accum=False, dsem=None):
        for k in reads:
            r = self.res.get(k)
            if r and r["w"] is not None:
                self._wait(e, r["w"])
        for k in writes:
            r = self.res.get(k)
            if r:
                w = r["w"]
                if w is not None:
                    skip = (pe_accum and r["we"] == "pe") or (dsem is not None and w[0] is dsem)
                    if not skip:
                        self._wait(e, w)
                for t in r["r"]:
                    self._wait(e, t)

    def _record(self, e, tok, reads, writes):
        for k in reads:
            r = self.res.setdefault(k, {"w": None, "r": [], "we": None})
            r["r"] = [t for t in r["r"] if t[0] is not tok[0]] + [tok]
        for k in writes:
            self.res[k] = {"w": tok, "r": [], "we": e}

    def op(self, e, fn, reads=(), writes=(), pe_accum=False):
        reads = [k for ks in reads if ks is not None for k in _kt(ks)]
        writes = [k for ks in writes for k in _kt(ks)]
        self._deps(e, reads, writes, pe_accum)
        if self.ecnt[e] >= EPOCH:
            self._new_epoch(e)
        inst = fn()
        self.ecnt[e] += 1
        inst.then_inc(self.esem[e], 1)
        self._record(e, (self.esem[e], self.ecnt[e]), reads, writes)
        self.ninst += 1
        return inst

    def dma(self, e, out, in_, **kw):
        reads, writes = list(in_.k), list(out.k)
        sk = out.k[0]
        if sk not in self.dsem:
            if self.dfree:
                self.dsem[sk] = self.dfree.pop()
            else:
                self.dsem[sk] = [self._alloc(), 0]
        ds = self.dsem[sk]
        self._deps(e, reads, writes, dsem=ds[0])
        inst = self.eng[e].dma_start(out=out.ap, in_=in_.ap, **kw)
        ds[1] += 16
        inst.then_inc(ds[0], 16)
        self._record(e, (ds[0], ds[1]), reads, writes)
        self.ninst += 1
        return inst

    def barrier(self):
        toks = [(self.esem[e], self.ecnt[e]) for e in self.eng if self.ecnt[e] > 0]
        toks += [(d[0], d[1]) for d in self.dsem.values() if d[1] > 0]
        for e in self.eng:
            for t in toks:
                if t[0] is not self.esem[e]:
                    self._wait(e, t)
        for d in self.dsem.values():
            if d[1] < 40000:
                self.dfree.append(d)
        self.dsem = {}
        self.res = {}

    def mm(self, out, lhsT, rhs, start=True, stop=True):
        nc = self.nc
        return self.op("pe", lambda: nc.tensor.matmul(out.ap, lhsT=lhsT.ap, rhs=rhs.ap, start=start, stop=stop),
                       reads=[lhsT.k, rhs.k], writes=[out.k], pe_accum=True)

    def tr(self, out, in_, ident):
        nc = self.nc
        return self.op("pe", lambda: nc.tensor.transpose(out.ap, in_.ap, ident.ap),
                       reads=[in_.k, ident.k], writes=[out.k], pe_accum=True)

    def act(self, out, in_, func, bias=None, scale=None, accum=None):
        nc = self.nc
        kw = {}
        rd = [in_.k]
        wr = [out.k]
        if bias is not None:
            if isinstance(bias, V):
                kw["bias"] = bias.ap
                rd.append(bias.k)
            else:
                kw["bias"] = bias
        if scale is not None:
            if isinstance(scale, V):
                kw["scale"] = scale.ap
                rd.append(scale.k)
            else:
                kw["scale"] = scale
        if accum is not None:
            kw["accum_out"] = accum.ap
            wr.append(accum.k)
        return self.op("act", lambda: nc.scalar.activation(out=out.ap, in_=in_.ap, func=func, **kw),
                       reads=rd, writes=wr)

    def _e(self, e):
        return self.eng[e]

    def tt(self, e, out, in0, in1, op):
        return self.op(e, lambda: self._e(e).tensor_tensor(out=out.ap, in0=in0.ap, in1=in1.ap, op=op),
                       reads=[in0.k, in1.k], writes=[out.k])

    def ts(self, e, out, in0, s1, s2, op0, op1=None):
        rd = [in0.k]
        a1 = s1.ap if isinstance(s1, V) else s1
        a2 = s2.ap if isinstance(s2, V) else s2
        if isinstance(s1, V):
            rd.append(s1.k)
        if isinstance(s2, V):
            rd.append(s2.k)
        if op1 is None:
            return self.op(e, lambda: self._e(e).tensor_scalar(out=out.ap, in0=in0.ap, scalar1=a1, scalar2=None, op0=op0),
                           reads=rd, writes=[out.k])
        return self.op(e, lambda: self._e(e).tensor_scalar(out=out.ap, in0=in0.ap, scalar1=a1, scalar2=a2, op0=op0, op1=op1),
                       reads=rd, writes=[out.k])

    def stt(self, e, out, in0, scalar, in1, op0, op1):
        rd = [in0.k, in1.k]
        a = scalar.ap if isinstance(scalar, V) else scalar
        if isinstance(scalar, V):
            rd.append(scalar.k)
        return self.op(e, lambda: self._e(e).scalar_tensor_tensor(out=out.ap, in0=in0.ap, scalar=a, in1=in1.ap, op0=op0, op1=op1),
                       reads=rd, writes=[out.k])

    def cp(self, e, out, in_):
        if e == "act":
            return self.op(e, lambda: self.nc.scalar.copy(out=out.ap, in_=in_.ap), reads=[in_.k], writes=[out.k])
        return self.op(e, lambda: self._e(e).tensor_copy(out=out.ap, in_=in_.ap), reads=[in_.k], writes=[out.k])

    def memset(self, e, out, val):
        return self.op(e, lambda: self._e(e).memset(out.ap, val), reads=[], writes=[out.k])

    def red(self, e, out, in_, op, axis=AX.X):
        return self.op(e, lambda: self._e(e).tensor_reduce(out=out.ap, in_=in_.ap, axis=axis, op=op),
                       reads=[in_.k], writes=[out.k])

    def recip(self, e, out, in_):
        return self.op(e, lambda: self._e(e).reciprocal(out=out.ap, in_=in_.ap), reads=[in_.k], writes=[out.k])


class Ctx:
    pass


class Ring:
    def __init__(self, bufs):
        self.bufs = bufs
        self.i = 0

    def next(self):
        b = self.bufs[self.i % len(self.bufs)]
        self.i += 1
        return b


_UID = [0]


def sb(es, nc, name, shape, dt):
    _UID[0] += 1
    name = f"{name}_{_UID[0]}"
    t = es.enter_context(nc.sbuf_tensor(name, shape, dt))
    return Buf(t, name)


def sbring(es, nc, name, shape, dt, n):
    return Ring([sb(es, nc, f"{name}{i}", shape, dt) for i in range(n)])


def bc(v, shape):
    return V(v.ap.to_broadcast(shape), v.k)


def phase_norm(c, hin, gvec, uT):
    nc, s = c.nc, c.s
    with ExitStack() as es:
        gt = sb(es, nc, "n_g", [128, D], F32)
        xr = sbring(es, nc, "n_x", [128, D], F32, 2)
        sq = sb(es, nc, "n_sq", [128, D], F32)
        ur = sbring(es, nc, "n_u", [128, D], BF16, 2)
        st = sb(es, nc, "n_st", [128, NT, 4], F32)
        s.dma("sp", gt[:], V(gvec.partition_broadcast(128), "w_const"))
        for i in range(NT):
            xt = xr.next()
            ub = ur.next()
            stv = st.sub(str(i))
            s.dma("sp", xt[:], hin[i * 128:(i + 1) * 128, :])
            s.act(sq[:], xt[:], AF.Square, accum=stv[:, i, 0:1])
            s.ts("dve", stv[:, i, 1:2], stv[:, i, 0:1], 1.0 / D, EPS, ALU.mult, ALU.add)
            s.act(stv[:, i, 2:3], stv[:, i, 1:2], AF.Sqrt)
            s.recip("dve", stv[:, i, 3:4], stv[:, i, 2:3])
            s.stt("dve", ub[:], xt[:], stv[:, i, 3:4], gt[:], ALU.mult, ALU.mult)
            pt = c.ptr.next()
            for k in range(8):
                s.tr(pt[:, k, :], ub[:, k * 128:(k + 1) * 128], c.identb[:])
            s.cp("act", uT[:, :, i * 128:(i + 1) * 128], pt[:, 0:8, :])
    s.barrier()


def phase_project(c, uT, w_ap, segs):
    nc, s = c.nc, c.s
    with ExitStack() as es:
        wfr = sbring(es, nc, "p_wf", [128, 8, 512], F32, 2)
        wbr = sbring(es, nc, "p_wb", [128, 8, 512], BF16, 2)
        ofr = sbring(es, nc, "p_of", [128, 512], F32, 3)
        obr = sbring(es, nc, "p_ob", [128, 512], BF16, 3)
        wv = w_ap.rearrange("(ko p) n -> p ko n", p=128)
        ev = 0
        for (col0, ncols, mode, dst, dt, scale) in segs:
            for cb in range(0, ncols, 512):
                nb = min(512, ncols - cb)
                wf = wfr.next()
                wb = wbr.next()
                s.dma("sp", wf[:, :, 0:nb], V(wv[:, :, col0 + cb:col0 + cb + nb], "w_const"))
                s.cp("pool", wb[:, :, 0:nb], wf[:, :, 0:nb])
                if mode == "tm":
                    for i in range(NT):
                        ps = c.pr.next()
                        for ko in range(8):
                            s.mm(ps[:, 0:nb], uT[:, ko, i * 128:(i + 1) * 128], wb[:, ko, 0:nb],
                                 start=(ko == 0), stop=(ko == 7))
                        ot = (ofr if dt == F32 else obr).next()
                        if ev % 2 == 0:
                            s.act(ot[:, 0:nb], ps[:, 0:nb], AF.Copy, scale=scale)
                        else:
                            s.ts("dve", ot[:, 0:nb], ps[:, 0:nb], scale, None, ALU.mult)
                        ev += 1
                        s.dma("pool", dst[i * 128:(i + 1) * 128, cb:cb + nb], ot[:, 0:nb])
                else:
                    for fb in range(0, nb, 128):
                        fn = min(128, nb - fb)
                        for tb in range(T // 512):
                            ps = c.pr.next()
                            for ko in range(8):
                                s.mm(ps[0:fn, :], wb[:, ko, fb:fb + fn], uT[:, ko, tb * 512:(tb + 1) * 512],
                                     start=(ko == 0), stop=(ko == 7))
                            ot = (ofr if dt == F32 else obr).next()
                            if ev % 2 == 0:
                                s.act(ot[0:fn, :], ps[0:fn, :], AF.Copy, scale=scale)
                            else:
                                s.ts("dve", ot[0:fn, :], ps[0:fn, :], scale, None, ALU.mult)
                            ev += 1
                            s.dma("pool", dst[cb + fb:cb + fb + fn, tb * 512:(tb + 1) * 512], ot[0:fn, :])
    s.barrier()


class Finalizer:
    def __init__(self, c, es, w_out_ap, hin, hout):
        nc = c.nc
        self.c = c
        self.wo = sb(es, nc, "f_wo", [128, 16, D], BF16)
        self.yT = sbring(es, nc, "f_yT", [128, 16, 128], BF16, 2)
        self.hr = sbring(es, nc, "f_h", [128, D], F32, 2)
        self.hin, self.hout = hin, hout
        with ExitStack() as es2:
            stg = sbring(es2, nc, "f_stg", [128, 2, D], F32, 2)
            wv = w_out_ap.rearrange("(ko p) n -> p ko n", p=128)
            for k0 in range(0, 16, 2):
                st = stg.next()
                c.s.dma("sp", st[:], V(wv[:, k0:k0 + 2, :], "w_const"))
                c.s.cp("pool", self.wo[:, k0:k0 + 2, :], st[:])
            c.s.barrier()

    def run(self, y, i):
        c, s = self.c, self.c.s
        yT = self.yT.next()
        for half in range(2):
            pt = c.ptr.next()
            for k in range(8):
                kk = half * 8 + k
                s.tr(pt[:, k, :], V(y.t[:, kk * 128:(kk + 1) * 128], y.key), c.identb[:])
            s.cp("act" if half == 0 else "dve", yT[:, half * 8:half * 8 + 8, :], pt[:, 0:8, :])
        ht = self.hr.next()
        s.dma("sp", ht[:], self.hin[i * 128:(i + 1) * 128, :])
        for n in range(2):
            ps = c.pr.next()
            for kc in range(16):
                s.mm(ps[:, :], yT[:, kc, :], self.wo[:, kc, n * 512:(n + 1) * 512],
                     start=(kc == 0), stop=(kc == 15))
            s.tt("dve", ht[:, n * 512:(n + 1) * 512], ps[:, :], ht[:, n * 512:(n + 1) * 512], ALU.add)
        s.dma("pool", self.hout[i * 128:(i + 1) * 128, :], ht[:])


def phase_final_norm(c, hin, gvec, out):
    nc, s = c.nc, c.s
    with ExitStack() as es:
        gt = sb(es, nc, "fn_g", [128, D], F32)
        xr = sbring(es, nc, "fn_x", [128, D], F32, 2)
        sq = sb(es, nc, "fn_sq", [128, D], F32)
        orr = sbring(es, nc, "fn_o", [128, D], F32, 2)
        st = sb(es, nc, "fn_st", [128, NT, 4], F32)
        s.dma("sp", gt[:], V(gvec.partition_broadcast(128), "w_const"))
        for i in range(NT):
            xt = xr.next()
            ot = orr.next()
            stv = st.sub(str(i))
            s.dma("sp", xt[:], hin[i * 128:(i + 1) * 128, :])
            s.act(sq[:], xt[:], AF.Square, accum=stv[:, i, 0:1])
            s.ts("dve", stv[:, i, 1:2], stv[:, i, 0:1], 1.0 / D, EPS, ALU.mult, ALU.add)
            s.act(stv[:, i, 2:3], stv[:, i, 1:2], AF.Sqrt)
            s.recip("dve", stv[:, i, 3:4], stv[:, i, 2:3])
            s.stt("dve", ot[:], xt[:], stv[:, i, 3:4], gt[:], ALU.mult, ALU.mult)
            s.dma("pool", out[i * 128:(i + 1) * 128, :], ot[:])
    s.barrier()


def conv_fm(c, srcT, nch_tiles, K, cw_ap, cb_ap, silu, emit):
    nc, s = c.nc, c.s
    pad = (K - 1) // 2
    with ExitStack() as es:
        cw = sb(es, nc, "cv_w", [128, nch_tiles, K], F32)
        cbias = sb(es, nc, "cv_b", [128, nch_tiles], F32)
        xr = sbring(es, nc, "cv_x", [128, L + 2 * pad], BF16, 2)
        dg = sbring(es, nc, "cv_dg", [128, K, 128], BF16, 2)
        cvr = sbring(es, nc, "cv_o", [128, 512], BF16, 3)
        s.dma("sp", cw[:], V(cw_ap.rearrange("(ct p) k -> p ct k", p=128), "w_const"))
        s.dma("sp", cbias[:], V(cb_ap.rearrange("(ct p) -> p ct", p=128), "w_const"), allow_slow_non_contiguous=True)
        for xb in xr.bufs:
            s.memset("pool", xb[:, 0:pad], 0.0)
            s.memset("pool", xb[:, L + pad:L + 2 * pad], 0.0)
        for ct in range(nch_tiles):
            d = dg.next()
            for k in range(K):
                s.ts("dve", d[:, k, :], c.identf[:], cw[:, ct, k:k + 1], None, ALU.mult)
            for b in range(BL):
                xt = xr.next()
                s.dma("sp", V(xt.t[:, pad:L + pad], xt.key + ".d"), srcT[ct * 128:(ct + 1) * 128, b * L:(b + 1) * L])
                for tb in range(L // 512):
                    ps = c.pr.next()
                    for k in range(K):
                        s.mm(ps[:, :], d[:, k, :],
                             V(xt.t[:, tb * 512 + k:tb * 512 + k + 512], (xt.key, xt.key + ".d")),
                             start=(k == 0), stop=(k == K - 1))
                    cv = cvr.next()
                    s.act(cv[:, :], ps[:, :], AF.Silu if silu else AF.Identity, bias=cbias[:, ct:ct + 1])
                    emit(ct, b, tb, cv[:, :])


def to_tm_store(c, cv, dst, row0, col0, stg_ring):
    s = c.s
    pt = c.ptr.next()
    for j in range(4):
        s.tr(pt[:, j, :], V(cv.ap[:, j * 128:(j + 1) * 128], cv.k), c.identb[:])
    st = stg_ring.next()
    s.cp("dve", st[:, 0:4, :], pt[:, 0:4, :])
    s.dma("pool", V(dst.t[row0:row0 + 512, col0:col0 + 128].rearrange("(j p) c -> p j c", p=128), dst.key),
          st[:, 0:4, :])


def layer_ssd(c, W, hin, hout):
    nc, s = c.nc, c.s
    H, P, G, N = 32, 64, 8, 128
    dr = c.dram
    z_tm = dr("ssd_z", [T, DI], BF16)
    xbcT = dr("ssd_xbcT", [4096, T], BF16)
    dt_tm = dr("ssd_dt", [T, 64], F32)
    x_tm = dr("ssd_x", [T, DI], BF16)
    B_tm = dr("ssd_B", [T, 1024], BF16)
    BT = dr("ssd_BT", [1024, T], BF16)
    CT = dr("ssd_CT", [1024, T], BF16)
    yf = dr("ssd_yf", [T, DI], F32)

    with ExitStack() as es:
        uT = sb(es, nc, "uT", [128, 8, T], BF16)
        phase_norm(c, hin, W["ssd_norm"], uT)
        phase_project(c, uT, W["ssd_w_in"], [
            (0, DI, "tm", z_tm, BF16, 1.0),
            (DI, 4096, "fm", xbcT, BF16, 1.0),
            (DI + 4096, 64, "tm", dt_tm, F32, 1.0),
        ])

    if DEBUG_STOP == "proj":
        return
    with ExitStack() as es:
        stg = sbring(es, nc, "sc_stg", [128, 4, 128], BF16, 3)

        def emit(ct, b, tb, cv):
            col = b * L + tb * 512
            if ct < 16:
                to_tm_store(c, cv, x_tm, col, ct * 128, stg)
            elif ct < 24:
                to_tm_store(c, cv, B_tm, col, (ct - 16) * 128, stg)
                s.dma("pool", BT[(ct - 16) * 128:(ct - 15) * 128, col:col + 512], cv)
            else:
                s.dma("pool", CT[(ct - 24) * 128:(ct - 23) * 128, col:col + 512], cv)

        conv_fm(c, xbcT, 32, 5, W["ssd_conv_wT"], W["ssd_conv_b"], True, emit)
    s.barrier()

    if DEBUG_STOP == "conv":
        return
    with ExitStack() as es:
        c.pr = Ring(c.pbanks[0:4])
        fin = Finalizer(c, es, W["ssd_w_out"], hin, hout)
        dtb = sb(es, nc, "ss_dtb", [128, 64], F32)
        aneg = sb(es, nc, "ss_a", [128, 64], F32)
        dsk = sb(es, nc, "ss_dsk", [128, 32], F32)
        gn = sb(es, nc, "ss_gn", [128, DI], F32)
        s.dma("sp", dtb[:], V(W["ssd_dt_bias"].rearrange("d h -> (d h)").partition_broadcast(128), "w_const"))
        s.dma("sp", aneg[:], V(W["ssd_a_log"].rearrange("d h -> (d h)").partition_broadcast(128), "w_const"))
        s.dma("sp", dsk[:], V(W["ssd_d"].partition_broadcast(128), "w_const"))
        s.dma("sp", gn[:], V(W["ssd_gnorm"].partition_broadcast(128), "w_const"))
        s.act(aneg[:], aneg[:], AF.Exp)
        s.ts("dve", aneg[:], aneg[:], -1.0, None, ALU.mult)
        xr = sbring(es, nc, "ss_x", [128, DI], BF16, 2)
        Br = sbring(es, nc, "ss_B", [128, 1024], BF16, 2)
        BTr = sbring(es, nc, "ss_BT", [128, G, 128], BF16, 2)
        CTr = sbring(es, nc, "ss_CT", [128, G, 128], BF16, 2)
        dtr = sbring(es, nc, "ss_dt", [128, 64], F32, 2)
        sm = sbring(es, nc, "ss_sm", [128, 8, 32], F32, 2)
        labc = sb(es, nc, "ss_labc", [128, H, 128], F32)
        latri = sb(es, nc, "ss_latri", [128, H, 128], F32)
        xdt = sbring(es, nc, "ss_xdt", [128, DI], BF16, 1)
        xw = sbring(es, nc, "ss_xw", [128, DI], BF16, 1)
        Er = sbring(es, nc, "ss_E", [128, 512], F32, 2)
        PTr = sbring(es, nc, "ss_PT", [128, 4, 128], BF16, 2)
        t1r = sbring(es, nc, "ss_t1", [128, 256], F32, 2)
        yacc = sbring(es, nc, "ss_yacc", [128, DI], F32, 1)
        st32 = sb(es, nc, "ss_st32", [128, G, 256], F32)
        stbf = sb(es, nc, "ss_stbf", [128, G, 256], BF16)
        zr = sbring(es, nc, "ss_z", [128, DI], BF16, 1)
        yfr = sbring(es, nc, "ss_yf", [128, DI], F32, 1)
        tmp = sb(es, nc, "ss_tmp", [128, DI], F32)
        gst = sbring(es, nc, "ss_gst", [128, 4, 8], F32, 2)
        yb = sbring(es, nc, "ss_yb", [128, DI], BF16, 1)
        allg = [str(g) for g in range(G)]

        def r3(buf, q):
            return V(buf.t[:].rearrange("p (h q) -> p h q", q=q), buf.key)

        for b in range(BL):
            for d in range(2):
                s.memset("dve", st32.subs(allg)[:], 0.0)
                s.memset("pool", stbf.subs(allg)[:], 0.0)
                tri = c.tri[:, d, :]
                order = range(NQ) if d == 0 else range(NQ - 1, -1, -1)
                for ci in order:
                    if DEBUG_STOP and DEBUG_STOP.startswith("scan") and (b * 2 + d) * NQ + (ci if d == 0 else NQ - 1 - ci) >= int(DEBUG_STOP[4:]):
                        continue
                    i = b * NQ + ci
                    r0 = i * 128
                    xt, Bt, BTt, CTt, dtt = xr.next(), Br.next(), BTr.next(), CTr.next(), dtr.next()
                    s.dma("sp", xt[:], x_tm[r0:r0 + 128, :])
                    s.dma("sp", Bt[:], B_tm[r0:r0 + 128, :])
                    s.dma("sp", BTt[:], V(BT.t[:, r0:r0 + 128].rearrange("(g n) t -> n g t", n=128), BT.key))
                    s.dma("sp", CTt[:], V(CT.t[:, r0:r0 + 128].rearrange("(g n) t -> n g t", n=128), CT.key))
                    s.dma("sp", dtt[:], dt_tm[r0:r0 + 128, :])
                    m = sm.next()
                    s.tt("dve", m[:, 0:2, :], V(dtt.t[:].rearrange("p (a h) -> p a h", a=2), dtt.key),
                         V(dtb.t[:].rearrange("p (a h) -> p a h", a=2), dtb.key), ALU.add)
                    s.act(m[:, 0:2, :], m[:, 0:2, :], AF.Exp)
                    s.act(m[:, 0:2, :], m[:, 0:2, :], AF.Ln, bias=c.one[:, 0:1])
                    dtd = m[:, d, :]
                    s.tt("dve", m[:, 2, :], dtd, aneg[:, d * 32:(d + 1) * 32], ALU.mult)
                    la = m[:, 2, :]
                    pc = c.pr.next()
                    s.mm(pc[:, 0:32], tri, la)
                    s.mm(pc[:, 32:64], c.onesf[:], la)
                    s.act(m[:, 3, :], pc[:, 0:32], AF.Exp)
                    s.cp("dve", m[:, 5, :], pc[:, 0:32])
                    s.tt("dve", m[:, 4, :], pc[:, 32:64], m[:, 5, :], ALU.subtract)
                    s.act(m[:, 4, :], m[:, 4, :], AF.Exp)
                    s.act(m[:, 6, :], pc[:, 32:64], AF.Exp)
                    s.tt("dve", m[:, 7, :], m[:, 4, :], dtd, ALU.mult)
                    xd, xwt = xdt.next(), xw.next()
                    x3 = r3(xt, P)
                    s.tt("dve", r3(xd, P), x3, bc(V(dtd.ap.unsqueeze(2), dtd.k), [128, H, P]), ALU.mult)
                    s.tt("pool", r3(xwt, P), x3, bc(V(m.t[:, 7, :].unsqueeze(2), m.key), [128, H, P]), ALU.mult)
                    s.cp("dve", labc[:], bc(V(la.ap.unsqueeze(2), la.k), [128, H, 128]))
                    s.tt("pool", latri[:], bc(V(la.ap.unsqueeze(2), la.k), [128, H, 128]),
                         bc(V(tri.ap.unsqueeze(1), tri.k), [128, H, 128]), ALU.mult)
                    pcb = c.pbanks[4:6]
                    for g in range(G):
                        s.mm(V(pcb[g // 4].t[:, (g % 4) * 128:(g % 4 + 1) * 128], pcb[g // 4].key),
                             BTt[:, g, :], CTt[:, g, :])
                    ya = yacc.next()
                    for g in range(G):
                        pa = c.pr.next()
                        for hh in range(4):
                            h = g * 4 + hh
                            o = V(pa.t[:, hh * 128:(hh + 1) * 128], pa.key)
                            s.mm(o, labc[:, h, :], tri, start=True, stop=False)
                            s.mm(o, latri[:, h, :], c.nonesf[:], start=False, stop=False)
                            s.mm(o, c.identf[:], c.negm[:, d, :], start=False, stop=True)
                        E = Er.next()
                        s.act(E[:], pa[:], AF.Exp)
                        PT = PTr.next()
                        cbv = V(pcb[g // 4].t[:, (g % 4) * 128:(g % 4 + 1) * 128].unsqueeze(1), pcb[g // 4].key)
                        s.tt("dve", PT[:], V(E.t[:].rearrange("p (a l) -> p a l", a=4), E.key),
                             bc(cbv, [128, 4, 128]), ALU.mult)
                        py = c.pr.next()
                        for hh in range(4):
                            h = g * 4 + hh
                            s.mm(V(py.t[:, hh * 64:(hh + 1) * 64], py.key), PT[:, hh, :], xd[:, h * 64:(h + 1) * 64])
                        s.mm(V(py.t[:, 256:512], py.key), CTt[:, g, :], stbf.sub(str(g))[:, g, :])
                        t1 = t1r.next()
                        s.tt("dve", V(t1.t[:].rearrange("p (a q) -> p a q", a=4), t1.key),
                             V(py.t[:, 256:512].rearrange("p (a q) -> p a q", a=4), py.key),
                             bc(V(m.t[:, 3, g * 4:(g + 1) * 4].unsqueeze(2), m.key), [128, 4, P]), ALU.mult)
                        s.tt("dve", ya[:, g * 256:(g + 1) * 256], py[:, 0:256], t1[:], ALU.add)
                        pst = c.pr.next()
                        s.mm(pst[:, 0:256], Bt[:, g * 128:(g + 1) * 128], xwt[:, g * 256:(g + 1) * 256])
                        sg = st32.sub(str(g))
                        sg3 = V(sg.t[:, g, :].rearrange("p (a q) -> p a q", a=4), sg.key)
                        s.tt("pool", sg3, sg3,
                             bc(V(m.t[:, 6, g * 4:(g + 1) * 4].unsqueeze(2), m.key), [128, 4, P]), ALU.mult)
                        s.tt("dve", sg[:, g, :], sg[:, g, :], pst[:, 0:256], ALU.add)
                        s.cp("act", stbf.sub(str(g))[:, g, :], sg[:, g, :])
                    if d == 0:
                        s.dma("pool", yf[r0:r0 + 128, :], ya[:])
                    else:
                        yft, zt = yfr.next(), zr.next()
                        s.dma("sp", yft[:], yf[r0:r0 + 128, :])
                        s.dma("sp", zt[:], z_tm[r0:r0 + 128, :])
                        s.tt("pool", yft[:], yft[:], ya[:], ALU.add)
                        s.tt("dve", r3(tmp, P), x3, bc(V(dsk.t[:].unsqueeze(2), dsk.key), [128, H, P]), ALU.mult)
                        s.tt("dve", yft[:], yft[:], tmp[:], ALU.add)
                        s.act(tmp[:], zt[:], AF.Silu)
                        s.tt("dve", yft[:], yft[:], tmp[:], ALU.mult)
                        gs = gst.next()
                        s.tt("pool", tmp[:], yft[:], yft[:], ALU.mult)
                        s.red("dve", gs[:, 0, :], r3(tmp, 256), ALU.add)
                        s.ts("dve", gs[:, 1, :], gs[:, 0, :], 1.0 / 256, EPS, ALU.mult, ALU.add)
                        s.act(gs[:, 2, :], gs[:, 1, :], AF.Sqrt)
                        s.recip("dve", gs[:, 3, :], gs[:, 2, :])
                        s.tt("dve", r3(yft, 256), r3(yft, 256),
                             bc(V(gs.t[:, 3, :].unsqueeze(2), gs.key), [128, 8, 256]), ALU.mult)
                        ybt = yb.next()
                        s.tt("dve", ybt[:], yft[:], gn[:], ALU.mult)
                        fin.run(ybt, i)
    s.barrier()
    c.pr = Ring(c.pbanks)


def layer_gla(c, W, hin, hout):
    nc, s = c.nc, c.s
    H, DK, DV = 4, 128, 512
    dr = c.dram
    q_tm = dr("gla_q", [T, 512], BF16)
    k_tm = dr("gla_k", [T, 512], BF16)
    v_tm = dr("gla_v", [T, DI], BF16)
    z_tm = dr("gla_z", [T, DI], BF16)
    glT = dr("gla_glT", [32, T], F32)
    of = dr("gla_of", [T, DI], F32)

    with ExitStack() as es:
        uT = sb(es, nc, "uT", [128, 8, T], BF16)
        phase_norm(c, hin, W["gla_norm"], uT)
        phase_project(c, uT, W["gla_w_in"], [
            (0, 512, "tm", q_tm, BF16, DK ** -0.5),
            (512, 512, "tm", k_tm, BF16, 1.0),
            (1024, DI, "tm", v_tm, BF16, 1.0),
            (3072, DI, "tm", z_tm, BF16, 1.0),
            (5120, 32, "fm", glT, F32, 1.0),
        ])

    with ExitStack() as es:
        fin = Finalizer(c, es, W["gla_w_out"], hin, hout)
        wg = sb(es, nc, "g_wg", [16, 2, 512], F32)
        bg = sb(es, nc, "g_bg", [128, 2, 512], F32)
        on = sb(es, nc, "g_on", [128, 512], F32)
        s.dma("sp", wg[:], V(W["gla_w_gate"].rearrange("d r k -> r d k"), "w_const"))
        s.dma("sp", V(bg.t[:].rearrange("p d k -> p (d k)"), bg.key),
              V(W["gla_b_gate"].rearrange("d k -> (d k)").partition_broadcast(128), "w_const"))
        s.dma("sp", on[:], V(W["gla_onorm"].partition_broadcast(128), "w_const"))
        qr = sbring(es, nc, "g_q", [128, 512], BF16, 2)
        kr = sbring(es, nc, "g_k", [128, 512], BF16, 2)
        vr = sbring(es, nc, "g_v", [128, DI], BF16, 2)
        glr = sbring(es, nc, "g_gl", [16, 128], F32, 2)
        Lp = sb(es, nc, "g_Lp", [128, 512], F32)
        Eq = sb(es, nc, "g_Eq", [128, 512], F32)
        Ek = sb(es, nc, "g_Ek", [128, 512], F32)
        Ee = sbring(es, nc, "g_Ee", [128, 4], F32, 2)
        qs = sb(es, nc, "g_qs", [128, 512], BF16)
        ks = sb(es, nc, "g_ks", [128, 512], BF16)
        qkT = sb(es, nc, "g_qkT", [128, 8, 128], BF16)
        PT = sb(es, nc, "g_PT", [128, 4, 128], BF16)
        oacc = sb(es, nc, "g_oacc", [128, DI], F32)
        S32 = sb(es, nc, "g_S32", [128, H, DV], F32)
        Sbf = sb(es, nc, "g_Sbf", [128, H, DV], BF16)
        oft = sb(es, nc, "g_of", [128, DI], F32)
        zt = sb(es, nc, "g_z", [128, DI], BF16)
        tmp = sb(es, nc, "g_tmp", [128, DI], F32)
        gst = sbring(es, nc, "g_st", [128, 4, 4], F32, 2)
        yb = sb(es, nc, "g_yb", [128, DI], BF16)
        allh = [str(h) for h in range(H)]

        def r3(buf, q):
            return V(buf.t[:].rearrange("p (h q) -> p h q", q=q), buf.key)

        for b in range(BL):
            for d in range(2):
                s.memset("dve", S32.subs(allh)[:], 0.0)
                s.memset("pool", Sbf.subs(allh)[:], 0.0)
                tri = c.tri[:, d, :]
                order = range(NQ) if d == 0 else range(NQ - 1, -1, -1)
                for ci in order:
                    i = b * NQ + ci
                    r0 = i * 128
                    qt, kt, vt, gt = qr.next(), kr.next(), vr.next(), glr.next()
                    s.dma("sp", qt[:], q_tm[r0:r0 + 128, :])
                    s.dma("sp", kt[:], k_tm[r0:r0 + 128, :])
                    s.dma("sp", vt[:], v_tm[r0:r0 + 128, :])
                    s.dma("sp", gt[:], glT[d * 16:(d + 1) * 16, r0:r0 + 128])
                    pg = c.pr.next()
                    s.mm(pg[:, :], gt[:, :], wg[:, d, :])
                    s.tt("dve", Lp[:], pg[:, :], bg[:, d, :], ALU.add)
                    s.act(Lp[:], Lp[:], AF.Exp, scale=-1.0)
                    s.act(Lp[:], Lp[:], AF.Ln, bias=c.one[:, 0:1])
                    pcum = c.pr.next()
                    s.mm(pcum[:, :], tri, Lp[:])
                    s.act(Eq[:], pcum[:, :], AF.Exp, scale=-1.0 / 16)
                    s.act(Ek[:], pcum[:, :], AF.Exp, scale=1.0 / 16)
                    ptot = c.pr.next()
                    for h in range(H):
                        s.mm(ptot[:, h:h + 1], Lp[:, h * 128:(h + 1) * 128], c.onesf[:, 0:1])
                    ee = Ee.next()
                    s.act(ee[:], ptot[:, 0:4], AF.Exp, scale=-1.0 / 16)
                    s.tt("dve", qs[:], qt[:], Eq[:], ALU.mult)
                    s.tt("pool", ks[:], kt[:], Ek[:], ALU.mult)
                    pt = c.ptr.next()
                    for h in range(H):
                        s.tr(pt[:, h, :], qs[:, h * 128:(h + 1) * 128], c.identb[:])
                        s.tr(pt[:, 4 + h, :], ks[:, h * 128:(h + 1) * 128], c.identb[:])
                    s.cp("act", qkT[:], pt[:, :, :])
                    pS = c.pr.next()
                    for h in range(H):
                        s.mm(pS[:, h * 128:(h + 1) * 128], qkT[:, 4 + h, :], qkT[:, h, :])
                    s.tt("dve", PT[:], V(pS.t[:, :].rearrange("p (a l) -> p a l", a=4), pS.key),
                         bc(V(tri.ap.unsqueeze(1), tri.k), [128, 4, 128]), ALU.mult)
                    for h in range(H):
                        po = c.pr.next()
                        s.mm(po[:, :], PT[:, h, :], vt[:, h * DV:(h + 1) * DV], start=True, stop=False)
                        s.mm(po[:, :], qkT[:, h, :], Sbf.sub(str(h))[:, h, :], start=False, stop=True)
                        s.cp("act", oacc[:, h * DV:(h + 1) * DV], po[:, :])
                        pu = c.pr.next()
                        s.mm(pu[:, :], ks[:, h * 128:(h + 1) * 128], vt[:, h * DV:(h + 1) * DV])
                        sh = S32.sub(str(h))
                        s.tt("dve", sh[:, h, :], sh[:, h, :], pu[:, :], ALU.add)
                        s.ts("pool", sh[:, h, :], sh[:, h, :], ee[:, h:h + 1], None, ALU.mult)
                        s.cp("act", Sbf.sub(str(h))[:, h, :], sh[:, h, :])
                    if d == 0:
                        s.dma("pool", of[r0:r0 + 128, :], oacc[:])
                    else:
                        s.dma("sp", oft[:], of[r0:r0 + 128, :])
                        s.dma("sp", zt[:], z_tm[r0:r0 + 128, :])
                        s.tt("pool", oft[:], oft[:], oacc[:], ALU.add)
                        gs = gst.next()
                        s.tt("pool", tmp[:], oft[:], oft[:], ALU.mult)
                        s.red("dve", gs[:, 0, :], r3(tmp, DV), ALU.add)
                        s.ts("dve", gs[:, 1, :], gs[:, 0, :], 1.0 / DV, EPS, ALU.mult, ALU.add)
                        s.act(gs[:, 2, :], gs[:, 1, :], AF.Sqrt)
                        s.recip("dve", gs[:, 3, :], gs[:, 2, :])
                        s.tt("dve", r3(oft, DV), r3(oft, DV), bc(V(gs.t[:, 3, :].unsqueeze(2), gs.key), [128, H, DV]), ALU.mult)
                        s.tt("pool", r3(oft, DV), r3(oft, DV), bc(V(on.t[:].unsqueeze(1), on.key), [128, H, DV]), ALU.mult)
                        s.act(tmp[:], zt[:], AF.Silu)
                        s.tt("dve", yb[:], oft[:], tmp[:], ALU.mult)
                        fin.run(yb, i)
    s.barrier()


def layer_mlstm(c, W, hin, hout):
    nc, s = c.nc, c.s
    H, DK, DV = 4, 256, 512
    dr = c.dram
    xmT = dr("ml_xmT", [DI, T], BF16)
    z_tm = dr("ml_z", [T, DI], BF16)
    og_tm = dr("ml_og", [T, DI], BF16)
    gt_tm = dr("ml_gates", [T, 16], F32)
    chT = dr("ml_chT", [DI, T], BF16)
    ch_tm = dr("ml_ch", [T, DI], BF16)
    qk_tm = dr("ml_qk", [T, H * 512], BF16)
    v_tm = dr("ml_v", [T, DI], BF16)
    hf = dr("ml_hf", [T, DI], F32)

    with ExitStack() as es:
        uT = sb(es, nc, "uT", [128, 8, T], BF16)
        phase_norm(c, hin, W["ml_norm"], uT)
        phase_project(c, uT, W["ml_w_in"], [
            (0, DI, "fm", xmT, BF16, 1.0),
            (DI, DI, "tm", z_tm, BF16, 1.0),
            (2 * DI, DI, "tm", og_tm, BF16, 1.0),
            (3 * DI, 16, "tm", gt_tm, F32, 1.0),
        ])

    with ExitStack() as es:
        stg = sbring(es, nc, "mc_stg", [128, 4, 128], BF16, 3)

        def emit(ct, b, tb, cv):
            col = b * L + tb * 512
            to_tm_store(c, cv, ch_tm, col, ct * 128, stg)
            s.dma("pool", chT[ct * 128:(ct + 1) * 128, col:col + 512], cv)

        conv_fm(c, xmT, 16, 5, W["ml_conv_wT"], W["ml_conv_b"], True, emit)
    s.barrier()

    with ExitStack() as es:
        wq = sb(es, nc, "mq_wq", [128, H, 4, 256], BF16)
        wk = sb(es, nc, "mq_wk", [128, H, 4, 256], BF16)
        wv = sb(es, nc, "mq_wv", [128, H, 4, 512], BF16)
        with ExitStack() as es2:
            stf = sbring(es2, nc, "mq_stg", [128, 4096], F32, 2)
            st = stf.next()
            sv = V(st.t[:].rearrange("p (h k n) -> p h k n", h=4, k=4), st.key)
            s.dma("sp", sv, V(W["ml_w_q"].rearrange("h (k p) n -> p h k n", p=128), "w_const"))
            s.cp("pool", wq[:], sv)
            st = stf.next()
            sv = V(st.t[:].rearrange("p (h k n) -> p h k n", h=4, k=4), st.key)
            s.dma("sp", sv, V(W["ml_w_k"].rearrange("h (k p) n -> p h k n", p=128), "w_const"))
            s.cp("pool", wk[:], sv)
            for hh in range(2):
                st = stf.next()
                sv = V(st.t[:].rearrange("p (h k n) -> p h k n", h=2, k=4), st.key)
                s.dma("sp", sv, V(W["ml_w_v"][hh * 2:hh * 2 + 2].rearrange("h (k p) n -> p h k n", p=128), "w_const"))
                s.cp("pool", wv[:, hh * 2:hh * 2 + 2, :, :], sv)
            s.barrier()
        chr_ = sbring(es, nc, "mq_ch", [128, 16, 128], BF16, 2)
        xmr = sbring(es, nc, "mq_xm", [128, 16, 128], BF16, 2)
        qko = sbring(es, nc, "mq_qko", [128, H * 512], BF16, 2)
        vo = sbring(es, nc, "mq_vo", [128, DI], BF16, 2)
        for i in range(NT):
            cht, xmt, qo, vot = chr_.next(), xmr.next(), qko.next(), vo.next()
            s.dma("sp", cht[:], V(chT.t[:, i * 128:(i + 1) * 128].rearrange("(j p) t -> p j t", p=128), chT.key))
            s.dma("sp", xmt[:], V(xmT.t[:, i * 128:(i + 1) * 128].rearrange("(j p) t -> p j t", p=128), xmT.key))
            for h in range(H):
                pq = c.pr.next()
                for kc in range(4):
                    s.mm(pq[:, 0:256], cht[:, h * 4 + kc, :], wq[:, h, kc, :], start=(kc == 0), stop=(kc == 3))
                for kc in range(4):
                    s.mm(pq[:, 256:512], cht[:, h * 4 + kc, :], wk[:, h, kc, :], start=(kc == 0), stop=(kc == 3))
                s.cp("act", qo[:, h * 512:(h + 1) * 512], pq[:, :])
                pv = c.pr.next()
                for kc in range(4):
                    s.mm(pv[:, :], xmt[:, h * 4 + kc, :], wv[:, h, kc, :], start=(kc == 0), stop=(kc == 3))
                s.cp("dve", vot[:, h * 512:(h + 1) * 512], pv[:, :])
            s.dma("pool", qk_tm[i * 128:(i + 1) * 128, :], qo[:])
            s.dma("pool", v_tm[i * 128:(i + 1) * 128, :], vot[:])
    s.barrier()

    with ExitStack() as es:
        c.pr = Ring(c.pbanks[0:5])
        psm = c.pbanks[5]
        fin = Finalizer(c, es, W["ml_w_out"], hin, hout)
        gb = sb(es, nc, "m_gb", [128, 16], F32)
        on = sb(es, nc, "m_on", [128, 512], F32)
        skp = sb(es, nc, "m_skip", [128, DI], F32)
        s.dma("sp", gb[:], V(W["ml_gate_b"].rearrange("a b c -> (a b c)").partition_broadcast(128), "w_const"))
        s.dma("sp", on[:], V(W["ml_onorm"].partition_broadcast(128), "w_const"))
        s.dma("sp", skp[:], V(W["ml_skip"].rearrange("h d -> (h d)").partition_broadcast(128), "w_const"))
        onesb = sb(es, nc, "m_onesb", [128, 2], BF16)
        s.memset("dve", onesb[:], 1.0)
        qkr = sbring(es, nc, "m_qk", [128, H, 512], BF16, 2)
        vr = sbring(es, nc, "m_v", [128, DI], BF16, 2)
        gr = sbring(es, nc, "m_g", [128, 16], F32, 2)
        sm = sbring(es, nc, "m_sm", [128, 8, 4], F32, 2)
        qs = sb(es, nc, "m_qs", [128, H, 256], BF16)
        ks = sb(es, nc, "m_ks", [128, H, 256], BF16)
        qT = sb(es, nc, "m_qT", [128, 8, 128], BF16)
        kT = sb(es, nc, "m_kT", [128, 8, 128], BF16)
        PT = sb(es, nc, "m_PT", [128, 4, 128], BF16)
        hacc = sb(es, nc, "m_hacc", [128, DI], F32)
        C32 = sb(es, nc, "m_C32", [128, 8, DV], F32)
        Cbf = sb(es, nc, "m_Cbf", [128, 8, DV], BF16)
        n32 = sb(es, nc, "m_n32", [128, 8], F32)
        nbf = sb(es, nc, "m_nbf", [128, 8], BF16)
        hft = sb(es, nc, "m_hf", [128, DI], F32)
        ogt = sb(es, nc, "m_og", [128, DI], BF16)
        zt = sb(es, nc, "m_z", [128, DI], BF16)
        cht = sb(es, nc, "m_ch", [128, DI], BF16)
        tmp = sb(es, nc, "m_tmp", [128, DI], F32)
        gst = sbring(es, nc, "m_st", [128, 4, 4], F32, 2)
        yb = sb(es, nc, "m_yb", [128, DI], BF16)
        allj = [str(j) for j in range(8)]

        def r3(buf, q):
            return V(buf.t[:].rearrange("p (h q) -> p h q", q=q), buf.key)

        for b in range(BL):
            for d in range(2):
                s.memset("dve", C32.subs(allj)[:], 0.0)
                s.memset("pool", Cbf.subs(allj)[:], 0.0)
                s.memset("dve", n32[:], 0.0)
                s.memset("pool", nbf[:], 0.0)
                tri = c.tri[:, d, :]
                order = range(NQ) if d == 0 else range(NQ - 1, -1, -1)
                for ci in order:
                    i = b * NQ + ci
                    r0 = i * 128
                    qkt, vt, gt = qkr.next(), vr.next(), gr.next()
                    s.dma("sp", V(qkt.t[:].rearrange("p h n -> p (h n)"), qkt.key), qk_tm[r0:r0 + 128, :])
                    s.dma("sp", vt[:], v_tm[r0:r0 + 128, :])
                    s.dma("sp", gt[:], gt_tm[r0:r0 + 128, :])
                    m = sm.next()
                    s.tt("dve", gt[:], gt[:], gb[:], ALU.add)
                    ig = gt[:, d * 8:d * 8 + 4]
                    fr = gt[:, d * 8 + 4:d * 8 + 8]
                    s.act(m[:, 0, :], fr, AF.Exp, scale=-1.0)
                    s.act(m[:, 0, :], m[:, 0, :], AF.Ln, bias=c.one[:, 0:1])
                    s.mm(psm[:, 0:4], tri, m[:, 0, :])
                    s.mm(psm[:, 4:8], c.onesf[:], m[:, 0, :])
                    s.act(m[:, 1, :], psm[:, 0:4], AF.Exp, scale=-1.0)
                    s.tt("dve", m[:, 2, :], psm[:, 0:4], ig, ALU.add)
                    s.act(m[:, 2, :], m[:, 2, :], AF.Exp, bias=c.one[:, 3:4])
                    s.act(m[:, 3, :], psm[:, 4:8], AF.Exp, scale=-1.0)
                    s.tt("dve", qs[:], V(qkt.t[:, :, 0:256], qkt.key),
                         bc(V(m.t[:, 1, :].unsqueeze(2), m.key), [128, H, 256]), ALU.mult)
                    s.tt("pool", ks[:], V(qkt.t[:, :, 256:512], qkt.key),
                         bc(V(m.t[:, 2, :].unsqueeze(2), m.key), [128, H, 256]), ALU.mult)
                    pt = c.ptr.next()
                    for j in range(8):
                        s.tr(pt[:, j, :], qs[:, j // 2, (j % 2) * 128:(j % 2 + 1) * 128], c.identb[:])
                    s.cp("act", qT[:], pt[:, :, :])
                    pt = c.ptr.next()
                    for j in range(8):
                        s.tr(pt[:, j, :], ks[:, j // 2, (j % 2) * 128:(j % 2 + 1) * 128], c.identb[:])
                    s.cp("dve", kT[:], pt[:, :, :])
                    pS = c.pr.next()
                    for h in range(H):
                        for kc in range(2):
                            s.mm(pS[:, h * 128:(h + 1) * 128], kT[:, h * 2 + kc, :], qT[:, h * 2 + kc, :],
                                 start=(kc == 0), stop=(kc == 1))
                    s.tt("dve", PT[:], V(pS.t[:, :].rearrange("p (a l) -> p a l", a=4), pS.key),
                         bc(V(tri.ap.unsqueeze(1), tri.k), [128, 4, 128]), ALU.mult)
                    for h in range(H):
                        s.mm(psm[:, 8 + h:9 + h], PT[:, h, :], onesb[:, 0:1], start=True, stop=False)
                        for kc in range(2):
                            s.mm(psm[:, 8 + h:9 + h], qT[:, h * 2 + kc, :], nbf[:, h * 2 + kc:h * 2 + kc + 1],
                                 start=False, stop=(kc == 1))
                    s.act(m[:, 4, :], psm[:, 8:12], AF.Abs)
                    s.ts("dve", m[:, 4, :], m[:, 4, :], 1.0, None, ALU.max)
                    s.recip("dve", m[:, 5, :], m[:, 4, :])
                    for h in range(H):
                        po = c.pr.next()
                        s.mm(po[:, :], PT[:, h, :], vt[:, h * DV:(h + 1) * DV], start=True, stop=False)
                        for kc in range(2):
                            s.mm(po[:, :], qT[:, h * 2 + kc, :], Cbf.sub(str(h * 2 + kc))[:, h * 2 + kc, :],
                                 start=False, stop=(kc == 1))
                        s.act(hacc[:, h * DV:(h + 1) * DV], po[:, :], AF.Copy, scale=m[:, 5, h:h + 1])
                        for kc in range(2):
                            j = h * 2 + kc
                            pu = c.pr.next()
                            s.mm(pu[:, :], ks[:, h, kc * 128:(kc + 1) * 128], vt[:, h * DV:(h + 1) * DV])
                            s.mm(psm[:, 16 + j:17 + j], ks[:, h, kc * 128:(kc + 1) * 128], onesb[:, 0:1])
                            cj = C32.sub(str(j))
                            s.tt("dve", cj[:, j, :], cj[:, j, :], pu[:, :], ALU.add)
                            s.ts("pool", cj[:, j, :], cj[:, j, :], m[:, 3, h:h + 1], None, ALU.mult)
                            s.cp("act", Cbf.sub(str(j))[:, j, :], cj[:, j, :])
                    s.tt("dve", n32[:], n32[:], psm[:, 16:24], ALU.add)
                    s.tt("dve", V(n32.t[:].rearrange("p (h k) -> p h k", k=2), n32.key),
                         V(n32.t[:].rearrange("p (h k) -> p h k", k=2), n32.key),
                         bc(V(m.t[:, 3, :].unsqueeze(2), m.key), [128, H, 2]), ALU.mult)
                    s.cp("dve", nbf[:], n32[:])
                    if d == 0:
                        s.dma("pool", hf[r0:r0 + 128, :], hacc[:])
                    else:
                        s.dma("sp", hft[:], hf[r0:r0 + 128, :])
                        s.dma("sp", ogt[:], og_tm[r0:r0 + 128, :])
                        s.dma("sp", zt[:], z_tm[r0:r0 + 128, :])
                        s.dma("sp", cht[:], ch_tm[r0:r0 + 128, :])
                        s.tt("pool", hft[:], hft[:], hacc[:], ALU.add)
                        s.act(tmp[:], ogt[:], AF.Sigmoid)
                        s.tt("dve", hft[:], hft[:], tmp[:], ALU.mult)
                        gs = gst.next()
                        s.tt("pool", tmp[:], hft[:], hft[:], ALU.mult)
                        s.red("dve", gs[:, 0, :], r3(tmp, DV), ALU.add)
                        s.ts("dve", gs[:, 1, :], gs[:, 0, :], 1.0 / DV, EPS, ALU.mult, ALU.add)
                        s.act(gs[:, 2, :], gs[:, 1, :], AF.Sqrt)
                        s.recip("dve", gs[:, 3, :], gs[:, 2, :])
                        s.tt("dve", r3(hft, DV), r3(hft, DV), bc(V(gs.t[:, 3, :].unsqueeze(2), gs.key), [128, H, DV]), ALU.mult)
                        s.tt("pool", r3(hft, DV), r3(hft, DV), bc(V(on.t[:].unsqueeze(1), on.key), [128, H, DV]), ALU.mult)
                        s.tt("dve", tmp[:], cht[:], skp[:], ALU.mult)
                        s.tt("dve", hft[:], hft[:], tmp[:], ALU.add)
                        s.act(tmp[:], zt[:], AF.Silu)
                        s.tt("dve", yb[:], hft[:], tmp[:], ALU.mult)
                        fin.run(yb, i)
    s.barrier()
    c.pr = Ring(c.pbanks)


def load_mat(c, dst, src_ap):
    v = src_ap.rearrange("(st p) f -> p st f", p=128)
    for q in range(4):
        c.s.dma("sp", dst[:, q * 4:(q + 1) * 4, :], V(v[:, q * 4:(q + 1) * 4, :], "w_const"))


def layer_hyena(c, W, hin, hout):
    nc, s = c.nc, c.s
    C = c.C
    dr = c.dram
    vxT = dr("hy_vxT", [3 * DI, T], BF16)
    z_tm = dr("hy_z", [T, DI], BF16)
    sig = [dr("hy_v", [T, DI], BF16), dr("hy_x1", [T, DI], BF16), dr("hy_x2", [T, DI], BF16)]
    y1_tm = dr("hy_y1", [T, DI], BF16)
    y2_tm = dr("hy_y2", [T, DI], BF16)
    Kf = dr("hy_Kf", [2, 2, L, DI], BF16)
    Zd = dr("hy_Z", [BL, 2, L, DI], BF16)

    with ExitStack() as es:
        uT = sb(es, nc, "uT", [128, 8, T], BF16)
        phase_norm(c, hin, W["hy_norm"], uT)
        phase_project(c, uT, W["hy_w_in"], [
            (0, 3 * DI, "fm", vxT, BF16, 1.0),
            (3 * DI, DI, "tm", z_tm, BF16, 1.0),
        ])

    with ExitStack() as es:
        stg = sbring(es, nc, "hc_stg", [128, 4, 128], BF16, 3)

        def emit(ct, b, tb, cv):
            to_tm_store(c, cv, sig[ct // 16], b * L + tb * 512, (ct % 16) * 128, stg)

        conv_fm(c, vxT, 48, 3, W["hy_conv_wT"], W["hy_conv_b"], False, emit)
    s.barrier()

    def fwd_transform(Cm, Sn, o, ysrc):
        with ExitStack() as es:
            yr = sbring(es, nc, "hf_y", [128, 16, 512], BF16, 2)
            kr = sbring(es, nc, "hf_k", [128, 2, 512], BF16, 2)
            yre = sb(es, nc, "hf_yre", [128, 512], F32)
            yim = sb(es, nc, "hf_yim", [128, 512], F32)
            t1 = sb(es, nc, "hf_t1", [128, 512], F32)
            t2 = sb(es, nc, "hf_t2", [128, 512], F32)
            t3 = sb(es, nc, "hf_t3", [128, 512], F32)
            t4 = sb(es, nc, "hf_t4", [128, 512], F32)
            zr = sbring(es, nc, "hf_z", [128, 2, 512], BF16, 2)
            for b in range(BL):
                for cb in range(4):
                    yt = yr.next()
                    cs = slice(cb * 512, (cb + 1) * 512)
                    s.dma("sp", yt[:], V(ysrc.t[b * L:(b + 1) * L, cs].rearrange("(st p) c -> p st c", p=128), ysrc.key))
                    for ft in range(16):
                        fs = slice(ft * 128, (ft + 1) * 128)
                        kt = kr.next()
                        s.dma("sp", kt[:], V(Kf.t[o, :, fs, cs].rearrange("a p c -> p a c"), Kf.key))
                        pre, pim = c.pr.next(), c.pr.next()
                        for st in range(16):
                            s.mm(pre[:, :], Cm[:, st, fs], yt[:, st, :], start=(st == 0), stop=(st == 15))
                        for st in range(16):
                            s.mm(pim[:, :], Sn[:, st, fs], yt[:, st, :], start=(st == 0), stop=(st == 15))
                        s.cp("act", yre[:], pre[:, :])
                        s.cp("act", yim[:], pim[:, :])
                        s.tt("dve", t1[:], yre[:], kt[:, 0, :], ALU.mult)
                        s.tt("dve", t2[:], yim[:], kt[:, 1, :], ALU.mult)
                        s.tt("pool", t3[:], yre[:], kt[:, 1, :], ALU.mult)
                        s.tt("pool", t4[:], yim[:], kt[:, 0, :], ALU.mult)
                        zt = zr.next()
                        s.tt("dve", zt[:, 0, :], t1[:], t2[:], ALU.subtract)
                        s.tt("pool", zt[:, 1, :], t3[:], t4[:], ALU.add)
                        s.dma("pool", V(Zd.t[b, :, fs, cs].rearrange("a p c -> p a c"), Zd.key), zt[:])
        s.barrier()

    def inv_transform(o, ysrc, xg_src, ydst):
        with ExitStack() as es:
            CI = sb(es, nc, "hi_CI", [128, 16, L], BF16)
            SI = sb(es, nc, "hi_SI", [128, 16, L], BF16)
            load_mat(c, CI, C["c_CI"])
            load_mat(c, SI, C["c_SI"])
            dbc = sb(es, nc, "hi_d", [128, DI], F32)
            s.dma("sp", dbc[:], V(W["hy_d"][o].partition_broadcast(128), "w_const"))
            zb = sb(es, nc, "hi_zb", [128, 2, 16, 512], BF16)
            ypr = sbring(es, nc, "hi_yp", [128, 512], BF16, 2)
            xgr = sbring(es, nc, "hi_xg", [128, 512], BF16, 2)
            zzr = sbring(es, nc, "hi_zz", [128, 512], BF16, 2)
            ta = sbring(es, nc, "hi_ta", [128, 512], F32, 2)
            tb_ = sbring(es, nc, "hi_tb", [128, 512], F32, 2)
            yo = sbring(es, nc, "hi_yo", [128, 512], BF16, 2)
            for b in range(BL):
                for cb in range(4):
                    cs = slice(cb * 512, (cb + 1) * 512)
                    for a in range(2):
                        for q in range(2):
                            s.dma("sp", zb[:, a, q * 8:(q + 1) * 8, :],
                                  V(Zd.t[b, a, q * 1024:(q + 1) * 1024, cs].rearrange("(ft p) c -> p ft c", p=128), Zd.key))
                    for tt in range(16):
                        ts_ = slice(tt * 128, (tt + 1) * 128)
                        rows = slice(b * L + tt * 128, b * L + (tt + 1) * 128)
                        po = c.pr.next()
                        for ft in range(16):
                            s.mm(po[:, :], CI[:, ft, ts_], zb[:, 0, ft, :], start=(ft == 0), stop=False)
                        for ft in range(16):
                            s.mm(po[:, :], SI[:, ft, ts_], zb[:, 1, ft, :], start=False, stop=(ft == 15))
                        yp, xg = ypr.next(), xgr.next()
                        s.dma("sp", yp[:], ysrc[rows, cs])
                        s.dma("sp", xg[:], xg_src[rows, cs])
                        t_a = ta.next()
                        s.tt("dve", t_a[:], yp[:], dbc[:, cs], ALU.mult)
                        s.tt("dve", t_a[:], t_a[:], po[:, :], ALU.add)
                        yot = yo.next()
                        if o == 0:
                            s.tt("pool", yot[:], t_a[:], xg[:], ALU.mult)
                        else:
                            zz, t_b = zzr.next(), tb_.next()
                            s.dma("sp", zz[:], z_tm[rows, cs])
                            s.act(t_b[:], zz[:], AF.Silu)
                            s.tt("pool", t_a[:], t_a[:], xg[:], ALU.mult)
                            s.tt("dve", yot[:], t_a[:], t_b[:], ALU.mult)
                        s.dma("pool", ydst[rows, cs], yot[:])
        s.barrier()

    with ExitStack() as es:
        Cm = sb(es, nc, "hy_Cm", [128, 16, L], BF16)
        Sn = sb(es, nc, "hy_Sn", [128, 16, L], BF16)
        load_mat(c, Cm, C["c_Cm"])
        load_mat(c, Sn, C["c_Sn"])
        with ExitStack() as es2:
            hA = sb(es2, nc, "hm_hA", [64, L], F32)
            hB = sb(es2, nc, "hm_hB", [64, L], F32)
            with ExitStack() as es3:
                feats = sb(es3, nc, "hm_feats", [33, L], F32)
                w1 = sb(es3, nc, "hm_w1", [33, 64], F32)
                wh = sb(es3, nc, "hm_wh", [64, 2, 64], F32)
                prm = sb(es3, nc, "hm_prm", [64, 8], F32)
                tr_ = sb(es3, nc, "hm_t", [64, 512], F32)
                tki = sb(es3, nc, "hm_ki", [64, 512], mybir.dt.int32)
                tkf = sb(es3, nc, "hm_kf", [64, 512], F32)
                s.dma("sp", feats[:], V(C["c_featsT"], "w_const"))
                s.dma("sp", w1[:], V(W["hy_ffn_w_in"], "w_const"))
                s.dma("sp", wh[:], V(W["hy_ffn_w_hid"].rearrange("j a b -> a j b"), "w_const"))
                s.dma("sp", prm[:, 0:1], V(W["hy_ffn_b_in"].rearrange("(p o) -> p o", o=1), "w_const"))
                s.dma("sp", prm[:, 1:3], V(W["hy_ffn_b_hidT"], "w_const"))
                s.dma("sp", prm[:, 3:6], V(W["hy_ffn_freqT"], "w_const"))
                cur, nxt = hA, hB
                for layer in range(3):
                    for blk in range(4):
                        bs = slice(blk * 512, (blk + 1) * 512)
                        ps = c.pr.next()
                        if layer == 0:
                            s.mm(ps[0:64, :], w1[:, :], feats[:, bs])
                            dst = cur
                        else:
                            s.mm(ps[0:64, :], wh[:, layer - 1, :], cur[:, bs])
                            dst = nxt
                        s.ts("dve", tr_[:], ps[0:64, :], prm[:, layer:layer + 1], prm[:, 3 + layer:4 + layer], ALU.add, ALU.mult)
                        s.ts("dve", tr_[:], tr_[:], 1.0 / (2.0 * PI), 8.5, ALU.mult, ALU.add)
                        s.cp("dve", tki[:], tr_[:])
                        s.cp("dve", tkf[:], tki[:])
                        s.tt("dve", tr_[:], tr_[:], tkf[:], ALU.subtract)
                        s.ts("dve", tkf[:], tr_[:], 0.0, None, ALU.is_lt)
                        s.tt("dve", tr_[:], tr_[:], tkf[:], ALU.add)
                        s.act(dst[:, bs], tr_[:], AF.Sin, bias=c.one[0:64, 1:2], scale=2.0 * PI)
                    if layer > 0:
                        cur, nxt = nxt, cur
                h3 = cur
                s.barrier()
            dlt = sb(es2, nc, "hm_dlt", [128, DI], F32)
            ngt = sb(es2, nc, "hm_negt", [128, 16], F32)
            s.dma("sp", dlt[:], V(C["c_deltas"].partition_broadcast(128), "w_const"))
            s.dma("sp", ngt[:], V(C["c_negt"], "w_const"))
            wor = sbring(es2, nc, "hm_wo", [64, 2, 256], F32, 2)
            Ar = sb(es2, nc, "hm_A", [128, 16, 256], BF16)
            Br_ = sb(es2, nc, "hm_B", [128, 16, 256], BF16)
            dec = sbring(es2, nc, "hm_dec", [128, 256], F32, 2)
            hbs = sbring(es2, nc, "hm_hb", [128, 256], F32, 2)
            sa = sbring(es2, nc, "hm_sa", [128, 256], F32, 2)
            sbm = sbring(es2, nc, "hm_sb", [128, 256], F32, 2)
            ko = sbring(es2, nc, "hm_ko", [128, 512], BF16, 2)
            wov = W["hy_ffn_w_out"]
            for o in range(2):
                for cb in range(8):
                    cs = slice(cb * 256, (cb + 1) * 256)
                    wo = wor.next()
                    for dd in range(2):
                        c0 = dd * 2 * DI + o * DI + cb * 256
                        s.dma("sp", wo[:, dd, :], V(wov[:, c0:c0 + 256], "w_const"))
                    for tt in range(16):
                        ps = c.pr.next()
                        s.mm(ps[:, 0:256], h3[:, tt * 128:(tt + 1) * 128], wo[:, 0, :])
                        s.mm(ps[:, 256:512], h3[:, tt * 128:(tt + 1) * 128], wo[:, 1, :])
                        dc, hb_, a_, b_ = dec.next(), hbs.next(), sa.next(), sbm.next()
                        s.act(dc[:], dlt[:, cs], AF.Exp, scale=ngt[:, tt:tt + 1])
                        s.cp("dve", hb_[:], ps[:, 256:512])
                        if tt == 0:
                            s.memset("dve", hb_[0:1, :], 0.0)
                        s.tt("dve", a_[:], ps[:, 0:256], hb_[:], ALU.add)
                        s.tt("dve", b_[:], ps[:, 0:256], hb_[:], ALU.subtract)
                        s.tt("pool", Ar[:, tt, :], a_[:], dc[:], ALU.mult)
                        s.tt("pool", Br_[:, tt, :], b_[:], dc[:], ALU.mult)
                    for ft in range(16):
                        fs = slice(ft * 128, (ft + 1) * 128)
                        pk = c.pr.next()
                        for tt in range(16):
                            s.mm(pk[:, 0:256], Cm[:, tt, fs], Ar[:, tt, :], start=(tt == 0), stop=(tt == 15))
                        for tt in range(16):
                            s.mm(pk[:, 256:512], Sn[:, tt, fs], Br_[:, tt, :], start=(tt == 0), stop=(tt == 15))
                        kt = ko.next()
                        s.cp("act", kt[:], pk[:, :])
                        s.dma("pool", V(Kf.t[o, :, fs, cs].rearrange("a p c -> p a c"), Kf.key),
                              V(kt.t[:].rearrange("p (a c) -> p a c", a=2), kt.key))
            s.barrier()
        fwd_transform(Cm, Sn, 0, sig[0])
    inv_transform(0, sig[0], sig[1], y1_tm)
    with ExitStack() as es:
        Cm = sb(es, nc, "hy_Cm2", [128, 16, L], BF16)
        Sn = sb(es, nc, "hy_Sn2", [128, 16, L], BF16)
        load_mat(c, Cm, C["c_Cm"])
        load_mat(c, Sn, C["c_Sn"])
        fwd_transform(Cm, Sn, 1, y1_tm)
    inv_transform(1, y1_tm, sig[2], y2_tm)

    with ExitStack() as es:
        fin = Finalizer(c, es, W["hy_w_out"], hin, hout)
        yr = sbring(es, nc, "ho_y", [128, DI], BF16, 2)
        for i in range(NT):
            yt = yr.next()
            s.dma("sp", yt[:], y2_tm[i * 128:(i + 1) * 128, :])
            fin.run(yt, i)
    s.barrier()


N_IMPL = 4
DEBUG_STOP = None


def host_constants():
    cst = {}
    cst["c_identb"] = np.eye(128, dtype=np.float32).astype(ml_dtypes.bfloat16)
    cst["c_identf"] = np.eye(128, dtype=np.float32)
    sidx = np.arange(128)[:, None]
    lidx = np.arange(128)[None, :]
    tri = np.stack([(sidx <= lidx), (sidx >= lidx)], axis=1).astype(np.float32)
    cst["c_tri"] = tri
    cst["c_negm"] = ((1.0 - tri) * -1.0e5).astype(np.float32)
    sI = np.arange(L, dtype=np.int64)[:, None]
    fI = np.arange(L, dtype=np.int64)[None, :]
    ph = ((2 * fI + 1) * sI) % (4 * L)
    th = (2.0 * np.pi / (4 * L)) * ph.astype(np.float64)
    cm = np.cos(th)
    sn = np.sin(th)
    bf = ml_dtypes.bfloat16
    cst["c_Cm"] = cm.astype(np.float32).astype(bf)
    cst["c_Sn"] = (-sn).astype(np.float32).astype(bf)
    cst["c_CI"] = np.ascontiguousarray((cm.T / L)).astype(np.float32).astype(bf)
    cst["c_SI"] = np.ascontiguousarray((-sn.T / L)).astype(np.float32).astype(bf)
    t = np.linspace(0.0, 1.0, L, dtype=np.float32)[:, None]
    pos = np.arange(L, dtype=np.float32)[:, None]
    bands = np.linspace(1e-4, 15.0, 16, dtype=np.float32)[None]
    ang = (np.float32(2.0 * math.pi / L) * pos * bands).astype(np.float32)
    feats = np.concatenate([t, np.cos(ang), -np.sin(ang)], axis=-1).astype(np.float32)
    cst["c_featsT"] = np.ascontiguousarray(feats.T)
    max_decay = math.log(1e-2) / 0.3
    min_decay = math.log(1e-2) / 1.5
    cst["c_deltas"] = np.abs(np.linspace(min_decay, max_decay, DI, dtype=np.float32)).astype(np.float32)
    cst["c_negt"] = np.ascontiguousarray(-(t[:, 0].reshape(16, 128).T)).astype(np.float32)
    return cst


def host_prepare(inputs):
    W = {}
    for k, v in inputs.items():
        if k in ("x", "final_norm"):
            continue
        W[k] = np.ascontiguousarray(v[0])
    W["final_norm"] = np.ascontiguousarray(inputs["final_norm"])
    W["ssd_conv_wT"] = np.ascontiguousarray(W.pop("ssd_conv_w").T)
    W["ml_conv_wT"] = np.ascontiguousarray(W.pop("ml_conv_w").T)
    W["hy_conv_wT"] = np.ascontiguousarray(W.pop("hy_conv_w").T)
    W["hy_ffn_b_hidT"] = np.ascontiguousarray(W.pop("hy_ffn_b_hid").T)
    W["hy_ffn_freqT"] = np.ascontiguousarray(W.pop("hy_ffn_freq").T)
    return W


def build_program(wshapes, cshapes, n_layers=4, final_norm=True):
    n_layers = min(n_layers, N_IMPL)
    nc = bass.Bass("TRN2", target_bir_lowering=False)
    c = Ctx()
    c.nc = nc
    c.s = Sched(nc)
    s = c.s
    x_in = Buf(nc.dram_tensor("x", [T, D], F32, kind="ExternalInput").ap(), "x_in")
    out = Buf(nc.dram_tensor("out", [T, D], F32, kind="ExternalOutput").ap(), "out")
    W = {k: nc.dram_tensor(k, list(shp), F32 if dt == np.float32 else BF16, kind="ExternalInput").ap()
         for k, (shp, dt) in wshapes.items()}
    C = {k: nc.dram_tensor(k, list(shp), F32 if dt == np.float32 else BF16, kind="ExternalInput").ap()
         for k, (shp, dt) in cshapes.items()}

    def dram(name, shape, dt):
        return Buf(nc.dram_tensor(name, shape, dt, kind="Internal").ap(), name)

    c.dram = dram
    c.C = C
    hA = dram("hA", [T, D], F32)
    hB = dram("hB", [T, D], F32)

    with ExitStack() as es:
        c.identb = sb(es, nc, "k_identb", [128, 128], BF16)
        c.identf = sb(es, nc, "k_identf", [128, 128], F32)
        c.tri = sb(es, nc, "k_tri", [128, 2, 128], F32)
        c.negm = sb(es, nc, "k_negm", [128, 2, 128], F32)
        c.onesf = sb(es, nc, "k_onesf", [128, 128], F32)
        c.nonesf = sb(es, nc, "k_nonesf", [128, 128], F32)
        c.one = sb(es, nc, "k_one", [128, 4], F32)
        s.dma("sp", c.identb[:], V(C["c_identb"], "w_const"))
        s.dma("sp", c.identf[:], V(C["c_identf"], "w_const"))
        s.dma("sp", c.tri[:], V(C["c_tri"], "w_const"))
        s.dma("sp", c.negm[:], V(C["c_negm"], "w_const"))
        s.memset("dve", c.onesf[:], 1.0)
        s.memset("dve", c.nonesf[:], -1.0)
        s.memset("dve", c.one[:, 0:1], 1.0)
        s.memset("dve", c.one[:, 1:2], -PI)
        s.memset("dve", c.one[:, 2:3], 0.0)
        s.memset("dve", c.one[:, 3:4], math.log(1.0 / 16.0))
        pbanks = [Buf(es.enter_context(nc.psum_tensor(f"ps{i}", [128, 512], F32)), f"ps{i}") for i in range(6)]
        tbanks = [Buf(es.enter_context(nc.psum_tensor(f"pt{i}", [128, 8, 128], BF16)), f"pt{i}") for i in range(2)]
        c.pbanks = pbanks
        c.pr = Ring(pbanks)
        c.ptr = Ring(tbanks)
        s.barrier()
        c.identb.key = c.identf.key = c.tri.key = c.negm.key = "konst"
        c.onesf.key = c.nonesf.key = c.one.key = "konst"

        layers = [layer_ssd, layer_gla, layer_hyena, layer_mlstm]
        hs = [x_in, hA, hB, hA, hB]
        hcur = x_in
        for li in range(n_layers):
            hnext = hA if (li % 2 == 0) else hB
            layers[li](c, W, hcur, hnext)
            hcur = hnext
        if final_norm:
            phase_final_norm(c, hcur, W["final_norm"], out)
        else:
            with ExitStack() as es2:
                cr = sbring(es2, nc, "cp_x", [128, D], F32, 2)
                for i in range(NT):
                    t = cr.next()
                    s.dma("sp", t[:], hcur[i * 128:(i + 1) * 128, :])
                    s.dma("pool", out[i * 128:(i + 1) * 128, :], t[:])
            s.barrier()
    return nc


_CACHE = {}


def kernel(**inputs):
    x = np.ascontiguousarray(inputs["x"], dtype=np.float32)
    W = host_prepare(inputs)
    Cst = host_constants()
    wshapes = {k: (v.shape, v.dtype.type if v.dtype != ml_dtypes.bfloat16 else "bf16") for k, v in W.items()}
    cshapes = {k: (v.shape, v.dtype.type if v.dtype != ml_dtypes.bfloat16 else "bf16") for k, v in Cst.items()}
    nc = build_program(wshapes, cshapes)
    in_maps = []
    for i in range(NCORES):
        m = {"x": x[i * BL:(i + 1) * BL].reshape(T, D)}
        m.update(W)
        m.update(Cst)
        in_maps.append(m)
    res = run_bass_kernel_spmd(nc, in_maps, core_ids=list(range(NCORES)))
    outs = [r["out"].reshape(BL, L, D) for r in res.results]
    return np.concatenate(outs, axis=0).astype(np.float32)
```

```python
import math
from contextlib import ExitStack
import numpy as np
import ml_dtypes
import concourse.bass as bass
import concourse.mybir as mybir
from concourse.bass_utils import run_bass_kernel_spmd

F32 = mybir.dt.float32
BF16 = mybir.dt.bfloat16
AF = mybir.ActivationFunctionType
ALU = mybir.AluOpType
AX = mybir.AxisListType

NCORES = 8
BL = 2
L = 2048
T = BL * L
D = 1024
DI = 2048
Q = 128
NQ = L // Q
NT = T // 128
EPS = 1e-6
EPOCH = 30000
PI = math.pi


def _kt(k):
    if isinstance(k, str):
        return (k,)
    return tuple(k)


class V:
    __slots__ = ("ap", "k")

    def __init__(self, ap, k):
        self.ap = ap
        self.k = _kt(k)


class Buf:
    def __init__(self, t, key):
        self.t = t
        self.key = key

    def __getitem__(self, idx):
        return V(self.t[idx], self.key)

    def sub(self, sub):
        return Buf(self.t, f"{self.key}.{sub}")

    def subs(self, subs):
        return Buf(self.t, tuple(f"{self.key}.{x}" for x in subs))


class Sched:
    def __init__(self, nc):
        self.nc = nc
        self.eng = {"pe": nc.tensor, "dve": nc.vector, "act": nc.scalar,
                    "pool": nc.gpsimd, "sp": nc.sync}
        self.nsem = 0
        self.esem, self.ecnt = {}, {}
        for e in self.eng:
            self._new_epoch(e)
        self.seen = {e: {} for e in self.eng}
        self.res = {}
        self.dsem = {}
        self.dfree = []
        self.ninst = 0

    def _alloc(self):
        self.nsem += 1
        return self.nc.alloc_semaphore(name=f"s{self.nsem}")

    def _new_epoch(self, e):
        self.esem[e] = self._alloc()
        self.ecnt[e] = 0

    def _wait(self, e, tok):
        sem, val = tok
        sid = id(sem)
        if self.seen[e].get(sid, 0) >= val:
            return
        self.eng[e].wait_ge(sem, val)
        self.seen[e][sid] = val

    def _deps(self, e, reads, writes, pe_accum=False, dsem=None):
        for k in reads:
            r = self.res.get(k)
            if r and r["w"] is not None:
                self._wait(e, r["w"])
        for k in writes:
            r = self.res.get(k)
            if r:
                w = r["w"]
                if w is not None:
                    skip = (pe_accum and r["we"] == "pe") or (dsem is not None and w[0] is dsem)
                    if not skip:
                        self._wait(e, w)
                for t in r["r"]:
                    self._wait(e, t)

    def _record(self, e, tok, reads, writes):
        for k in reads:
            r = self.res.setdefault(k, {"w": None, "r": [], "we": None})
            r["r"] = [t for t in r["r"] if t[0] is not tok[0]] + [tok]
        for k in writes:
            self.res[k] = {"w": tok, "r": [], "we": e}

    def op(self, e, fn, reads=(), writes=(), pe_accum=False):
        reads = [k for ks in reads if ks is not None for k in _kt(ks)]
        writes = [k for ks in writes for k in _kt(ks)]
        self._deps(e, reads, writes, pe_accum)
        if self.ecnt[e] >= EPOCH:
            self._new_epoch(e)
        inst = fn()
        self.ecnt[e] += 1
        inst.then_inc(self.esem[e], 1)
        self._record(e, (self.esem[e], self.ecnt[e]), reads, writes)
        self.ninst += 1
        return inst

    def dma(self, e, out, in_, **kw):
        reads, writes = list(in_.k), list(out.k)
        sk = out.k[0]
        if sk not in self.dsem:
            if self.dfree:
                self.dsem[sk] = self.dfree.pop()
            else:
                self.dsem[sk] = [self._alloc(), 0]
        ds = self.dsem[sk]
        self._deps(e, reads, writes, dsem=ds[0])
        inst = self.eng[e].dma_start(out=out.ap, in_=in_.ap, **kw)
        ds[1] += 16
        inst.then_inc(ds[0], 16)
        self._record(e, (ds[0], ds[1]), reads, writes)
        self.ninst += 1
        return inst

    def barrier(self):
        toks = [(self.esem[e], self.ecnt[e]) for e in self.eng if self.ecnt[e] > 0]
        toks += [(d[0], d[1]) for d in self.dsem.values() if d[1] > 0]
        for e in self.eng:
            for t in toks:
                if t[0] is not self.esem[e]:
                    self._wait(e, t)
        for d in self.dsem.values():
            if d[1] < 40000:
                self.dfree.append(d)
        self.dsem = {}
        self.res = {}

    def mm(self, out, lhsT, rhs, start=True, stop=True):
        nc = self.nc
        return self.op("pe", lambda: nc.tensor.matmul(out.ap, lhsT=lhsT.ap, rhs=rhs.ap, start=start, stop=stop),
                       reads=[lhsT.k, rhs.k], writes=[out.k], pe_accum=True)

    def tr(self, out, in_, ident):
        nc = self.nc
        return self.op("pe", lambda: nc.tensor.transpose(out.ap, in_.ap, ident.ap),
                       reads=[in_.k, ident.k], writes=[out.k], pe_accum=True)

    def act(self, out, in_, func, bias=None, scale=None, accum=None):
        nc = self.nc
        kw = {}
        rd = [in_.k]
        wr = [out.k]
        if bias is not None:
            if isinstance(bias, V):
                kw["bias"] = bias.ap
                rd.append(bias.k)
            else:
                kw["bias"] = bias
        if scale is not None:
            if isinstance(scale, V):
                kw["scale"] = scale.ap
                rd.append(scale.k)
            else:
                kw["scale"] = scale
        if accum is not None:
            kw["accum_out"] = accum.ap
            wr.append(accum.k)
        return self.op("act", lambda: nc.scalar.activation(out=out.ap, in_=in_.ap, func=func, **kw),
                       reads=rd, writes=wr)

    def _e(self, e):
        return self.eng[e]

    def tt(self, e, out, in0, in1, op):
        return self.op(e, lambda: self._e(e).tensor_tensor(out=out.ap, in0=in0.ap, in1=in1.ap, op=op),
                       reads=[in0.k, in1.k], writes=[out.k])

    def ts(self, e, out, in0, s1, s2, op0, op1=None):
        rd = [in0.k]
        a1 = s1.ap if isinstance(s1, V) else s1
        a2 = s2.ap if isinstance(s2, V) else s2
        if isinstance(s1, V):
            rd.append(s1.k)
        if isinstance(s2, V):
            rd.append(s2.k)
        if op1 is None:
            return self.op(e, lambda: self._e(e).tensor_scalar(out=out.ap, in0=in0.ap, scalar1=a1, scalar2=None, op0=op0),
                           reads=rd, writes=[out.k])
        return self.op(e, lambda: self._e(e).tensor_scalar(out=out.ap, in0=in0.ap, scalar1=a1, scalar2=a2, op0=op0, op1=op1),
                       reads=rd, writes=[out.k])

    def stt(self, e, out, in0, scalar, in1, op0, op1):
        rd = [in0.k, in1.k]
        a = scalar.ap if isinstance(scalar, V) else scalar
        if isinstance(scalar, V):
            rd.append(scalar.k)
        return self.op(e, lambda: self._e(e).scalar_tensor_tensor(out=out.ap, in0=in0.ap, scalar=a, in1=in1.ap, op0=op0, op1=op1),
                       reads=rd, writes=[out.k])

    def cp(self, e, out, in_):
        if e == "act":
            return self.op(e, lambda: self.nc.scalar.copy(out=out.ap, in_=in_.ap), reads=[in_.k], writes=[out.k])
        return self.op(e, lambda: self._e(e).tensor_copy(out=out.ap, in_=in_.ap), reads=[in_.k], writes=[out.k])

    def memset(self, e, out, val):
        return self.op(e, lambda: self._e(e).memset(out.ap, val), reads=[], writes=[out.k])

    def red(self, e, out, in_, op, axis=AX.X):
        return self.op(e, lambda: self._e(e).tensor_reduce(out=out.ap, in_=in_.ap, axis=axis, op=op),
                       reads=[in_.k], writes=[out.k])

    def recip(self, e, out, in_):
        return self.op(e, lambda: self._e(e).reciprocal(out=out.ap, in_=in_.ap), reads=[in_.k], writes=[out.k])


class Ctx:
    pass


class Ring:
    def __init__(self, bufs):
        self.bufs = bufs
        self.i = 0

    def next(self):
        b = self.bufs[self.i % len(self.bufs)]
        self.i += 1
        return b


_UID = [0]


def sb(es, nc, name, shape, dt):
    _UID[0] += 1
    name = f"{name}_{_UID[0]}"
    t = es.enter_context(nc.sbuf_tensor(name, shape, dt))
    return Buf(t, name)


def sbring(es, nc, name, shape, dt, n):
    return Ring([sb(es, nc, f"{name}{i}", shape, dt) for i in range(n)])


def bc(v, shape):
    return V(v.ap.to_broadcast(shape), v.k)


def phase_norm(c, hin, gvec, uT):
    nc, s = c.nc, c.s
    with ExitStack() as es:
        gt = sb(es, nc, "n_g", [128, D], F32)
        xr = sbring(es, nc, "n_x", [128, D], F32, 2)
        sq = sb(es, nc, "n_sq", [128, D], F32)
        ur = sbring(es, nc, "n_u", [128, D], BF16, 2)
        st = sb(es, nc, "n_st", [128, NT, 4], F32)
        s.dma("sp", gt[:], V(gvec.partition_broadcast(128), "w_const"))
        for i in range(NT):
            xt = xr.next()
            ub = ur.next()
            stv = st.sub(str(i))
            s.dma("sp", xt[:], hin[i * 128:(i + 1) * 128, :])
            s.act(sq[:], xt[:], AF.Square, accum=stv[:, i, 0:1])
            s.ts("dve", stv[:, i, 1:2], stv[:, i, 0:1], 1.0 / D, EPS, ALU.mult, ALU.add)
            s.act(stv[:, i, 2:3], stv[:, i, 1:2], AF.Sqrt)
            s.recip("dve", stv[:, i, 3:4], stv[:, i, 2:3])
            s.stt("dve", ub[:], xt[:], stv[:, i, 3:4], gt[:], ALU.mult, ALU.mult)
            pt = c.ptr.next()
            for k in range(8):
                s.tr(pt[:, k, :], ub[:, k * 128:(k + 1) * 128], c.identb[:])
            s.cp("act", uT[:, :, i * 128:(i + 1) * 128], pt[:, 0:8, :])
    s.barrier()


def phase_project(c, uT, w_ap, segs):
    nc, s = c.nc, c.s
    with ExitStack() as es:
        wfr = sbring(es, nc, "p_wf", [128, 8, 512], F32, 2)
        wbr = sbring(es, nc, "p_wb", [128, 8, 512], BF16, 2)
        ofr = sbring(es, nc, "p_of", [128, 512], F32, 3)
        obr = sbring(es, nc, "p_ob", [128, 512], BF16, 3)
        wv = w_ap.rearrange("(ko p) n -> p ko n", p=128)
        ev = 0
        for (col0, ncols, mode, dst, dt, scale) in segs:
            for cb in range(0, ncols, 512):
                nb = min(512, ncols - cb)
                wf = wfr.next()
                wb = wbr.next()
                s.dma("sp", wf[:, :, 0:nb], V(wv[:, :, col0 + cb:col0 + cb + nb], "w_const"))
                s.cp("dve", wb.sub("a")[:, 0:4, 0:nb], wf[:, 0:4, 0:nb])
                s.cp("act", wb.sub("b")[:, 4:8, 0:nb], wf[:, 4:8, 0:nb])
                if mode == "tm":
                    for i in range(NT):
                        ps = c.pr.next()
                        for ko in range(8):
                            s.mm(ps[:, 0:nb], uT[:, ko, i * 128:(i + 1) * 128], wb.sub("a" if ko < 4 else "b")[:, ko, 0:nb],
                                 start=(ko == 0), stop=(ko == 7))
                        ot = (ofr if dt == F32 else obr).next()
                        if ev % 2 == 0:
                            s.act(ot[:, 0:nb], ps[:, 0:nb], AF.Copy, scale=scale)
                        else:
                            s.ts("dve", ot[:, 0:nb], ps[:, 0:nb], scale, None, ALU.mult)
                        ev += 1
                        s.dma(STQ, dst[i * 128:(i + 1) * 128, cb:cb + nb], ot[:, 0:nb])
                else:
                    for fb in range(0, nb, 128):
                        fn = min(128, nb - fb)
                        for tb in range(T // 512):
                            ps = c.pr.next()
                            for ko in range(8):
                                s.mm(ps[0:fn, :], wb.sub("a" if ko < 4 else "b")[:, ko, fb:fb + fn], uT[:, ko, tb * 512:(tb + 1) * 512],
                                     start=(ko == 0), stop=(ko == 7))
                            ot = (ofr if dt == F32 else obr).next()
                            if ev % 2 == 0:
                                s.act(ot[0:fn, :], ps[0:fn, :], AF.Copy, scale=scale)
                            else:
                                s.ts("dve", ot[0:fn, :], ps[0:fn, :], scale, None, ALU.mult)
                            ev += 1
                            s.dma(STQ, dst[cb + fb:cb + fb + fn, tb * 512:(tb + 1) * 512], ot[0:fn, :])
    s.barrier()


class Finalizer:
    def __init__(self, c, es, w_out_ap, hin, hout):
        nc = c.nc
        self.c = c
        self.wo = sb(es, nc, "f_wo", [128, 16, D], BF16)
        self.yT = sbring(es, nc, "f_yT", [128, 16, 128], BF16, 2)
        self.hr = sbring(es, nc, "f_h", [128, D], F32, 2)
        self.hin, self.hout = hin, hout
        with ExitStack() as es2:
            stg = sbring(es2, nc, "f_stg", [128, 2, D], F32, 2)
            wv = w_out_ap.rearrange("(ko p) n -> p ko n", p=128)
            for k0 in range(0, 16, 2):
                st = stg.next()
                c.s.dma("sp", st[:], V(wv[:, k0:k0 + 2, :], "w_const"))
                c.s.cp("dve" if (k0 // 2) % 2 == 0 else "act", self.wo[:, k0:k0 + 2, :], st[:])
            c.s.barrier()

    def run(self, y, i):
        c, s = self.c, self.c.s
        yT = self.yT.next()
        for half in range(2):
            pt = c.ptr.next()
            for k in range(8):
                kk = half * 8 + k
                s.tr(pt[:, k, :], V(y.t[:, kk * 128:(kk + 1) * 128], y.key), c.identb[:])
            s.cp("act" if half == 0 else "dve", yT[:, half * 8:half * 8 + 8, :], pt[:, 0:8, :])
        ht = self.hr.next()
        s.dma("sp", ht[:], self.hin[i * 128:(i + 1) * 128, :])
        for n in range(2):
            ps = c.pr.next()
            for kc in range(16):
                s.mm(ps[:, :], yT[:, kc, :], self.wo[:, kc, n * 512:(n + 1) * 512],
                     start=(kc == 0), stop=(kc == 15))
            s.tt("dve", ht[:, n * 512:(n + 1) * 512], ps[:, :], ht[:, n * 512:(n + 1) * 512], ALU.add)
        s.dma(STQ, self.hout[i * 128:(i + 1) * 128, :], ht[:])


def phase_final_norm(c, hin, gvec, out):
    nc, s = c.nc, c.s
    with ExitStack() as es:
        gt = sb(es, nc, "fn_g", [128, D], F32)
        xr = sbring(es, nc, "fn_x", [128, D], F32, 2)
        sq = sb(es, nc, "fn_sq", [128, D], F32)
        orr = sbring(es, nc, "fn_o", [128, D], F32, 2)
        st = sb(es, nc, "fn_st", [128, NT, 4], F32)
        s.dma("sp", gt[:], V(gvec.partition_broadcast(128), "w_const"))
        for i in range(NT):
            xt = xr.next()
            ot = orr.next()
            stv = st.sub(str(i))
            s.dma("sp", xt[:], hin[i * 128:(i + 1) * 128, :])
            s.act(sq[:], xt[:], AF.Square, accum=stv[:, i, 0:1])
            s.ts("dve", stv[:, i, 1:2], stv[:, i, 0:1], 1.0 / D, EPS, ALU.mult, ALU.add)
            s.act(stv[:, i, 2:3], stv[:, i, 1:2], AF.Sqrt)
            s.recip("dve", stv[:, i, 3:4], stv[:, i, 2:3])
            s.stt("dve", ot[:], xt[:], stv[:, i, 3:4], gt[:], ALU.mult, ALU.mult)
            s.dma(STQ, out[i * 128:(i + 1) * 128, :], ot[:])
    s.barrier()


def conv_fm(c, srcT, nch_tiles, K, cw_ap, cb_ap, silu, emit):
    nc, s = c.nc, c.s
    pad = (K - 1) // 2
    with ExitStack() as es:
        cw = sb(es, nc, "cv_w", [128, nch_tiles, K], F32)
        cbias = sb(es, nc, "cv_b", [128, nch_tiles], F32)
        xr = sbring(es, nc, "cv_x", [128, L + 2 * pad], BF16, 2)
        dg = sbring(es, nc, "cv_dg", [128, K, 128], BF16, 2)
        cvr = sbring(es, nc, "cv_o", [128, 512], BF16, 3)
        s.dma("sp", cw[:], V(cw_ap.rearrange("(ct p) k -> p ct k", p=128), "w_const"))
        s.dma("sp", cbias[:], V(cb_ap.rearrange("(ct p) -> p ct", p=128), "w_const"), allow_slow_non_contiguous=True)
        for xb in xr.bufs:
            s.memset("pool", xb[:, 0:pad], 0.0)
            s.memset("pool", xb[:, L + pad:L + 2 * pad], 0.0)
        for ct in range(nch_tiles):
            d = dg.next()
            for k in range(K):
                s.ts("dve", d[:, k, :], c.identf[:], cw[:, ct, k:k + 1], None, ALU.mult)
            for b in range(BL):
                xt = xr.next()
                s.dma("sp", V(xt.t[:, pad:L + pad], xt.key + ".d"), srcT[ct * 128:(ct + 1) * 128, b * L:(b + 1) * L])
                for tb in range(L // 512):
                    ps = c.pr.next()
                    for k in range(K):
                        s.mm(ps[:, :], d[:, k, :],
                             V(xt.t[:, tb * 512 + k:tb * 512 + k + 512], (xt.key, xt.key + ".d")),
                             start=(k == 0), stop=(k == K - 1))
                    cv = cvr.next()
                    s.act(cv[:, :], ps[:, :], AF.Silu if silu else AF.Identity, bias=cbias[:, ct:ct + 1])
                    emit(ct, b, tb, cv[:, :])


def to_tm_store(c, cv, dst, row0, col0, stg_ring):
    s = c.s
    pt = c.ptr.next()
    for j in range(4):
        s.tr(pt[:, j, :], V(cv.ap[:, j * 128:(j + 1) * 128], cv.k), c.identb[:])
    st = stg_ring.next()
    s.cp("dve", st[:, 0:4, :], pt[:, 0:4, :])
    s.dma(STQ, V(dst.t[row0:row0 + 512, col0:col0 + 128].rearrange("(j p) c -> p j c", p=128), dst.key),
          st[:, 0:4, :])


def layer_ssd(c, W, hin, hout):
    nc, s = c.nc, c.s
    H, P, G, N = 32, 64, 8, 128
    dr = c.dram
    z_tm = dr("ssd_z", [T, DI], BF16)
    xbcT = dr("ssd_xbcT", [4096, T], BF16)
    dt_tm = dr("ssd_dt", [T, 64], F32)
    x_tm = dr("ssd_x", [T, DI], BF16)
    B_tm = dr("ssd_B", [T, 1024], BF16)
    BT = dr("ssd_BT", [1024, T], BF16)
    CT = dr("ssd_CT", [1024, T], BF16)
    yf = dr("ssd_yf", [T, DI], F32)

    with ExitStack() as es:
        uT = sb(es, nc, "uT", [128, 8, T], BF16)
        phase_norm(c, hin, W["ssd_norm"], uT)
        phase_project(c, uT, W["ssd_w_in"], [
            (0, DI, "tm", z_tm, BF16, 1.0),
            (DI, 4096, "fm", xbcT, BF16, 1.0),
            (DI + 4096, 64, "tm", dt_tm, F32, 1.0),
        ])

    if DEBUG_STOP == "proj":
        return
    with ExitStack() as es:
        stg = sbring(es, nc, "sc_stg", [128, 4, 128], BF16, 3)

        def emit(ct, b, tb, cv):
            col = b * L + tb * 512
            if ct < 16:
                to_tm_store(c, cv, x_tm, col, ct * 128, stg)
            elif ct < 24:
                to_tm_store(c, cv, B_tm, col, (ct - 16) * 128, stg)
                s.dma(STQ, BT[(ct - 16) * 128:(ct - 15) * 128, col:col + 512], cv)
            else:
                s.dma(STQ, CT[(ct - 24) * 128:(ct - 23) * 128, col:col + 512], cv)

        conv_fm(c, xbcT, 32, 5, W["ssd_conv_wT"], W["ssd_conv_b"], True, emit)
    s.barrier()

    if DEBUG_STOP == "conv":
        return
    with ExitStack() as es:
        c.pr = Ring(c.pbanks[0:4])
        fin = Finalizer(c, es, W["ssd_w_out"], hin, hout)
        dtb = sb(es, nc, "ss_dtb", [128, 64], F32)
        aneg = sb(es, nc, "ss_a", [128, 64], F32)
        dsk = sb(es, nc, "ss_dsk", [128, 32], F32)
        gn = sb(es, nc, "ss_gn", [128, DI], F32)
        s.dma("sp", dtb[:], V(W["ssd_dt_bias"].rearrange("d h -> (d h)").partition_broadcast(128), "w_const"))
        s.dma("sp", aneg[:], V(W["ssd_a_log"].rearrange("d h -> (d h)").partition_broadcast(128), "w_const"))
        s.dma("sp", dsk[:], V(W["ssd_d"].partition_broadcast(128), "w_const"))
        s.dma("sp", gn[:], V(W["ssd_gnorm"].partition_broadcast(128), "w_const"))
        s.act(aneg[:], aneg[:], AF.Exp)
        s.ts("dve", aneg[:], aneg[:], -1.0, None, ALU.mult)
        xr = sbring(es, nc, "ss_x", [128, DI], BF16, 2)
        Br = sbring(es, nc, "ss_B", [128, 1024], BF16, 2)
        BTr = sbring(es, nc, "ss_BT", [128, G, 128], BF16, 2)
        CTr = sbring(es, nc, "ss_CT", [128, G, 128], BF16, 2)
        dtr = sbring(es, nc, "ss_dt", [128, 64], F32, 2)
        sm = sbring(es, nc, "ss_sm", [128, 8, 32], F32, 2)
        labcr = sbring(es, nc, "ss_labc", [128, H, 128], F32, 1)
        xdt = sbring(es, nc, "ss_xdt", [128, DI], BF16, 2)
        xw = sbring(es, nc, "ss_xw", [128, DI], BF16, 2)
        negm4 = sb(es, nc, "ss_negm4", [128, 2, 4, 128], F32)
        for d in range(2):
            s.cp("dve", negm4[:, d, :, :], bc(V(c.negm.t[:, d, :].unsqueeze(1), c.negm.key), [128, 4, 128]))
        Er = sbring(es, nc, "ss_E", [128, 512], F32, 2)
        PTr = sbring(es, nc, "ss_PT", [128, 4, 128], BF16, 2)
        t1r = sbring(es, nc, "ss_t1", [128, 256], F32, 2)
        yacc = sbring(es, nc, "ss_yacc", [128, DI], F32, 2)
        st32 = sb(es, nc, "ss_st32", [128, G, 256], F32)
        stbf = sb(es, nc, "ss_stbf", [128, G, 256], BF16)
        zr = sbring(es, nc, "ss_z", [128, DI], BF16, 1)
        yfr = sbring(es, nc, "ss_yf", [128, DI], F32, 1)
        tmp = sb(es, nc, "ss_tmp", [128, DI], F32)
        gst = sbring(es, nc, "ss_gst", [128, 4, 8], F32, 2)
        yb = sbring(es, nc, "ss_yb", [128, DI], BF16, 1)
        allg = [str(g) for g in range(G)]

        def r3(buf, q):
            return V(buf.t[:].rearrange("p (h q) -> p h q", q=q), buf.key)

        for b in range(BL):
            for d in range(2):
                s.memset("dve", st32.subs(allg)[:], 0.0)
                s.memset("pool", stbf.subs(allg)[:], 0.0)
                tri = c.tri[:, d, :]
                order = range(NQ) if d == 0 else range(NQ - 1, -1, -1)
                for ci in order:
                    if DEBUG_STOP and DEBUG_STOP.startswith("scan") and (b * 2 + d) * NQ + (ci if d == 0 else NQ - 1 - ci) >= int(DEBUG_STOP[4:]):
                        continue
                    i = b * NQ + ci
                    r0 = i * 128
                    xt, Bt, BTt, CTt, dtt = xr.next(), Br.next(), BTr.next(), CTr.next(), dtr.next()
                    s.dma("sp", xt[:], x_tm[r0:r0 + 128, :])
                    s.dma("sp", Bt[:], B_tm[r0:r0 + 128, :])
                    s.dma("sp", BTt[:], V(BT.t[:, r0:r0 + 128].rearrange("(g n) t -> n g t", n=128), BT.key))
                    s.dma("sp", CTt[:], V(CT.t[:, r0:r0 + 128].rearrange("(g n) t -> n g t", n=128), CT.key))
                    s.dma("sp", dtt[:], dt_tm[r0:r0 + 128, :])
                    m = sm.next()
                    s.tt("dve", m[:, 0:2, :], V(dtt.t[:].rearrange("p (a h) -> p a h", a=2), dtt.key),
                         V(dtb.t[:].rearrange("p (a h) -> p a h", a=2), dtb.key), ALU.add)
                    s.act(m[:, 0:2, :], m[:, 0:2, :], AF.Exp)
                    s.act(m[:, 0:2, :], m[:, 0:2, :], AF.Ln, bias=c.one[:, 0:1])
                    dtd = m[:, d, :]
                    s.tt("dve", m[:, 2, :], dtd, aneg[:, d * 32:(d + 1) * 32], ALU.mult)
                    la = m[:, 2, :]
                    pc = c.pr.next()
                    s.mm(pc[:, 0:32], tri, la)
                    s.mm(pc[:, 32:64], c.onesf[:], la)
                    s.act(m[:, 3, :], pc[:, 0:32], AF.Exp)
                    s.cp("dve", m[:, 5, :], pc[:, 0:32])
                    s.tt("dve", m[:, 4, :], pc[:, 32:64], m[:, 5, :], ALU.subtract)
                    s.act(m[:, 4, :], m[:, 4, :], AF.Exp)
                    s.act(m[:, 6, :], pc[:, 32:64], AF.Exp)
                    s.tt("dve", m[:, 7, :], m[:, 4, :], dtd, ALU.mult)
                    xd, xwt = xdt.next(), xw.next()
                    x3 = r3(xt, P)
                    s.tt("dve", r3(xd, P), x3, bc(V(dtd.ap.unsqueeze(2), dtd.k), [128, H, P]), ALU.mult)
                    s.tt("dve", r3(xwt, P), x3, bc(V(m.t[:, 7, :].unsqueeze(2), m.key), [128, H, P]), ALU.mult)
                    labc = labcr.next()
                    s.cp("act", labc[:], bc(V(la.ap.unsqueeze(2), la.k), [128, H, 128]))
                    s.ts("dve", m[:, 5, :], m[:, 5, :], -1.0, None, ALU.mult)
                    pcb = c.pbanks[4:6]
                    for g in range(G):
                        s.mm(V(pcb[g // 4].t[:, (g % 4) * 128:(g % 4 + 1) * 128], pcb[g // 4].key),
                             BTt[:, g, :], CTt[:, g, :])
                    ya = yacc.next()
                    for g in range(G):
                        pa = c.pr.next()
                        s.mm(pa[:, :], c.identf[:], V(negm4.t[:, d, :, :].rearrange("p a l -> p (a l)"), negm4.key),
                             start=True, stop=False)
                        for hh in range(4):
                            h = g * 4 + hh
                            o = V(pa.t[:, hh * 128:(hh + 1) * 128], pa.key)
                            s.mm(o, labc[:, h, :], tri, start=False, stop=(hh == 3))
                        E = Er.next()
                        for hh in range(4):
                            h = g * 4 + hh
                            s.act(E[:, hh * 128:(hh + 1) * 128], V(pa.t[:, hh * 128:(hh + 1) * 128], pa.key), AF.Exp,
                                  bias=m[:, 5, h:h + 1])
                        PT = PTr.next()
                        cbv = V(pcb[g // 4].t[:, (g % 4) * 128:(g % 4 + 1) * 128].unsqueeze(1), pcb[g // 4].key)
                        s.tt("dve", PT[:], V(E.t[:].rearrange("p (a l) -> p a l", a=4), E.key),
                             bc(cbv, [128, 4, 128]), ALU.mult)
                        py = c.pr.next()
                        for hh in range(4):
                            h = g * 4 + hh
                            s.mm(V(py.t[:, hh * 64:(hh + 1) * 64], py.key), PT[:, hh, :], xd[:, h * 64:(h + 1) * 64])
                        s.mm(V(py.t[:, 256:512], py.key), CTt[:, g, :], stbf.sub(str(g))[:, g, :])
                        t1 = t1r.next()
                        s.tt("dve", V(t1.t[:].rearrange("p (a q) -> p a q", a=4), t1.key),
                             V(py.t[:, 256:512].rearrange("p (a q) -> p a q", a=4), py.key),
                             bc(V(m.t[:, 3, g * 4:(g + 1) * 4].unsqueeze(2), m.key), [128, 4, P]), ALU.mult)
                        s.tt("dve", ya[:, g * 256:(g + 1) * 256], py[:, 0:256], t1[:], ALU.add)
                        pst = c.pr.next()
                        s.mm(pst[:, 0:256], Bt[:, g * 128:(g + 1) * 128], xwt[:, g * 256:(g + 1) * 256])
                        sg = st32.sub(str(g))
                        sg3 = V(sg.t[:, g, :].rearrange("p (a q) -> p a q", a=4), sg.key)
                        s.tt("dve", sg3, sg3,
                             bc(V(m.t[:, 6, g * 4:(g + 1) * 4].unsqueeze(2), m.key), [128, 4, P]), ALU.mult)
                        s.tt("dve", sg[:, g, :], sg[:, g, :], pst[:, 0:256], ALU.add)
                        s.cp("act", stbf.sub(str(g))[:, g, :], sg[:, g, :])
                    if d == 0:
                        s.dma(STQ, yf[r0:r0 + 128, :], ya[:])
                    else:
                        yft, zt = yfr.next(), zr.next()
                        s.dma("sp", yft[:], yf[r0:r0 + 128, :])
                        s.dma("sp", zt[:], z_tm[r0:r0 + 128, :])
                        s.tt("dve", yft[:], yft[:], ya[:], ALU.add)
                        s.tt("dve", r3(tmp, P), x3, bc(V(dsk.t[:].unsqueeze(2), dsk.key), [128, H, P]), ALU.mult)
                        s.tt("dve", yft[:], yft[:], tmp[:], ALU.add)
                        s.act(tmp[:], zt[:], AF.Silu)
                        s.tt("dve", yft[:], yft[:], tmp[:], ALU.mult)
                        gs = gst.next()
                        s.act(tmp[:], yft[:], AF.Square)
                        s.red("dve", gs[:, 0, :], r3(tmp, 256), ALU.add)
                        s.ts("dve", gs[:, 1, :], gs[:, 0, :], 1.0 / 256, EPS, ALU.mult, ALU.add)
                        s.act(gs[:, 2, :], gs[:, 1, :], AF.Sqrt)
                        s.recip("dve", gs[:, 3, :], gs[:, 2, :])
                        s.tt("dve", r3(yft, 256), r3(yft, 256),
                             bc(V(gs.t[:, 3, :].unsqueeze(2), gs.key), [128, 8, 256]), ALU.mult)
                        ybt = yb.next()
                        s.tt("dve", ybt[:], yft[:], gn[:], ALU.mult)
                        fin.run(ybt, i)
    s.barrier()
    c.pr = Ring(c.pbanks)


def layer_gla(c, W, hin, hout):
    nc, s = c.nc, c.s
    H, DK, DV = 4, 128, 512
    dr = c.dram
    q_tm = dr("gla_q", [T, 512], BF16)
    k_tm = dr("gla_k", [T, 512], BF16)
    v_tm = dr("gla_v", [T, DI], BF16)
    z_tm = dr("gla_z", [T, DI], BF16)
    glT = dr("gla_glT", [32, T], F32)
    of = dr("gla_of", [T, DI], F32)

    with ExitStack() as es:
        uT = sb(es, nc, "uT", [128, 8, T], BF16)
        phase_norm(c, hin, W["gla_norm"], uT)
        phase_project(c, uT, W["gla_w_in"], [
            (0, 512, "tm", q_tm, BF16, DK ** -0.5),
            (512, 512, "tm", k_tm, BF16, 1.0),
            (1024, DI, "tm", v_tm, BF16, 1.0),
            (3072, DI, "tm", z_tm, BF16, 1.0),
            (5120, 32, "fm", glT, F32, 1.0),
        ])

    with ExitStack() as es:
        fin = Finalizer(c, es, W["gla_w_out"], hin, hout)
        wg = sb(es, nc, "g_wg", [16, 2, 512], F32)
        bg = sb(es, nc, "g_bg", [128, 2, 512], F32)
        on = sb(es, nc, "g_on", [128, 512], F32)
        s.dma("sp", wg[:], V(W["gla_w_gate"].rearrange("d r k -> r d k"), "w_const"))
        s.dma("sp", V(bg.t[:].rearrange("p d k -> p (d k)"), bg.key),
              V(W["gla_b_gate"].rearrange("d k -> (d k)").partition_broadcast(128), "w_const"))
        s.dma("sp", on[:], V(W["gla_onorm"].partition_broadcast(128), "w_const"))
        qr = sbring(es, nc, "g_q", [128, 512], BF16, 2)
        kr = sbring(es, nc, "g_k", [128, 512], BF16, 2)
        vr = sbring(es, nc, "g_v", [128, DI], BF16, 2)
        glr = sbring(es, nc, "g_gl", [16, 128], F32, 2)
        Lpr = sbring(es, nc, "g_Lp", [128, 512], F32, 2)
        Eqr = sbring(es, nc, "g_Eq", [128, 512], F32, 2)
        Ekr = sbring(es, nc, "g_Ek", [128, 512], F32, 2)
        Ee = sbring(es, nc, "g_Ee", [128, 4], F32, 2)
        qsr = sbring(es, nc, "g_qs", [128, 512], BF16, 2)
        ksr = sbring(es, nc, "g_ks", [128, 512], BF16, 2)
        qkTr = sbring(es, nc, "g_qkT", [128, 8, 128], BF16, 2)
        PTr = sbring(es, nc, "g_PT", [128, 4, 128], BF16, 2)
        oaccr = sbring(es, nc, "g_oacc", [128, DI], F32, 2)
        S32 = sb(es, nc, "g_S32", [128, H, DV], F32)
        Sbf = sb(es, nc, "g_Sbf", [128, H, DV], BF16)
        oft = sb(es, nc, "g_of", [128, DI], F32)
        zt = sb(es, nc, "g_z", [128, DI], BF16)
        tmp = sb(es, nc, "g_tmp", [128, DI], F32)
        gst = sbring(es, nc, "g_st", [128, 4, 4], F32, 2)
        yb = sb(es, nc, "g_yb", [128, DI], BF16)
        allh = [str(h) for h in range(H)]

        def r3(buf, q):
            return V(buf.t[:].rearrange("p (h q) -> p h q", q=q), buf.key)

        for b in range(BL):
            for d in range(2):
                s.memset("dve", S32.subs(allh)[:], 0.0)
                s.memset("pool", Sbf.subs(allh)[:], 0.0)
                tri = c.tri[:, d, :]
                order = range(NQ) if d == 0 else range(NQ - 1, -1, -1)
                for ci in order:
                    i = b * NQ + ci
                    r0 = i * 128
                    qt, kt, vt, gt = qr.next(), kr.next(), vr.next(), glr.next()
                    Lp, Eq, Ek, qs, ks = Lpr.next(), Eqr.next(), Ekr.next(), qsr.next(), ksr.next()
                    qkT, PT, oacc = qkTr.next(), PTr.next(), oaccr.next()
                    s.dma("sp", qt[:], q_tm[r0:r0 + 128, :])
                    s.dma("sp", kt[:], k_tm[r0:r0 + 128, :])
                    s.dma("sp", vt[:], v_tm[r0:r0 + 128, :])
                    s.dma("sp", gt[:], glT[d * 16:(d + 1) * 16, r0:r0 + 128])
                    pg = c.pr.next()
                    s.mm(pg[:, :], gt[:, :], wg[:, d, :])
                    s.tt("dve", Lp[:], pg[:, :], bg[:, d, :], ALU.add)
                    s.act(Lp[:], Lp[:], AF.Exp, scale=-1.0)
                    s.act(Lp[:], Lp[:], AF.Ln, bias=c.one[:, 0:1])
                    pcum = c.pr.next()
                    s.mm(pcum[:, :], tri, Lp[:])
                    s.act(Eq[:], pcum[:, :], AF.Exp, scale=-1.0 / 16)
                    s.act(Ek[:], pcum[:, :], AF.Exp, scale=1.0 / 16)
                    ptot = c.pr.next()
                    for h in range(H):
                        s.mm(ptot[:, h:h + 1], Lp[:, h * 128:(h + 1) * 128], c.onesf[:, 0:1])
                    ee = Ee.next()
                    s.act(ee[:], ptot[:, 0:4], AF.Exp, scale=-1.0 / 16)
                    s.tt("dve", qs[:], qt[:], Eq[:], ALU.mult)
                    s.tt("dve", ks[:], kt[:], Ek[:], ALU.mult)
                    pt = c.ptr.next()
                    for h in range(H):
                        s.tr(pt[:, h, :], qs[:, h * 128:(h + 1) * 128], c.identb[:])
                        s.tr(pt[:, 4 + h, :], ks[:, h * 128:(h + 1) * 128], c.identb[:])
                    s.cp("act", qkT[:], pt[:, :, :])
                    pS = c.pr.next()
                    for h in range(H):
                        s.mm(pS[:, h * 128:(h + 1) * 128], qkT[:, 4 + h, :], qkT[:, h, :])
                    s.tt("dve", PT[:], V(pS.t[:, :].rearrange("p (a l) -> p a l", a=4), pS.key),
                         bc(V(tri.ap.unsqueeze(1), tri.k), [128, 4, 128]), ALU.mult)
                    for h in range(H):
                        po = c.pr.next()
                        s.mm(po[:, :], PT[:, h, :], vt[:, h * DV:(h + 1) * DV], start=True, stop=False)
                        s.mm(po[:, :], qkT[:, h, :], Sbf.sub(str(h))[:, h, :], start=False, stop=True)
                        s.cp("act", oacc[:, h * DV:(h + 1) * DV], po[:, :])
                        pu = c.pr.next()
                        s.mm(pu[:, :], ks[:, h * 128:(h + 1) * 128], vt[:, h * DV:(h + 1) * DV])
                        sh = S32.sub(str(h))
                        s.tt("dve", sh[:, h, :], sh[:, h, :], pu[:, :], ALU.add)
                        s.act(sh[:, h, :], sh[:, h, :], AF.Copy, scale=ee[:, h:h + 1])
                        s.cp("dve", Sbf.sub(str(h))[:, h, :], sh[:, h, :])
                    if d == 0:
                        s.dma(STQ, of[r0:r0 + 128, :], oacc[:])
                    else:
                        s.dma("sp", oft[:], of[r0:r0 + 128, :])
                        s.dma("sp", zt[:], z_tm[r0:r0 + 128, :])
                        s.tt("dve", oft[:], oft[:], oacc[:], ALU.add)
                        gs = gst.next()
                        s.act(tmp[:], oft[:], AF.Square)
                        s.red("dve", gs[:, 0, :], r3(tmp, DV), ALU.add)
                        s.ts("dve", gs[:, 1, :], gs[:, 0, :], 1.0 / DV, EPS, ALU.mult, ALU.add)
                        s.act(gs[:, 2, :], gs[:, 1, :], AF.Sqrt)
                        s.recip("dve", gs[:, 3, :], gs[:, 2, :])
                        s.tt("dve", r3(oft, DV), r3(oft, DV), bc(V(gs.t[:, 3, :].unsqueeze(2), gs.key), [128, H, DV]), ALU.mult)
                        s.tt("dve", r3(oft, DV), r3(oft, DV), bc(V(on.t[:].unsqueeze(1), on.key), [128, H, DV]), ALU.mult)
                        s.act(tmp[:], zt[:], AF.Silu)
                        s.tt("dve", yb[:], oft[:], tmp[:], ALU.mult)
                        fin.run(yb, i)
    s.barrier()


def layer_mlstm(c, W, hin, hout):
    nc, s = c.nc, c.s
    H, DK, DV = 4, 256, 512
    dr = c.dram
    xmT = dr("ml_xmT", [DI, T], BF16)
    z_tm = dr("ml_z", [T, DI], BF16)
    og_tm = dr("ml_og", [T, DI], BF16)
    gt_tm = dr("ml_gates", [T, 16], F32)
    chT = dr("ml_chT", [DI, T], BF16)
    ch_tm = dr("ml_ch", [T, DI], BF16)
    qk_tm = dr("ml_qk", [T, H * 512], BF16)
    v_tm = dr("ml_v", [T, DI], BF16)
    hf = dr("ml_hf", [T, DI], F32)

    with ExitStack() as es:
        uT = sb(es, nc, "uT", [128, 8, T], BF16)
        phase_norm(c, hin, W["ml_norm"], uT)
        phase_project(c, uT, W["ml_w_in"], [
            (0, DI, "fm", xmT, BF16, 1.0),
            (DI, DI, "tm", z_tm, BF16, 1.0),
            (2 * DI, DI, "tm", og_tm, BF16, 1.0),
            (3 * DI, 16, "tm", gt_tm, F32, 1.0),
        ])

    with ExitStack() as es:
        stg = sbring(es, nc, "mc_stg", [128, 4, 128], BF16, 3)

        def emit(ct, b, tb, cv):
            col = b * L + tb * 512
            to_tm_store(c, cv, ch_tm, col, ct * 128, stg)
            s.dma(STQ, chT[ct * 128:(ct + 1) * 128, col:col + 512], cv)

        conv_fm(c, xmT, 16, 5, W["ml_conv_wT"], W["ml_conv_b"], True, emit)
    s.barrier()

    with ExitStack() as es:
        wq = sb(es, nc, "mq_wq", [128, H, 4, 256], BF16)
        wk = sb(es, nc, "mq_wk", [128, H, 4, 256], BF16)
        wv = sb(es, nc, "mq_wv", [128, H, 4, 512], BF16)
        with ExitStack() as es2:
            stf = sbring(es2, nc, "mq_stg", [128, 4096], F32, 2)
            st = stf.next()
            sv = V(st.t[:].rearrange("p (h k n) -> p h k n", h=4, k=4), st.key)
            s.dma("sp", sv, V(W["ml_w_q"].rearrange("h (k p) n -> p h k n", p=128), "w_const"))
            s.cp("dve", wq[:], sv)
            st = stf.next()
            sv = V(st.t[:].rearrange("p (h k n) -> p h k n", h=4, k=4), st.key)
            s.dma("sp", sv, V(W["ml_w_k"].rearrange("h (k p) n -> p h k n", p=128), "w_const"))
            s.cp("act", wk[:], sv)
            for hh in range(2):
                st = stf.next()
                sv = V(st.t[:].rearrange("p (h k n) -> p h k n", h=2, k=4), st.key)
                s.dma("sp", sv, V(W["ml_w_v"][hh * 2:hh * 2 + 2].rearrange("h (k p) n -> p h k n", p=128), "w_const"))
                s.cp("dve" if hh == 0 else "act", wv[:, hh * 2:hh * 2 + 2, :, :], sv)
            s.barrier()
        chr_ = sbring(es, nc, "mq_ch", [128, 16, 128], BF16, 2)
        xmr = sbring(es, nc, "mq_xm", [128, 16, 128], BF16, 2)
        qko = sbring(es, nc, "mq_qko", [128, H * 512], BF16, 2)
        vo = sbring(es, nc, "mq_vo", [128, DI], BF16, 2)
        for i in range(NT):
            cht, xmt, qo, vot = chr_.next(), xmr.next(), qko.next(), vo.next()
            s.dma("sp", cht[:], V(chT.t[:, i * 128:(i + 1) * 128].rearrange("(j p) t -> p j t", p=128), chT.key))
            s.dma("sp", xmt[:], V(xmT.t[:, i * 128:(i + 1) * 128].rearrange("(j p) t -> p j t", p=128), xmT.key))
            for h in range(H):
                pq = c.pr.next()
                for kc in range(4):
                    s.mm(pq[:, 0:256], cht[:, h * 4 + kc, :], wq[:, h, kc, :], start=(kc == 0), stop=(kc == 3))
                for kc in range(4):
                    s.mm(pq[:, 256:512], cht[:, h * 4 + kc, :], wk[:, h, kc, :], start=(kc == 0), stop=(kc == 3))
                s.cp("act", qo[:, h * 512:(h + 1) * 512], pq[:, :])
                pv = c.pr.next()
                for kc in range(4):
                    s.mm(pv[:, :], xmt[:, h * 4 + kc, :], wv[:, h, kc, :], start=(kc == 0), stop=(kc == 3))
                s.cp("dve", vot[:, h * 512:(h + 1) * 512], pv[:, :])
            s.dma(STQ, qk_tm[i * 128:(i + 1) * 128, :], qo[:])
            s.dma(STQ, v_tm[i * 128:(i + 1) * 128, :], vot[:])
    s.barrier()

    with ExitStack() as es:
        c.pr = Ring(c.pbanks[0:5])
        psm = c.pbanks[5]
        fin = Finalizer(c, es, W["ml_w_out"], hin, hout)
        gb = sb(es, nc, "m_gb", [128, 16], F32)
        on = sb(es, nc, "m_on", [128, 512], F32)
        skp = sb(es, nc, "m_skip", [128, DI], F32)
        s.dma("sp", gb[:], V(W["ml_gate_b"].rearrange("a b c -> (a b c)").partition_broadcast(128), "w_const"))
        s.dma("sp", on[:], V(W["ml_onorm"].partition_broadcast(128), "w_const"))
        s.dma("sp", skp[:], V(W["ml_skip"].rearrange("h d -> (h d)").partition_broadcast(128), "w_const"))
        onesb = sb(es, nc, "m_onesb", [128, 2], BF16)
        s.memset("dve", onesb[:], 1.0)
        qkr = sbring(es, nc, "m_qk", [128, H, 512], BF16, 2)
        vr = sbring(es, nc, "m_v", [128, DI], BF16, 2)
        gr = sbring(es, nc, "m_g", [128, 16], F32, 2)
        sm = sbring(es, nc, "m_sm", [128, 8, 4], F32, 2)
        qsr = sbring(es, nc, "m_qs", [128, H, 256], BF16, 2)
        ksr = sbring(es, nc, "m_ks", [128, H, 256], BF16, 2)
        qTr = sbring(es, nc, "m_qT", [128, 8, 128], BF16, 2)
        kTr = sbring(es, nc, "m_kT", [128, 8, 128], BF16, 2)
        PTr = sbring(es, nc, "m_PT", [128, 4, 128], BF16, 2)
        haccr = sbring(es, nc, "m_hacc", [128, DI], F32, 2)
        C32 = sb(es, nc, "m_C32", [128, 8, DV], F32)
        Cbf = sb(es, nc, "m_Cbf", [128, 8, DV], BF16)
        n32 = sb(es, nc, "m_n32", [128, 8], F32)
        nbf = sb(es, nc, "m_nbf", [128, 8], BF16)
        hft = sb(es, nc, "m_hf", [128, DI], F32)
        ogt = sb(es, nc, "m_og", [128, DI], BF16)
        zt = sb(es, nc, "m_z", [128, DI], BF16)
        cht = sb(es, nc, "m_ch", [128, DI], BF16)
        tmp = sb(es, nc, "m_tmp", [128, DI], F32)
        gst = sbring(es, nc, "m_st", [128, 4, 4], F32, 2)
        yb = sb(es, nc, "m_yb", [128, DI], BF16)
        allj = [str(j) for j in range(8)]

        def r3(buf, q):
            return V(buf.t[:].rearrange("p (h q) -> p h q", q=q), buf.key)

        for b in range(BL):
            for d in range(2):
                s.memset("dve", C32.subs(allj)[:], 0.0)
                s.memset("pool", Cbf.subs(allj)[:], 0.0)
                s.memset("dve", n32[:], 0.0)
                s.memset("pool", nbf[:], 0.0)
                tri = c.tri[:, d, :]
                order = range(NQ) if d == 0 else range(NQ - 1, -1, -1)
                for ci in order:
                    i = b * NQ + ci
                    r0 = i * 128
                    qkt, vt, gt = qkr.next(), vr.next(), gr.next()
                    qs, ks, qT, kT, PT, hacc = qsr.next(), ksr.next(), qTr.next(), kTr.next(), PTr.next(), haccr.next()
                    s.dma("sp", V(qkt.t[:].rearrange("p h n -> p (h n)"), qkt.key), qk_tm[r0:r0 + 128, :])
                    s.dma("sp", vt[:], v_tm[r0:r0 + 128, :])
                    s.dma("sp", gt[:], gt_tm[r0:r0 + 128, :])
                    m = sm.next()
                    s.tt("dve", gt[:], gt[:], gb[:], ALU.add)
                    ig = gt[:, d * 8:d * 8 + 4]
                    fr = gt[:, d * 8 + 4:d * 8 + 8]
                    s.act(m[:, 0, :], fr, AF.Exp, scale=-1.0)
                    s.act(m[:, 0, :], m[:, 0, :], AF.Ln, bias=c.one[:, 0:1])
                    s.mm(psm[:, 0:4], tri, m[:, 0, :])
                    s.mm(psm[:, 4:8], c.onesf[:], m[:, 0, :])
                    s.act(m[:, 1, :], psm[:, 0:4], AF.Exp, scale=-1.0)
                    s.tt("dve", m[:, 2, :], psm[:, 0:4], ig, ALU.add)
                    s.act(m[:, 2, :], m[:, 2, :], AF.Exp, bias=c.one[:, 3:4])
                    s.act(m[:, 3, :], psm[:, 4:8], AF.Exp, scale=-1.0)
                    s.tt("dve", qs[:], V(qkt.t[:, :, 0:256], qkt.key),
                         bc(V(m.t[:, 1, :].unsqueeze(2), m.key), [128, H, 256]), ALU.mult)
                    s.tt("dve", ks[:], V(qkt.t[:, :, 256:512], qkt.key),
                         bc(V(m.t[:, 2, :].unsqueeze(2), m.key), [128, H, 256]), ALU.mult)
                    pt = c.ptr.next()
                    for j in range(8):
                        s.tr(pt[:, j, :], qs[:, j // 2, (j % 2) * 128:(j % 2 + 1) * 128], c.identb[:])
                    s.cp("act", qT[:], pt[:, :, :])
                    pt = c.ptr.next()
                    for j in range(8):
                        s.tr(pt[:, j, :], ks[:, j // 2, (j % 2) * 128:(j % 2 + 1) * 128], c.identb[:])
                    s.cp("dve", kT[:], pt[:, :, :])
                    pS = c.pr.next()
                    for h in range(H):
                        for kc in range(2):
                            s.mm(pS[:, h * 128:(h + 1) * 128], kT[:, h * 2 + kc, :], qT[:, h * 2 + kc, :],
                                 start=(kc == 0), stop=(kc == 1))
                    s.tt("dve", PT[:], V(pS.t[:, :].rearrange("p (a l) -> p a l", a=4), pS.key),
                         bc(V(tri.ap.unsqueeze(1), tri.k), [128, 4, 128]), ALU.mult)
                    for h in range(H):
                        s.mm(psm[:, 8 + h:9 + h], PT[:, h, :], onesb[:, 0:1], start=True, stop=False)
                        for kc in range(2):
                            s.mm(psm[:, 8 + h:9 + h], qT[:, h * 2 + kc, :], nbf[:, h * 2 + kc:h * 2 + kc + 1],
                                 start=False, stop=(kc == 1))
                    s.act(m[:, 4, :], psm[:, 8:12], AF.Abs)
                    s.ts("dve", m[:, 4, :], m[:, 4, :], 1.0, None, ALU.max)
                    s.recip("dve", m[:, 5, :], m[:, 4, :])
                    for h in range(H):
                        po = c.pr.next()
                        s.mm(po[:, :], PT[:, h, :], vt[:, h * DV:(h + 1) * DV], start=True, stop=False)
                        for kc in range(2):
                            s.mm(po[:, :], qT[:, h * 2 + kc, :], Cbf.sub(str(h * 2 + kc))[:, h * 2 + kc, :],
                                 start=False, stop=(kc == 1))
                        s.act(hacc[:, h * DV:(h + 1) * DV], po[:, :], AF.Copy, scale=m[:, 5, h:h + 1])
                        for kc in range(2):
                            j = h * 2 + kc
                            pu = c.pr.next()
                            s.mm(pu[:, :], ks[:, h, kc * 128:(kc + 1) * 128], vt[:, h * DV:(h + 1) * DV])
                            s.mm(psm[:, 16 + j:17 + j], ks[:, h, kc * 128:(kc + 1) * 128], onesb[:, 0:1])
                            cj = C32.sub(str(j))
                            s.tt("dve", cj[:, j, :], cj[:, j, :], pu[:, :], ALU.add)
                            s.act(cj[:, j, :], cj[:, j, :], AF.Copy, scale=m[:, 3, h:h + 1])
                            s.cp("dve" if kc == 0 else "act", Cbf.sub(str(j))[:, j, :], cj[:, j, :])
                    s.tt("dve", n32[:], n32[:], psm[:, 16:24], ALU.add)
                    s.tt("dve", V(n32.t[:].rearrange("p (h k) -> p h k", k=2), n32.key),
                         V(n32.t[:].rearrange("p (h k) -> p h k", k=2), n32.key),
                         bc(V(m.t[:, 3, :].unsqueeze(2), m.key), [128, H, 2]), ALU.mult)
                    s.cp("dve", nbf[:], n32[:])
                    if d == 0:
                        s.dma(STQ, hf[r0:r0 + 128, :], hacc[:])
                    else:
                        s.dma("sp", hft[:], hf[r0:r0 + 128, :])
                        s.dma("sp", ogt[:], og_tm[r0:r0 + 128, :])
                        s.dma("sp", zt[:], z_tm[r0:r0 + 128, :])
                        s.dma("sp", cht[:], ch_tm[r0:r0 + 128, :])
                        s.tt("dve", hft[:], hft[:], hacc[:], ALU.add)
                        s.act(tmp[:], ogt[:], AF.Sigmoid)
                        s.tt("dve", hft[:], hft[:], tmp[:], ALU.mult)
                        gs = gst.next()
                        s.act(tmp[:], hft[:], AF.Square)
                        s.red("dve", gs[:, 0, :], r3(tmp, DV), ALU.add)
                        s.ts("dve", gs[:, 1, :], gs[:, 0, :], 1.0 / DV, EPS, ALU.mult, ALU.add)
                        s.act(gs[:, 2, :], gs[:, 1, :], AF.Sqrt)
                        s.recip("dve", gs[:, 3, :], gs[:, 2, :])
                        s.tt("dve", r3(hft, DV), r3(hft, DV), bc(V(gs.t[:, 3, :].unsqueeze(2), gs.key), [128, H, DV]), ALU.mult)
                        s.tt("dve", r3(hft, DV), r3(hft, DV), bc(V(on.t[:].unsqueeze(1), on.key), [128, H, DV]), ALU.mult)
                        s.tt("dve", tmp[:], cht[:], skp[:], ALU.mult)
                        s.tt("dve", hft[:], hft[:], tmp[:], ALU.add)
                        s.act(tmp[:], zt[:], AF.Silu)
                        s.tt("dve", yb[:], hft[:], tmp[:], ALU.mult)
                        fin.run(yb, i)
    s.barrier()
    c.pr = Ring(c.pbanks)


def load_mat(c, dst, src_ap):
    v = src_ap.rearrange("(st p) f -> p st f", p=128)
    for q in range(4):
        c.s.dma("sp", dst[:, q * 4:(q + 1) * 4, :], V(v[:, q * 4:(q + 1) * 4, :], "w_const"))


def layer_hyena(c, W, hin, hout):
    nc, s = c.nc, c.s
    C = c.C
    dr = c.dram
    vxT = dr("hy_vxT", [3 * DI, T], BF16)
    z_tm = dr("hy_z", [T, DI], BF16)
    sig = [dr("hy_v", [T, DI], BF16), dr("hy_x1", [T, DI], BF16), dr("hy_x2", [T, DI], BF16)]
    y1_tm = dr("hy_y1", [T, DI], BF16)
    y2_tm = dr("hy_y2", [T, DI], BF16)
    Kf = dr("hy_Kf", [2, 2, L, DI], BF16)
    Zd = dr("hy_Z", [BL, 2, L, DI], BF16)

    with ExitStack() as es:
        uT = sb(es, nc, "uT", [128, 8, T], BF16)
        phase_norm(c, hin, W["hy_norm"], uT)
        phase_project(c, uT, W["hy_w_in"], [
            (0, 3 * DI, "fm", vxT, BF16, 1.0),
            (3 * DI, DI, "tm", z_tm, BF16, 1.0),
        ])

    with ExitStack() as es:
        stg = sbring(es, nc, "hc_stg", [128, 4, 128], BF16, 3)

        def emit(ct, b, tb, cv):
            to_tm_store(c, cv, sig[ct // 16], b * L + tb * 512, (ct % 16) * 128, stg)

        conv_fm(c, vxT, 48, 3, W["hy_conv_wT"], W["hy_conv_b"], False, emit)
    s.barrier()

    def fwd_transform(Cm, Sn, o, ysrc):
        with ExitStack() as es:
            yr = sbring(es, nc, "hf_y", [128, 16, 512], BF16, 2)
            kr = sbring(es, nc, "hf_k", [128, 2, 512], BF16, 2)
            yre = sb(es, nc, "hf_yre", [128, 512], F32)
            yim = sb(es, nc, "hf_yim", [128, 512], F32)
            t1 = sb(es, nc, "hf_t1", [128, 512], F32)
            t2 = sb(es, nc, "hf_t2", [128, 512], F32)
            t3 = sb(es, nc, "hf_t3", [128, 512], F32)
            t4 = sb(es, nc, "hf_t4", [128, 512], F32)
            zr = sbring(es, nc, "hf_z", [128, 2, 512], BF16, 2)
            for b in range(BL):
                for cb in range(4):
                    yt = yr.next()
                    cs = slice(cb * 512, (cb + 1) * 512)
                    s.dma("sp", yt[:], V(ysrc.t[b * L:(b + 1) * L, cs].rearrange("(st p) c -> p st c", p=128), ysrc.key))
                    for ft in range(16):
                        fs = slice(ft * 128, (ft + 1) * 128)
                        kt = kr.next()
                        s.dma("sp", kt[:], V(Kf.t[o, :, fs, cs].rearrange("a p c -> p a c"), Kf.key))
                        pre, pim = c.pr.next(), c.pr.next()
                        for st in range(16):
                            s.mm(pre[:, :], Cm[:, st, fs], yt[:, st, :], start=(st == 0), stop=(st == 15))
                        for st in range(16):
                            s.mm(pim[:, :], Sn[:, st, fs], yt[:, st, :], start=(st == 0), stop=(st == 15))
                        s.cp("act", yre[:], pre[:, :])
                        s.cp("act", yim[:], pim[:, :])
                        s.tt("dve", t1[:], yre[:], kt[:, 0, :], ALU.mult)
                        s.tt("dve", t2[:], yim[:], kt[:, 1, :], ALU.mult)
                        s.tt("dve", t3[:], yre[:], kt[:, 1, :], ALU.mult)
                        s.tt("dve", t4[:], yim[:], kt[:, 0, :], ALU.mult)
                        zt = zr.next()
                        s.tt("dve", zt[:, 0, :], t1[:], t2[:], ALU.subtract)
                        s.tt("pool", zt[:, 1, :], t3[:], t4[:], ALU.add)
                        s.dma(STQ, V(Zd.t[b, :, fs, cs].rearrange("a p c -> p a c"), Zd.key), zt[:])
        s.barrier()

    def inv_transform(o, ysrc, xg_src, ydst):
        with ExitStack() as es:
            CI = sb(es, nc, "hi_CI", [128, 16, L], BF16)
            SI = sb(es, nc, "hi_SI", [128, 16, L], BF16)
            load_mat(c, CI, C["c_CI"])
            load_mat(c, SI, C["c_SI"])
            dbc = sb(es, nc, "hi_d", [128, DI], F32)
            s.dma("sp", dbc[:], V(W["hy_d"][o].partition_broadcast(128), "w_const"))
            zb = sb(es, nc, "hi_zb", [128, 2, 16, 512], BF16)
            ypr = sbring(es, nc, "hi_yp", [128, 512], BF16, 2)
            xgr = sbring(es, nc, "hi_xg", [128, 512], BF16, 2)
            zzr = sbring(es, nc, "hi_zz", [128, 512], BF16, 2)
            ta = sbring(es, nc, "hi_ta", [128, 512], F32, 2)
            tb_ = sbring(es, nc, "hi_tb", [128, 512], F32, 2)
            yo = sbring(es, nc, "hi_yo", [128, 512], BF16, 2)
            for b in range(BL):
                for cb in range(4):
                    cs = slice(cb * 512, (cb + 1) * 512)
                    for a in range(2):
                        for q in range(2):
                            s.dma("sp", zb[:, a, q * 8:(q + 1) * 8, :],
                                  V(Zd.t[b, a, q * 1024:(q + 1) * 1024, cs].rearrange("(ft p) c -> p ft c", p=128), Zd.key))
                    for tt in range(16):
                        ts_ = slice(tt * 128, (tt + 1) * 128)
                        rows = slice(b * L + tt * 128, b * L + (tt + 1) * 128)
                        po = c.pr.next()
                        for ft in range(16):
                            s.mm(po[:, :], CI[:, ft, ts_], zb[:, 0, ft, :], start=(ft == 0), stop=False)
                        for ft in range(16):
                            s.mm(po[:, :], SI[:, ft, ts_], zb[:, 1, ft, :], start=False, stop=(ft == 15))
                        yp, xg = ypr.next(), xgr.next()
                        s.dma("sp", yp[:], ysrc[rows, cs])
                        s.dma("sp", xg[:], xg_src[rows, cs])
                        t_a = ta.next()
                        s.tt("dve", t_a[:], yp[:], dbc[:, cs], ALU.mult)
                        s.tt("dve", t_a[:], t_a[:], po[:, :], ALU.add)
                        yot = yo.next()
                        if o == 0:
                            s.tt("dve", yot[:], t_a[:], xg[:], ALU.mult)
                        else:
                            zz, t_b = zzr.next(), tb_.next()
                            s.dma("sp", zz[:], z_tm[rows, cs])
                            s.act(t_b[:], zz[:], AF.Silu)
                            s.tt("dve", t_a[:], t_a[:], xg[:], ALU.mult)
                            s.tt("dve", yot[:], t_a[:], t_b[:], ALU.mult)
                        s.dma(STQ, ydst[rows, cs], yot[:])
        s.barrier()

    with ExitStack() as es:
        Cm = sb(es, nc, "hy_Cm", [128, 16, L], BF16)
        Sn = sb(es, nc, "hy_Sn", [128, 16, L], BF16)
        load_mat(c, Cm, C["c_Cm"])
        load_mat(c, Sn, C["c_Sn"])
        with ExitStack() as es2:
            hA = sb(es2, nc, "hm_hA", [64, L], F32)
            hB = sb(es2, nc, "hm_hB", [64, L], F32)
            with ExitStack() as es3:
                feats = sb(es3, nc, "hm_feats", [33, L], F32)
                w1 = sb(es3, nc, "hm_w1", [33, 64], F32)
                wh = sb(es3, nc, "hm_wh", [64, 2, 64], F32)
                prm = sb(es3, nc, "hm_prm", [64, 8], F32)
                tr_ = sb(es3, nc, "hm_t", [64, 512], F32)
                tki = sb(es3, nc, "hm_ki", [64, 512], mybir.dt.int32)
                tkf = sb(es3, nc, "hm_kf", [64, 512], F32)
                s.dma("sp", feats[:], V(C["c_featsT"], "w_const"))
                s.dma("sp", w1[:], V(W["hy_ffn_w_in"], "w_const"))
                s.dma("sp", wh[:], V(W["hy_ffn_w_hid"].rearrange("j a b -> a j b"), "w_const"))
                s.dma("sp", prm[:, 0:1], V(W["hy_ffn_b_in"].rearrange("(p o) -> p o", o=1), "w_const"))
                s.dma("sp", prm[:, 1:3], V(W["hy_ffn_b_hidT"], "w_const"))
                s.dma("sp", prm[:, 3:6], V(W["hy_ffn_freqT"], "w_const"))
                cur, nxt = hA, hB
                for layer in range(3):
                    for blk in range(4):
                        bs = slice(blk * 512, (blk + 1) * 512)
                        ps = c.pr.next()
                        if layer == 0:
                            s.mm(ps[0:64, :], w1[:, :], feats[:, bs])
                            dst = cur
                        else:
                            s.mm(ps[0:64, :], wh[:, layer - 1, :], cur[:, bs])
                            dst = nxt
                        s.ts("dve", tr_[:], ps[0:64, :], prm[:, layer:layer + 1], prm[:, 3 + layer:4 + layer], ALU.add, ALU.mult)
                        s.ts("dve", tr_[:], tr_[:], 1.0 / (2.0 * PI), 8.5, ALU.mult, ALU.add)
                        s.cp("dve", tki[:], tr_[:])
                        s.cp("dve", tkf[:], tki[:])
                        s.tt("dve", tr_[:], tr_[:], tkf[:], ALU.subtract)
                        s.ts("dve", tkf[:], tr_[:], 0.0, None, ALU.is_lt)
                        s.tt("dve", tr_[:], tr_[:], tkf[:], ALU.add)
                        s.act(dst[:, bs], tr_[:], AF.Sin, bias=c.one[0:64, 1:2], scale=2.0 * PI)
                    if layer > 0:
                        cur, nxt = nxt, cur
                h3 = cur
                s.barrier()
            dlt = sb(es2, nc, "hm_dlt", [128, DI], F32)
            ngt = sb(es2, nc, "hm_negt", [128, 16], F32)
            s.dma("sp", dlt[:], V(C["c_deltas"].partition_broadcast(128), "w_const"))
            s.dma("sp", ngt[:], V(C["c_negt"], "w_const"))
            wor = sbring(es2, nc, "hm_wo", [64, 2, 256], F32, 2)
            Ar = sb(es2, nc, "hm_A", [128, 16, 256], BF16)
            Br_ = sb(es2, nc, "hm_B", [128, 16, 256], BF16)
            dec = sbring(es2, nc, "hm_dec", [128, 256], F32, 2)
            hbs = sbring(es2, nc, "hm_hb", [128, 256], F32, 2)
            sa = sbring(es2, nc, "hm_sa", [128, 256], F32, 2)
            sbm = sbring(es2, nc, "hm_sb", [128, 256], F32, 2)
            ko = sbring(es2, nc, "hm_ko", [128, 512], BF16, 2)
            wov = W["hy_ffn_w_out"]
            for o in range(2):
                for cb in range(8):
                    cs = slice(cb * 256, (cb + 1) * 256)
                    wo = wor.next()
                    for dd in range(2):
                        c0 = dd * 2 * DI + o * DI + cb * 256
                        s.dma("sp", wo[:, dd, :], V(wov[:, c0:c0 + 256], "w_const"))
                    for tt in range(16):
                        ps = c.pr.next()
                        s.mm(ps[:, 0:256], h3[:, tt * 128:(tt + 1) * 128], wo[:, 0, :])
                        s.mm(ps[:, 256:512], h3[:, tt * 128:(tt + 1) * 128], wo[:, 1, :])
                        dc, hb_, a_, b_ = dec.next(), hbs.next(), sa.next(), sbm.next()
                        s.act(dc[:], dlt[:, cs], AF.Exp, scale=ngt[:, tt:tt + 1])
                        s.cp("dve", hb_[:], ps[:, 256:512])
                        if tt == 0:
                            s.memset("dve", hb_[0:1, :], 0.0)
                        s.tt("dve", a_[:], ps[:, 0:256], hb_[:], ALU.add)
                        s.tt("dve", b_[:], ps[:, 0:256], hb_[:], ALU.subtract)
                        s.tt("dve", Ar[:, tt, :], a_[:], dc[:], ALU.mult)
                        s.tt("pool", Br_[:, tt, :], b_[:], dc[:], ALU.mult)
                    for ft in range(16):
                        fs = slice(ft * 128, (ft + 1) * 128)
                        pk = c.pr.next()
                        for tt in range(16):
                            s.mm(pk[:, 0:256], Cm[:, tt, fs], Ar[:, tt, :], start=(tt == 0), stop=(tt == 15))
                        for tt in range(16):
                            s.mm(pk[:, 256:512], Sn[:, tt, fs], Br_[:, tt, :], start=(tt == 0), stop=(tt == 15))
                        kt = ko.next()
                        s.cp("act", kt[:], pk[:, :])
                        s.dma(STQ, V(Kf.t[o, :, fs, cs].rearrange("a p c -> p a c"), Kf.key),
                              V(kt.t[:].rearrange("p (a c) -> p a c", a=2), kt.key))
            s.barrier()
        fwd_transform(Cm, Sn, 0, sig[0])
    inv_transform(0, sig[0], sig[1], y1_tm)
    with ExitStack() as es:
        Cm = sb(es, nc, "hy_Cm2", [128, 16, L], BF16)
        Sn = sb(es, nc, "hy_Sn2", [128, 16, L], BF16)
        load_mat(c, Cm, C["c_Cm"])
        load_mat(c, Sn, C["c_Sn"])
        fwd_transform(Cm, Sn, 1, y1_tm)
    inv_transform(1, y1_tm, sig[2], y2_tm)

    with ExitStack() as es:
        fin = Finalizer(c, es, W["hy_w_out"], hin, hout)
        yr = sbring(es, nc, "ho_y", [128, DI], BF16, 2)
        for i in range(NT):
            yt = yr.next()
            s.dma("sp", yt[:], y2_tm[i * 128:(i + 1) * 128, :])
            fin.run(yt, i)
    s.barrier()


N_IMPL = 4
STQ = "act"
DEBUG_STOP = None


def host_constants():
    cst = {}
    cst["c_identb"] = np.eye(128, dtype=np.float32).astype(ml_dtypes.bfloat16)
    cst["c_identf"] = np.eye(128, dtype=np.float32)
    sidx = np.arange(128)[:, None]
    lidx = np.arange(128)[None, :]
    tri = np.stack([(sidx <= lidx), (sidx >= lidx)], axis=1).astype(np.float32)
    cst["c_tri"] = tri
    cst["c_negm"] = ((1.0 - tri) * -1.0e5).astype(np.float32)
    sI = np.arange(L, dtype=np.int64)[:, None]
    fI = np.arange(L, dtype=np.int64)[None, :]
    ph = ((2 * fI + 1) * sI) % (4 * L)
    th = (2.0 * np.pi / (4 * L)) * ph.astype(np.float64)
    cm = np.cos(th)
    sn = np.sin(th)
    bf = ml_dtypes.bfloat16
    cst["c_Cm"] = cm.astype(np.float32).astype(bf)
    cst["c_Sn"] = (-sn).astype(np.float32).astype(bf)
    cst["c_CI"] = np.ascontiguousarray((cm.T / L)).astype(np.float32).astype(bf)
    cst["c_SI"] = np.ascontiguousarray((-sn.T / L)).astype(np.float32).astype(bf)
    t = np.linspace(0.0, 1.0, L, dtype=np.float32)[:, None]
    pos = np.arange(L, dtype=np.float32)[:, None]
    bands = np.linspace(1e-4, 15.0, 16, dtype=np.float32)[None]
    ang = (np.float32(2.0 * math.pi / L) * pos * bands).astype(np.float32)
    feats = np.concatenate([t, np.cos(ang), -np.sin(ang)], axis=-1).astype(np.float32)
    cst["c_featsT"] = np.ascontiguousarray(feats.T)
    max_decay = math.log(1e-2) / 0.3
    min_decay = math.log(1e-2) / 1.5
    cst["c_deltas"] = np.abs(np.linspace(min_decay, max_decay, DI, dtype=np.float32)).astype(np.float32)
    cst["c_negt"] = np.ascontiguousarray(-(t[:, 0].reshape(16, 128).T)).astype(np.float32)
    return cst


def host_prepare(inputs):
    W = {}
    for k, v in inputs.items():
        if k in ("x", "final_norm"):
            continue
        W[k] = np.ascontiguousarray(v[0])
    W["final_norm"] = np.ascontiguousarray(inputs["final_norm"])
    W["ssd_conv_wT"] = np.ascontiguousarray(W.pop("ssd_conv_w").T)
    W["ml_conv_wT"] = np.ascontiguousarray(W.pop("ml_conv_w").T)
    W["hy_conv_wT"] = np.ascontiguousarray(W.pop("hy_conv_w").T)
    W["hy_ffn_b_hidT"] = np.ascontiguousarray(W.pop("hy_ffn_b_hid").T)
    W["hy_ffn_freqT"] = np.ascontiguousarray(W.pop("hy_ffn_freq").T)
    return W


def build_program(wshapes, cshapes, n_layers=4, final_norm=True):
    n_layers = min(n_layers, N_IMPL)
    nc = bass.Bass("TRN2", target_bir_lowering=False)
    c = Ctx()
    c.nc = nc
    c.s = Sched(nc)
    s = c.s
    x_in = Buf(nc.dram_tensor("x", [T, D], F32, kind="ExternalInput").ap(), "x_in")
    out = Buf(nc.dram_tensor("out", [T, D], F32, kind="ExternalOutput").ap(), "out")
    W = {k: nc.dram_tensor(k, list(shp), F32 if dt == np.float32 else BF16, kind="ExternalInput").ap()
         for k, (shp, dt) in wshapes.items()}
    C = {k: nc.dram_tensor(k, list(shp), F32 if dt == np.float32 else BF16, kind="ExternalInput").ap()
         for k, (shp, dt) in cshapes.items()}

    def dram(name, shape, dt):
        return Buf(nc.dram_tensor(name, shape, dt, kind="Internal").ap(), name)

    c.dram = dram
    c.C = C
    hA = dram("hA", [T, D], F32)
    hB = dram("hB", [T, D], F32)

    with ExitStack() as es:
        c.identb = sb(es, nc, "k_identb", [128, 128], BF16)
        c.identf = sb(es, nc, "k_identf", [128, 128], F32)
        c.tri = sb(es, nc, "k_tri", [128, 2, 128], F32)
        c.negm = sb(es, nc, "k_negm", [128, 2, 128], F32)
        c.onesf = sb(es, nc, "k_onesf", [128, 128], F32)
        c.nonesf = sb(es, nc, "k_nonesf", [128, 128], F32)
        c.one = sb(es, nc, "k_one", [128, 4], F32)
        s.dma("sp", c.identb[:], V(C["c_identb"], "w_const"))
        s.dma("sp", c.identf[:], V(C["c_identf"], "w_const"))
        s.dma("sp", c.tri[:], V(C["c_tri"], "w_const"))
        s.dma("sp", c.negm[:], V(C["c_negm"], "w_const"))
        s.memset("dve", c.onesf[:], 1.0)
        s.memset("dve", c.nonesf[:], -1.0)
        s.memset("dve", c.one[:, 0:1], 1.0)
        s.memset("dve", c.one[:, 1:2], -PI)
        s.memset("dve", c.one[:, 2:3], 0.0)
        s.memset("dve", c.one[:, 3:4], math.log(1.0 / 16.0))
        pbanks = [Buf(es.enter_context(nc.psum_tensor(f"ps{i}", [128, 512], F32)), f"ps{i}") for i in range(6)]
        tbanks = [Buf(es.enter_context(nc.psum_tensor(f"pt{i}", [128, 8, 128], BF16)), f"pt{i}") for i in range(2)]
        c.pbanks = pbanks
        c.pr = Ring(pbanks)
        c.ptr = Ring(tbanks)
        s.barrier()
        c.identb.key = c.identf.key = c.tri.key = c.negm.key = "konst"
        c.onesf.key = c.nonesf.key = c.one.key = "konst"

        layers = [layer_ssd, layer_gla, layer_hyena, layer_mlstm]
        hs = [x_in, hA, hB, hA, hB]
        hcur = x_in
        for li in range(n_layers):
            hnext = hA if (li % 2 == 0) else hB
            layers[li](c, W, hcur, hnext)
            hcur = hnext
        if final_norm:
            phase_final_norm(c, hcur, W["final_norm"], out)
        else:
            with ExitStack() as es2:
                cr = sbring(es2, nc, "cp_x", [128, D], F32, 2)
                for i in range(NT):
                    t = cr.next()
                    s.dma("sp", t[:], hcur[i * 128:(i + 1) * 128, :])
                    s.dma(STQ, out[i * 128:(i + 1) * 128, :], t[:])
            s.barrier()
    return nc


_CACHE = {}


def kernel(**inputs):
    x = np.ascontiguousarray(inputs["x"], dtype=np.float32)
    W = host_prepare(inputs)
    Cst = host_constants()
    wshapes = {k: (v.shape, v.dtype.type if v.dtype != ml_dtypes.bfloat16 else "bf16") for k, v in W.items()}
    cshapes = {k: (v.shape, v.dtype.type if v.dtype != ml_dtypes.bfloat16 else "bf16") for k, v in Cst.items()}
    nc = build_program(wshapes, cshapes)
    in_maps = []
    for i in range(NCORES):
        m = {"x": x[i * BL:(i + 1) * BL].reshape(T, D)}
        m.update(W)
        m.update(Cst)
        in_maps.append(m)
    res = run_bass_kernel_spmd(nc, in_maps, core_ids=list(range(NCORES)))
    outs = [r["out"].reshape(BL, L, D) for r in res.results]
    return np.concatenate(outs, axis=0).astype(np.float32)
```

```python
import math
from contextlib import ExitStack
import numpy as np
import ml_dtypes
import concourse.bass as bass
import concourse.mybir as mybir
from concourse.bass_utils import run_bass_kernel_spmd

F32 = mybir.dt.float32
BF16 = mybir.dt.bfloat16
AF = mybir.ActivationFunctionType
ALU = mybir.AluOpType
AX = mybir.AxisListType

NCORES = 8
BL = 2
L = 2048
T = BL * L
D = 1024
DI = 2048
Q = 128
NQ = L // Q
NT = T // 128
EPS = 1e-6
EPOCH = 30000
PI = math.pi


def _kt(k):
    if isinstance(k, str):
        return (k,)
    return tuple(k)


class V:
    __slots__ = ("ap", "k")

    def __init__(self, ap, k):
        self.ap = ap
        self.k = _kt(k)


class Buf:
    def __init__(self, t, key):
        self.t = t
        self.key = key

    def __getitem__(self, idx):
        return V(self.t[idx], self.key)

    def sub(self, sub):
        return Buf(self.t, f"{self.key}.{sub}")

    def subs(self, subs):
        return Buf(self.t, tuple(f"{self.key}.{x}" for x in subs))


class Sched:
    def __init__(self, nc):
        self.nc = nc
        self.eng = {"pe": nc.tensor, "dve": nc.vector, "act": nc.scalar,
                    "pool": nc.gpsimd, "sp": nc.sync}
        self.nsem = 0
        self.esem, self.ecnt = {}, {}
        for e in self.eng:
            self._new_epoch(e)
        self.seen = {e: {} for e in self.eng}
        self.res = {}
        self.dsem = {}
        self.dfree = []
        self.ninst = 0

    def _alloc(self):
        self.nsem += 1
        return self.nc.alloc_semaphore(name=f"s{self.nsem}")

    def _new_epoch(self, e):
        self.esem[e] = self._alloc()
        self.ecnt[e] = 0

    def _wait(self, e, tok):
        sem, val = tok
        sid = id(sem)
        if self.seen[e].get(sid, 0) >= val:
            return
        self.eng[e].wait_ge(sem, val)
        self.seen[e][sid] = val

    def _deps(self, e, reads, writes, pe_accum=False, dsem=None):
        for k in reads:
            r = self.res.get(k)
            if r and r["w"] is not None:
                self._wait(e, r["w"])
        for k in writes:
            r = self.res.get(k)
            if r:
                w = r["w"]
                if w is not None:
                    skip = (pe_accum and r["we"] == "pe") or (dsem is not None and w[0] is dsem)
                    if not skip:
                        self._wait(e, w)
                for t in r["r"]:
                    self._wait(e, t)

    def _record(self, e, tok, reads, writes):
        for k in reads:
            r = self.res.setdefault(k, {"w": None, "r": [], "we": None})
            r["r"] = [t for t in r["r"] if t[0] is not tok[0]] + [tok]
        for k in writes:
            self.res[k] = {"w": tok, "r": [], "we": e}

    def op(self, e, fn, reads=(), writes=(), pe_accum=False):
        reads = [k for ks in reads if ks is not None for k in _kt(ks)]
        writes = [k for ks in writes for k in _kt(ks)]
        self._deps(e, reads, writes, pe_accum)
        if self.ecnt[e] >= EPOCH:
            self._new_epoch(e)
        inst = fn()
        self.ecnt[e] += 1
        inst.then_inc(self.esem[e], 1)
        self._record(e, (self.esem[e], self.ecnt[e]), reads, writes)
        self.ninst += 1
        return inst

    def dma(self, e, out, in_, **kw):
        reads, writes = list(in_.k), list(out.k)
        sk = out.k[0]
        if sk not in self.dsem:
            if self.dfree:
                self.dsem[sk] = self.dfree.pop()
            else:
                self.dsem[sk] = [self._alloc(), 0]
        ds = self.dsem[sk]
        self._deps(e, reads, writes, dsem=ds[0])
        inst = self.eng[e].dma_start(out=out.ap, in_=in_.ap, **kw)
        ds[1] += 16
        inst.then_inc(ds[0], 16)
        self._record(e, (ds[0], ds[1]), reads, writes)
        self.ninst += 1
        return inst

    def barrier(self):
        toks = [(self.esem[e], self.ecnt[e]) for e in self.eng if self.ecnt[e] > 0]
        toks += [(d[0], d[1]) for d in self.dsem.values() if d[1] > 0]
        for e in self.eng:
            for t in toks:
                if t[0] is not self.esem[e]:
                    self._wait(e, t)
        for d in self.dsem.values():
            if d[1] < 40000:
                self.dfree.append(d)
        self.dsem = {}
        self.res = {}

    def mm(self, out, lhsT, rhs, start=True, stop=True):
        nc = self.nc
        return self.op("pe", lambda: nc.tensor.matmul(out.ap, lhsT=lhsT.ap, rhs=rhs.ap, start=start, stop=stop),
                       reads=[lhsT.k, rhs.k], writes=[out.k], pe_accum=True)

    def tr(self, out, in_, ident):
        nc = self.nc
        return self.op("pe", lambda: nc.tensor.transpose(out.ap, in_.ap, ident.ap),
                       reads=[in_.k, ident.k], writes=[out.k], pe_accum=True)

    def act(self, out, in_, func, bias=None, scale=None, accum=None):
        nc = self.nc
        kw = {}
        rd = [in_.k]
        wr = [out.k]
        if bias is not None:
            if isinstance(bias, V):
                kw["bias"] = bias.ap
                rd.append(bias.k)
            else:
                kw["bias"] = bias
        if scale is not None:
            if isinstance(scale, V):
                kw["scale"] = scale.ap
                rd.append(scale.k)
            else:
                kw["scale"] = scale
        if accum is not None:
            kw["accum_out"] = accum.ap
            wr.append(accum.k)
        return self.op("act", lambda: nc.scalar.activation(out=out.ap, in_=in_.ap, func=func, **kw),
                       reads=rd, writes=wr)

    def _e(self, e):
        return self.eng[e]

    def tt(self, e, out, in0, in1, op):
        return self.op(e, lambda: self._e(e).tensor_tensor(out=out.ap, in0=in0.ap, in1=in1.ap, op=op),
                       reads=[in0.k, in1.k], writes=[out.k])

    def ts(self, e, out, in0, s1, s2, op0, op1=None):
        rd = [in0.k]
        a1 = s1.ap if isinstance(s1, V) else s1
        a2 = s2.ap if isinstance(s2, V) else s2
        if isinstance(s1, V):
            rd.append(s1.k)
        if isinstance(s2, V):
            rd.append(s2.k)
        if op1 is None:
            return self.op(e, lambda: self._e(e).tensor_scalar(out=out.ap, in0=in0.ap, scalar1=a1, scalar2=None, op0=op0),
                           reads=rd, writes=[out.k])
        return self.op(e, lambda: self._e(e).tensor_scalar(out=out.ap, in0=in0.ap, scalar1=a1, scalar2=a2, op0=op0, op1=op1),
                       reads=rd, writes=[out.k])

    def stt(self, e, out, in0, scalar, in1, op0, op1):
        rd = [in0.k, in1.k]
        a = scalar.ap if isinstance(scalar, V) else scalar
        if isinstance(scalar, V):
            rd.append(scalar.k)
        return self.op(e, lambda: self._e(e).scalar_tensor_tensor(out=out.ap, in0=in0.ap, scalar=a, in1=in1.ap, op0=op0, op1=op1),
                       reads=rd, writes=[out.k])

    def cp(self, e, out, in_):
        if e == "act":
            return self.op(e, lambda: self.nc.scalar.copy(out=out.ap, in_=in_.ap), reads=[in_.k], writes=[out.k])
        return self.op(e, lambda: self._e(e).tensor_copy(out=out.ap, in_=in_.ap), reads=[in_.k], writes=[out.k])

    def memset(self, e, out, val):
        return self.op(e, lambda: self._e(e).memset(out.ap, val), reads=[], writes=[out.k])

    def red(self, e, out, in_, op, axis=AX.X):
        return self.op(e, lambda: self._e(e).tensor_reduce(out=out.ap, in_=in_.ap, axis=axis, op=op),
                       reads=[in_.k], writes=[out.k])

    def recip(self, e, out, in_):
        return self.op(e, lambda: self._e(e).reciprocal(out=out.ap, in_=in_.ap), reads=[in_.k], writes=[out.k])


class Ctx:
    pass


class Ring:
    def __init__(self, bufs):
        self.bufs = bufs
        self.i = 0

    def next(self):
        b = self.bufs[self.i % len(self.bufs)]
        self.i += 1
        return b


_UID = [0]


def sb(es, nc, name, shape, dt):
    _UID[0] += 1
    name = f"{name}_{_UID[0]}"
    t = es.enter_context(nc.sbuf_tensor(name, shape, dt))
    return Buf(t, name)


def sbring(es, nc, name, shape, dt, n):
    return Ring([sb(es, nc, f"{name}{i}", shape, dt) for i in range(n)])


def bc(v, shape):
    return V(v.ap.to_broadcast(shape), v.k)


def phase_norm(c, hin, gvec, uT):
    nc, s = c.nc, c.s
    with ExitStack() as es:
        gt = sb(es, nc, "n_g", [128, D], F32)
        xr = sbring(es, nc, "n_x", [128, D], F32, 2)
        sq = sb(es, nc, "n_sq", [128, D], F32)
        ur = sbring(es, nc, "n_u", [128, D], BF16, 2)
        st = sb(es, nc, "n_st", [128, NT, 4], F32)
        s.dma("sp", gt[:], V(gvec.partition_broadcast(128), "w_const"))
        for i in range(NT):
            xt = xr.next()
            ub = ur.next()
            stv = st.sub(str(i))
            s.dma("sp", xt[:], hin[i * 128:(i + 1) * 128, :])
            s.act(sq[:], xt[:], AF.Square, accum=stv[:, i, 0:1])
            s.ts("dve", stv[:, i, 1:2], stv[:, i, 0:1], 1.0 / D, EPS, ALU.mult, ALU.add)
            s.act(stv[:, i, 2:3], stv[:, i, 1:2], AF.Sqrt)
            s.recip("dve", stv[:, i, 3:4], stv[:, i, 2:3])
            s.stt("dve", ub[:], xt[:], stv[:, i, 3:4], gt[:], ALU.mult, ALU.mult)
            pt = c.ptr.next()
            for k in range(8):
                s.tr(pt[:, k, :], ub[:, k * 128:(k + 1) * 128], c.identb[:])
            s.cp("act", uT[:, :, i * 128:(i + 1) * 128], pt[:, 0:8, :])
    s.barrier()


def phase_project(c, uT, w_ap, segs):
    nc, s = c.nc, c.s
    with ExitStack() as es:
        wfr = sbring(es, nc, "p_wf", [128, 8, 512], F32, 2)
        wbr = sbring(es, nc, "p_wb", [128, 8, 512], BF16, 2)
        ofr = sbring(es, nc, "p_of", [128, 512], F32, 3)
        obr = sbring(es, nc, "p_ob", [128, 512], BF16, 3)
        wv = w_ap.rearrange("(ko p) n -> p ko n", p=128)
        ev = 0
        for seg in segs:
            (col0, ncols, mode, dst, dt, scale) = seg[:6]
            func = seg[6] if len(seg) > 6 else None
            for cb in range(0, ncols, 512):
                nb = min(512, ncols - cb)
                wf = wfr.next()
                wb = wbr.next()
                s.dma("sp", wf[:, :, 0:nb], V(wv[:, :, col0 + cb:col0 + cb + nb], "w_const"))
                s.cp("dve", wb.sub("a")[:, 0:4, 0:nb], wf[:, 0:4, 0:nb])
                s.cp("act", wb.sub("b")[:, 4:8, 0:nb], wf[:, 4:8, 0:nb])
                if mode == "tm":
                    for i in range(NT):
                        ps = c.pr.next()
                        for ko in range(8):
                            s.mm(ps[:, 0:nb], uT[:, ko, i * 128:(i + 1) * 128], wb.sub("a" if ko < 4 else "b")[:, ko, 0:nb],
                                 start=(ko == 0), stop=(ko == 7))
                        ot = (ofr if dt == F32 else obr).next()
                        if func is not None:
                            s.act(ot[:, 0:nb], ps[:, 0:nb], func, scale=scale)
                        elif ev % 2 == 0:
                            s.act(ot[:, 0:nb], ps[:, 0:nb], AF.Copy, scale=scale)
                        else:
                            s.ts("dve", ot[:, 0:nb], ps[:, 0:nb], scale, None, ALU.mult)
                        ev += 1
                        s.dma(STQ, dst[i * 128:(i + 1) * 128, cb:cb + nb], ot[:, 0:nb])
                else:
                    for fb in range(0, nb, 128):
                        fn = min(128, nb - fb)
                        for tb in range(T // 512):
                            ps = c.pr.next()
                            for ko in range(8):
                                s.mm(ps[0:fn, :], wb.sub("a" if ko < 4 else "b")[:, ko, fb:fb + fn], uT[:, ko, tb * 512:(tb + 1) * 512],
                                     start=(ko == 0), stop=(ko == 7))
                            ot = (ofr if dt == F32 else obr).next()
                            if ev % 2 == 0:
                                s.act(ot[0:fn, :], ps[0:fn, :], AF.Copy, scale=scale)
                            else:
                                s.ts("dve", ot[0:fn, :], ps[0:fn, :], scale, None, ALU.mult)
                            ev += 1
                            s.dma(STQ, dst[cb + fb:cb + fb + fn, tb * 512:(tb + 1) * 512], ot[0:fn, :])
    s.barrier()


class Finalizer:
    def __init__(self, c, es, w_out_ap, hin, hout):
        nc = c.nc
        self.c = c
        self.wo = sb(es, nc, "f_wo", [128, 16, D], BF16)
        self.yT = sbring(es, nc, "f_yT", [128, 16, 128], BF16, 2)
        self.hr = sbring(es, nc, "f_h", [128, D], F32, 2)
        self.hin, self.hout = hin, hout
        with ExitStack() as es2:
            stg = sbring(es2, nc, "f_stg", [128, 2, D], F32, 2)
            wv = w_out_ap.rearrange("(ko p) n -> p ko n", p=128)
            for k0 in range(0, 16, 2):
                st = stg.next()
                c.s.dma("sp", st[:], V(wv[:, k0:k0 + 2, :], "w_const"))
                c.s.cp("dve" if (k0 // 2) % 2 == 0 else "act", self.wo[:, k0:k0 + 2, :], st[:])
            c.s.barrier()

    def run(self, y, i):
        c, s = self.c, self.c.s
        yT = self.yT.next()
        for half in range(2):
            pt = c.ptr.next()
            for k in range(8):
                kk = half * 8 + k
                s.tr(pt[:, k, :], V(y.t[:, kk * 128:(kk + 1) * 128], y.key), c.identb[:])
            s.cp("act" if half == 0 else "dve", yT[:, half * 8:half * 8 + 8, :], pt[:, 0:8, :])
        ht = self.hr.next()
        s.dma("sp", ht[:], self.hin[i * 128:(i + 1) * 128, :])
        for n in range(2):
            ps = c.pr.next()
            for kc in range(16):
                s.mm(ps[:, :], yT[:, kc, :], self.wo[:, kc, n * 512:(n + 1) * 512],
                     start=(kc == 0), stop=(kc == 15))
            s.tt("dve", ht[:, n * 512:(n + 1) * 512], ps[:, :], ht[:, n * 512:(n + 1) * 512], ALU.add)
        s.dma(STQ, self.hout[i * 128:(i + 1) * 128, :], ht[:])


def phase_final_norm(c, hin, gvec, out):
    nc, s = c.nc, c.s
    with ExitStack() as es:
        gt = sb(es, nc, "fn_g", [128, D], F32)
        xr = sbring(es, nc, "fn_x", [128, D], F32, 2)
        sq = sb(es, nc, "fn_sq", [128, D], F32)
        orr = sbring(es, nc, "fn_o", [128, D], F32, 2)
        st = sb(es, nc, "fn_st", [128, NT, 4], F32)
        s.dma("sp", gt[:], V(gvec.partition_broadcast(128), "w_const"))
        for i in range(NT):
            xt = xr.next()
            ot = orr.next()
            stv = st.sub(str(i))
            s.dma("sp", xt[:], hin[i * 128:(i + 1) * 128, :])
            s.act(sq[:], xt[:], AF.Square, accum=stv[:, i, 0:1])
            s.ts("dve", stv[:, i, 1:2], stv[:, i, 0:1], 1.0 / D, EPS, ALU.mult, ALU.add)
            s.act(stv[:, i, 2:3], stv[:, i, 1:2], AF.Sqrt)
            s.recip("dve", stv[:, i, 3:4], stv[:, i, 2:3])
            s.stt("dve", ot[:], xt[:], stv[:, i, 3:4], gt[:], ALU.mult, ALU.mult)
            s.dma(STQ, out[i * 128:(i + 1) * 128, :], ot[:])
    s.barrier()


def conv_fm(c, srcT, nch_tiles, K, cw_ap, cb_ap, silu, emit):
    nc, s = c.nc, c.s
    pad = (K - 1) // 2
    with ExitStack() as es:
        cw = sb(es, nc, "cv_w", [128, nch_tiles, K], F32)
        cbias = sb(es, nc, "cv_b", [128, nch_tiles], F32)
        xr = sbring(es, nc, "cv_x", [128, L + 2 * pad], BF16, 2)
        dg = sbring(es, nc, "cv_dg", [128, K, 128], BF16, 2)
        cvr = sbring(es, nc, "cv_o", [128, 512], BF16, 3)
        s.dma("sp", cw[:], V(cw_ap.rearrange("(ct p) k -> p ct k", p=128), "w_const"))
        s.dma("sp", cbias[:], V(cb_ap.rearrange("(ct p) -> p ct", p=128), "w_const"), allow_slow_non_contiguous=True)
        for xb in xr.bufs:
            s.memset("pool", xb[:, 0:pad], 0.0)
            s.memset("pool", xb[:, L + pad:L + 2 * pad], 0.0)
        for ct in range(nch_tiles):
            d = dg.next()
            for k in range(K):
                s.ts("dve", d[:, k, :], c.identf[:], cw[:, ct, k:k + 1], None, ALU.mult)
            for b in range(BL):
                xt = xr.next()
                s.dma("sp", V(xt.t[:, pad:L + pad], xt.key + ".d"), srcT[ct * 128:(ct + 1) * 128, b * L:(b + 1) * L])
                for tb in range(L // 512):
                    ps = c.pr.next()
                    for k in range(K):
                        s.mm(ps[:, :], d[:, k, :],
                             V(xt.t[:, tb * 512 + k:tb * 512 + k + 512], (xt.key, xt.key + ".d")),
                             start=(k == 0), stop=(k == K - 1))
                    cv = cvr.next()
                    s.act(cv[:, :], ps[:, :], AF.Silu if silu else AF.Identity, bias=cbias[:, ct:ct + 1])
                    emit(ct, b, tb, cv[:, :])


def to_tm_store(c, cv, dst, row0, col0, stg_ring, mul=None):
    s = c.s
    pt = c.ptr.next()
    for j in range(4):
        s.tr(pt[:, j, :], V(cv.ap[:, j * 128:(j + 1) * 128], cv.k), c.identb[:])
    st = stg_ring.next()
    if mul is None:
        s.cp("dve", st[:, 0:4, :], pt[:, 0:4, :])
    else:
        s.tt("dve", st[:, 0:4, :], pt[:, 0:4, :], bc(V(mul.ap.unsqueeze(1), mul.k), [128, 4, 128]), ALU.mult)
    s.dma(STQ, V(dst.t[row0:row0 + 512, col0:col0 + 128].rearrange("(j p) c -> p j c", p=128), dst.key),
          st[:, 0:4, :])


def layer_ssd(c, W, hin, hout):
    nc, s = c.nc, c.s
    H, P, G, N = 32, 64, 8, 128
    dr = c.dram
    z_tm = dr("ssd_z", [T, DI], BF16)
    xbcT = dr("ssd_xbcT", [4096, T], BF16)
    dt_tm = dr("ssd_dt", [T, 64], F32)
    x_tm = dr("ssd_x", [T, DI], BF16)
    B_tm = dr("ssd_B", [T, 1024], BF16)
    BT = dr("ssd_BT", [1024, T], BF16)
    CT = dr("ssd_CT", [1024, T], BF16)
    yf = dr("ssd_yf", [T, DI], F32)

    with ExitStack() as es:
        uT = sb(es, nc, "uT", [128, 8, T], BF16)
        phase_norm(c, hin, W["ssd_norm"], uT)
        phase_project(c, uT, W["ssd_w_in"], [
            (0, DI, "tm", z_tm, BF16, 1.0, AF.Silu),
            (DI, 4096, "fm", xbcT, BF16, 1.0),
            (DI + 4096, 64, "tm", dt_tm, F32, 1.0),
        ])

    if DEBUG_STOP == "proj":
        return
    with ExitStack() as es:
        stg = sbring(es, nc, "sc_stg", [128, 4, 128], BF16, 3)

        def emit(ct, b, tb, cv):
            col = b * L + tb * 512
            if ct < 16:
                to_tm_store(c, cv, x_tm, col, ct * 128, stg)
            elif ct < 24:
                to_tm_store(c, cv, B_tm, col, (ct - 16) * 128, stg)
                s.dma(STQ, BT[(ct - 16) * 128:(ct - 15) * 128, col:col + 512], cv)
            else:
                s.dma(STQ, CT[(ct - 24) * 128:(ct - 23) * 128, col:col + 512], cv)

        conv_fm(c, xbcT, 32, 5, W["ssd_conv_wT"], W["ssd_conv_b"], True, emit)
    s.barrier()

    if DEBUG_STOP == "conv":
        return
    with ExitStack() as es:
        c.pr = Ring([c.pbanks[5], c.pbanks[4]])
        fin = Finalizer(c, es, W["ssd_w_out"], hin, hout)
        dtb = sb(es, nc, "ss_dtb", [128, 64], F32)
        aneg = sb(es, nc, "ss_a", [128, 64], F32)
        dsk = sb(es, nc, "ss_dsk", [128, 32], F32)
        gn = sb(es, nc, "ss_gn", [128, DI], F32)
        s.dma("sp", dtb[:], V(W["ssd_dt_bias"].rearrange("d h -> (d h)").partition_broadcast(128), "w_const"))
        s.dma("sp", aneg[:], V(W["ssd_a_log"].rearrange("d h -> (d h)").partition_broadcast(128), "w_const"))
        s.dma("sp", dsk[:], V(W["ssd_d"].partition_broadcast(128), "w_const"))
        s.dma("sp", gn[:], V(W["ssd_gnorm"].partition_broadcast(128), "w_const"))
        s.act(aneg[:], aneg[:], AF.Exp)
        s.ts("dve", aneg[:], aneg[:], -1.0, None, ALU.mult)
        xr = sbring(es, nc, "ss_x", [128, DI], BF16, 2)
        Br = sbring(es, nc, "ss_B", [128, 1024], BF16, 2)
        BTr = sbring(es, nc, "ss_BT", [128, G, 128], BF16, 2)
        CTr = sbring(es, nc, "ss_CT", [128, G, 128], BF16, 2)
        dtr = sbring(es, nc, "ss_dt", [128, 64], F32, 2)
        sm = sbring(es, nc, "ss_sm", [128, 8, 32], F32, 2)
        labcr = sbring(es, nc, "ss_labc", [128, H, 128], F32, 2)
        xdt = sbring(es, nc, "ss_xdt", [128, DI], BF16, 2)
        xw = sbring(es, nc, "ss_xw", [128, DI], BF16, 2)
        negm4 = sb(es, nc, "ss_negm4", [128, 2, 4, 128], F32)
        for d in range(2):
            s.cp("dve", negm4[:, d, :, :], bc(V(c.negm.t[:, d, :].unsqueeze(1), c.negm.key), [128, 4, 128]))
        Er = sbring(es, nc, "ss_E", [128, 512], F32, 2)
        PTr = sbring(es, nc, "ss_PT", [128, 4, 128], BF16, 2)
        t1r = sbring(es, nc, "ss_t1", [128, 256], F32, 2)
        yacc = sbring(es, nc, "ss_yacc", [128, DI], F32, 2)
        st32 = sb(es, nc, "ss_st32", [128, G, 256], F32)
        stbf = sb(es, nc, "ss_stbf", [128, G, 256], BF16)
        zr = sbring(es, nc, "ss_z", [128, DI], BF16, 1)
        yfr = sbring(es, nc, "ss_yf", [128, DI], F32, 1)
        tmp = sb(es, nc, "ss_tmp", [128, DI], F32)
        gst = sbring(es, nc, "ss_gst", [128, 4, 8], F32, 2)
        yb = sbring(es, nc, "ss_yb", [128, DI], BF16, 1)
        allg = [str(g) for g in range(G)]

        def r3(buf, q):
            return V(buf.t[:].rearrange("p (h q) -> p h q", q=q), buf.key)

        pab = c.pbanks[0:2]
        pyb = c.pbanks[2:4]
        pstb = c.pbanks[4]
        pmisc = c.pbanks[5]
        c.pr = Ring([c.pbanks[5], c.pbanks[4]])
        cbs = sbring(es, nc, "ss_cbs", [128, G, 128], F32, 2)

        def prep(b, d, ci):
            i = b * NQ + ci
            r0 = i * 128
            tri = c.tri[:, d, :]
            xt, Bt, BTt, CTt, dtt = xr.next(), Br.next(), BTr.next(), CTr.next(), dtr.next()
            s.dma("sp", xt[:], x_tm[r0:r0 + 128, :])
            s.dma("sp", Bt[:], B_tm[r0:r0 + 128, :])
            s.dma("sp", BTt[:], V(BT.t[:, r0:r0 + 128].rearrange("(g n) t -> n g t", n=128), BT.key))
            s.dma("sp", CTt[:], V(CT.t[:, r0:r0 + 128].rearrange("(g n) t -> n g t", n=128), CT.key))
            s.dma("sp", dtt[:], dt_tm[r0:r0 + 128, :])
            m = sm.next()
            s.tt("dve", m[:, 0:2, :], V(dtt.t[:].rearrange("p (a h) -> p a h", a=2), dtt.key),
                 V(dtb.t[:].rearrange("p (a h) -> p a h", a=2), dtb.key), ALU.add)
            s.act(m[:, 0:2, :], m[:, 0:2, :], AF.Exp)
            s.act(m[:, 0:2, :], m[:, 0:2, :], AF.Ln, bias=c.one[:, 0:1])
            dtd = m[:, d, :]
            s.tt("dve", m[:, 2, :], dtd, aneg[:, d * 32:(d + 1) * 32], ALU.mult)
            la = m[:, 2, :]
            pc = pmisc
            s.mm(pc[:, 0:32], tri, la)
            s.mm(pc[:, 32:64], c.onesf[:], la)
            s.act(m[:, 3, :], pc[:, 0:32], AF.Exp)
            s.cp("dve", m[:, 5, :], pc[:, 0:32])
            s.tt("dve", m[:, 4, :], pc[:, 32:64], m[:, 5, :], ALU.subtract)
            s.act(m[:, 4, :], m[:, 4, :], AF.Exp)
            s.act(m[:, 6, :], pc[:, 32:64], AF.Exp)
            s.tt("dve", m[:, 7, :], m[:, 4, :], dtd, ALU.mult)
            xd, xwt = xdt.next(), xw.next()
            x3 = r3(xt, P)
            s.tt("dve", r3(xd, P), x3, bc(V(dtd.ap.unsqueeze(2), dtd.k), [128, H, P]), ALU.mult)
            s.tt("dve", r3(xwt, P), x3, bc(V(m.t[:, 7, :].unsqueeze(2), m.key), [128, H, P]), ALU.mult)
            labc = labcr.next()
            s.cp("act", labc[:], bc(V(la.ap.unsqueeze(2), la.k), [128, H, 128]))
            s.ts("dve", m[:, 5, :], m[:, 5, :], -1.0, None, ALU.mult)
            cb = cbs.next()
            for half in range(2):
                for gq in range(4):
                    g = half * 4 + gq
                    s.mm(V(pmisc.t[:, gq * 128:(gq + 1) * 128], pmisc.key), BTt[:, g, :], CTt[:, g, :])
                s.cp("act", V(cb.t[:, half * 4:half * 4 + 4, :].rearrange("p g l -> p (g l)"), cb.key), pmisc[:, :])
            return dict(i=i, r0=r0, xt=xt, Bt=Bt, CTt=CTt, m=m, xd=xd, xwt=xwt, labc=labc, cb=cb, x3=x3)

        def heavy(b, d, ci, t):
            i, r0, m = t["i"], t["r0"], t["m"]
            tri = c.tri[:, d, :]
            xd, xwt, labc, cb, CTt, Bt, x3 = t["xd"], t["xwt"], t["labc"], t["cb"], t["CTt"], t["Bt"], t["x3"]
            ya = yacc.next()
            Es, PTs = {}, {}

            def stage_a(g):
                pa = pab[g % 2]
                s.mm(pa[:, :], c.identf[:], V(negm4.t[:, d, :, :].rearrange("p a l -> p (a l)"), negm4.key),
                     start=True, stop=False)
                for hh in range(4):
                    h = g * 4 + hh
                    s.mm(V(pa.t[:, hh * 128:(hh + 1) * 128], pa.key), labc[:, h, :], tri, start=False, stop=(hh == 3))
                E = Er.next()
                for hh in range(4):
                    h = g * 4 + hh
                    s.act(E[:, hh * 128:(hh + 1) * 128], V(pa.t[:, hh * 128:(hh + 1) * 128], pa.key), AF.Exp,
                          bias=m[:, 5, h:h + 1])
                Es[g] = E

            def stage_b(g):
                E = Es.pop(g)
                PT = PTr.next()
                s.tt("dve", PT[:], V(E.t[:].rearrange("p (a l) -> p a l", a=4), E.key),
                     bc(V(cb.t[:, g, :].unsqueeze(1), cb.key), [128, 4, 128]), ALU.mult)
                py = pyb[g % 2]
                for hh in range(4):
                    h = g * 4 + hh
                    s.mm(V(py.t[:, hh * 64:(hh + 1) * 64], py.key), PT[:, hh, :], xd[:, h * 64:(h + 1) * 64])
                s.mm(V(py.t[:, 256:512], py.key), CTt[:, g, :], stbf.sub(str(g))[:, g, :])
                t1 = t1r.next()
                s.tt("dve", V(t1.t[:].rearrange("p (a q) -> p a q", a=4), t1.key),
                     V(py.t[:, 256:512].rearrange("p (a q) -> p a q", a=4), py.key),
                     bc(V(m.t[:, 3, g * 4:(g + 1) * 4].unsqueeze(2), m.key), [128, 4, P]), ALU.mult)
                s.tt("dve", ya[:, g * 256:(g + 1) * 256], py[:, 0:256], t1[:], ALU.add)
                s.mm(pstb[:, 0:256], Bt[:, g * 128:(g + 1) * 128], xwt[:, g * 256:(g + 1) * 256])
                sg = st32.sub(str(g))
                sg3 = V(sg.t[:, g, :].rearrange("p (a q) -> p a q", a=4), sg.key)
                s.tt("dve", sg3, sg3,
                     bc(V(m.t[:, 6, g * 4:(g + 1) * 4].unsqueeze(2), m.key), [128, 4, P]), ALU.mult)
                s.tt("dve", sg[:, g, :], sg[:, g, :], pstb[:, 0:256], ALU.add)
                s.cp("act", stbf.sub(str(g))[:, g, :], sg[:, g, :])

            for gg in range(G + 1):
                if gg < G:
                    stage_a(gg)
                if gg >= 1:
                    stage_b(gg - 1)

            if d == 0:
                s.dma(STQ, yf[r0:r0 + 128, :], ya[:])
            else:
                yft, zt = yfr.next(), zr.next()
                s.dma("sp", yft[:], yf[r0:r0 + 128, :])
                s.dma("sp", zt[:], z_tm[r0:r0 + 128, :])
                s.tt("dve", yft[:], yft[:], ya[:], ALU.add)
                s.tt("dve", r3(tmp, P), x3, bc(V(dsk.t[:].unsqueeze(2), dsk.key), [128, H, P]), ALU.mult)
                s.tt("dve", yft[:], yft[:], tmp[:], ALU.add)
                s.tt("dve", yft[:], yft[:], zt[:], ALU.mult)
                gs = gst.next()
                s.act(tmp[:], yft[:], AF.Square)
                s.red("dve", gs[:, 0, :], r3(tmp, 256), ALU.add)
                s.ts("dve", gs[:, 1, :], gs[:, 0, :], 1.0 / 256, EPS, ALU.mult, ALU.add)
                s.act(gs[:, 2, :], gs[:, 1, :], AF.Ln)
                s.act(gs[:, 3, :], gs[:, 2, :], AF.Exp, scale=-0.5)
                s.tt("dve", r3(yft, 256), r3(yft, 256),
                     bc(V(gs.t[:, 3, :].unsqueeze(2), gs.key), [128, 8, 256]), ALU.mult)
                ybt = yb.next()
                s.tt("dve", ybt[:], yft[:], gn[:], ALU.mult)
                fin.run(ybt, i)

        seq = []
        for b in range(BL):
            for d in range(2):
                order = list(range(NQ)) if d == 0 else list(range(NQ - 1, -1, -1))
                for k, ci in enumerate(order):
                    seq.append((b, d, ci, k == 0))
        nxt = prep(*seq[0][:3])
        for idx, (b, d, ci, first) in enumerate(seq):
            cur = nxt
            if first:
                s.memset("dve", st32.subs(allg)[:], 0.0)
                s.memset("pool", stbf.subs(allg)[:], 0.0)
            if idx + 1 < len(seq):
                nxt = prep(*seq[idx + 1][:3])
            heavy(b, d, ci, cur)
    s.barrier()
    c.pr = Ring(c.pbanks)


def layer_gla(c, W, hin, hout):
    nc, s = c.nc, c.s
    H, DK, DV = 4, 128, 512
    dr = c.dram
    q_tm = dr("gla_q", [T, 512], BF16)
    k_tm = dr("gla_k", [T, 512], BF16)
    v_tm = dr("gla_v", [T, DI], BF16)
    z_tm = dr("gla_z", [T, DI], BF16)
    glT = dr("gla_glT", [32, T], F32)
    of = dr("gla_of", [T, DI], F32)

    with ExitStack() as es:
        uT = sb(es, nc, "uT", [128, 8, T], BF16)
        phase_norm(c, hin, W["gla_norm"], uT)
        phase_project(c, uT, W["gla_w_in"], [
            (0, 512, "tm", q_tm, BF16, DK ** -0.5),
            (512, 512, "tm", k_tm, BF16, 1.0),
            (1024, DI, "tm", v_tm, BF16, 1.0),
            (3072, DI, "tm", z_tm, BF16, 1.0, AF.Silu),
            (5120, 32, "fm", glT, F32, 1.0),
        ])

    with ExitStack() as es:
        fin = Finalizer(c, es, W["gla_w_out"], hin, hout)
        wg = sb(es, nc, "g_wg", [16, 2, 512], F32)
        bg = sb(es, nc, "g_bg", [128, 2, 512], F32)
        on = sb(es, nc, "g_on", [128, 512], F32)
        s.dma("sp", wg[:], V(W["gla_w_gate"].rearrange("d r k -> r d k"), "w_const"))
        s.dma("sp", V(bg.t[:].rearrange("p d k -> p (d k)"), bg.key),
              V(W["gla_b_gate"].rearrange("d k -> (d k)").partition_broadcast(128), "w_const"))
        s.dma("sp", on[:], V(W["gla_onorm"].partition_broadcast(128), "w_const"))
        qr = sbring(es, nc, "g_q", [128, 512], BF16, 2)
        kr = sbring(es, nc, "g_k", [128, 512], BF16, 2)
        vr = sbring(es, nc, "g_v", [128, DI], BF16, 2)
        glr = sbring(es, nc, "g_gl", [16, 128], F32, 2)
        Lpr = sbring(es, nc, "g_Lp", [128, 512], F32, 2)
        Eqr = sbring(es, nc, "g_Eq", [128, 512], F32, 2)
        Ekr = sbring(es, nc, "g_Ek", [128, 512], F32, 2)
        Ee = sbring(es, nc, "g_Ee", [128, 4], F32, 2)
        qsr = sbring(es, nc, "g_qs", [128, 512], BF16, 2)
        ksr = sbring(es, nc, "g_ks", [128, 512], BF16, 2)
        qkTr = sbring(es, nc, "g_qkT", [128, 8, 128], BF16, 2)
        PTr = sbring(es, nc, "g_PT", [128, 4, 128], BF16, 2)
        oaccr = sbring(es, nc, "g_oacc", [128, DI], F32, 2)
        S32 = sb(es, nc, "g_S32", [128, H, DV], F32)
        Sbf = sb(es, nc, "g_Sbf", [128, H, DV], BF16)
        oft = sb(es, nc, "g_of", [128, DI], F32)
        zt = sb(es, nc, "g_z", [128, DI], BF16)
        tmp = sb(es, nc, "g_tmp", [128, DI], F32)
        gst = sbring(es, nc, "g_st", [128, 4, 4], F32, 2)
        yb = sb(es, nc, "g_yb", [128, DI], BF16)
        allh = [str(h) for h in range(H)]

        def r3(buf, q):
            return V(buf.t[:].rearrange("p (h q) -> p h q", q=q), buf.key)

        def prep(b, d, ci):
            tri = c.tri[:, d, :]
            i = b * NQ + ci
            r0 = i * 128
            qt, kt, vt, gt = qr.next(), kr.next(), vr.next(), glr.next()
            Lp, Eq, Ek, qs, ks = Lpr.next(), Eqr.next(), Ekr.next(), qsr.next(), ksr.next()
            qkT, PT, oacc = qkTr.next(), PTr.next(), oaccr.next()
            s.dma("sp", qt[:], q_tm[r0:r0 + 128, :])
            s.dma("sp", kt[:], k_tm[r0:r0 + 128, :])
            s.dma("sp", vt[:], v_tm[r0:r0 + 128, :])
            s.dma("sp", gt[:], glT[d * 16:(d + 1) * 16, r0:r0 + 128])
            pg = c.pr.next()
            s.mm(pg[:, :], gt[:, :], wg[:, d, :])
            s.tt("dve", Lp[:], pg[:, :], bg[:, d, :], ALU.add)
            s.act(Lp[:], Lp[:], AF.Exp, scale=-1.0)
            s.act(Lp[:], Lp[:], AF.Ln, bias=c.one[:, 0:1])
            pcum = c.pr.next()
            s.mm(pcum[:, :], tri, Lp[:])
            s.act(Eq[:], pcum[:, :], AF.Exp, scale=-1.0 / 16)
            s.act(Ek[:], pcum[:, :], AF.Exp, scale=1.0 / 16)
            ptot = c.pr.next()
            for h in range(H):
                s.mm(ptot[:, h:h + 1], Lp[:, h * 128:(h + 1) * 128], c.onesf[:, 0:1])
            ee = Ee.next()
            s.act(ee[:], ptot[:, 0:4], AF.Exp, scale=-1.0 / 16)
            s.tt("dve", qs[:], qt[:], Eq[:], ALU.mult)
            s.tt("dve", ks[:], kt[:], Ek[:], ALU.mult)
            pt = c.ptr.next()
            for h in range(H):
                s.tr(pt[:, h, :], qs[:, h * 128:(h + 1) * 128], c.identb[:])
                s.tr(pt[:, 4 + h, :], ks[:, h * 128:(h + 1) * 128], c.identb[:])
            s.cp("act", qkT[:], pt[:, :, :])
            pS = c.pr.next()
            for h in range(H):
                s.mm(pS[:, h * 128:(h + 1) * 128], qkT[:, 4 + h, :], qkT[:, h, :])
            s.tt("dve", PT[:], V(pS.t[:, :].rearrange("p (a l) -> p a l", a=4), pS.key),
                 bc(V(tri.ap.unsqueeze(1), tri.k), [128, 4, 128]), ALU.mult)
            return dict(i=i, r0=r0, vt=vt, qkT=qkT, PT=PT, ks=ks, ee=ee, oacc=oacc)

        def heavy(b, d, ci, t):
            i, r0, vt, qkT, PT, ks, ee, oacc = (t[k] for k in ("i", "r0", "vt", "qkT", "PT", "ks", "ee", "oacc"))
            for h in range(H):
                po = c.pr.next()
                s.mm(po[:, :], PT[:, h, :], vt[:, h * DV:(h + 1) * DV], start=True, stop=False)
                s.mm(po[:, :], qkT[:, h, :], Sbf.sub(str(h))[:, h, :], start=False, stop=True)
                s.cp("act", oacc[:, h * DV:(h + 1) * DV], po[:, :])
                pu = c.pr.next()
                s.mm(pu[:, :], ks[:, h * 128:(h + 1) * 128], vt[:, h * DV:(h + 1) * DV])
                sh = S32.sub(str(h))
                s.tt("dve", sh[:, h, :], sh[:, h, :], pu[:, :], ALU.add)
                s.act(sh[:, h, :], sh[:, h, :], AF.Copy, scale=ee[:, h:h + 1])
                s.cp("dve", Sbf.sub(str(h))[:, h, :], sh[:, h, :])
            if d == 0:
                s.dma(STQ, of[r0:r0 + 128, :], oacc[:])
            else:
                s.dma("sp", oft[:], of[r0:r0 + 128, :])
                s.dma("sp", zt[:], z_tm[r0:r0 + 128, :])
                s.tt("dve", oft[:], oft[:], oacc[:], ALU.add)
                gs = gst.next()
                s.act(tmp[:], oft[:], AF.Square)
                s.red("dve", gs[:, 0, :], r3(tmp, DV), ALU.add)
                s.ts("dve", gs[:, 1, :], gs[:, 0, :], 1.0 / DV, EPS, ALU.mult, ALU.add)
                s.act(gs[:, 2, :], gs[:, 1, :], AF.Ln)
                s.act(gs[:, 3, :], gs[:, 2, :], AF.Exp, scale=-0.5)
                s.tt("dve", r3(oft, DV), r3(oft, DV), bc(V(gs.t[:, 3, :].unsqueeze(2), gs.key), [128, H, DV]), ALU.mult)
                s.tt("dve", r3(oft, DV), r3(oft, DV), bc(V(on.t[:].unsqueeze(1), on.key), [128, H, DV]), ALU.mult)
                s.tt("dve", yb[:], oft[:], zt[:], ALU.mult)
                fin.run(yb, i)


        seq = []
        for b in range(BL):
            for d in range(2):
                order = list(range(NQ)) if d == 0 else list(range(NQ - 1, -1, -1))
                for k, ci in enumerate(order):
                    seq.append((b, d, ci, k == 0))
        nxt = prep(*seq[0][:3])
        for idx, (b, d, ci, first) in enumerate(seq):
            cur = nxt
            if first:
                s.memset("dve", S32.subs(allh)[:], 0.0)
                s.memset("pool", Sbf.subs(allh)[:], 0.0)
            if idx + 1 < len(seq):
                nxt = prep(*seq[idx + 1][:3])
            heavy(b, d, ci, cur)
    s.barrier()


def layer_mlstm(c, W, hin, hout):
    nc, s = c.nc, c.s
    H, DK, DV = 4, 256, 512
    dr = c.dram
    xmT = dr("ml_xmT", [DI, T], BF16)
    z_tm = dr("ml_z", [T, DI], BF16)
    og_tm = dr("ml_og", [T, DI], BF16)
    gt_tm = dr("ml_gates", [T, 16], F32)
    chT = dr("ml_chT", [DI, T], BF16)
    ch_tm = dr("ml_ch", [T, DI], BF16)
    qk_tm = dr("ml_qk", [T, H * 512], BF16)
    v_tm = dr("ml_v", [T, DI], BF16)
    hf = dr("ml_hf", [T, DI], F32)

    with ExitStack() as es:
        uT = sb(es, nc, "uT", [128, 8, T], BF16)
        phase_norm(c, hin, W["ml_norm"], uT)
        phase_project(c, uT, W["ml_w_in"], [
            (0, DI, "fm", xmT, BF16, 1.0),
            (DI, DI, "tm", z_tm, BF16, 1.0, AF.Silu),
            (2 * DI, DI, "tm", og_tm, BF16, 1.0, AF.Sigmoid),
            (3 * DI, 16, "tm", gt_tm, F32, 1.0),
        ])

    with ExitStack() as es:
        stg = sbring(es, nc, "mc_stg", [128, 4, 128], BF16, 3)
        skc = sb(es, nc, "mc_skip", [128, DI], F32)
        s.dma("sp", skc[:], V(W["ml_skip"].rearrange("h d -> (h d)").partition_broadcast(128), "w_const"))

        def emit(ct, b, tb, cv):
            col = b * L + tb * 512
            to_tm_store(c, cv, ch_tm, col, ct * 128, stg, mul=skc[:, ct * 128:(ct + 1) * 128])
            s.dma(STQ, chT[ct * 128:(ct + 1) * 128, col:col + 512], cv)

        conv_fm(c, xmT, 16, 5, W["ml_conv_wT"], W["ml_conv_b"], True, emit)
    s.barrier()

    with ExitStack() as es:
        wq = sb(es, nc, "mq_wq", [128, H, 4, 256], BF16)
        wk = sb(es, nc, "mq_wk", [128, H, 4, 256], BF16)
        wv = sb(es, nc, "mq_wv", [128, H, 4, 512], BF16)
        with ExitStack() as es2:
            stf = sbring(es2, nc, "mq_stg", [128, 4096], F32, 2)
            st = stf.next()
            sv = V(st.t[:].rearrange("p (h k n) -> p h k n", h=4, k=4), st.key)
            s.dma("sp", sv, V(W["ml_w_q"].rearrange("h (k p) n -> p h k n", p=128), "w_const"))
            s.cp("dve", wq[:], sv)
            st = stf.next()
            sv = V(st.t[:].rearrange("p (h k n) -> p h k n", h=4, k=4), st.key)
            s.dma("sp", sv, V(W["ml_w_k"].rearrange("h (k p) n -> p h k n", p=128), "w_const"))
            s.cp("act", wk[:], sv)
            for hh in range(2):
                st = stf.next()
                sv = V(st.t[:].rearrange("p (h k n) -> p h k n", h=2, k=4), st.key)
                s.dma("sp", sv, V(W["ml_w_v"][hh * 2:hh * 2 + 2].rearrange("h (k p) n -> p h k n", p=128), "w_const"))
                s.cp("dve" if hh == 0 else "act", wv[:, hh * 2:hh * 2 + 2, :, :], sv)
            s.barrier()
        chr_ = sbring(es, nc, "mq_ch", [128, 16, 128], BF16, 2)
        xmr = sbring(es, nc, "mq_xm", [128, 16, 128], BF16, 2)
        qko = sbring(es, nc, "mq_qko", [128, H * 512], BF16, 2)
        vo = sbring(es, nc, "mq_vo", [128, DI], BF16, 2)
        for i in range(NT):
            cht, xmt, qo, vot = chr_.next(), xmr.next(), qko.next(), vo.next()
            s.dma("sp", cht[:], V(chT.t[:, i * 128:(i + 1) * 128].rearrange("(j p) t -> p j t", p=128), chT.key))
            s.dma("sp", xmt[:], V(xmT.t[:, i * 128:(i + 1) * 128].rearrange("(j p) t -> p j t", p=128), xmT.key))
            for h in range(H):
                pq = c.pr.next()
                for kc in range(4):
                    s.mm(pq[:, 0:256], cht[:, h * 4 + kc, :], wq[:, h, kc, :], start=(kc == 0), stop=(kc == 3))
                for kc in range(4):
                    s.mm(pq[:, 256:512], cht[:, h * 4 + kc, :], wk[:, h, kc, :], start=(kc == 0), stop=(kc == 3))
                s.cp("act", qo[:, h * 512:(h + 1) * 512], pq[:, :])
                pv = c.pr.next()
                for kc in range(4):
                    s.mm(pv[:, :], xmt[:, h * 4 + kc, :], wv[:, h, kc, :], start=(kc == 0), stop=(kc == 3))
                s.cp("dve", vot[:, h * 512:(h + 1) * 512], pv[:, :])
            s.dma(STQ, qk_tm[i * 128:(i + 1) * 128, :], qo[:])
            s.dma(STQ, v_tm[i * 128:(i + 1) * 128, :], vot[:])
    s.barrier()

    with ExitStack() as es:
        c.pr = Ring(c.pbanks[0:5])
        psm = c.pbanks[5]
        fin = Finalizer(c, es, W["ml_w_out"], hin, hout)
        gb = sb(es, nc, "m_gb", [128, 16], F32)
        on = sb(es, nc, "m_on", [128, 512], F32)
        skp = sb(es, nc, "m_skip", [128, DI], F32)
        s.dma("sp", gb[:], V(W["ml_gate_b"].rearrange("a b c -> (a b c)").partition_broadcast(128), "w_const"))
        s.dma("sp", on[:], V(W["ml_onorm"].partition_broadcast(128), "w_const"))
        s.dma("sp", skp[:], V(W["ml_skip"].rearrange("h d -> (h d)").partition_broadcast(128), "w_const"))
        onesb = sb(es, nc, "m_onesb", [128, 2], BF16)
        s.memset("dve", onesb[:], 1.0)
        qkr = sbring(es, nc, "m_qk", [128, H, 512], BF16, 2)
        vr = sbring(es, nc, "m_v", [128, DI], BF16, 2)
        gr = sbring(es, nc, "m_g", [128, 16], F32, 2)
        sm = sbring(es, nc, "m_sm", [128, 8, 4], F32, 2)
        qsr = sbring(es, nc, "m_qs", [128, H, 256], BF16, 2)
        ksr = sbring(es, nc, "m_ks", [128, H, 256], BF16, 2)
        qTr = sbring(es, nc, "m_qT", [128, 8, 128], BF16, 2)
        kTr = sbring(es, nc, "m_kT", [128, 8, 128], BF16, 2)
        PTr = sbring(es, nc, "m_PT", [128, 4, 128], BF16, 2)
        haccr = sbring(es, nc, "m_hacc", [128, DI], F32, 2)
        C32 = sb(es, nc, "m_C32", [128, 8, DV], F32)
        Cbf = sb(es, nc, "m_Cbf", [128, 8, DV], BF16)
        n32 = sb(es, nc, "m_n32", [128, 8], F32)
        nbf = sb(es, nc, "m_nbf", [128, 8], BF16)
        hft = sb(es, nc, "m_hf", [128, DI], F32)
        ogt = sb(es, nc, "m_og", [128, DI], BF16)
        zt = sb(es, nc, "m_z", [128, DI], BF16)
        cht = sb(es, nc, "m_ch", [128, DI], BF16)
        tmp = sb(es, nc, "m_tmp", [128, DI], F32)
        gst = sbring(es, nc, "m_st", [128, 4, 4], F32, 2)
        yb = sb(es, nc, "m_yb", [128, DI], BF16)
        allj = [str(j) for j in range(8)]

        def r3(buf, q):
            return V(buf.t[:].rearrange("p (h q) -> p h q", q=q), buf.key)

        def prep(b, d, ci):
            tri = c.tri[:, d, :]
            i = b * NQ + ci
            r0 = i * 128
            qkt, vt, gt = qkr.next(), vr.next(), gr.next()
            qs, ks, qT, kT, PT, hacc = qsr.next(), ksr.next(), qTr.next(), kTr.next(), PTr.next(), haccr.next()
            s.dma("sp", V(qkt.t[:].rearrange("p h n -> p (h n)"), qkt.key), qk_tm[r0:r0 + 128, :])
            s.dma("sp", vt[:], v_tm[r0:r0 + 128, :])
            s.dma("sp", gt[:], gt_tm[r0:r0 + 128, :])
            m = sm.next()
            s.tt("dve", gt[:], gt[:], gb[:], ALU.add)
            ig = gt[:, d * 8:d * 8 + 4]
            fr = gt[:, d * 8 + 4:d * 8 + 8]
            s.act(m[:, 0, :], fr, AF.Exp, scale=-1.0)
            s.act(m[:, 0, :], m[:, 0, :], AF.Ln, bias=c.one[:, 0:1])
            s.mm(psm[:, 0:4], tri, m[:, 0, :])
            s.mm(psm[:, 4:8], c.onesf[:], m[:, 0, :])
            s.act(m[:, 1, :], psm[:, 0:4], AF.Exp, scale=-1.0)
            s.tt("dve", m[:, 2, :], psm[:, 0:4], ig, ALU.add)
            s.act(m[:, 2, :], m[:, 2, :], AF.Exp, bias=c.one[:, 3:4])
            s.act(m[:, 3, :], psm[:, 4:8], AF.Exp, scale=-1.0)
            s.tt("dve", qs[:], V(qkt.t[:, :, 0:256], qkt.key),
                 bc(V(m.t[:, 1, :].unsqueeze(2), m.key), [128, H, 256]), ALU.mult)
            s.tt("dve", ks[:], V(qkt.t[:, :, 256:512], qkt.key),
                 bc(V(m.t[:, 2, :].unsqueeze(2), m.key), [128, H, 256]), ALU.mult)
            pt = c.ptr.next()
            for j in range(8):
                s.tr(pt[:, j, :], qs[:, j // 2, (j % 2) * 128:(j % 2 + 1) * 128], c.identb[:])
            s.cp("act", qT[:], pt[:, :, :])
            pt = c.ptr.next()
            for j in range(8):
                s.tr(pt[:, j, :], ks[:, j // 2, (j % 2) * 128:(j % 2 + 1) * 128], c.identb[:])
            s.cp("dve", kT[:], pt[:, :, :])
            pS = c.pr.next()
            for h in range(H):
                for kc in range(2):
                    s.mm(pS[:, h * 128:(h + 1) * 128], kT[:, h * 2 + kc, :], qT[:, h * 2 + kc, :],
                         start=(kc == 0), stop=(kc == 1))
            s.tt("dve", PT[:], V(pS.t[:, :].rearrange("p (a l) -> p a l", a=4), pS.key),
                 bc(V(tri.ap.unsqueeze(1), tri.k), [128, 4, 128]), ALU.mult)
            return dict(i=i, r0=r0, vt=vt, qT=qT, kT=kT, PT=PT, ks=ks, m=m, hacc=hacc)

        def heavy(b, d, ci, t):
            i, r0, vt, qT, kT, PT, ks, m, hacc = (t[k] for k in ("i", "r0", "vt", "qT", "kT", "PT", "ks", "m", "hacc"))
            for h in range(H):
                s.mm(psm[:, 8 + h:9 + h], PT[:, h, :], onesb[:, 0:1], start=True, stop=False)
                for kc in range(2):
                    s.mm(psm[:, 8 + h:9 + h], qT[:, h * 2 + kc, :], nbf[:, h * 2 + kc:h * 2 + kc + 1],
                         start=False, stop=(kc == 1))
            s.act(m[:, 4, :], psm[:, 8:12], AF.Abs)
            s.ts("dve", m[:, 4, :], m[:, 4, :], 1.0, None, ALU.max)
            s.recip("dve", m[:, 5, :], m[:, 4, :])
            for h in range(H):
                po = c.pr.next()
                s.mm(po[:, :], PT[:, h, :], vt[:, h * DV:(h + 1) * DV], start=True, stop=False)
                for kc in range(2):
                    s.mm(po[:, :], qT[:, h * 2 + kc, :], Cbf.sub(str(h * 2 + kc))[:, h * 2 + kc, :],
                         start=False, stop=(kc == 1))
                s.act(hacc[:, h * DV:(h + 1) * DV], po[:, :], AF.Copy, scale=m[:, 5, h:h + 1])
                for kc in range(2):
                    j = h * 2 + kc
                    pu = c.pr.next()
                    s.mm(pu[:, :], ks[:, h, kc * 128:(kc + 1) * 128], vt[:, h * DV:(h + 1) * DV])
                    s.mm(psm[:, 16 + j:17 + j], ks[:, h, kc * 128:(kc + 1) * 128], onesb[:, 0:1])
                    cj = C32.sub(str(j))
                    s.tt("dve", cj[:, j, :], cj[:, j, :], pu[:, :], ALU.add)
                    s.act(cj[:, j, :], cj[:, j, :], AF.Copy, scale=m[:, 3, h:h + 1])
                    s.cp("dve" if kc == 0 else "act", Cbf.sub(str(j))[:, j, :], cj[:, j, :])
            s.tt("dve", n32[:], n32[:], psm[:, 16:24], ALU.add)
            s.tt("dve", V(n32.t[:].rearrange("p (h k) -> p h k", k=2), n32.key),
                 V(n32.t[:].rearrange("p (h k) -> p h k", k=2), n32.key),
                 bc(V(m.t[:, 3, :].unsqueeze(2), m.key), [128, H, 2]), ALU.mult)
            s.cp("dve", nbf[:], n32[:])
            if d == 0:
                s.dma(STQ, hf[r0:r0 + 128, :], hacc[:])
            else:
                s.dma("sp", hft[:], hf[r0:r0 + 128, :])
                s.dma("sp", ogt[:], og_tm[r0:r0 + 128, :])
                s.dma("sp", zt[:], z_tm[r0:r0 + 128, :])
                s.dma("sp", cht[:], ch_tm[r0:r0 + 128, :])
                s.tt("dve", hft[:], hft[:], hacc[:], ALU.add)
                s.tt("dve", hft[:], hft[:], ogt[:], ALU.mult)
                gs = gst.next()
                s.act(tmp[:], hft[:], AF.Square)
                s.red("dve", gs[:, 0, :], r3(tmp, DV), ALU.add)
                s.ts("dve", gs[:, 1, :], gs[:, 0, :], 1.0 / DV, EPS, ALU.mult, ALU.add)
                s.act(gs[:, 2, :], gs[:, 1, :], AF.Ln)
                s.act(gs[:, 3, :], gs[:, 2, :], AF.Exp, scale=-0.5)
                s.tt("dve", r3(hft, DV), r3(hft, DV), bc(V(gs.t[:, 3, :].unsqueeze(2), gs.key), [128, H, DV]), ALU.mult)
                s.tt("dve", r3(hft, DV), r3(hft, DV), bc(V(on.t[:].unsqueeze(1), on.key), [128, H, DV]), ALU.mult)
                s.tt("dve", hft[:], hft[:], cht[:], ALU.add)
                s.tt("dve", yb[:], hft[:], zt[:], ALU.mult)
                fin.run(yb, i)


        seq = []
        for b in range(BL):
            for d in range(2):
                order = list(range(NQ)) if d == 0 else list(range(NQ - 1, -1, -1))
                for k, ci in enumerate(order):
                    seq.append((b, d, ci, k == 0))
        nxt = prep(*seq[0][:3])
        for idx, (b, d, ci, first) in enumerate(seq):
            cur = nxt
            if first:
                s.memset("dve", C32.subs(allj)[:], 0.0)
                s.memset("pool", Cbf.subs(allj)[:], 0.0)
                s.memset("dve", n32[:], 0.0)
                s.memset("pool", nbf[:], 0.0)
            if idx + 1 < len(seq):
                nxt = prep(*seq[idx + 1][:3])
            heavy(b, d, ci, cur)
    s.barrier()
    c.pr = Ring(c.pbanks)


def load_mat(c, dst, src_ap):
    v = src_ap.rearrange("(st p) f -> p st f", p=128)
    for q in range(4):
        c.s.dma("sp", dst[:, q * 4:(q + 1) * 4, :], V(v[:, q * 4:(q + 1) * 4, :], "w_const"))


def layer_hyena(c, W, hin, hout):
    nc, s = c.nc, c.s
    C = c.C
    dr = c.dram
    vxT = dr("hy_vxT", [3 * DI, T], BF16)
    z_tm = dr("hy_z", [T, DI], BF16)
    sig = [dr("hy_v", [T, DI], BF16), dr("hy_x1", [T, DI], BF16), dr("hy_x2", [T, DI], BF16)]
    y1_tm = dr("hy_y1", [T, DI], BF16)
    y2_tm = dr("hy_y2", [T, DI], BF16)
    Kf = dr("hy_Kf", [2, 2, L, DI], BF16)
    Zd = dr("hy_Z", [BL, 2, L, DI], BF16)

    with ExitStack() as es:
        uT = sb(es, nc, "uT", [128, 8, T], BF16)
        phase_norm(c, hin, W["hy_norm"], uT)
        phase_project(c, uT, W["hy_w_in"], [
            (0, 3 * DI, "fm", vxT, BF16, 1.0),
            (3 * DI, DI, "tm", z_tm, BF16, 1.0, AF.Silu),
        ])

    with ExitStack() as es:
        stg = sbring(es, nc, "hc_stg", [128, 4, 128], BF16, 3)

        def emit(ct, b, tb, cv):
            to_tm_store(c, cv, sig[ct // 16], b * L + tb * 512, (ct % 16) * 128, stg)

        conv_fm(c, vxT, 48, 3, W["hy_conv_wT"], W["hy_conv_b"], False, emit)
    s.barrier()

    def fwd_transform(Cm, Sn, o, ysrc):
        with ExitStack() as es:
            yr = sbring(es, nc, "hf_y", [128, 16, 512], BF16, 2)
            kr = sbring(es, nc, "hf_k", [128, 2, 512], BF16, 2)
            yre = sb(es, nc, "hf_yre", [128, 512], F32)
            yim = sb(es, nc, "hf_yim", [128, 512], F32)
            t1 = sb(es, nc, "hf_t1", [128, 512], F32)
            t2 = sb(es, nc, "hf_t2", [128, 512], F32)
            t3 = sb(es, nc, "hf_t3", [128, 512], F32)
            t4 = sb(es, nc, "hf_t4", [128, 512], F32)
            zr = sbring(es, nc, "hf_z", [128, 2, 512], BF16, 2)
            for b in range(BL):
                for cb in range(4):
                    yt = yr.next()
                    cs = slice(cb * 512, (cb + 1) * 512)
                    s.dma("sp", yt[:], V(ysrc.t[b * L:(b + 1) * L, cs].rearrange("(st p) c -> p st c", p=128), ysrc.key))
                    for ft in range(16):
                        fs = slice(ft * 128, (ft + 1) * 128)
                        kt = kr.next()
                        s.dma("sp", kt[:], V(Kf.t[o, :, fs, cs].rearrange("a p c -> p a c"), Kf.key))
                        pre, pim = c.pr.next(), c.pr.next()
                        for st in range(16):
                            s.mm(pre[:, :], Cm[:, st, fs], yt[:, st, :], start=(st == 0), stop=(st == 15))
                        for st in range(16):
                            s.mm(pim[:, :], Sn[:, st, fs], yt[:, st, :], start=(st == 0), stop=(st == 15))
                        s.cp("act", yre[:], pre[:, :])
                        s.cp("act", yim[:], pim[:, :])
                        s.tt("dve", t1[:], yre[:], kt[:, 0, :], ALU.mult)
                        s.tt("dve", t2[:], yim[:], kt[:, 1, :], ALU.mult)
                        s.tt("dve", t3[:], yre[:], kt[:, 1, :], ALU.mult)
                        s.tt("dve", t4[:], yim[:], kt[:, 0, :], ALU.mult)
                        zt = zr.next()
                        s.tt("dve", zt[:, 0, :], t1[:], t2[:], ALU.subtract)
                        s.tt("pool", zt[:, 1, :], t3[:], t4[:], ALU.add)
                        s.dma(STQ, V(Zd.t[b, :, fs, cs].rearrange("a p c -> p a c"), Zd.key), zt[:])
        s.barrier()

    def inv_transform(o, ysrc, xg_src, ydst):
        with ExitStack() as es:
            CI = sb(es, nc, "hi_CI", [128, 16, L], BF16)
            SI = sb(es, nc, "hi_SI", [128, 16, L], BF16)
            load_mat(c, CI, C["c_CI"])
            load_mat(c, SI, C["c_SI"])
            dbc = sb(es, nc, "hi_d", [128, DI], F32)
            s.dma("sp", dbc[:], V(W["hy_d"][o].partition_broadcast(128), "w_const"))
            zb = sb(es, nc, "hi_zb", [128, 2, 16, 512], BF16)
            ypr = sbring(es, nc, "hi_yp", [128, 512], BF16, 2)
            xgr = sbring(es, nc, "hi_xg", [128, 512], BF16, 2)
            zzr = sbring(es, nc, "hi_zz", [128, 512], BF16, 2)
            ta = sbring(es, nc, "hi_ta", [128, 512], F32, 2)
            tb_ = sbring(es, nc, "hi_tb", [128, 512], F32, 2)
            yo = sbring(es, nc, "hi_yo", [128, 512], BF16, 2)
            for b in range(BL):
                for cb in range(4):
                    cs = slice(cb * 512, (cb + 1) * 512)
                    for a in range(2):
                        for q in range(2):
                            s.dma("sp", zb[:, a, q * 8:(q + 1) * 8, :],
                                  V(Zd.t[b, a, q * 1024:(q + 1) * 1024, cs].rearrange("(ft p) c -> p ft c", p=128), Zd.key))
                    for tt in range(16):
                        ts_ = slice(tt * 128, (tt + 1) * 128)
                        rows = slice(b * L + tt * 128, b * L + (tt + 1) * 128)
                        po = c.pr.next()
                        for ft in range(16):
                            s.mm(po[:, :], CI[:, ft, ts_], zb[:, 0, ft, :], start=(ft == 0), stop=False)
                        for ft in range(16):
                            s.mm(po[:, :], SI[:, ft, ts_], zb[:, 1, ft, :], start=False, stop=(ft == 15))
                        yp, xg = ypr.next(), xgr.next()
                        s.dma("sp", yp[:], ysrc[rows, cs])
                        s.dma("sp", xg[:], xg_src[rows, cs])
                        t_a = ta.next()
                        s.tt("dve", t_a[:], yp[:], dbc[:, cs], ALU.mult)
                        s.tt("dve", t_a[:], t_a[:], po[:, :], ALU.add)
                        yot = yo.next()
                        if o == 0:
                            s.tt("dve", yot[:], t_a[:], xg[:], ALU.mult)
                        else:
                            zz, t_b = zzr.next(), tb_.next()
                            s.dma("sp", zz[:], z_tm[rows, cs])
                            s.tt("dve", t_a[:], t_a[:], xg[:], ALU.mult)
                            s.tt("dve", yot[:], t_a[:], zz[:], ALU.mult)
                        s.dma(STQ, ydst[rows, cs], yot[:])
        s.barrier()

    with ExitStack() as es:
        Cm = sb(es, nc, "hy_Cm", [128, 16, L], BF16)
        Sn = sb(es, nc, "hy_Sn", [128, 16, L], BF16)
        load_mat(c, Cm, C["c_Cm"])
        load_mat(c, Sn, C["c_Sn"])
        with ExitStack() as es2:
            hA = sb(es2, nc, "hm_hA", [64, L], F32)
            hB = sb(es2, nc, "hm_hB", [64, L], F32)
            with ExitStack() as es3:
                feats = sb(es3, nc, "hm_feats", [33, L], F32)
                w1 = sb(es3, nc, "hm_w1", [33, 64], F32)
                wh = sb(es3, nc, "hm_wh", [64, 2, 64], F32)
                prm = sb(es3, nc, "hm_prm", [64, 8], F32)
                tr_ = sb(es3, nc, "hm_t", [64, 512], F32)
                tki = sb(es3, nc, "hm_ki", [64, 512], mybir.dt.int32)
                tkf = sb(es3, nc, "hm_kf", [64, 512], F32)
                s.dma("sp", feats[:], V(C["c_featsT"], "w_const"))
                s.dma("sp", w1[:], V(W["hy_ffn_w_in"], "w_const"))
                s.dma("sp", wh[:], V(W["hy_ffn_w_hid"].rearrange("j a b -> a j b"), "w_const"))
                s.dma("sp", prm[:, 0:1], V(W["hy_ffn_b_in"].rearrange("(p o) -> p o", o=1), "w_const"))
                s.dma("sp", prm[:, 1:3], V(W["hy_ffn_b_hidT"], "w_const"))
                s.dma("sp", prm[:, 3:6], V(W["hy_ffn_freqT"], "w_const"))
                cur, nxt = hA, hB
                for layer in range(3):
                    for blk in range(4):
                        bs = slice(blk * 512, (blk + 1) * 512)
                        ps = c.pr.next()
                        if layer == 0:
                            s.mm(ps[0:64, :], w1[:, :], feats[:, bs])
                            dst = cur
                        else:
                            s.mm(ps[0:64, :], wh[:, layer - 1, :], cur[:, bs])
                            dst = nxt
                        s.ts("dve", tr_[:], ps[0:64, :], prm[:, layer:layer + 1], prm[:, 3 + layer:4 + layer], ALU.add, ALU.mult)
                        s.ts("dve", tr_[:], tr_[:], 1.0 / (2.0 * PI), 8.5, ALU.mult, ALU.add)
                        s.cp("dve", tki[:], tr_[:])
                        s.cp("dve", tkf[:], tki[:])
                        s.tt("dve", tr_[:], tr_[:], tkf[:], ALU.subtract)
                        s.ts("dve", tkf[:], tr_[:], 0.0, None, ALU.is_lt)
                        s.tt("dve", tr_[:], tr_[:], tkf[:], ALU.add)
                        s.act(dst[:, bs], tr_[:], AF.Sin, bias=c.one[0:64, 1:2], scale=2.0 * PI)
                    if layer > 0:
                        cur, nxt = nxt, cur
                h3 = cur
                s.barrier()
            dlt = sb(es2, nc, "hm_dlt", [128, DI], F32)
            ngt = sb(es2, nc, "hm_negt", [128, 16], F32)
            s.dma("sp", dlt[:], V(C["c_deltas"].partition_broadcast(128), "w_const"))
            s.dma("sp", ngt[:], V(C["c_negt"], "w_const"))
            wor = sbring(es2, nc, "hm_wo", [64, 2, 256], F32, 2)
            Ar = sb(es2, nc, "hm_A", [128, 16, 256], BF16)
            Br_ = sb(es2, nc, "hm_B", [128, 16, 256], BF16)
            dec = sbring(es2, nc, "hm_dec", [128, 256], F32, 2)
            hbs = sbring(es2, nc, "hm_hb", [128, 256], F32, 2)
            sa = sbring(es2, nc, "hm_sa", [128, 256], F32, 2)
            sbm = sbring(es2, nc, "hm_sb", [128, 256], F32, 2)
            ko = sbring(es2, nc, "hm_ko", [128, 512], BF16, 2)
            wov = W["hy_ffn_w_out"]
            for o in range(2):
                for cb in range(8):
                    cs = slice(cb * 256, (cb + 1) * 256)
                    wo = wor.next()
                    for dd in range(2):
                        c0 = dd * 2 * DI + o * DI + cb * 256
                        s.dma("sp", wo[:, dd, :], V(wov[:, c0:c0 + 256], "w_const"))
                    for tt in range(16):
                        ps = c.pr.next()
                        s.mm(ps[:, 0:256], h3[:, tt * 128:(tt + 1) * 128], wo[:, 0, :])
                        s.mm(ps[:, 256:512], h3[:, tt * 128:(tt + 1) * 128], wo[:, 1, :])
                        dc, hb_, a_, b_ = dec.next(), hbs.next(), sa.next(), sbm.next()
                        s.act(dc[:], dlt[:, cs], AF.Exp, scale=ngt[:, tt:tt + 1])
                        s.cp("dve", hb_[:], ps[:, 256:512])
                        if tt == 0:
                            s.memset("dve", hb_[0:1, :], 0.0)
                        s.tt("dve", a_[:], ps[:, 0:256], hb_[:], ALU.add)
                        s.tt("dve", b_[:], ps[:, 0:256], hb_[:], ALU.subtract)
                        s.tt("dve", Ar[:, tt, :], a_[:], dc[:], ALU.mult)
                        s.tt("pool", Br_[:, tt, :], b_[:], dc[:], ALU.mult)
                    for ft in range(16):
                        fs = slice(ft * 128, (ft + 1) * 128)
                        pk = c.pr.next()
                        for tt in range(16):
                            s.mm(pk[:, 0:256], Cm[:, tt, fs], Ar[:, tt, :], start=(tt == 0), stop=(tt == 15))
                        for tt in range(16):
                            s.mm(pk[:, 256:512], Sn[:, tt, fs], Br_[:, tt, :], start=(tt == 0), stop=(tt == 15))
                        kt = ko.next()
                        s.cp("act", kt[:], pk[:, :])
                        s.dma(STQ, V(Kf.t[o, :, fs, cs].rearrange("a p c -> p a c"), Kf.key),
                              V(kt.t[:].rearrange("p (a c) -> p a c", a=2), kt.key))
            s.barrier()
        fwd_transform(Cm, Sn, 0, sig[0])
    inv_transform(0, sig[0], sig[1], y1_tm)
    with ExitStack() as es:
        Cm = sb(es, nc, "hy_Cm2", [128, 16, L], BF16)
        Sn = sb(es, nc, "hy_Sn2", [128, 16, L], BF16)
        load_mat(c, Cm, C["c_Cm"])
        load_mat(c, Sn, C["c_Sn"])
        fwd_transform(Cm, Sn, 1, y1_tm)
    inv_transform(1, y1_tm, sig[2], y2_tm)

    with ExitStack() as es:
        fin = Finalizer(c, es, W["hy_w_out"], hin, hout)
        yr = sbring(es, nc, "ho_y", [128, DI], BF16, 2)
        for i in range(NT):
            yt = yr.next()
            s.dma("sp", yt[:], y2_tm[i * 128:(i + 1) * 128, :])
            fin.run(yt, i)
    s.barrier()


N_IMPL = 4
STQ = "act"
DEBUG_STOP = None


def host_constants():
    cst = {}
    cst["c_identb"] = np.eye(128, dtype=np.float32).astype(ml_dtypes.bfloat16)
    cst["c_identf"] = np.eye(128, dtype=np.float32)
    sidx = np.arange(128)[:, None]
    lidx = np.arange(128)[None, :]
    tri = np.stack([(sidx <= lidx), (sidx >= lidx)], axis=1).astype(np.float32)
    cst["c_tri"] = tri
    cst["c_negm"] = ((1.0 - tri) * -1.0e5).astype(np.float32)
    sI = np.arange(L, dtype=np.int64)[:, None]
    fI = np.arange(L, dtype=np.int64)[None, :]
    ph = ((2 * fI + 1) * sI) % (4 * L)
    th = (2.0 * np.pi / (4 * L)) * ph.astype(np.float64)
    cm = np.cos(th)
    sn = np.sin(th)
    bf = ml_dtypes.bfloat16
    cst["c_Cm"] = cm.astype(np.float32).astype(bf)
    cst["c_Sn"] = (-sn).astype(np.float32).astype(bf)
    cst["c_CI"] = np.ascontiguousarray((cm.T / L)).astype(np.float32).astype(bf)
    cst["c_SI"] = np.ascontiguousarray((-sn.T / L)).astype(np.float32).astype(bf)
    t = np.linspace(0.0, 1.0, L, dtype=np.float32)[:, None]
    pos = np.arange(L, dtype=np.float32)[:, None]
    bands = np.linspace(1e-4, 15.0, 16, dtype=np.float32)[None]
    ang = (np.float32(2.0 * math.pi / L) * pos * bands).astype(np.float32)
    feats = np.concatenate([t, np.cos(ang), -np.sin(ang)], axis=-1).astype(np.float32)
    cst["c_featsT"] = np.ascontiguousarray(feats.T)
    max_decay = math.log(1e-2) / 0.3
    min_decay = math.log(1e-2) / 1.5
    cst["c_deltas"] = np.abs(np.linspace(min_decay, max_decay, DI, dtype=np.float32)).astype(np.float32)
    cst["c_negt"] = np.ascontiguousarray(-(t[:, 0].reshape(16, 128).T)).astype(np.float32)
    return cst


def host_prepare(inputs):
    W = {}
    for k, v in inputs.items():
        if k in ("x", "final_norm"):
            continue
        W[k] = np.ascontiguousarray(v[0])
    W["final_norm"] = np.ascontiguousarray(inputs["final_norm"])
    W["ssd_conv_wT"] = np.ascontiguousarray(W.pop("ssd_conv_w").T)
    W["ml_conv_wT"] = np.ascontiguousarray(W.pop("ml_conv_w").T)
    W["hy_conv_wT"] = np.ascontiguousarray(W.pop("hy_conv_w").T)
    W["hy_ffn_b_hidT"] = np.ascontiguousarray(W.pop("hy_ffn_b_hid").T)
    W["hy_ffn_freqT"] = np.ascontiguousarray(W.pop("hy_ffn_freq").T)
    return W


def build_program(wshapes, cshapes, n_layers=4, final_norm=True):
    n_layers = min(n_layers, N_IMPL)
    nc = bass.Bass("TRN2", target_bir_lowering=False)
    c = Ctx()
    c.nc = nc
    c.s = Sched(nc)
    s = c.s
    x_in = Buf(nc.dram_tensor("x", [T, D], F32, kind="ExternalInput").ap(), "x_in")
    out = Buf(nc.dram_tensor("out", [T, D], F32, kind="ExternalOutput").ap(), "out")
    W = {k: nc.dram_tensor(k, list(shp), F32 if dt == np.float32 else BF16, kind="ExternalInput").ap()
         for k, (shp, dt) in wshapes.items()}
    C = {k: nc.dram_tensor(k, list(shp), F32 if dt == np.float32 else BF16, kind="ExternalInput").ap()
         for k, (shp, dt) in cshapes.items()}

    def dram(name, shape, dt):
        return Buf(nc.dram_tensor(name, shape, dt, kind="Internal").ap(), name)

    c.dram = dram
    c.C = C
    hA = dram("hA", [T, D], F32)
    hB = dram("hB", [T, D], F32)

    with ExitStack() as es:
        c.identb = sb(es, nc, "k_identb", [128, 128], BF16)
        c.identf = sb(es, nc, "k_identf", [128, 128], F32)
        c.tri = sb(es, nc, "k_tri", [128, 2, 128], F32)
        c.negm = sb(es, nc, "k_negm", [128, 2, 128], F32)
        c.onesf = sb(es, nc, "k_onesf", [128, 128], F32)
        c.nonesf = sb(es, nc, "k_nonesf", [128, 128], F32)
        c.one = sb(es, nc, "k_one", [128, 4], F32)
        s.dma("sp", c.identb[:], V(C["c_identb"], "w_const"))
        s.dma("sp", c.identf[:], V(C["c_identf"], "w_const"))
        s.dma("sp", c.tri[:], V(C["c_tri"], "w_const"))
        s.dma("sp", c.negm[:], V(C["c_negm"], "w_const"))
        s.memset("dve", c.onesf[:], 1.0)
        s.memset("dve", c.nonesf[:], -1.0)
        s.memset("dve", c.one[:, 0:1], 1.0)
        s.memset("dve", c.one[:, 1:2], -PI)
        s.memset("dve", c.one[:, 2:3], 0.0)
        s.memset("dve", c.one[:, 3:4], math.log(1.0 / 16.0))
        pbanks = [Buf(es.enter_context(nc.psum_tensor(f"ps{i}", [128, 512], F32)), f"ps{i}") for i in range(6)]
        tbanks = [Buf(es.enter_context(nc.psum_tensor(f"pt{i}", [128, 8, 128], BF16)), f"pt{i}") for i in range(2)]
        c.pbanks = pbanks
        c.pr = Ring(pbanks)
        c.ptr = Ring(tbanks)
        s.barrier()
        c.identb.key = c.identf.key = c.tri.key = c.negm.key = "konst"
        c.onesf.key = c.nonesf.key = c.one.key = "konst"

        layers = [layer_ssd, layer_gla, layer_hyena, layer_mlstm]
        hs = [x_in, hA, hB, hA, hB]
        hcur = x_in
        for li in range(n_layers):
            hnext = hA if (li % 2 == 0) else hB
            layers[li](c, W, hcur, hnext)
            hcur = hnext
        if final_norm:
            phase_final_norm(c, hcur, W["final_norm"], out)
        else:
            with ExitStack() as es2:
                cr = sbring(es2, nc, "cp_x", [128, D], F32, 2)
                for i in range(NT):
                    t = cr.next()
                    s.dma("sp", t[:], hcur[i * 128:(i + 1) * 128, :])
                    s.dma(STQ, out[i * 128:(i + 1) * 128, :], t[:])
            s.barrier()
    return nc


_CACHE = {}


def kernel(**inputs):
    x = np.ascontiguousarray(inputs["x"], dtype=np.float32)
    W = host_prepare(inputs)
    Cst = host_constants()
    wshapes = {k: (v.shape, v.dtype.type if v.dtype != ml_dtypes.bfloat16 else "bf16") for k, v in W.items()}
    cshapes = {k: (v.shape, v.dtype.type if v.dtype != ml_dtypes.bfloat16 else "bf16") for k, v in Cst.items()}
    nc = build_program(wshapes, cshapes)
    in_maps = []
    for i in range(NCORES):
        m = {"x": x[i * BL:(i + 1) * BL].reshape(T, D)}
        m.update(W)
        m.update(Cst)
        in_maps.append(m)
    res = run_bass_kernel_spmd(nc, in_maps, core_ids=list(range(NCORES)))
    outs = [r["out"].reshape(BL, L, D) for r in res.results]
    return np.concatenate(outs, axis=0).astype(np.float32)
```

```python
import math
from contextlib import ExitStack
import numpy as np
import ml_dtypes
import concourse.bass as bass
import concourse.mybir as mybir
from concourse.bass_utils import run_bass_kernel_spmd

F32 = mybir.dt.float32
BF16 = mybir.dt.bfloat16
AF = mybir.ActivationFunctionType
ALU = mybir.AluOpType
AX = mybir.AxisListType

NCORES = 8
BL = 2
L = 2048
T = BL * L
D = 1024
DI = 2048
Q = 128
NQ = L // Q
NT = T // 128
EPS = 1e-6
EPOCH = 30000
PI = math.pi


def _kt(k):
    if isinstance(k, str):
        return (k,)
    return tuple(k)


class V:
    __slots__ = ("ap", "k")

    def __init__(self, ap, k):
        self.ap = ap
        self.k = _kt(k)


class Buf:
    def __init__(self, t, key):
        self.t = t
        self.key = key

    def __getitem__(self, idx):
        return V(self.t[idx], self.key)

    def sub(self, sub):
        return Buf(self.t, f"{self.key}.{sub}")

    def subs(self, subs):
        return Buf(self.t, tuple(f"{self.key}.{x}" for x in subs))


class Sched:
    def __init__(self, nc):
        self.nc = nc
        self.eng = {"pe": nc.tensor, "dve": nc.vector, "act": nc.scalar,
                    "pool": nc.gpsimd, "sp": nc.sync}
        self.nsem = 0
        self.esem, self.ecnt = {}, {}
        for e in self.eng:
            self._new_epoch(e)
        self.seen = {e: {} for e in self.eng}
        self.res = {}
        self.dsem = {}
        self.dfree = []
        self.ninst = 0

    def _alloc(self):
        self.nsem += 1
        return self.nc.alloc_semaphore(name=f"s{self.nsem}")

    def _new_epoch(self, e):
        self.esem[e] = self._alloc()
        self.ecnt[e] = 0

    def _wait(self, e, tok):
        sem, val = tok
        sid = id(sem)
        if self.seen[e].get(sid, 0) >= val:
            return
        self.eng[e].wait_ge(sem, val)
        self.seen[e][sid] = val

    def _deps(self, e, reads, writes, pe_accum=False, dsem=None):
        for k in reads:
            r = self.res.get(k)
            if r and r["w"] is not None:
                self._wait(e, r["w"])
        for k in writes:
            r = self.res.get(k)
            if r:
                w = r["w"]
                if w is not None:
                    skip = (pe_accum and r["we"] == "pe") or (dsem is not None and w[0] is dsem)
                    if not skip:
                        self._wait(e, w)
                for t in r["r"]:
                    self._wait(e, t)

    def _record(self, e, tok, reads, writes):
        for k in reads:
            r = self.res.setdefault(k, {"w": None, "r": [], "we": None})
            r["r"] = [t for t in r["r"] if t[0] is not tok[0]] + [tok]
        for k in writes:
            self.res[k] = {"w": tok, "r": [], "we": e}

    def op(self, e, fn, reads=(), writes=(), pe_accum=False):
        reads = [k for ks in reads if ks is not None for k in _kt(ks)]
        writes = [k for ks in writes for k in _kt(ks)]
        self._deps(e, reads, writes, pe_accum)
        if self.ecnt[e] >= EPOCH:
            self._new_epoch(e)
        inst = fn()
        self.ecnt[e] += 1
        inst.then_inc(self.esem[e], 1)
        self._record(e, (self.esem[e], self.ecnt[e]), reads, writes)
        self.ninst += 1
        return inst

    def dma(self, e, out, in_, **kw):
        reads, writes = list(in_.k), list(out.k)
        sk = out.k[0]
        if sk not in self.dsem:
            if self.dfree:
                self.dsem[sk] = self.dfree.pop()
            else:
                self.dsem[sk] = [self._alloc(), 0]
        ds = self.dsem[sk]
        self._deps(e, reads, writes, dsem=ds[0])
        inst = self.eng[e].dma_start(out=out.ap, in_=in_.ap, **kw)
        ds[1] += 16
        inst.then_inc(ds[0], 16)
        self._record(e, (ds[0], ds[1]), reads, writes)
        self.ninst += 1
        return inst

    def barrier(self):
        toks = [(self.esem[e], self.ecnt[e]) for e in self.eng if self.ecnt[e] > 0]
        toks += [(d[0], d[1]) for d in self.dsem.values() if d[1] > 0]
        for e in self.eng:
            for t in toks:
                if t[0] is not self.esem[e]:
                    self._wait(e, t)
        for d in self.dsem.values():
            if d[1] < 40000:
                self.dfree.append(d)
        self.dsem = {}
        self.res = {}

    def mm(self, out, lhsT, rhs, start=True, stop=True):
        nc = self.nc
        return self.op("pe", lambda: nc.tensor.matmul(out.ap, lhsT=lhsT.ap, rhs=rhs.ap, start=start, stop=stop),
                       reads=[lhsT.k, rhs.k], writes=[out.k], pe_accum=True)

    def tr(self, out, in_, ident):
        nc = self.nc
        return self.op("pe", lambda: nc.tensor.transpose(out.ap, in_.ap, ident.ap),
                       reads=[in_.k, ident.k], writes=[out.k], pe_accum=True)

    def act(self, out, in_, func, bias=None, scale=None, accum=None):
        nc = self.nc
        kw = {}
        rd = [in_.k]
        wr = [out.k]
        if bias is not None:
            if isinstance(bias, V):
                kw["bias"] = bias.ap
                rd.append(bias.k)
            else:
                kw["bias"] = bias
        if scale is not None:
            if isinstance(scale, V):
                kw["scale"] = scale.ap
                rd.append(scale.k)
            else:
                kw["scale"] = scale
        if accum is not None:
            kw["accum_out"] = accum.ap
            wr.append(accum.k)
        return self.op("act", lambda: nc.scalar.activation(out=out.ap, in_=in_.ap, func=func, **kw),
                       reads=rd, writes=wr)

    def _e(self, e):
        return self.eng[e]

    def tt(self, e, out, in0, in1, op):
        return self.op(e, lambda: self._e(e).tensor_tensor(out=out.ap, in0=in0.ap, in1=in1.ap, op=op),
                       reads=[in0.k, in1.k], writes=[out.k])

    def ts(self, e, out, in0, s1, s2, op0, op1=None):
        rd = [in0.k]
        a1 = s1.ap if isinstance(s1, V) else s1
        a2 = s2.ap if isinstance(s2, V) else s2
        if isinstance(s1, V):
            rd.append(s1.k)
        if isinstance(s2, V):
            rd.append(s2.k)
        if op1 is None:
            return self.op(e, lambda: self._e(e).tensor_scalar(out=out.ap, in0=in0.ap, scalar1=a1, scalar2=None, op0=op0),
                           reads=rd, writes=[out.k])
        return self.op(e, lambda: self._e(e).tensor_scalar(out=out.ap, in0=in0.ap, scalar1=a1, scalar2=a2, op0=op0, op1=op1),
                       reads=rd, writes=[out.k])

    def stt(self, e, out, in0, scalar, in1, op0, op1):
        rd = [in0.k, in1.k]
        a = scalar.ap if isinstance(scalar, V) else scalar
        if isinstance(scalar, V):
            rd.append(scalar.k)
        return self.op(e, lambda: self._e(e).scalar_tensor_tensor(out=out.ap, in0=in0.ap, scalar=a, in1=in1.ap, op0=op0, op1=op1),
                       reads=rd, writes=[out.k])

    def cp(self, e, out, in_):
        if e == "act":
            return self.op(e, lambda: self.nc.scalar.copy(out=out.ap, in_=in_.ap), reads=[in_.k], writes=[out.k])
        return self.op(e, lambda: self._e(e).tensor_copy(out=out.ap, in_=in_.ap), reads=[in_.k], writes=[out.k])

    def memset(self, e, out, val):
        return self.op(e, lambda: self._e(e).memset(out.ap, val), reads=[], writes=[out.k])

    def red(self, e, out, in_, op, axis=AX.X):
        return self.op(e, lambda: self._e(e).tensor_reduce(out=out.ap, in_=in_.ap, axis=axis, op=op),
                       reads=[in_.k], writes=[out.k])

    def recip(self, e, out, in_):
        return self.op(e, lambda: self._e(e).reciprocal(out=out.ap, in_=in_.ap), reads=[in_.k], writes=[out.k])


class Ctx:
    pass


class Ring:
    def __init__(self, bufs):
        self.bufs = bufs
        self.i = 0

    def next(self):
        b = self.bufs[self.i % len(self.bufs)]
        self.i += 1
        return b


_UID = [0]


def drain(gens, n):
    for g in gens:
        for _ in range(n):
            try:
                next(g)
            except StopIteration:
                break


def drain_all(gens):
    for g in gens:
        for _ in g:
            pass


def run_scan(s, seq, prep, heavy, reset_state):
    t_next = {}
    drain_all([prep(*seq[0][:3], t_next)])
    epi = None
    for idx, (b, d, ci, first) in enumerate(seq):
        cur = t_next
        if first:
            reset_state()
        bgs = []
        if idx + 1 < len(seq):
            t_next = {}
            bgs.append(prep(*seq[idx + 1][:3], t_next))
        if epi is not None:
            bgs.append(epi)
        epi = heavy(b, d, ci, cur, bgs)
    drain_all([epi])


def sb(es, nc, name, shape, dt):
    _UID[0] += 1
    name = f"{name}_{_UID[0]}"
    t = es.enter_context(nc.sbuf_tensor(name, shape, dt))
    return Buf(t, name)


def sbring(es, nc, name, shape, dt, n):
    return Ring([sb(es, nc, f"{name}{i}", shape, dt) for i in range(n)])


def bc(v, shape):
    return V(v.ap.to_broadcast(shape), v.k)


def phase_norm(c, hin, gvec, uT):
    nc, s = c.nc, c.s
    with ExitStack() as es:
        gt = sb(es, nc, "n_g", [128, D], F32)
        xr = sbring(es, nc, "n_x", [128, D], F32, 2)
        sq = sb(es, nc, "n_sq", [128, D], F32)
        ur = sbring(es, nc, "n_u", [128, D], BF16, 2)
        st = sb(es, nc, "n_st", [128, NT, 4], F32)
        s.dma("sp", gt[:], V(gvec.partition_broadcast(128), "w_const"))
        for i in range(NT):
            xt = xr.next()
            ub = ur.next()
            stv = st.sub(str(i))
            s.dma("sp", xt[:], hin[i * 128:(i + 1) * 128, :])
            s.act(sq[:], xt[:], AF.Square, accum=stv[:, i, 0:1])
            s.ts("dve", stv[:, i, 1:2], stv[:, i, 0:1], 1.0 / D, EPS, ALU.mult, ALU.add)
            s.act(stv[:, i, 2:3], stv[:, i, 1:2], AF.Sqrt)
            s.recip("dve", stv[:, i, 3:4], stv[:, i, 2:3])
            s.stt("dve", ub[:], xt[:], stv[:, i, 3:4], gt[:], ALU.mult, ALU.mult)
            pt = c.ptr.next()
            for k in range(8):
                s.tr(pt[:, k, :], ub[:, k * 128:(k + 1) * 128], c.identb[:])
            s.cp("act", uT[:, :, i * 128:(i + 1) * 128], pt[:, 0:8, :])
    s.barrier()


def phase_project(c, uT, w_ap, segs):
    nc, s = c.nc, c.s
    with ExitStack() as es:
        wfr = sbring(es, nc, "p_wf", [128, 8, 512], F32, 2)
        wbr = sbring(es, nc, "p_wb", [128, 8, 512], BF16, 2)
        ofr = sbring(es, nc, "p_of", [128, 512], F32, 3)
        obr = sbring(es, nc, "p_ob", [128, 512], BF16, 3)
        wv = w_ap.rearrange("(ko p) n -> p ko n", p=128)
        ev = 0
        for seg in segs:
            (col0, ncols, mode, dst, dt, scale) = seg[:6]
            func = seg[6] if len(seg) > 6 else None
            for cb in range(0, ncols, 512):
                nb = min(512, ncols - cb)
                wf = wfr.next()
                wb = wbr.next()
                s.dma("sp", wf[:, :, 0:nb], V(wv[:, :, col0 + cb:col0 + cb + nb], "w_const"))
                s.cp("dve", wb.sub("a")[:, 0:4, 0:nb], wf[:, 0:4, 0:nb])
                s.cp("act", wb.sub("b")[:, 4:8, 0:nb], wf[:, 4:8, 0:nb])
                if mode == "tm":
                    for i in range(NT):
                        ps = c.pr.next()
                        for ko in range(8):
                            s.mm(ps[:, 0:nb], uT[:, ko, i * 128:(i + 1) * 128], wb.sub("a" if ko < 4 else "b")[:, ko, 0:nb],
                                 start=(ko == 0), stop=(ko == 7))
                        ot = (ofr if dt == F32 else obr).next()
                        if func is not None:
                            s.act(ot[:, 0:nb], ps[:, 0:nb], func, scale=scale)
                        elif ev % 2 == 0:
                            s.act(ot[:, 0:nb], ps[:, 0:nb], AF.Copy, scale=scale)
                        else:
                            s.ts("dve", ot[:, 0:nb], ps[:, 0:nb], scale, None, ALU.mult)
                        ev += 1
                        s.dma(STQ, dst[i * 128:(i + 1) * 128, cb:cb + nb], ot[:, 0:nb])
                else:
                    for fb in range(0, nb, 128):
                        fn = min(128, nb - fb)
                        for tb in range(T // 512):
                            ps = c.pr.next()
                            for ko in range(8):
                                s.mm(ps[0:fn, :], wb.sub("a" if ko < 4 else "b")[:, ko, fb:fb + fn], uT[:, ko, tb * 512:(tb + 1) * 512],
                                     start=(ko == 0), stop=(ko == 7))
                            ot = (ofr if dt == F32 else obr).next()
                            if ev % 2 == 0:
                                s.act(ot[0:fn, :], ps[0:fn, :], AF.Copy, scale=scale)
                            else:
                                s.ts("dve", ot[0:fn, :], ps[0:fn, :], scale, None, ALU.mult)
                            ev += 1
                            s.dma(STQ, dst[cb + fb:cb + fb + fn, tb * 512:(tb + 1) * 512], ot[0:fn, :])
    s.barrier()


class Finalizer:
    def __init__(self, c, es, w_out_ap, hin, hout):
        nc = c.nc
        self.c = c
        self.wo = sb(es, nc, "f_wo", [128, 16, D], BF16)
        self.yT = sbring(es, nc, "f_yT", [128, 16, 128], BF16, 2)
        self.hr = sbring(es, nc, "f_h", [128, D], F32, 2)
        self.hin, self.hout = hin, hout
        with ExitStack() as es2:
            stg = sbring(es2, nc, "f_stg", [128, 2, D], F32, 2)
            wv = w_out_ap.rearrange("(ko p) n -> p ko n", p=128)
            for k0 in range(0, 16, 2):
                st = stg.next()
                c.s.dma("sp", st[:], V(wv[:, k0:k0 + 2, :], "w_const"))
                c.s.cp("dve" if (k0 // 2) % 2 == 0 else "act", self.wo[:, k0:k0 + 2, :], st[:])
            c.s.barrier()

    def run(self, y, i):
        for _ in self.run_gen(y, i):
            pass

    def run_gen(self, y, i):
        c, s = self.c, self.c.s
        yT = self.yT.next()
        for half in range(2):
            pt = c.ptr.next()
            for k in range(8):
                kk = half * 8 + k
                s.tr(pt[:, k, :], V(y.t[:, kk * 128:(kk + 1) * 128], y.key), c.identb[:])
            s.cp("act" if half == 0 else "dve", yT[:, half * 8:half * 8 + 8, :], pt[:, 0:8, :])
            yield
        ht = self.hr.next()
        s.dma("sp", ht[:], self.hin[i * 128:(i + 1) * 128, :])
        for n in range(2):
            ps = c.pr.next()
            for kc in range(16):
                s.mm(ps[:, :], yT[:, kc, :], self.wo[:, kc, n * 512:(n + 1) * 512],
                     start=(kc == 0), stop=(kc == 15))
            s.tt("dve", ht[:, n * 512:(n + 1) * 512], ps[:, :], ht[:, n * 512:(n + 1) * 512], ALU.add)
            yield
        s.dma(STQ, self.hout[i * 128:(i + 1) * 128, :], ht[:])


def phase_final_norm(c, hin, gvec, out):
    nc, s = c.nc, c.s
    with ExitStack() as es:
        gt = sb(es, nc, "fn_g", [128, D], F32)
        xr = sbring(es, nc, "fn_x", [128, D], F32, 2)
        sq = sb(es, nc, "fn_sq", [128, D], F32)
        orr = sbring(es, nc, "fn_o", [128, D], F32, 2)
        st = sb(es, nc, "fn_st", [128, NT, 4], F32)
        s.dma("sp", gt[:], V(gvec.partition_broadcast(128), "w_const"))
        for i in range(NT):
            xt = xr.next()
            ot = orr.next()
            stv = st.sub(str(i))
            s.dma("sp", xt[:], hin[i * 128:(i + 1) * 128, :])
            s.act(sq[:], xt[:], AF.Square, accum=stv[:, i, 0:1])
            s.ts("dve", stv[:, i, 1:2], stv[:, i, 0:1], 1.0 / D, EPS, ALU.mult, ALU.add)
            s.act(stv[:, i, 2:3], stv[:, i, 1:2], AF.Sqrt)
            s.recip("dve", stv[:, i, 3:4], stv[:, i, 2:3])
            s.stt("dve", ot[:], xt[:], stv[:, i, 3:4], gt[:], ALU.mult, ALU.mult)
            s.dma(STQ, out[i * 128:(i + 1) * 128, :], ot[:])
    s.barrier()


def conv_fm(c, srcT, nch_tiles, K, cw_ap, cb_ap, silu, emit):
    nc, s = c.nc, c.s
    pad = (K - 1) // 2
    with ExitStack() as es:
        cw = sb(es, nc, "cv_w", [128, nch_tiles, K], F32)
        cbias = sb(es, nc, "cv_b", [128, nch_tiles], F32)
        xr = sbring(es, nc, "cv_x", [128, L + 2 * pad], BF16, 2)
        dg = sbring(es, nc, "cv_dg", [128, K, 128], BF16, 2)
        cvr = sbring(es, nc, "cv_o", [128, 512], BF16, 3)
        s.dma("sp", cw[:], V(cw_ap.rearrange("(ct p) k -> p ct k", p=128), "w_const"))
        s.dma("sp", cbias[:], V(cb_ap.rearrange("(ct p) -> p ct", p=128), "w_const"), allow_slow_non_contiguous=True)
        for xb in xr.bufs:
            s.memset("pool", xb[:, 0:pad], 0.0)
            s.memset("pool", xb[:, L + pad:L + 2 * pad], 0.0)
        for ct in range(nch_tiles):
            d = dg.next()
            for k in range(K):
                s.ts("dve", d[:, k, :], c.identf[:], cw[:, ct, k:k + 1], None, ALU.mult)
            for b in range(BL):
                xt = xr.next()
                s.dma("sp", V(xt.t[:, pad:L + pad], xt.key + ".d"), srcT[ct * 128:(ct + 1) * 128, b * L:(b + 1) * L])
                for tb in range(L // 512):
                    ps = c.pr.next()
                    for k in range(K):
                        s.mm(ps[:, :], d[:, k, :],
                             V(xt.t[:, tb * 512 + k:tb * 512 + k + 512], (xt.key, xt.key + ".d")),
                             start=(k == 0), stop=(k == K - 1))
                    cv = cvr.next()
                    s.act(cv[:, :], ps[:, :], AF.Silu if silu else AF.Identity, bias=cbias[:, ct:ct + 1])
                    emit(ct, b, tb, cv[:, :])


def to_tm_store(c, cv, dst, row0, col0, stg_ring, mul=None):
    s = c.s
    pt = c.ptr.next()
    for j in range(4):
        s.tr(pt[:, j, :], V(cv.ap[:, j * 128:(j + 1) * 128], cv.k), c.identb[:])
    st = stg_ring.next()
    if mul is None:
        s.cp("dve", st[:, 0:4, :], pt[:, 0:4, :])
    else:
        s.tt("dve", st[:, 0:4, :], pt[:, 0:4, :], bc(V(mul.ap.unsqueeze(1), mul.k), [128, 4, 128]), ALU.mult)
    s.dma(STQ, V(dst.t[row0:row0 + 512, col0:col0 + 128].rearrange("(j p) c -> p j c", p=128), dst.key),
          st[:, 0:4, :])


def layer_ssd(c, W, hin, hout):
    nc, s = c.nc, c.s
    H, P, G, N = 32, 64, 8, 128
    dr = c.dram
    z_tm = dr("ssd_z", [T, DI], BF16)
    xbcT = dr("ssd_xbcT", [4096, T], BF16)
    dt_tm = dr("ssd_dt", [T, 64], F32)
    x_tm = dr("ssd_x", [T, DI], BF16)
    B_tm = dr("ssd_B", [T, 1024], BF16)
    BT = dr("ssd_BT", [1024, T], BF16)
    CT = dr("ssd_CT", [1024, T], BF16)
    yf = dr("ssd_yf", [T, DI], F32)

    with ExitStack() as es:
        uT = sb(es, nc, "uT", [128, 8, T], BF16)
        phase_norm(c, hin, W["ssd_norm"], uT)
        phase_project(c, uT, W["ssd_w_in"], [
            (0, DI, "tm", z_tm, BF16, 1.0, AF.Silu),
            (DI, 4096, "fm", xbcT, BF16, 1.0),
            (DI + 4096, 64, "tm", dt_tm, F32, 1.0),
        ])

    if DEBUG_STOP == "proj":
        return
    with ExitStack() as es:
        stg = sbring(es, nc, "sc_stg", [128, 4, 128], BF16, 3)

        def emit(ct, b, tb, cv):
            col = b * L + tb * 512
            if ct < 16:
                to_tm_store(c, cv, x_tm, col, ct * 128, stg)
            elif ct < 24:
                to_tm_store(c, cv, B_tm, col, (ct - 16) * 128, stg)
                s.dma(STQ, BT[(ct - 16) * 128:(ct - 15) * 128, col:col + 512], cv)
            else:
                s.dma(STQ, CT[(ct - 24) * 128:(ct - 23) * 128, col:col + 512], cv)

        conv_fm(c, xbcT, 32, 5, W["ssd_conv_wT"], W["ssd_conv_b"], True, emit)
    s.barrier()

    if DEBUG_STOP == "conv":
        return
    with ExitStack() as es:
        c.pr = Ring([c.pbanks[5], c.pbanks[4]])
        fin = Finalizer(c, es, W["ssd_w_out"], hin, hout)
        dtb = sb(es, nc, "ss_dtb", [128, 64], F32)
        aneg = sb(es, nc, "ss_a", [128, 64], F32)
        dsk = sb(es, nc, "ss_dsk", [128, 32], F32)
        gn = sb(es, nc, "ss_gn", [128, DI], F32)
        s.dma("sp", dtb[:], V(W["ssd_dt_bias"].rearrange("d h -> (d h)").partition_broadcast(128), "w_const"))
        s.dma("sp", aneg[:], V(W["ssd_a_log"].rearrange("d h -> (d h)").partition_broadcast(128), "w_const"))
        s.dma("sp", dsk[:], V(W["ssd_d"].partition_broadcast(128), "w_const"))
        s.dma("sp", gn[:], V(W["ssd_gnorm"].partition_broadcast(128), "w_const"))
        s.act(aneg[:], aneg[:], AF.Exp)
        s.ts("dve", aneg[:], aneg[:], -1.0, None, ALU.mult)
        xr = sbring(es, nc, "ss_x", [128, DI], BF16, 3)
        Br = sbring(es, nc, "ss_B", [128, 1024], BF16, 2)
        BTr = sbring(es, nc, "ss_BT", [128, G, 128], BF16, 2)
        CTr = sbring(es, nc, "ss_CT", [128, G, 128], BF16, 2)
        dtr = sbring(es, nc, "ss_dt", [128, 64], F32, 2)
        sm = sbring(es, nc, "ss_sm", [128, 8, 32], F32, 2)
        labcr = sbring(es, nc, "ss_labc", [128, H, 128], F32, 2)
        xdt = sbring(es, nc, "ss_xdt", [128, DI], BF16, 2)
        xw = sbring(es, nc, "ss_xw", [128, DI], BF16, 2)
        negm4 = sb(es, nc, "ss_negm4", [128, 2, 4, 128], F32)
        for d in range(2):
            s.cp("dve", negm4[:, d, :, :], bc(V(c.negm.t[:, d, :].unsqueeze(1), c.negm.key), [128, 4, 128]))
        Er = sbring(es, nc, "ss_E", [128, 512], F32, 2)
        PTr = sbring(es, nc, "ss_PT", [128, 4, 128], BF16, 2)
        t1r = sbring(es, nc, "ss_t1", [128, 256], F32, 2)
        yacc = sbring(es, nc, "ss_yacc", [128, DI], F32, 2)
        st32 = sb(es, nc, "ss_st32", [128, G, 256], F32)
        stbf = sb(es, nc, "ss_stbf", [128, G, 256], BF16)
        zr = sbring(es, nc, "ss_z", [128, DI], BF16, 1)
        yfr = sbring(es, nc, "ss_yf", [128, DI], F32, 1)
        tmp = sb(es, nc, "ss_tmp", [128, DI], F32)
        gst = sbring(es, nc, "ss_gst", [128, 4, 8], F32, 2)
        yb = sbring(es, nc, "ss_yb", [128, DI], BF16, 1)
        allg = [str(g) for g in range(G)]

        def r3(buf, q):
            return V(buf.t[:].rearrange("p (h q) -> p h q", q=q), buf.key)

        pab = c.pbanks[0:2]
        pyb = c.pbanks[2:4]
        pstb = c.pbanks[4]
        pmisc = c.pbanks[5]
        c.pr = Ring([c.pbanks[5], c.pbanks[4]])
        cbs = sbring(es, nc, "ss_cbs", [128, G, 128], F32, 2)

        def prep(b, d, ci, tout):
            i = b * NQ + ci
            r0 = i * 128
            tri = c.tri[:, d, :]
            xt, Bt, BTt, CTt, dtt = xr.next(), Br.next(), BTr.next(), CTr.next(), dtr.next()
            s.dma("sp", xt[:], x_tm[r0:r0 + 128, :])
            s.dma("sp", Bt[:], B_tm[r0:r0 + 128, :])
            s.dma("sp", BTt[:], V(BT.t[:, r0:r0 + 128].rearrange("(g n) t -> n g t", n=128), BT.key))
            s.dma("sp", CTt[:], V(CT.t[:, r0:r0 + 128].rearrange("(g n) t -> n g t", n=128), CT.key))
            s.dma("sp", dtt[:], dt_tm[r0:r0 + 128, :])
            m = sm.next()
            s.tt("dve", m[:, 0:2, :], V(dtt.t[:].rearrange("p (a h) -> p a h", a=2), dtt.key),
                 V(dtb.t[:].rearrange("p (a h) -> p a h", a=2), dtb.key), ALU.add)
            s.act(m[:, 0:2, :], m[:, 0:2, :], AF.Exp)
            s.act(m[:, 0:2, :], m[:, 0:2, :], AF.Ln, bias=c.one[:, 0:1])
            dtd = m[:, d, :]
            s.tt("dve", m[:, 2, :], dtd, aneg[:, d * 32:(d + 1) * 32], ALU.mult)
            la = m[:, 2, :]
            yield
            pc = pmisc
            s.mm(pc[:, 0:32], tri, la)
            s.mm(pc[:, 32:64], c.onesf[:], la)
            s.act(m[:, 3, :], pc[:, 0:32], AF.Exp)
            s.cp("dve", m[:, 5, :], pc[:, 0:32])
            s.tt("dve", m[:, 4, :], pc[:, 32:64], m[:, 5, :], ALU.subtract)
            s.act(m[:, 4, :], m[:, 4, :], AF.Exp)
            s.act(m[:, 6, :], pc[:, 32:64], AF.Exp)
            s.tt("dve", m[:, 7, :], m[:, 4, :], dtd, ALU.mult)
            yield
            xd, xwt = xdt.next(), xw.next()
            x3 = r3(xt, P)
            s.tt("dve", r3(xd, P), x3, bc(V(dtd.ap.unsqueeze(2), dtd.k), [128, H, P]), ALU.mult)
            yield
            s.tt("dve", r3(xwt, P), x3, bc(V(m.t[:, 7, :].unsqueeze(2), m.key), [128, H, P]), ALU.mult)
            yield
            labc = labcr.next()
            s.cp("act", labc[:], bc(V(la.ap.unsqueeze(2), la.k), [128, H, 128]))
            s.ts("dve", m[:, 5, :], m[:, 5, :], -1.0, None, ALU.mult)
            yield
            cb = cbs.next()
            for half in range(2):
                for gq in range(4):
                    g = half * 4 + gq
                    s.mm(V(pmisc.t[:, gq * 128:(gq + 1) * 128], pmisc.key), BTt[:, g, :], CTt[:, g, :])
                s.cp("act", V(cb.t[:, half * 4:half * 4 + 4, :].rearrange("p g l -> p (g l)"), cb.key), pmisc[:, :])
                yield
            tout.update(dict(i=i, r0=r0, xt=xt, Bt=Bt, CTt=CTt, m=m, xd=xd, xwt=xwt, labc=labc, cb=cb, x3=x3))

        def heavy(b, d, ci, t, bgs):
            i, r0, m = t["i"], t["r0"], t["m"]
            tri = c.tri[:, d, :]
            xd, xwt, labc, cb, CTt, Bt, x3 = t["xd"], t["xwt"], t["labc"], t["cb"], t["CTt"], t["Bt"], t["x3"]
            ya = yacc.next()
            Es, PTs = {}, {}

            def stage_a(g):
                pa = pab[g % 2]
                s.mm(pa[:, :], c.identf[:], V(negm4.t[:, d, :, :].rearrange("p a l -> p (a l)"), negm4.key),
                     start=True, stop=False)
                for hh in range(4):
                    h = g * 4 + hh
                    s.mm(V(pa.t[:, hh * 128:(hh + 1) * 128], pa.key), labc[:, h, :], tri, start=False, stop=(hh == 3))
                E = Er.next()
                for hh in range(4):
                    h = g * 4 + hh
                    s.act(E[:, hh * 128:(hh + 1) * 128], V(pa.t[:, hh * 128:(hh + 1) * 128], pa.key), AF.Exp,
                          bias=m[:, 5, h:h + 1])
                Es[g] = E

            def stage_b(g):
                E = Es.pop(g)
                PT = PTr.next()
                s.tt("dve", PT[:], V(E.t[:].rearrange("p (a l) -> p a l", a=4), E.key),
                     bc(V(cb.t[:, g, :].unsqueeze(1), cb.key), [128, 4, 128]), ALU.mult)
                py = pyb[g % 2]
                for hh in range(4):
                    h = g * 4 + hh
                    s.mm(V(py.t[:, hh * 64:(hh + 1) * 64], py.key), PT[:, hh, :], xd[:, h * 64:(h + 1) * 64])
                s.mm(V(py.t[:, 256:512], py.key), CTt[:, g, :], stbf.sub(str(g))[:, g, :])
                t1 = t1r.next()
                s.tt("dve", V(t1.t[:].rearrange("p (a q) -> p a q", a=4), t1.key),
                     V(py.t[:, 256:512].rearrange("p (a q) -> p a q", a=4), py.key),
                     bc(V(m.t[:, 3, g * 4:(g + 1) * 4].unsqueeze(2), m.key), [128, 4, P]), ALU.mult)
                s.tt("dve", ya[:, g * 256:(g + 1) * 256], py[:, 0:256], t1[:], ALU.add)
                s.mm(pstb[:, 0:256], Bt[:, g * 128:(g + 1) * 128], xwt[:, g * 256:(g + 1) * 256])
                sg = st32.sub(str(g))
                sg3 = V(sg.t[:, g, :].rearrange("p (a q) -> p a q", a=4), sg.key)
                s.tt("dve", sg3, sg3,
                     bc(V(m.t[:, 6, g * 4:(g + 1) * 4].unsqueeze(2), m.key), [128, 4, P]), ALU.mult)
                s.tt("dve", sg[:, g, :], sg[:, g, :], pstb[:, 0:256], ALU.add)
                s.cp("act", stbf.sub(str(g))[:, g, :], sg[:, g, :])

            for gg in range(G + 1):
                if gg < G:
                    stage_a(gg)
                if gg >= 1:
                    stage_b(gg - 1)
                drain(bgs, 2)
            drain_all(bgs)

            def epilogue():
                if d == 0:
                    s.dma(STQ, yf[r0:r0 + 128, :], ya[:])
                    return
                yft, zt = yfr.next(), zr.next()
                s.dma("sp", yft[:], yf[r0:r0 + 128, :])
                s.dma("sp", zt[:], z_tm[r0:r0 + 128, :])
                s.tt("dve", yft[:], yft[:], ya[:], ALU.add)
                yield
                s.tt("dve", r3(tmp, P), x3, bc(V(dsk.t[:].unsqueeze(2), dsk.key), [128, H, P]), ALU.mult)
                yield
                s.tt("dve", yft[:], yft[:], tmp[:], ALU.add)
                yield
                s.tt("dve", yft[:], yft[:], zt[:], ALU.mult)
                yield
                gs = gst.next()
                s.act(tmp[:], yft[:], AF.Square)
                s.red("dve", gs[:, 0, :], r3(tmp, 256), ALU.add)
                s.ts("dve", gs[:, 1, :], gs[:, 0, :], 1.0 / 256, EPS, ALU.mult, ALU.add)
                s.act(gs[:, 2, :], gs[:, 1, :], AF.Ln)
                s.act(gs[:, 3, :], gs[:, 2, :], AF.Exp, scale=-0.5)
                yield
                s.tt("dve", r3(yft, 256), r3(yft, 256),
                     bc(V(gs.t[:, 3, :].unsqueeze(2), gs.key), [128, 8, 256]), ALU.mult)
                yield
                ybt = yb.next()
                s.tt("dve", ybt[:], yft[:], gn[:], ALU.mult)
                yield
                yield from fin.run_gen(ybt, i)

            return epilogue()

        seq = []
        for b in range(BL):
            for d in range(2):
                order = list(range(NQ)) if d == 0 else list(range(NQ - 1, -1, -1))
                for k, ci in enumerate(order):
                    seq.append((b, d, ci, k == 0))

        def reset_state():
            s.memset("dve", st32.subs(allg)[:], 0.0)
            s.memset("pool", stbf.subs(allg)[:], 0.0)

        run_scan(s, seq, prep, heavy, reset_state)
    s.barrier()
    c.pr = Ring(c.pbanks)


def layer_gla(c, W, hin, hout):
    nc, s = c.nc, c.s
    H, DK, DV = 4, 128, 512
    dr = c.dram
    q_tm = dr("gla_q", [T, 512], BF16)
    k_tm = dr("gla_k", [T, 512], BF16)
    v_tm = dr("gla_v", [T, DI], BF16)
    z_tm = dr("gla_z", [T, DI], BF16)
    glT = dr("gla_glT", [32, T], F32)
    of = dr("gla_of", [T, DI], F32)

    with ExitStack() as es:
        uT = sb(es, nc, "uT", [128, 8, T], BF16)
        phase_norm(c, hin, W["gla_norm"], uT)
        phase_project(c, uT, W["gla_w_in"], [
            (0, 512, "tm", q_tm, BF16, DK ** -0.5),
            (512, 512, "tm", k_tm, BF16, 1.0),
            (1024, DI, "tm", v_tm, BF16, 1.0),
            (3072, DI, "tm", z_tm, BF16, 1.0, AF.Silu),
            (5120, 32, "fm", glT, F32, 1.0),
        ])

    with ExitStack() as es:
        fin = Finalizer(c, es, W["gla_w_out"], hin, hout)
        wg = sb(es, nc, "g_wg", [16, 2, 512], F32)
        bg = sb(es, nc, "g_bg", [128, 2, 512], F32)
        on = sb(es, nc, "g_on", [128, 512], F32)
        s.dma("sp", wg[:], V(W["gla_w_gate"].rearrange("d r k -> r d k"), "w_const"))
        s.dma("sp", V(bg.t[:].rearrange("p d k -> p (d k)"), bg.key),
              V(W["gla_b_gate"].rearrange("d k -> (d k)").partition_broadcast(128), "w_const"))
        s.dma("sp", on[:], V(W["gla_onorm"].partition_broadcast(128), "w_const"))
        qr = sbring(es, nc, "g_q", [128, 512], BF16, 2)
        kr = sbring(es, nc, "g_k", [128, 512], BF16, 2)
        vr = sbring(es, nc, "g_v", [128, DI], BF16, 2)
        glr = sbring(es, nc, "g_gl", [16, 128], F32, 2)
        Lpr = sbring(es, nc, "g_Lp", [128, 512], F32, 2)
        Eqr = sbring(es, nc, "g_Eq", [128, 512], F32, 2)
        Ekr = sbring(es, nc, "g_Ek", [128, 512], F32, 2)
        Ee = sbring(es, nc, "g_Ee", [128, 4], F32, 2)
        qsr = sbring(es, nc, "g_qs", [128, 512], BF16, 2)
        ksr = sbring(es, nc, "g_ks", [128, 512], BF16, 2)
        qkTr = sbring(es, nc, "g_qkT", [128, 8, 128], BF16, 2)
        PTr = sbring(es, nc, "g_PT", [128, 4, 128], BF16, 2)
        oaccr = sbring(es, nc, "g_oacc", [128, DI], F32, 2)
        S32 = sb(es, nc, "g_S32", [128, H, DV], F32)
        Sbf = sb(es, nc, "g_Sbf", [128, H, DV], BF16)
        oft = sb(es, nc, "g_of", [128, DI], F32)
        zt = sb(es, nc, "g_z", [128, DI], BF16)
        tmp = sb(es, nc, "g_tmp", [128, DI], F32)
        gst = sbring(es, nc, "g_st", [128, 4, 4], F32, 2)
        yb = sb(es, nc, "g_yb", [128, DI], BF16)
        allh = [str(h) for h in range(H)]

        def r3(buf, q):
            return V(buf.t[:].rearrange("p (h q) -> p h q", q=q), buf.key)

        def prep(b, d, ci, tout):
            tri = c.tri[:, d, :]
            i = b * NQ + ci
            r0 = i * 128
            qt, kt, vt, gt = qr.next(), kr.next(), vr.next(), glr.next()
            Lp, Eq, Ek, qs, ks = Lpr.next(), Eqr.next(), Ekr.next(), qsr.next(), ksr.next()
            qkT, PT, oacc = qkTr.next(), PTr.next(), oaccr.next()
            s.dma("sp", qt[:], q_tm[r0:r0 + 128, :])
            s.dma("sp", kt[:], k_tm[r0:r0 + 128, :])
            s.dma("sp", vt[:], v_tm[r0:r0 + 128, :])
            s.dma("sp", gt[:], glT[d * 16:(d + 1) * 16, r0:r0 + 128])
            pg = c.pr.next()
            s.mm(pg[:, :], gt[:, :], wg[:, d, :])
            s.tt("dve", Lp[:], pg[:, :], bg[:, d, :], ALU.add)
            s.act(Lp[:], Lp[:], AF.Exp, scale=-1.0)
            s.act(Lp[:], Lp[:], AF.Ln, bias=c.one[:, 0:1])
            yield
            pcum = c.pr.next()
            s.mm(pcum[:, :], tri, Lp[:])
            s.act(Eq[:], pcum[:, :], AF.Exp, scale=-1.0 / 16)
            s.act(Ek[:], pcum[:, :], AF.Exp, scale=1.0 / 16)
            yield
            ptot = c.pr.next()
            for h in range(H):
                s.mm(ptot[:, h:h + 1], Lp[:, h * 128:(h + 1) * 128], c.onesf[:, 0:1])
            ee = Ee.next()
            s.act(ee[:], ptot[:, 0:4], AF.Exp, scale=-1.0 / 16)
            s.tt("dve", qs[:], qt[:], Eq[:], ALU.mult)
            s.tt("dve", ks[:], kt[:], Ek[:], ALU.mult)
            yield
            pt = c.ptr.next()
            for h in range(H):
                s.tr(pt[:, h, :], qs[:, h * 128:(h + 1) * 128], c.identb[:])
                s.tr(pt[:, 4 + h, :], ks[:, h * 128:(h + 1) * 128], c.identb[:])
            s.cp("act", qkT[:], pt[:, :, :])
            yield
            pS = c.pr.next()
            for h in range(H):
                s.mm(pS[:, h * 128:(h + 1) * 128], qkT[:, 4 + h, :], qkT[:, h, :])
            s.tt("dve", PT[:], V(pS.t[:, :].rearrange("p (a l) -> p a l", a=4), pS.key),
                 bc(V(tri.ap.unsqueeze(1), tri.k), [128, 4, 128]), ALU.mult)
            tout.update(dict(i=i, r0=r0, vt=vt, qkT=qkT, PT=PT, ks=ks, ee=ee, oacc=oacc))

        def heavy(b, d, ci, t, bgs):
            i, r0, vt, qkT, PT, ks, ee, oacc = (t[k] for k in ("i", "r0", "vt", "qkT", "PT", "ks", "ee", "oacc"))
            for h in range(H):
                po = c.pr.next()
                s.mm(po[:, :], PT[:, h, :], vt[:, h * DV:(h + 1) * DV], start=True, stop=False)
                s.mm(po[:, :], qkT[:, h, :], Sbf.sub(str(h))[:, h, :], start=False, stop=True)
                s.cp("act", oacc[:, h * DV:(h + 1) * DV], po[:, :])
                pu = c.pr.next()
                s.mm(pu[:, :], ks[:, h * 128:(h + 1) * 128], vt[:, h * DV:(h + 1) * DV])
                sh = S32.sub(str(h))
                s.tt("dve", sh[:, h, :], sh[:, h, :], pu[:, :], ALU.add)
                s.act(sh[:, h, :], sh[:, h, :], AF.Copy, scale=ee[:, h:h + 1])
                s.cp("dve", Sbf.sub(str(h))[:, h, :], sh[:, h, :])
                drain(bgs, 3)
            drain_all(bgs)

            def epilogue():
                if d == 0:
                    s.dma(STQ, of[r0:r0 + 128, :], oacc[:])
                    return
                s.dma("sp", oft[:], of[r0:r0 + 128, :])
                s.dma("sp", zt[:], z_tm[r0:r0 + 128, :])
                s.tt("dve", oft[:], oft[:], oacc[:], ALU.add)
                yield
                gs = gst.next()
                s.act(tmp[:], oft[:], AF.Square)
                s.red("dve", gs[:, 0, :], r3(tmp, DV), ALU.add)
                s.ts("dve", gs[:, 1, :], gs[:, 0, :], 1.0 / DV, EPS, ALU.mult, ALU.add)
                s.act(gs[:, 2, :], gs[:, 1, :], AF.Ln)
                s.act(gs[:, 3, :], gs[:, 2, :], AF.Exp, scale=-0.5)
                yield
                s.tt("dve", r3(oft, DV), r3(oft, DV), bc(V(gs.t[:, 3, :].unsqueeze(2), gs.key), [128, H, DV]), ALU.mult)
                yield
                s.tt("dve", r3(oft, DV), r3(oft, DV), bc(V(on.t[:].unsqueeze(1), on.key), [128, H, DV]), ALU.mult)
                yield
                s.tt("dve", yb[:], oft[:], zt[:], ALU.mult)
                yield
                yield from fin.run_gen(yb, i)

            return epilogue()


        seq = []
        for b in range(BL):
            for d in range(2):
                order = list(range(NQ)) if d == 0 else list(range(NQ - 1, -1, -1))
                for k, ci in enumerate(order):
                    seq.append((b, d, ci, k == 0))

        def reset_state():
            s.memset("dve", S32.subs(allh)[:], 0.0)
            s.memset("pool", Sbf.subs(allh)[:], 0.0)

        run_scan(s, seq, prep, heavy, reset_state)
    s.barrier()


def layer_mlstm(c, W, hin, hout):
    nc, s = c.nc, c.s
    H, DK, DV = 4, 256, 512
    dr = c.dram
    xmT = dr("ml_xmT", [DI, T], BF16)
    z_tm = dr("ml_z", [T, DI], BF16)
    og_tm = dr("ml_og", [T, DI], BF16)
    gt_tm = dr("ml_gates", [T, 16], F32)
    chT = dr("ml_chT", [DI, T], BF16)
    ch_tm = dr("ml_ch", [T, DI], BF16)
    qk_tm = dr("ml_qk", [T, H * 512], BF16)
    v_tm = dr("ml_v", [T, DI], BF16)
    hf = dr("ml_hf", [T, DI], F32)

    with ExitStack() as es:
        uT = sb(es, nc, "uT", [128, 8, T], BF16)
        phase_norm(c, hin, W["ml_norm"], uT)
        phase_project(c, uT, W["ml_w_in"], [
            (0, DI, "fm", xmT, BF16, 1.0),
            (DI, DI, "tm", z_tm, BF16, 1.0, AF.Silu),
            (2 * DI, DI, "tm", og_tm, BF16, 1.0, AF.Sigmoid),
            (3 * DI, 16, "tm", gt_tm, F32, 1.0),
        ])

    with ExitStack() as es:
        stg = sbring(es, nc, "mc_stg", [128, 4, 128], BF16, 3)
        skc = sb(es, nc, "mc_skip", [128, DI], F32)
        s.dma("sp", skc[:], V(W["ml_skip"].rearrange("h d -> (h d)").partition_broadcast(128), "w_const"))

        def emit(ct, b, tb, cv):
            col = b * L + tb * 512
            to_tm_store(c, cv, ch_tm, col, ct * 128, stg, mul=skc[:, ct * 128:(ct + 1) * 128])
            s.dma(STQ, chT[ct * 128:(ct + 1) * 128, col:col + 512], cv)

        conv_fm(c, xmT, 16, 5, W["ml_conv_wT"], W["ml_conv_b"], True, emit)
    s.barrier()

    with ExitStack() as es:
        wq = sb(es, nc, "mq_wq", [128, H, 4, 256], BF16)
        wk = sb(es, nc, "mq_wk", [128, H, 4, 256], BF16)
        wv = sb(es, nc, "mq_wv", [128, H, 4, 512], BF16)
        with ExitStack() as es2:
            stf = sbring(es2, nc, "mq_stg", [128, 4096], F32, 2)
            st = stf.next()
            sv = V(st.t[:].rearrange("p (h k n) -> p h k n", h=4, k=4), st.key)
            s.dma("sp", sv, V(W["ml_w_q"].rearrange("h (k p) n -> p h k n", p=128), "w_const"))
            s.cp("dve", wq[:], sv)
            st = stf.next()
            sv = V(st.t[:].rearrange("p (h k n) -> p h k n", h=4, k=4), st.key)
            s.dma("sp", sv, V(W["ml_w_k"].rearrange("h (k p) n -> p h k n", p=128), "w_const"))
            s.cp("act", wk[:], sv)
            for hh in range(2):
                st = stf.next()
                sv = V(st.t[:].rearrange("p (h k n) -> p h k n", h=2, k=4), st.key)
                s.dma("sp", sv, V(W["ml_w_v"][hh * 2:hh * 2 + 2].rearrange("h (k p) n -> p h k n", p=128), "w_const"))
                s.cp("dve" if hh == 0 else "act", wv[:, hh * 2:hh * 2 + 2, :, :], sv)
            s.barrier()
        chr_ = sbring(es, nc, "mq_ch", [128, 16, 128], BF16, 2)
        xmr = sbring(es, nc, "mq_xm", [128, 16, 128], BF16, 2)
        qko = sbring(es, nc, "mq_qko", [128, H * 512], BF16, 2)
        vo = sbring(es, nc, "mq_vo", [128, DI], BF16, 2)
        for i in range(NT):
            cht, xmt, qo, vot = chr_.next(), xmr.next(), qko.next(), vo.next()
            s.dma("sp", cht[:], V(chT.t[:, i * 128:(i + 1) * 128].rearrange("(j p) t -> p j t", p=128), chT.key))
            s.dma("sp", xmt[:], V(xmT.t[:, i * 128:(i + 1) * 128].rearrange("(j p) t -> p j t", p=128), xmT.key))
            for h in range(H):
                pq = c.pr.next()
                for kc in range(4):
                    s.mm(pq[:, 0:256], cht[:, h * 4 + kc, :], wq[:, h, kc, :], start=(kc == 0), stop=(kc == 3))
                for kc in range(4):
                    s.mm(pq[:, 256:512], cht[:, h * 4 + kc, :], wk[:, h, kc, :], start=(kc == 0), stop=(kc == 3))
                s.cp("act", qo[:, h * 512:(h + 1) * 512], pq[:, :])
                pv = c.pr.next()
                for kc in range(4):
                    s.mm(pv[:, :], xmt[:, h * 4 + kc, :], wv[:, h, kc, :], start=(kc == 0), stop=(kc == 3))
                s.cp("dve", vot[:, h * 512:(h + 1) * 512], pv[:, :])
            s.dma(STQ, qk_tm[i * 128:(i + 1) * 128, :], qo[:])
            s.dma(STQ, v_tm[i * 128:(i + 1) * 128, :], vot[:])
    s.barrier()

    with ExitStack() as es:
        c.pr = Ring(c.pbanks[0:5])
        psm = c.pbanks[5]
        fin = Finalizer(c, es, W["ml_w_out"], hin, hout)
        gb = sb(es, nc, "m_gb", [128, 16], F32)
        on = sb(es, nc, "m_on", [128, 512], F32)
        skp = sb(es, nc, "m_skip", [128, DI], F32)
        s.dma("sp", gb[:], V(W["ml_gate_b"].rearrange("a b c -> (a b c)").partition_broadcast(128), "w_const"))
        s.dma("sp", on[:], V(W["ml_onorm"].partition_broadcast(128), "w_const"))
        s.dma("sp", skp[:], V(W["ml_skip"].rearrange("h d -> (h d)").partition_broadcast(128), "w_const"))
        onesb = sb(es, nc, "m_onesb", [128, 2], BF16)
        s.memset("dve", onesb[:], 1.0)
        qkr = sbring(es, nc, "m_qk", [128, H, 512], BF16, 2)
        vr = sbring(es, nc, "m_v", [128, DI], BF16, 2)
        gr = sbring(es, nc, "m_g", [128, 16], F32, 2)
        sm = sbring(es, nc, "m_sm", [128, 8, 4], F32, 2)
        qsr = sbring(es, nc, "m_qs", [128, H, 256], BF16, 2)
        ksr = sbring(es, nc, "m_ks", [128, H, 256], BF16, 2)
        qTr = sbring(es, nc, "m_qT", [128, 8, 128], BF16, 2)
        kTr = sbring(es, nc, "m_kT", [128, 8, 128], BF16, 2)
        PTr = sbring(es, nc, "m_PT", [128, 4, 128], BF16, 2)
        haccr = sbring(es, nc, "m_hacc", [128, DI], F32, 2)
        C32 = sb(es, nc, "m_C32", [128, 8, DV], F32)
        Cbf = sb(es, nc, "m_Cbf", [128, 8, DV], BF16)
        n32 = sb(es, nc, "m_n32", [128, 8], F32)
        nbf = sb(es, nc, "m_nbf", [128, 8], BF16)
        hft = sb(es, nc, "m_hf", [128, DI], F32)
        ogt = sb(es, nc, "m_og", [128, DI], BF16)
        zt = sb(es, nc, "m_z", [128, DI], BF16)
        cht = sb(es, nc, "m_ch", [128, DI], BF16)
        tmp = sb(es, nc, "m_tmp", [128, DI], F32)
        gst = sbring(es, nc, "m_st", [128, 4, 4], F32, 2)
        yb = sb(es, nc, "m_yb", [128, DI], BF16)
        allj = [str(j) for j in range(8)]

        def r3(buf, q):
            return V(buf.t[:].rearrange("p (h q) -> p h q", q=q), buf.key)

        def prep(b, d, ci, tout):
            tri = c.tri[:, d, :]
            i = b * NQ + ci
            r0 = i * 128
            qkt, vt, gt = qkr.next(), vr.next(), gr.next()
            qs, ks, qT, kT, PT, hacc = qsr.next(), ksr.next(), qTr.next(), kTr.next(), PTr.next(), haccr.next()
            s.dma("sp", V(qkt.t[:].rearrange("p h n -> p (h n)"), qkt.key), qk_tm[r0:r0 + 128, :])
            s.dma("sp", vt[:], v_tm[r0:r0 + 128, :])
            s.dma("sp", gt[:], gt_tm[r0:r0 + 128, :])
            m = sm.next()
            s.tt("dve", gt[:], gt[:], gb[:], ALU.add)
            ig = gt[:, d * 8:d * 8 + 4]
            fr = gt[:, d * 8 + 4:d * 8 + 8]
            s.act(m[:, 0, :], fr, AF.Exp, scale=-1.0)
            s.act(m[:, 0, :], m[:, 0, :], AF.Ln, bias=c.one[:, 0:1])
            s.mm(psm[:, 0:4], tri, m[:, 0, :])
            s.mm(psm[:, 4:8], c.onesf[:], m[:, 0, :])
            s.act(m[:, 1, :], psm[:, 0:4], AF.Exp, scale=-1.0)
            s.tt("dve", m[:, 2, :], psm[:, 0:4], ig, ALU.add)
            s.act(m[:, 2, :], m[:, 2, :], AF.Exp, bias=c.one[:, 3:4])
            s.act(m[:, 3, :], psm[:, 4:8], AF.Exp, scale=-1.0)
            yield
            s.tt("dve", qs[:], V(qkt.t[:, :, 0:256], qkt.key),
                 bc(V(m.t[:, 1, :].unsqueeze(2), m.key), [128, H, 256]), ALU.mult)
            s.tt("dve", ks[:], V(qkt.t[:, :, 256:512], qkt.key),
                 bc(V(m.t[:, 2, :].unsqueeze(2), m.key), [128, H, 256]), ALU.mult)
            yield
            pt = c.ptr.next()
            for j in range(8):
                s.tr(pt[:, j, :], qs[:, j // 2, (j % 2) * 128:(j % 2 + 1) * 128], c.identb[:])
            s.cp("act", qT[:], pt[:, :, :])
            yield
            pt = c.ptr.next()
            for j in range(8):
                s.tr(pt[:, j, :], ks[:, j // 2, (j % 2) * 128:(j % 2 + 1) * 128], c.identb[:])
            s.cp("dve", kT[:], pt[:, :, :])
            yield
            pS = c.pr.next()
            for h in range(H):
                for kc in range(2):
                    s.mm(pS[:, h * 128:(h + 1) * 128], kT[:, h * 2 + kc, :], qT[:, h * 2 + kc, :],
                         start=(kc == 0), stop=(kc == 1))
            s.tt("dve", PT[:], V(pS.t[:, :].rearrange("p (a l) -> p a l", a=4), pS.key),
                 bc(V(tri.ap.unsqueeze(1), tri.k), [128, 4, 128]), ALU.mult)
            tout.update(dict(i=i, r0=r0, vt=vt, qT=qT, kT=kT, PT=PT, ks=ks, m=m, hacc=hacc))

        def heavy(b, d, ci, t, bgs):
            i, r0, vt, qT, kT, PT, ks, m, hacc = (t[k] for k in ("i", "r0", "vt", "qT", "kT", "PT", "ks", "m", "hacc"))
            for h in range(H):
                s.mm(psm[:, 8 + h:9 + h], PT[:, h, :], onesb[:, 0:1], start=True, stop=False)
                for kc in range(2):
                    s.mm(psm[:, 8 + h:9 + h], qT[:, h * 2 + kc, :], nbf[:, h * 2 + kc:h * 2 + kc + 1],
                         start=False, stop=(kc == 1))
            s.act(m[:, 4, :], psm[:, 8:12], AF.Abs)
            s.ts("dve", m[:, 4, :], m[:, 4, :], 1.0, None, ALU.max)
            s.recip("dve", m[:, 5, :], m[:, 4, :])
            for h in range(H):
                po = c.pr.next()
                s.mm(po[:, :], PT[:, h, :], vt[:, h * DV:(h + 1) * DV], start=True, stop=False)
                for kc in range(2):
                    s.mm(po[:, :], qT[:, h * 2 + kc, :], Cbf.sub(str(h * 2 + kc))[:, h * 2 + kc, :],
                         start=False, stop=(kc == 1))
                s.act(hacc[:, h * DV:(h + 1) * DV], po[:, :], AF.Copy, scale=m[:, 5, h:h + 1])
                for kc in range(2):
                    j = h * 2 + kc
                    pu = c.pr.next()
                    s.mm(pu[:, :], ks[:, h, kc * 128:(kc + 1) * 128], vt[:, h * DV:(h + 1) * DV])
                    s.mm(psm[:, 16 + j:17 + j], ks[:, h, kc * 128:(kc + 1) * 128], onesb[:, 0:1])
                    cj = C32.sub(str(j))
                    s.tt("dve", cj[:, j, :], cj[:, j, :], pu[:, :], ALU.add)
                    s.act(cj[:, j, :], cj[:, j, :], AF.Copy, scale=m[:, 3, h:h + 1])
                    s.cp("dve" if kc == 0 else "act", Cbf.sub(str(j))[:, j, :], cj[:, j, :])
                    drain(bgs, 2)
            s.tt("dve", n32[:], n32[:], psm[:, 16:24], ALU.add)
            s.tt("dve", V(n32.t[:].rearrange("p (h k) -> p h k", k=2), n32.key),
                 V(n32.t[:].rearrange("p (h k) -> p h k", k=2), n32.key),
                 bc(V(m.t[:, 3, :].unsqueeze(2), m.key), [128, H, 2]), ALU.mult)
            s.cp("dve", nbf[:], n32[:])
            drain_all(bgs)

            def epilogue():
                if d == 0:
                    s.dma(STQ, hf[r0:r0 + 128, :], hacc[:])
                    return
                s.dma("sp", hft[:], hf[r0:r0 + 128, :])
                s.dma("sp", ogt[:], og_tm[r0:r0 + 128, :])
                s.dma("sp", zt[:], z_tm[r0:r0 + 128, :])
                s.dma("sp", cht[:], ch_tm[r0:r0 + 128, :])
                s.tt("dve", hft[:], hft[:], hacc[:], ALU.add)
                yield
                s.tt("dve", hft[:], hft[:], ogt[:], ALU.mult)
                yield
                gs = gst.next()
                s.act(tmp[:], hft[:], AF.Square)
                s.red("dve", gs[:, 0, :], r3(tmp, DV), ALU.add)
                s.ts("dve", gs[:, 1, :], gs[:, 0, :], 1.0 / DV, EPS, ALU.mult, ALU.add)
                s.act(gs[:, 2, :], gs[:, 1, :], AF.Ln)
                s.act(gs[:, 3, :], gs[:, 2, :], AF.Exp, scale=-0.5)
                yield
                s.tt("dve", r3(hft, DV), r3(hft, DV), bc(V(gs.t[:, 3, :].unsqueeze(2), gs.key), [128, H, DV]), ALU.mult)
                yield
                s.tt("dve", r3(hft, DV), r3(hft, DV), bc(V(on.t[:].unsqueeze(1), on.key), [128, H, DV]), ALU.mult)
                yield
                s.tt("dve", hft[:], hft[:], cht[:], ALU.add)
                yield
                s.tt("dve", yb[:], hft[:], zt[:], ALU.mult)
                yield
                yield from fin.run_gen(yb, i)

            return epilogue()


        seq = []
        for b in range(BL):
            for d in range(2):
                order = list(range(NQ)) if d == 0 else list(range(NQ - 1, -1, -1))
                for k, ci in enumerate(order):
                    seq.append((b, d, ci, k == 0))

        def reset_state():
            s.memset("dve", C32.subs(allj)[:], 0.0)
            s.memset("pool", Cbf.subs(allj)[:], 0.0)
            s.memset("dve", n32[:], 0.0)
            s.memset("pool", nbf[:], 0.0)

        run_scan(s, seq, prep, heavy, reset_state)
    s.barrier()
    c.pr = Ring(c.pbanks)


def load_mat(c, dst, src_ap):
    v = src_ap.rearrange("(st p) f -> p st f", p=128)
    for q in range(4):
        c.s.dma("sp", dst[:, q * 4:(q + 1) * 4, :], V(v[:, q * 4:(q + 1) * 4, :], "w_const"))


def layer_hyena(c, W, hin, hout):
    nc, s = c.nc, c.s
    C = c.C
    dr = c.dram
    vxT = dr("hy_vxT", [3 * DI, T], BF16)
    z_tm = dr("hy_z", [T, DI], BF16)
    sig = [dr("hy_v", [T, DI], BF16), dr("hy_x1", [T, DI], BF16), dr("hy_x2", [T, DI], BF16)]
    y1_tm = dr("hy_y1", [T, DI], BF16)
    y2_tm = dr("hy_y2", [T, DI], BF16)
    Kf = dr("hy_Kf", [2, 2, L, DI], BF16)
    Zd = dr("hy_Z", [BL, 2, L, DI], BF16)

    with ExitStack() as es:
        uT = sb(es, nc, "uT", [128, 8, T], BF16)
        phase_norm(c, hin, W["hy_norm"], uT)
        phase_project(c, uT, W["hy_w_in"], [
            (0, 3 * DI, "fm", vxT, BF16, 1.0),
            (3 * DI, DI, "tm", z_tm, BF16, 1.0, AF.Silu),
        ])

    with ExitStack() as es:
        stg = sbring(es, nc, "hc_stg", [128, 4, 128], BF16, 3)

        def emit(ct, b, tb, cv):
            to_tm_store(c, cv, sig[ct // 16], b * L + tb * 512, (ct % 16) * 128, stg)

        conv_fm(c, vxT, 48, 3, W["hy_conv_wT"], W["hy_conv_b"], False, emit)
    s.barrier()

    def fwd_transform(Cm, Sn, o, ysrc):
        with ExitStack() as es:
            yr = sbring(es, nc, "hf_y", [128, 16, 512], BF16, 2)
            kr = sbring(es, nc, "hf_k", [128, 2, 512], BF16, 2)
            yre = sb(es, nc, "hf_yre", [128, 512], F32)
            yim = sb(es, nc, "hf_yim", [128, 512], F32)
            t1 = sb(es, nc, "hf_t1", [128, 512], F32)
            t2 = sb(es, nc, "hf_t2", [128, 512], F32)
            t3 = sb(es, nc, "hf_t3", [128, 512], F32)
            t4 = sb(es, nc, "hf_t4", [128, 512], F32)
            zr = sbring(es, nc, "hf_z", [128, 2, 512], BF16, 2)
            for b in range(BL):
                for cb in range(4):
                    yt = yr.next()
                    cs = slice(cb * 512, (cb + 1) * 512)
                    s.dma("sp", yt[:], V(ysrc.t[b * L:(b + 1) * L, cs].rearrange("(st p) c -> p st c", p=128), ysrc.key))
                    for ft in range(16):
                        fs = slice(ft * 128, (ft + 1) * 128)
                        kt = kr.next()
                        s.dma("sp", kt[:], V(Kf.t[o, :, fs, cs].rearrange("a p c -> p a c"), Kf.key))
                        pre, pim = c.pr.next(), c.pr.next()
                        for st in range(16):
                            s.mm(pre[:, :], Cm[:, st, fs], yt[:, st, :], start=(st == 0), stop=(st == 15))
                        for st in range(16):
                            s.mm(pim[:, :], Sn[:, st, fs], yt[:, st, :], start=(st == 0), stop=(st == 15))
                        s.cp("act", yre[:], pre[:, :])
                        s.cp("act", yim[:], pim[:, :])
                        s.tt("dve", t1[:], yre[:], kt[:, 0, :], ALU.mult)
                        s.tt("dve", t2[:], yim[:], kt[:, 1, :], ALU.mult)
                        s.tt("dve", t3[:], yre[:], kt[:, 1, :], ALU.mult)
                        s.tt("dve", t4[:], yim[:], kt[:, 0, :], ALU.mult)
                        zt = zr.next()
                        s.tt("dve", zt[:, 0, :], t1[:], t2[:], ALU.subtract)
                        s.tt("pool", zt[:, 1, :], t3[:], t4[:], ALU.add)
                        s.dma(STQ, V(Zd.t[b, :, fs, cs].rearrange("a p c -> p a c"), Zd.key), zt[:])
        s.barrier()

    def inv_transform(o, ysrc, xg_src, ydst):
        with ExitStack() as es:
            CI = sb(es, nc, "hi_CI", [128, 16, L], BF16)
            SI = sb(es, nc, "hi_SI", [128, 16, L], BF16)
            load_mat(c, CI, C["c_CI"])
            load_mat(c, SI, C["c_SI"])
            dbc = sb(es, nc, "hi_d", [128, DI], F32)
            s.dma("sp", dbc[:], V(W["hy_d"][o].partition_broadcast(128), "w_const"))
            zb = sb(es, nc, "hi_zb", [128, 2, 16, 512], BF16)
            ypr = sbring(es, nc, "hi_yp", [128, 512], BF16, 2)
            xgr = sbring(es, nc, "hi_xg", [128, 512], BF16, 2)
            zzr = sbring(es, nc, "hi_zz", [128, 512], BF16, 2)
            ta = sbring(es, nc, "hi_ta", [128, 512], F32, 2)
            tb_ = sbring(es, nc, "hi_tb", [128, 512], F32, 2)
            yo = sbring(es, nc, "hi_yo", [128, 512], BF16, 2)
            for b in range(BL):
                for cb in range(4):
                    cs = slice(cb * 512, (cb + 1) * 512)
                    for a in range(2):
                        for q in range(2):
                            s.dma("sp", zb[:, a, q * 8:(q + 1) * 8, :],
                                  V(Zd.t[b, a, q * 1024:(q + 1) * 1024, cs].rearrange("(ft p) c -> p ft c", p=128), Zd.key))
                    for tt in range(16):
                        ts_ = slice(tt * 128, (tt + 1) * 128)
                        rows = slice(b * L + tt * 128, b * L + (tt + 1) * 128)
                        po = c.pr.next()
                        for ft in range(16):
                            s.mm(po[:, :], CI[:, ft, ts_], zb[:, 0, ft, :], start=(ft == 0), stop=False)
                        for ft in range(16):
                            s.mm(po[:, :], SI[:, ft, ts_], zb[:, 1, ft, :], start=False, stop=(ft == 15))
                        yp, xg = ypr.next(), xgr.next()
                        s.dma("sp", yp[:], ysrc[rows, cs])
                        s.dma("sp", xg[:], xg_src[rows, cs])
                        t_a = ta.next()
                        s.tt("dve", t_a[:], yp[:], dbc[:, cs], ALU.mult)
                        s.tt("dve", t_a[:], t_a[:], po[:, :], ALU.add)
                        yot = yo.next()
                        if o == 0:
                            s.tt("dve", yot[:], t_a[:], xg[:], ALU.mult)
                        else:
                            zz, t_b = zzr.next(), tb_.next()
                            s.dma("sp", zz[:], z_tm[rows, cs])
                            s.tt("dve", t_a[:], t_a[:], xg[:], ALU.mult)
                            s.tt("dve", yot[:], t_a[:], zz[:], ALU.mult)
                        s.dma(STQ, ydst[rows, cs], yot[:])
        s.barrier()

    with ExitStack() as es:
        Cm = sb(es, nc, "hy_Cm", [128, 16, L], BF16)
        Sn = sb(es, nc, "hy_Sn", [128, 16, L], BF16)
        load_mat(c, Cm, C["c_Cm"])
        load_mat(c, Sn, C["c_Sn"])
        with ExitStack() as es2:
            hA = sb(es2, nc, "hm_hA", [64, L], F32)
            hB = sb(es2, nc, "hm_hB", [64, L], F32)
            with ExitStack() as es3:
                feats = sb(es3, nc, "hm_feats", [33, L], F32)
                w1 = sb(es3, nc, "hm_w1", [33, 64], F32)
                wh = sb(es3, nc, "hm_wh", [64, 2, 64], F32)
                prm = sb(es3, nc, "hm_prm", [64, 8], F32)
                tr_ = sb(es3, nc, "hm_t", [64, 512], F32)
                tki = sb(es3, nc, "hm_ki", [64, 512], mybir.dt.int32)
                tkf = sb(es3, nc, "hm_kf", [64, 512], F32)
                s.dma("sp", feats[:], V(C["c_featsT"], "w_const"))
                s.dma("sp", w1[:], V(W["hy_ffn_w_in"], "w_const"))
                s.dma("sp", wh[:], V(W["hy_ffn_w_hid"].rearrange("j a b -> a j b"), "w_const"))
                s.dma("sp", prm[:, 0:1], V(W["hy_ffn_b_in"].rearrange("(p o) -> p o", o=1), "w_const"))
                s.dma("sp", prm[:, 1:3], V(W["hy_ffn_b_hidT"], "w_const"))
                s.dma("sp", prm[:, 3:6], V(W["hy_ffn_freqT"], "w_const"))
                cur, nxt = hA, hB
                for layer in range(3):
                    for blk in range(4):
                        bs = slice(blk * 512, (blk + 1) * 512)
                        ps = c.pr.next()
                        if layer == 0:
                            s.mm(ps[0:64, :], w1[:, :], feats[:, bs])
                            dst = cur
                        else:
                            s.mm(ps[0:64, :], wh[:, layer - 1, :], cur[:, bs])
                            dst = nxt
                        s.ts("dve", tr_[:], ps[0:64, :], prm[:, layer:layer + 1], prm[:, 3 + layer:4 + layer], ALU.add, ALU.mult)
                        s.ts("dve", tr_[:], tr_[:], 1.0 / (2.0 * PI), 8.5, ALU.mult, ALU.add)
                        s.cp("dve", tki[:], tr_[:])
                        s.cp("dve", tkf[:], tki[:])
                        s.tt("dve", tr_[:], tr_[:], tkf[:], ALU.subtract)
                        s.ts("dve", tkf[:], tr_[:], 0.0, None, ALU.is_lt)
                        s.tt("dve", tr_[:], tr_[:], tkf[:], ALU.add)
                        s.act(dst[:, bs], tr_[:], AF.Sin, bias=c.one[0:64, 1:2], scale=2.0 * PI)
                    if layer > 0:
                        cur, nxt = nxt, cur
                h3 = cur
                s.barrier()
            dlt = sb(es2, nc, "hm_dlt", [128, DI], F32)
            ngt = sb(es2, nc, "hm_negt", [128, 16], F32)
            s.dma("sp", dlt[:], V(C["c_deltas"].partition_broadcast(128), "w_const"))
            s.dma("sp", ngt[:], V(C["c_negt"], "w_const"))
            wor = sbring(es2, nc, "hm_wo", [64, 2, 256], F32, 2)
            Ar = sb(es2, nc, "hm_A", [128, 16, 256], BF16)
            Br_ = sb(es2, nc, "hm_B", [128, 16, 256], BF16)
            dec = sbring(es2, nc, "hm_dec", [128, 256], F32, 2)
            hbs = sbring(es2, nc, "hm_hb", [128, 256], F32, 2)
            sa = sbring(es2, nc, "hm_sa", [128, 256], F32, 2)
            sbm = sbring(es2, nc, "hm_sb", [128, 256], F32, 2)
            ko = sbring(es2, nc, "hm_ko", [128, 512], BF16, 2)
            wov = W["hy_ffn_w_out"]
            for o in range(2):
                for cb in range(8):
                    cs = slice(cb * 256, (cb + 1) * 256)
                    wo = wor.next()
                    for dd in range(2):
                        c0 = dd * 2 * DI + o * DI + cb * 256
                        s.dma("sp", wo[:, dd, :], V(wov[:, c0:c0 + 256], "w_const"))
                    for tt in range(16):
                        ps = c.pr.next()
                        s.mm(ps[:, 0:256], h3[:, tt * 128:(tt + 1) * 128], wo[:, 0, :])
                        s.mm(ps[:, 256:512], h3[:, tt * 128:(tt + 1) * 128], wo[:, 1, :])
                        dc, hb_, a_, b_ = dec.next(), hbs.next(), sa.next(), sbm.next()
                        s.act(dc[:], dlt[:, cs], AF.Exp, scale=ngt[:, tt:tt + 1])
                        s.cp("dve", hb_[:], ps[:, 256:512])
                        if tt == 0:
                            s.memset("dve", hb_[0:1, :], 0.0)
                        s.tt("dve", a_[:], ps[:, 0:256], hb_[:], ALU.add)
                        s.tt("dve", b_[:], ps[:, 0:256], hb_[:], ALU.subtract)
                        s.tt("dve", Ar[:, tt, :], a_[:], dc[:], ALU.mult)
                        s.tt("pool", Br_[:, tt, :], b_[:], dc[:], ALU.mult)
                    for ft in range(16):
                        fs = slice(ft * 128, (ft + 1) * 128)
                        pk = c.pr.next()
                        for tt in range(16):
                            s.mm(pk[:, 0:256], Cm[:, tt, fs], Ar[:, tt, :], start=(tt == 0), stop=(tt == 15))
                        for tt in range(16):
                            s.mm(pk[:, 256:512], Sn[:, tt, fs], Br_[:, tt, :], start=(tt == 0), stop=(tt == 15))
                        kt = ko.next()
                        s.cp("act", kt[:], pk[:, :])
                        s.dma(STQ, V(Kf.t[o, :, fs, cs].rearrange("a p c -> p a c"), Kf.key),
                              V(kt.t[:].rearrange("p (a c) -> p a c", a=2), kt.key))
            s.barrier()
        fwd_transform(Cm, Sn, 0, sig[0])
    inv_transform(0, sig[0], sig[1], y1_tm)
    with ExitStack() as es:
        Cm = sb(es, nc, "hy_Cm2", [128, 16, L], BF16)
        Sn = sb(es, nc, "hy_Sn2", [128, 16, L], BF16)
        load_mat(c, Cm, C["c_Cm"])
        load_mat(c, Sn, C["c_Sn"])
        fwd_transform(Cm, Sn, 1, y1_tm)
    inv_transform(1, y1_tm, sig[2], y2_tm)

    with ExitStack() as es:
        fin = Finalizer(c, es, W["hy_w_out"], hin, hout)
        yr = sbring(es, nc, "ho_y", [128, DI], BF16, 2)
        for i in range(NT):
            yt = yr.next()
            s.dma("sp", yt[:], y2_tm[i * 128:(i + 1) * 128, :])
            fin.run(yt, i)
    s.barrier()


N_IMPL = 4
STQ = "act"
DEBUG_STOP = None


def host_constants():
    cst = {}
    cst["c_identb"] = np.eye(128, dtype=np.float32).astype(ml_dtypes.bfloat16)
    cst["c_identf"] = np.eye(128, dtype=np.float32)
    sidx = np.arange(128)[:, None]
    lidx = np.arange(128)[None, :]
    tri = np.stack([(sidx <= lidx), (sidx >= lidx)], axis=1).astype(np.float32)
    cst["c_tri"] = tri
    cst["c_negm"] = ((1.0 - tri) * -1.0e5).astype(np.float32)
    sI = np.arange(L, dtype=np.int64)[:, None]
    fI = np.arange(L, dtype=np.int64)[None, :]
    ph = ((2 * fI + 1) * sI) % (4 * L)
    th = (2.0 * np.pi / (4 * L)) * ph.astype(np.float64)
    cm = np.cos(th)
    sn = np.sin(th)
    bf = ml_dtypes.bfloat16
    cst["c_Cm"] = cm.astype(np.float32).astype(bf)
    cst["c_Sn"] = (-sn).astype(np.float32).astype(bf)
    cst["c_CI"] = np.ascontiguousarray((cm.T / L)).astype(np.float32).astype(bf)
    cst["c_SI"] = np.ascontiguousarray((-sn.T / L)).astype(np.float32).astype(bf)
    t = np.linspace(0.0, 1.0, L, dtype=np.float32)[:, None]
    pos = np.arange(L, dtype=np.float32)[:, None]
    bands = np.linspace(1e-4, 15.0, 16, dtype=np.float32)[None]
    ang = (np.float32(2.0 * math.pi / L) * pos * bands).astype(np.float32)
    feats = np.concatenate([t, np.cos(ang), -np.sin(ang)], axis=-1).astype(np.float32)
    cst["c_featsT"] = np.ascontiguousarray(feats.T)
    max_decay = math.log(1e-2) / 0.3
    min_decay = math.log(1e-2) / 1.5
    cst["c_deltas"] = np.abs(np.linspace(min_decay, max_decay, DI, dtype=np.float32)).astype(np.float32)
    cst["c_negt"] = np.ascontiguousarray(-(t[:, 0].reshape(16, 128).T)).astype(np.float32)
    return cst


def host_prepare(inputs):
    W = {}
    for k, v in inputs.items():
        if k in ("x", "final_norm"):
            continue
        W[k] = np.ascontiguousarray(v[0])
    W["final_norm"] = np.ascontiguousarray(inputs["final_norm"])
    W["ssd_conv_wT"] = np.ascontiguousarray(W.pop("ssd_conv_w").T)
    W["ml_conv_wT"] = np.ascontiguousarray(W.pop("ml_conv_w").T)
    W["hy_conv_wT"] = np.ascontiguousarray(W.pop("hy_conv_w").T)
    W["hy_ffn_b_hidT"] = np.ascontiguousarray(W.pop("hy_ffn_b_hid").T)
    W["hy_ffn_freqT"] = np.ascontiguousarray(W.pop("hy_ffn_freq").T)
    return W


def build_program(wshapes, cshapes, n_layers=4, final_norm=True):
    n_layers = min(n_layers, N_IMPL)
    nc = bass.Bass("TRN2", target_bir_lowering=False)
    c = Ctx()
    c.nc = nc
    c.s = Sched(nc)
    s = c.s
    x_in = Buf(nc.dram_tensor("x", [T, D], F32, kind="ExternalInput").ap(), "x_in")
    out = Buf(nc.dram_tensor("out", [T, D], F32, kind="ExternalOutput").ap(), "out")
    W = {k: nc.dram_tensor(k, list(shp), F32 if dt == np.float32 else BF16, kind="ExternalInput").ap()
         for k, (shp, dt) in wshapes.items()}
    C = {k: nc.dram_tensor(k, list(shp), F32 if dt == np.float32 else BF16, kind="ExternalInput").ap()
         for k, (shp, dt) in cshapes.items()}

    def dram(name, shape, dt):
        return Buf(nc.dram_tensor(name, shape, dt, kind="Internal").ap(), name)

    c.dram = dram
    c.C = C
    hA = dram("hA", [T, D], F32)
    hB = dram("hB", [T, D], F32)

    with ExitStack() as es:
        c.identb = sb(es, nc, "k_identb", [128, 128], BF16)
        c.identf = sb(es, nc, "k_identf", [128, 128], F32)
        c.tri = sb(es, nc, "k_tri", [128, 2, 128], F32)
        c.negm = sb(es, nc, "k_negm", [128, 2, 128], F32)
        c.onesf = sb(es, nc, "k_onesf", [128, 128], F32)
        c.nonesf = sb(es, nc, "k_nonesf", [128, 128], F32)
        c.one = sb(es, nc, "k_one", [128, 4], F32)
        s.dma("sp", c.identb[:], V(C["c_identb"], "w_const"))
        s.dma("sp", c.identf[:], V(C["c_identf"], "w_const"))
        s.dma("sp", c.tri[:], V(C["c_tri"], "w_const"))
        s.dma("sp", c.negm[:], V(C["c_negm"], "w_const"))
        s.memset("dve", c.onesf[:], 1.0)
        s.memset("dve", c.nonesf[:], -1.0)
        s.memset("dve", c.one[:, 0:1], 1.0)
        s.memset("dve", c.one[:, 1:2], -PI)
        s.memset("dve", c.one[:, 2:3], 0.0)
        s.memset("dve", c.one[:, 3:4], math.log(1.0 / 16.0))
        pbanks = [Buf(es.enter_context(nc.psum_tensor(f"ps{i}", [128, 512], F32)), f"ps{i}") for i in range(6)]
        tbanks = [Buf(es.enter_context(nc.psum_tensor(f"pt{i}", [128, 8, 128], BF16)), f"pt{i}") for i in range(2)]
        c.pbanks = pbanks
        c.pr = Ring(pbanks)
        c.ptr = Ring(tbanks)
        s.barrier()
        c.identb.key = c.identf.key = c.tri.key = c.negm.key = "konst"
        c.onesf.key = c.nonesf.key = c.one.key = "konst"

        layers = [layer_ssd, layer_gla, layer_hyena, layer_mlstm]
        hs = [x_in, hA, hB, hA, hB]
        hcur = x_in
        for li in range(n_layers):
            hnext = hA if (li % 2 == 0) else hB
            layers[li](c, W, hcur, hnext)
            hcur = hnext
        if final_norm:
            phase_final_norm(c, hcur, W["final_norm"], out)
        else:
            with ExitStack() as es2:
                cr = sbring(es2, nc, "cp_x", [128, D], F32, 2)
                for i in range(NT):
                    t = cr.next()
                    s.dma("sp", t[:], hcur[i * 128:(i + 1) * 128, :])
                    s.dma(STQ, out[i * 128:(i + 1) * 128, :], t[:])
            s.barrier()
    return nc


_CACHE = {}


def kernel(**inputs):
    x = np.ascontiguousarray(inputs["x"], dtype=np.float32)
    W = host_prepare(inputs)
    Cst = host_constants()
    wshapes = {k: (v.shape, v.dtype.type if v.dtype != ml_dtypes.bfloat16 else "bf16") for k, v in W.items()}
    cshapes = {k: (v.shape, v.dtype.type if v.dtype != ml_dtypes.bfloat16 else "bf16") for k, v in Cst.items()}
    nc = build_program(wshapes, cshapes)
    in_maps = []
    for i in range(NCORES):
        m = {"x": x[i * BL:(i + 1) * BL].reshape(T, D)}
        m.update(W)
        m.update(Cst)
        in_maps.append(m)
    res = run_bass_kernel_spmd(nc, in_maps, core_ids=list(range(NCORES)))
    outs = [r["out"].reshape(BL, L, D) for r in res.results]
    return np.concatenate(outs, axis=0).astype(np.float32)
```

```python
import math
from contextlib import ExitStack
import numpy as np
import ml_dtypes
import concourse.bass as bass
import concourse.mybir as mybir
from concourse.bass_utils import run_bass_kernel_spmd

F32 = mybir.dt.float32
BF16 = mybir.dt.bfloat16
AF = mybir.ActivationFunctionType
ALU = mybir.AluOpType
AX = mybir.AxisListType

NCORES = 8
BL = 2
L = 2048
T = BL * L
D = 1024
DI = 2048
Q = 128
NQ = L // Q
NT = T // 128
EPS = 1e-6
EPOCH = 30000
PI = math.pi


def _kt(k):
    if isinstance(k, str):
        return (k,)
    return tuple(k)


class V:
    __slots__ = ("ap", "k")

    def __init__(self, ap, k):
        self.ap = ap
        self.k = _kt(k)


class Buf:
    def __init__(self, t, key):
        self.t = t
        self.key = key

    def __getitem__(self, idx):
        return V(self.t[idx], self.key)

    def sub(self, sub):
        return Buf(self.t, f"{self.key}.{sub}")

    def subs(self, subs):
        return Buf(self.t, tuple(f"{self.key}.{x}" for x in subs))


class Sched:
    def __init__(self, nc):
        self.nc = nc
        self.eng = {"pe": nc.tensor, "dve": nc.vector, "act": nc.scalar,
                    "pool": nc.gpsimd, "sp": nc.sync}
        self.nsem = 0
        self.esem, self.ecnt = {}, {}
        for e in self.eng:
            self._new_epoch(e)
        self.seen = {e: {} for e in self.eng}
        self.res = {}
        self.dsem = {}
        self.dfree = []
        self.ninst = 0

    def _alloc(self):
        self.nsem += 1
        return self.nc.alloc_semaphore(name=f"s{self.nsem}")

    def _new_epoch(self, e):
        self.esem[e] = self._alloc()
        self.ecnt[e] = 0

    def _wait(self, e, tok):
        sem, val = tok
        sid = id(sem)
        if self.seen[e].get(sid, 0) >= val:
            return
        self.eng[e].wait_ge(sem, val)
        self.seen[e][sid] = val

    def _deps(self, e, reads, writes, pe_accum=False, dsem=None):
        for k in reads:
            r = self.res.get(k)
            if r and r["w"] is not None:
                self._wait(e, r["w"])
        for k in writes:
            r = self.res.get(k)
            if r:
                w = r["w"]
                if w is not None:
                    skip = (pe_accum and r["we"] == "pe") or (dsem is not None and w[0] is dsem)
                    if not skip:
                        self._wait(e, w)
                for t in r["r"]:
                    self._wait(e, t)

    def _record(self, e, tok, reads, writes):
        for k in reads:
            r = self.res.setdefault(k, {"w": None, "r": [], "we": None})
            r["r"] = [t for t in r["r"] if t[0] is not tok[0]] + [tok]
        for k in writes:
            self.res[k] = {"w": tok, "r": [], "we": e}

    def op(self, e, fn, reads=(), writes=(), pe_accum=False):
        reads = [k for ks in reads if ks is not None for k in _kt(ks)]
        writes = [k for ks in writes for k in _kt(ks)]
        self._deps(e, reads, writes, pe_accum)
        if self.ecnt[e] >= EPOCH:
            self._new_epoch(e)
        inst = fn()
        self.ecnt[e] += 1
        inst.then_inc(self.esem[e], 1)
        self._record(e, (self.esem[e], self.ecnt[e]), reads, writes)
        self.ninst += 1
        return inst

    def dma(self, e, out, in_, **kw):
        reads, writes = list(in_.k), list(out.k)
        sk = out.k[0]
        if sk not in self.dsem:
            if self.dfree:
                self.dsem[sk] = self.dfree.pop()
            else:
                self.dsem[sk] = [self._alloc(), 0]
        ds = self.dsem[sk]
        self._deps(e, reads, writes, dsem=ds[0])
        inst = self.eng[e].dma_start(out=out.ap, in_=in_.ap, **kw)
        ds[1] += 16
        inst.then_inc(ds[0], 16)
        self._record(e, (ds[0], ds[1]), reads, writes)
        self.ninst += 1
        return inst

    def barrier(self):
        toks = [(self.esem[e], self.ecnt[e]) for e in self.eng if self.ecnt[e] > 0]
        toks += [(d[0], d[1]) for d in self.dsem.values() if d[1] > 0]
        for e in self.eng:
            for t in toks:
                if t[0] is not self.esem[e]:
                    self._wait(e, t)
        for d in self.dsem.values():
            if d[1] < 40000:
                self.dfree.append(d)
        self.dsem = {}
        self.res = {}

    def mm(self, out, lhsT, rhs, start=True, stop=True):
        nc = self.nc
        return self.op("pe", lambda: nc.tensor.matmul(out.ap, lhsT=lhsT.ap, rhs=rhs.ap, start=start, stop=stop),
                       reads=[lhsT.k, rhs.k], writes=[out.k], pe_accum=True)

    def tr(self, out, in_, ident):
        nc = self.nc
        return self.op("pe", lambda: nc.tensor.transpose(out.ap, in_.ap, ident.ap),
                       reads=[in_.k, ident.k], writes=[out.k], pe_accum=True)

    def act(self, out, in_, func, bias=None, scale=None, accum=None):
        nc = self.nc
        kw = {}
        rd = [in_.k]
        wr = [out.k]
        if bias is not None:
            if isinstance(bias, V):
                kw["bias"] = bias.ap
                rd.append(bias.k)
            else:
                kw["bias"] = bias
        if scale is not None:
            if isinstance(scale, V):
                kw["scale"] = scale.ap
                rd.append(scale.k)
            else:
                kw["scale"] = scale
        if accum is not None:
            kw["accum_out"] = accum.ap
            wr.append(accum.k)
        return self.op("act", lambda: nc.scalar.activation(out=out.ap, in_=in_.ap, func=func, **kw),
                       reads=rd, writes=wr)

    def _e(self, e):
        return self.eng[e]

    def tt(self, e, out, in0, in1, op):
        return self.op(e, lambda: self._e(e).tensor_tensor(out=out.ap, in0=in0.ap, in1=in1.ap, op=op),
                       reads=[in0.k, in1.k], writes=[out.k])

    def ts(self, e, out, in0, s1, s2, op0, op1=None):
        rd = [in0.k]
        a1 = s1.ap if isinstance(s1, V) else s1
        a2 = s2.ap if isinstance(s2, V) else s2
        if isinstance(s1, V):
            rd.append(s1.k)
        if isinstance(s2, V):
            rd.append(s2.k)
        if op1 is None:
            return self.op(e, lambda: self._e(e).tensor_scalar(out=out.ap, in0=in0.ap, scalar1=a1, scalar2=None, op0=op0),
                           reads=rd, writes=[out.k])
        return self.op(e, lambda: self._e(e).tensor_scalar(out=out.ap, in0=in0.ap, scalar1=a1, scalar2=a2, op0=op0, op1=op1),
                       reads=rd, writes=[out.k])

    def stt(self, e, out, in0, scalar, in1, op0, op1):
        rd = [in0.k, in1.k]
        a = scalar.ap if isinstance(scalar, V) else scalar
        if isinstance(scalar, V):
            rd.append(scalar.k)
        return self.op(e, lambda: self._e(e).scalar_tensor_tensor(out=out.ap, in0=in0.ap, scalar=a, in1=in1.ap, op0=op0, op1=op1),
                       reads=rd, writes=[out.k])

    def cp(self, e, out, in_):
        if e == "act":
            return self.op(e, lambda: self.nc.scalar.copy(out=out.ap, in_=in_.ap), reads=[in_.k], writes=[out.k])
        return self.op(e, lambda: self._e(e).tensor_copy(out=out.ap, in_=in_.ap), reads=[in_.k], writes=[out.k])

    def memset(self, e, out, val):
        return self.op(e, lambda: self._e(e).memset(out.ap, val), reads=[], writes=[out.k])

    def red(self, e, out, in_, op, axis=AX.X):
        return self.op(e, lambda: self._e(e).tensor_reduce(out=out.ap, in_=in_.ap, axis=axis, op=op),
                       reads=[in_.k], writes=[out.k])

    def recip(self, e, out, in_):
        return self.op(e, lambda: self._e(e).reciprocal(out=out.ap, in_=in_.ap), reads=[in_.k], writes=[out.k])


class Ctx:
    pass


class Ring:
    def __init__(self, bufs):
        self.bufs = bufs
        self.i = 0

    def next(self):
        b = self.bufs[self.i % len(self.bufs)]
        self.i += 1
        return b


_UID = [0]


def drain(gens, n):
    for g in gens:
        for _ in range(n):
            try:
                next(g)
            except StopIteration:
                break


def drain_all(gens):
    for g in gens:
        for _ in g:
            pass


def run_scan(s, seq, prep, heavy, reset_state):
    t_next = {}
    drain_all([prep(*seq[0][:3], t_next)])
    epi = None
    for idx, (b, d, ci, first) in enumerate(seq):
        cur = t_next
        if first:
            reset_state()
        bgs = []
        if idx + 1 < len(seq):
            t_next = {}
            bgs.append(prep(*seq[idx + 1][:3], t_next))
        if epi is not None:
            bgs.append(epi)
        epi = heavy(b, d, ci, cur, bgs)
    drain_all([epi])


def sb(es, nc, name, shape, dt):
    _UID[0] += 1
    name = f"{name}_{_UID[0]}"
    t = es.enter_context(nc.sbuf_tensor(name, shape, dt))
    return Buf(t, name)


def sbring(es, nc, name, shape, dt, n):
    return Ring([sb(es, nc, f"{name}{i}", shape, dt) for i in range(n)])


def bc(v, shape):
    return V(v.ap.to_broadcast(shape), v.k)


def phase_norm(c, hin, gvec, uT):
    nc, s = c.nc, c.s
    with ExitStack() as es:
        gt = sb(es, nc, "n_g", [128, D], F32)
        xr = sbring(es, nc, "n_x", [128, D], F32, 2)
        sq = sb(es, nc, "n_sq", [128, D], F32)
        ur = sbring(es, nc, "n_u", [128, D], BF16, 2)
        st = sb(es, nc, "n_st", [128, NT, 4], F32)
        s.dma("sp", gt[:], V(gvec.partition_broadcast(128), "w_const"))
        for i in range(NT):
            xt = xr.next()
            ub = ur.next()
            stv = st.sub(str(i))
            s.dma("sp", xt[:], hin[i * 128:(i + 1) * 128, :])
            s.act(sq[:], xt[:], AF.Square, accum=stv[:, i, 0:1])
            s.ts("dve", stv[:, i, 1:2], stv[:, i, 0:1], 1.0 / D, EPS, ALU.mult, ALU.add)
            s.act(stv[:, i, 2:3], stv[:, i, 1:2], AF.Sqrt)
            s.recip("dve", stv[:, i, 3:4], stv[:, i, 2:3])
            s.stt("dve", ub[:], xt[:], stv[:, i, 3:4], gt[:], ALU.mult, ALU.mult)
            pt = c.ptr.next()
            for k in range(8):
                s.tr(pt[:, k, :], ub[:, k * 128:(k + 1) * 128], c.identb[:])
            s.cp("act", uT[:, :, i * 128:(i + 1) * 128], pt[:, 0:8, :])
    s.barrier()


def phase_project(c, uT, w_ap, segs):
    nc, s = c.nc, c.s
    with ExitStack() as es:
        wfr = sbring(es, nc, "p_wf", [128, 8, 512], F32, 2)
        wbr = sbring(es, nc, "p_wb", [128, 8, 512], BF16, 2)
        ofr = sbring(es, nc, "p_of", [128, 512], F32, 3)
        obr = sbring(es, nc, "p_ob", [128, 512], BF16, 3)
        wv = w_ap.rearrange("(ko p) n -> p ko n", p=128)
        ev = 0
        for seg in segs:
            (col0, ncols, mode, dst, dt, scale) = seg[:6]
            func = seg[6] if len(seg) > 6 else None
            for cb in range(0, ncols, 512):
                nb = min(512, ncols - cb)
                wf = wfr.next()
                wb = wbr.next()
                s.dma("sp", wf[:, :, 0:nb], V(wv[:, :, col0 + cb:col0 + cb + nb], "w_const"))
                s.cp("dve", wb.sub("a")[:, 0:4, 0:nb], wf[:, 0:4, 0:nb])
                s.cp("act", wb.sub("b")[:, 4:8, 0:nb], wf[:, 4:8, 0:nb])
                if mode == "tm":
                    for i in range(NT):
                        ps = c.pr.next()
                        for ko in range(8):
                            s.mm(ps[:, 0:nb], uT[:, ko, i * 128:(i + 1) * 128], wb.sub("a" if ko < 4 else "b")[:, ko, 0:nb],
                                 start=(ko == 0), stop=(ko == 7))
                        ot = (ofr if dt == F32 else obr).next()
                        if func is not None:
                            s.act(ot[:, 0:nb], ps[:, 0:nb], func, scale=scale)
                        elif ev % 2 == 0:
                            s.act(ot[:, 0:nb], ps[:, 0:nb], AF.Copy, scale=scale)
                        else:
                            s.ts("dve", ot[:, 0:nb], ps[:, 0:nb], scale, None, ALU.mult)
                        ev += 1
                        s.dma(STQ, dst[i * 128:(i + 1) * 128, cb:cb + nb], ot[:, 0:nb])
                else:
                    for fb in range(0, nb, 128):
                        fn = min(128, nb - fb)
                        for tb in range(T // 512):
                            ps = c.pr.next()
                            for ko in range(8):
                                s.mm(ps[0:fn, :], wb.sub("a" if ko < 4 else "b")[:, ko, fb:fb + fn], uT[:, ko, tb * 512:(tb + 1) * 512],
                                     start=(ko == 0), stop=(ko == 7))
                            ot = (ofr if dt == F32 else obr).next()
                            if ev % 2 == 0:
                                s.act(ot[0:fn, :], ps[0:fn, :], AF.Copy, scale=scale)
                            else:
                                s.ts("dve", ot[0:fn, :], ps[0:fn, :], scale, None, ALU.mult)
                            ev += 1
                            s.dma(STQ, dst[cb + fb:cb + fb + fn, tb * 512:(tb + 1) * 512], ot[0:fn, :])
    s.barrier()


class Finalizer:
    def __init__(self, c, es, w_out_ap, hin, hout):
        nc = c.nc
        self.c = c
        self.wo = sb(es, nc, "f_wo", [128, 16, D], BF16)
        self.yT = sbring(es, nc, "f_yT", [128, 16, 128], BF16, 2)
        self.hr = sbring(es, nc, "f_h", [128, D], F32, 2)
        self.hin, self.hout = hin, hout
        with ExitStack() as es2:
            stg = sbring(es2, nc, "f_stg", [128, 2, D], F32, 4)
            wv = w_out_ap.rearrange("(ko p) n -> p ko n", p=128)
            for k0 in range(0, 16, 2):
                st = stg.next()
                c.s.dma("sp" if (k0 // 2) % 2 == 0 else "pool", st[:], V(wv[:, k0:k0 + 2, :], "w_const"))
                c.s.cp("dve" if (k0 // 2) % 2 == 0 else "act", self.wo[:, k0:k0 + 2, :], st[:])
            c.s.barrier()

    def run(self, y, i):
        for _ in self.run_gen(y, i):
            pass

    def run_gen(self, y, i):
        c, s = self.c, self.c.s
        yT = self.yT.next()
        for half in range(2):
            pt = c.ptr.next()
            for k in range(8):
                kk = half * 8 + k
                s.tr(pt[:, k, :], V(y.t[:, kk * 128:(kk + 1) * 128], y.key), c.identb[:])
            s.cp("act" if half == 0 else "dve", yT[:, half * 8:half * 8 + 8, :], pt[:, 0:8, :])
            yield
        ht = self.hr.next()
        s.dma("sp", ht[:], self.hin[i * 128:(i + 1) * 128, :])
        for n in range(2):
            ps = c.pr.next()
            for kc in range(16):
                s.mm(ps[:, :], yT[:, kc, :], self.wo[:, kc, n * 512:(n + 1) * 512],
                     start=(kc == 0), stop=(kc == 15))
            s.tt("dve", ht[:, n * 512:(n + 1) * 512], ps[:, :], ht[:, n * 512:(n + 1) * 512], ALU.add)
            yield
        s.dma(STQ, self.hout[i * 128:(i + 1) * 128, :], ht[:])


def phase_final_norm(c, hin, gvec, out):
    nc, s = c.nc, c.s
    with ExitStack() as es:
        gt = sb(es, nc, "fn_g", [128, D], F32)
        xr = sbring(es, nc, "fn_x", [128, D], F32, 2)
        sq = sb(es, nc, "fn_sq", [128, D], F32)
        orr = sbring(es, nc, "fn_o", [128, D], F32, 2)
        st = sb(es, nc, "fn_st", [128, NT, 4], F32)
        s.dma("sp", gt[:], V(gvec.partition_broadcast(128), "w_const"))
        for i in range(NT):
            xt = xr.next()
            ot = orr.next()
            stv = st.sub(str(i))
            s.dma("sp", xt[:], hin[i * 128:(i + 1) * 128, :])
            s.act(sq[:], xt[:], AF.Square, accum=stv[:, i, 0:1])
            s.ts("dve", stv[:, i, 1:2], stv[:, i, 0:1], 1.0 / D, EPS, ALU.mult, ALU.add)
            s.act(stv[:, i, 2:3], stv[:, i, 1:2], AF.Sqrt)
            s.recip("dve", stv[:, i, 3:4], stv[:, i, 2:3])
            s.stt("dve", ot[:], xt[:], stv[:, i, 3:4], gt[:], ALU.mult, ALU.mult)
            s.dma(STQ, out[i * 128:(i + 1) * 128, :], ot[:])
    s.barrier()


def conv_fm(c, srcT, nch_tiles, K, cw_ap, cb_ap, silu, emit):
    nc, s = c.nc, c.s
    pad = (K - 1) // 2
    with ExitStack() as es:
        cw = sb(es, nc, "cv_w", [128, nch_tiles, K], F32)
        cbias = sb(es, nc, "cv_b", [128, nch_tiles], F32)
        xr = sbring(es, nc, "cv_x", [128, L + 2 * pad], BF16, 2)
        dg = sbring(es, nc, "cv_dg", [128, K, 128], BF16, 2)
        cvr = sbring(es, nc, "cv_o", [128, 512], BF16, 3)
        s.dma("sp", cw[:], V(cw_ap.rearrange("(ct p) k -> p ct k", p=128), "w_const"))
        s.dma("sp", cbias[:], V(cb_ap.rearrange("(ct p) -> p ct", p=128), "w_const"), allow_slow_non_contiguous=True)
        for xb in xr.bufs:
            s.memset("pool", xb[:, 0:pad], 0.0)
            s.memset("pool", xb[:, L + pad:L + 2 * pad], 0.0)
        for ct in range(nch_tiles):
            d = dg.next()
            for k in range(K):
                s.ts("dve", d[:, k, :], c.identf[:], cw[:, ct, k:k + 1], None, ALU.mult)
            for b in range(BL):
                xt = xr.next()
                s.dma("sp", V(xt.t[:, pad:L + pad], xt.key + ".d"), srcT[ct * 128:(ct + 1) * 128, b * L:(b + 1) * L])
                for tb in range(L // 512):
                    ps = c.pr.next()
                    for k in range(K):
                        s.mm(ps[:, :], d[:, k, :],
                             V(xt.t[:, tb * 512 + k:tb * 512 + k + 512], (xt.key, xt.key + ".d")),
                             start=(k == 0), stop=(k == K - 1))
                    cv = cvr.next()
                    s.act(cv[:, :], ps[:, :], AF.Silu if silu else AF.Identity, bias=cbias[:, ct:ct + 1])
                    emit(ct, b, tb, cv[:, :])


def conv_fm4(c, srcT, nct, K, cw_ap, cb_ap, silu, plan):
    nc, s = c.nc, c.s
    pad = (K - 1) // 2
    with ExitStack() as es:
        cw = sb(es, nc, "cv_w", [128, nct, K], F32)
        cbias = sb(es, nc, "cv_b", [128, nct], F32)
        xr = sbring(es, nc, "cv_x", [128, 4, L + 2 * pad], BF16, 2)
        dg = sbring(es, nc, "cv_dg", [128, 4, K, 128], BF16, 2)
        cvr = sbring(es, nc, "cv_o", [128, 512], BF16, 4)
        stg = sbring(es, nc, "cv_stg", [128, 4, 512], BF16, 2)
        s.dma("sp", cw[:], V(cw_ap.rearrange("(ct p) k -> p ct k", p=128), "w_const"))
        s.dma("sp", cbias[:], V(cb_ap.rearrange("(ct p) -> p ct", p=128), "w_const"), allow_slow_non_contiguous=True)
        for xb in xr.bufs:
            s.memset("pool", xb[:, :, 0:pad], 0.0)
            s.memset("pool", xb[:, :, L + pad:L + 2 * pad], 0.0)
        ncp = 0
        for cg in range(nct // 4):
            pl = plan(cg)
            tm, fm = pl.get("tm"), pl.get("fm")
            d = dg.next()
            for q in range(4):
                for k in range(K):
                    s.ts("dve", d[:, q, k, :], c.identf[:], cw[:, cg * 4 + q, k:k + 1], None, ALU.mult)
            for b in range(BL):
                xt = xr.next()
                s.dma("sp", V(xt.t[:, :, pad:L + pad], xt.key + ".d"),
                      V(srcT.t[cg * 512:(cg + 1) * 512, b * L:(b + 1) * L].rearrange("(q p) t -> p q t", p=128), srcT.key))
                for tb in range(L // 512):
                    col = b * L + tb * 512
                    st = stg.next() if tm else None
                    pt = None
                    for q in range(4):
                        ct = cg * 4 + q
                        ps = c.pr.next()
                        for k in range(K):
                            s.mm(ps[:, :], d[:, q, k, :],
                                 V(xt.t[:, q, tb * 512 + k:tb * 512 + k + 512], (xt.key, xt.key + ".d")),
                                 start=(k == 0), stop=(k == K - 1))
                        cv = cvr.next()
                        s.act(cv[:, :], ps[:, :], AF.Silu if silu else AF.Identity, bias=cbias[:, ct:ct + 1])
                        if fm:
                            dstT, row0 = fm
                            s.dma(STQ, dstT[row0 + q * 128:row0 + (q + 1) * 128, col:col + 512], cv[:, :])
                        if tm:
                            if q % 2 == 0:
                                pt = c.ptr.next()
                            for j in range(4):
                                s.tr(pt[:, (q % 2) * 4 + j, :], cv[:, j * 128:(j + 1) * 128], c.identb[:])
                            if q % 2 == 1:
                                for q2 in (q - 1, q):
                                    o = V(st.t[:, :, q2 * 128:(q2 + 1) * 128], st.key)
                                    i_ = V(pt.t[:, (q2 % 2) * 4:(q2 % 2) * 4 + 4, :], pt.key)
                                    mul = tm[2]
                                    if mul is not None:
                                        mv = V(mul.t[:, (cg * 4 + q2) * 128:(cg * 4 + q2 + 1) * 128].unsqueeze(1), mul.key)
                                        s.tt("dve", o, i_, bc(mv, [128, 4, 128]), ALU.mult)
                                    else:
                                        s.cp("dve" if ncp % 2 == 0 else "act", o, i_)
                                        ncp += 1
                    if tm:
                        dst, col0, _ = tm
                        s.dma(STQ, V(dst.t[col:col + 512, col0:col0 + 512].rearrange("(j p) c -> p j c", p=128), dst.key),
                              st[:, :, :])


def to_tm_store(c, cv, dst, row0, col0, stg_ring, mul=None):
    s = c.s
    pt = c.ptr.next()
    for j in range(4):
        s.tr(pt[:, j, :], V(cv.ap[:, j * 128:(j + 1) * 128], cv.k), c.identb[:])
    st = stg_ring.next()
    if mul is None:
        s.cp("dve", st[:, 0:4, :], pt[:, 0:4, :])
    else:
        s.tt("dve", st[:, 0:4, :], pt[:, 0:4, :], bc(V(mul.ap.unsqueeze(1), mul.k), [128, 4, 128]), ALU.mult)
    s.dma(STQ, V(dst.t[row0:row0 + 512, col0:col0 + 128].rearrange("(j p) c -> p j c", p=128), dst.key),
          st[:, 0:4, :])


def layer_ssd(c, W, hin, hout):
    nc, s = c.nc, c.s
    H, P, G, N = 32, 64, 8, 128
    dr = c.dram
    z_tm = dr("ssd_z", [T, DI], BF16)
    xbcT = dr("ssd_xbcT", [4096, T], BF16)
    dt_tm = dr("ssd_dt", [T, 64], F32)
    x_tm = dr("ssd_x", [T, DI], BF16)
    B_tm = dr("ssd_B", [T, 1024], BF16)
    BT = dr("ssd_BT", [1024, T], BF16)
    CT = dr("ssd_CT", [1024, T], BF16)
    yf = dr("ssd_yf", [T, DI], F32)

    with ExitStack() as es:
        uT = sb(es, nc, "uT", [128, 8, T], BF16)
        phase_norm(c, hin, W["ssd_norm"], uT)
        phase_project(c, uT, W["ssd_w_in"], [
            (0, DI, "tm", z_tm, BF16, 1.0, AF.Silu),
            (DI, 4096, "fm", xbcT, BF16, 1.0),
            (DI + 4096, 64, "tm", dt_tm, F32, 1.0),
        ])

    if DEBUG_STOP == "proj":
        return
    def plan(cg):
        if cg < 4:
            return dict(tm=(x_tm, cg * 512, None))
        if cg < 6:
            return dict(tm=(B_tm, (cg - 4) * 512, None), fm=(BT, (cg - 4) * 512))
        return dict(fm=(CT, (cg - 6) * 512))

    conv_fm4(c, xbcT, 32, 5, W["ssd_conv_wT"], W["ssd_conv_b"], True, plan)
    s.barrier()

    if DEBUG_STOP == "conv":
        return
    with ExitStack() as es:
        c.pr = Ring([c.pbanks[5], c.pbanks[4]])
        fin = Finalizer(c, es, W["ssd_w_out"], hin, hout)
        dtb = sb(es, nc, "ss_dtb", [128, 64], F32)
        aneg = sb(es, nc, "ss_a", [128, 64], F32)
        dsk = sb(es, nc, "ss_dsk", [128, 32], F32)
        gn = sb(es, nc, "ss_gn", [128, DI], F32)
        s.dma("sp", dtb[:], V(W["ssd_dt_bias"].rearrange("d h -> (d h)").partition_broadcast(128), "w_const"))
        s.dma("sp", aneg[:], V(W["ssd_a_log"].rearrange("d h -> (d h)").partition_broadcast(128), "w_const"))
        s.dma("sp", dsk[:], V(W["ssd_d"].partition_broadcast(128), "w_const"))
        s.dma("sp", gn[:], V(W["ssd_gnorm"].partition_broadcast(128), "w_const"))
        s.act(aneg[:], aneg[:], AF.Exp)
        s.ts("dve", aneg[:], aneg[:], -1.0, None, ALU.mult)
        xr = sbring(es, nc, "ss_x", [128, DI], BF16, 3)
        Br = sbring(es, nc, "ss_B", [128, 1024], BF16, 2)
        BTr = sbring(es, nc, "ss_BT", [128, G, 128], BF16, 2)
        CTr = sbring(es, nc, "ss_CT", [128, G, 128], BF16, 2)
        dtr = sbring(es, nc, "ss_dt", [128, 64], F32, 2)
        sm = sbring(es, nc, "ss_sm", [128, 8, 32], F32, 2)
        labcr = sbring(es, nc, "ss_labc", [128, H, 128], F32, 2)
        xdt = sbring(es, nc, "ss_xdt", [128, DI], BF16, 2)
        xw = sbring(es, nc, "ss_xw", [128, DI], BF16, 2)
        negm4 = sb(es, nc, "ss_negm4", [128, 2, 4, 128], F32)
        for d in range(2):
            s.cp("dve", negm4[:, d, :, :], bc(V(c.negm.t[:, d, :].unsqueeze(1), c.negm.key), [128, 4, 128]))
        Er = sbring(es, nc, "ss_E", [128, 512], F32, 2)
        PTr = sbring(es, nc, "ss_PT", [128, 4, 128], BF16, 2)
        t1r = sbring(es, nc, "ss_t1", [128, 256], F32, 2)
        yacc = sbring(es, nc, "ss_yacc", [128, DI], F32, 2)
        st32 = sb(es, nc, "ss_st32", [128, G, 256], F32)
        stbf = sb(es, nc, "ss_stbf", [128, G, 256], BF16)
        zr = sbring(es, nc, "ss_z", [128, DI], BF16, 1)
        yfr = sbring(es, nc, "ss_yf", [128, DI], F32, 1)
        tmp = sb(es, nc, "ss_tmp", [128, DI], F32)
        gst = sbring(es, nc, "ss_gst", [128, 4, 8], F32, 2)
        yb = sbring(es, nc, "ss_yb", [128, DI], BF16, 1)
        allg = [str(g) for g in range(G)]

        def r3(buf, q):
            return V(buf.t[:].rearrange("p (h q) -> p h q", q=q), buf.key)

        pab = c.pbanks[0:2]
        pyb = c.pbanks[2:4]
        pstb = c.pbanks[4]
        pmisc = c.pbanks[5]
        c.pr = Ring([c.pbanks[5], c.pbanks[4]])
        cbs = sbring(es, nc, "ss_cbs", [128, G, 128], F32, 2)

        def prep(b, d, ci, tout):
            i = b * NQ + ci
            r0 = i * 128
            tri = c.tri[:, d, :]
            xt, Bt, BTt, CTt, dtt = xr.next(), Br.next(), BTr.next(), CTr.next(), dtr.next()
            s.dma("sp", xt[:], x_tm[r0:r0 + 128, :])
            s.dma("sp", Bt[:], B_tm[r0:r0 + 128, :])
            s.dma("sp", BTt[:], V(BT.t[:, r0:r0 + 128].rearrange("(g n) t -> n g t", n=128), BT.key))
            s.dma("sp", CTt[:], V(CT.t[:, r0:r0 + 128].rearrange("(g n) t -> n g t", n=128), CT.key))
            s.dma("sp", dtt[:], dt_tm[r0:r0 + 128, :])
            m = sm.next()
            s.tt("dve", m[:, 0:2, :], V(dtt.t[:].rearrange("p (a h) -> p a h", a=2), dtt.key),
                 V(dtb.t[:].rearrange("p (a h) -> p a h", a=2), dtb.key), ALU.add)
            s.act(m[:, 0:2, :], m[:, 0:2, :], AF.Exp)
            s.act(m[:, 0:2, :], m[:, 0:2, :], AF.Ln, bias=c.one[:, 0:1])
            dtd = m[:, d, :]
            s.tt("dve", m[:, 2, :], dtd, aneg[:, d * 32:(d + 1) * 32], ALU.mult)
            la = m[:, 2, :]
            yield
            pc = pmisc
            s.mm(pc[:, 0:32], tri, la)
            s.mm(pc[:, 32:64], c.onesf[:], la)
            s.act(m[:, 3, :], pc[:, 0:32], AF.Exp)
            s.cp("dve", m[:, 5, :], pc[:, 0:32])
            s.tt("dve", m[:, 4, :], pc[:, 32:64], m[:, 5, :], ALU.subtract)
            s.act(m[:, 4, :], m[:, 4, :], AF.Exp)
            s.act(m[:, 6, :], pc[:, 32:64], AF.Exp)
            s.tt("dve", m[:, 7, :], m[:, 4, :], dtd, ALU.mult)
            yield
            xd, xwt = xdt.next(), xw.next()
            x3 = r3(xt, P)
            s.tt("dve", r3(xd, P), x3, bc(V(dtd.ap.unsqueeze(2), dtd.k), [128, H, P]), ALU.mult)
            yield
            s.tt("dve", r3(xwt, P), x3, bc(V(m.t[:, 7, :].unsqueeze(2), m.key), [128, H, P]), ALU.mult)
            yield
            labc = labcr.next()
            s.cp("act", labc[:], bc(V(la.ap.unsqueeze(2), la.k), [128, H, 128]))
            s.ts("dve", m[:, 5, :], m[:, 5, :], -1.0, None, ALU.mult)
            yield
            cb = cbs.next()
            for half in range(2):
                for gq in range(4):
                    g = half * 4 + gq
                    s.mm(V(pmisc.t[:, gq * 128:(gq + 1) * 128], pmisc.key), BTt[:, g, :], CTt[:, g, :])
                s.cp("act", V(cb.t[:, half * 4:half * 4 + 4, :].rearrange("p g l -> p (g l)"), cb.key), pmisc[:, :])
                yield
            tout.update(dict(i=i, r0=r0, xt=xt, Bt=Bt, CTt=CTt, m=m, xd=xd, xwt=xwt, labc=labc, cb=cb, x3=x3))

        def heavy(b, d, ci, t, bgs):
            i, r0, m = t["i"], t["r0"], t["m"]
            tri = c.tri[:, d, :]
            xd, xwt, labc, cb, CTt, Bt, x3 = t["xd"], t["xwt"], t["labc"], t["cb"], t["CTt"], t["Bt"], t["x3"]
            ya = yacc.next()
            Es, PTs = {}, {}

            def stage_a(g):
                pa = pab[g % 2]
                s.mm(pa[:, :], c.identf[:], V(negm4.t[:, d, :, :].rearrange("p a l -> p (a l)"), negm4.key),
                     start=True, stop=False)
                for hh in range(4):
                    h = g * 4 + hh
                    s.mm(V(pa.t[:, hh * 128:(hh + 1) * 128], pa.key), labc[:, h, :], tri, start=False, stop=(hh == 3))
                E = Er.next()
                for hh in range(4):
                    h = g * 4 + hh
                    s.act(E[:, hh * 128:(hh + 1) * 128], V(pa.t[:, hh * 128:(hh + 1) * 128], pa.key), AF.Exp,
                          bias=m[:, 5, h:h + 1])
                Es[g] = E

            def stage_b(g):
                E = Es.pop(g)
                PT = PTr.next()
                s.tt("dve", PT[:], V(E.t[:].rearrange("p (a l) -> p a l", a=4), E.key),
                     bc(V(cb.t[:, g, :].unsqueeze(1), cb.key), [128, 4, 128]), ALU.mult)
                py = pyb[g % 2]
                for hh in range(4):
                    h = g * 4 + hh
                    s.mm(V(py.t[:, hh * 64:(hh + 1) * 64], py.key), PT[:, hh, :], xd[:, h * 64:(h + 1) * 64])
                s.mm(V(py.t[:, 256:512], py.key), CTt[:, g, :], stbf.sub(str(g))[:, g, :])
                t1 = t1r.next()
                s.tt("dve", V(t1.t[:].rearrange("p (a q) -> p a q", a=4), t1.key),
                     V(py.t[:, 256:512].rearrange("p (a q) -> p a q", a=4), py.key),
                     bc(V(m.t[:, 3, g * 4:(g + 1) * 4].unsqueeze(2), m.key), [128, 4, P]), ALU.mult)
                s.tt("dve", ya[:, g * 256:(g + 1) * 256], py[:, 0:256], t1[:], ALU.add)
                s.mm(pstb[:, 0:256], Bt[:, g * 128:(g + 1) * 128], xwt[:, g * 256:(g + 1) * 256])
                sg = st32.sub(str(g))
                sg3 = V(sg.t[:, g, :].rearrange("p (a q) -> p a q", a=4), sg.key)
                s.tt("dve", sg3, sg3,
                     bc(V(m.t[:, 6, g * 4:(g + 1) * 4].unsqueeze(2), m.key), [128, 4, P]), ALU.mult)
                s.tt("dve", sg[:, g, :], sg[:, g, :], pstb[:, 0:256], ALU.add)
                s.cp("act", stbf.sub(str(g))[:, g, :], sg[:, g, :])

            for gg in range(G + 1):
                if gg < G:
                    stage_a(gg)
                if gg >= 1:
                    stage_b(gg - 1)
                drain(bgs, 2)
            drain_all(bgs)

            def epilogue():
                if d == 0:
                    s.dma(STQ, yf[r0:r0 + 128, :], ya[:])
                    return
                yft, zt = yfr.next(), zr.next()
                s.dma("sp", yft[:], yf[r0:r0 + 128, :])
                s.dma("sp", zt[:], z_tm[r0:r0 + 128, :])
                s.tt("dve", yft[:], yft[:], ya[:], ALU.add)
                yield
                s.tt("dve", r3(tmp, P), x3, bc(V(dsk.t[:].unsqueeze(2), dsk.key), [128, H, P]), ALU.mult)
                yield
                s.tt("dve", yft[:], yft[:], tmp[:], ALU.add)
                yield
                s.tt("dve", yft[:], yft[:], zt[:], ALU.mult)
                yield
                gs = gst.next()
                s.act(tmp[:], yft[:], AF.Square)
                s.red("dve", gs[:, 0, :], r3(tmp, 256), ALU.add)
                s.ts("dve", gs[:, 1, :], gs[:, 0, :], 1.0 / 256, EPS, ALU.mult, ALU.add)
                s.act(gs[:, 2, :], gs[:, 1, :], AF.Ln)
                s.act(gs[:, 3, :], gs[:, 2, :], AF.Exp, scale=-0.5)
                yield
                s.tt("dve", r3(yft, 256), r3(yft, 256),
                     bc(V(gs.t[:, 3, :].unsqueeze(2), gs.key), [128, 8, 256]), ALU.mult)
                yield
                ybt = yb.next()
                s.tt("dve", ybt[:], yft[:], gn[:], ALU.mult)
                yield
                yield from fin.run_gen(ybt, i)

            return epilogue()

        seq = []
        for b in range(BL):
            for d in range(2):
                order = list(range(NQ)) if d == 0 else list(range(NQ - 1, -1, -1))
                for k, ci in enumerate(order):
                    seq.append((b, d, ci, k == 0))

        def reset_state():
            s.memset("dve", st32.subs(allg)[:], 0.0)
            s.memset("pool", stbf.subs(allg)[:], 0.0)

        run_scan(s, seq, prep, heavy, reset_state)
    s.barrier()
    c.pr = Ring(c.pbanks)


def layer_gla(c, W, hin, hout):
    nc, s = c.nc, c.s
    H, DK, DV = 4, 128, 512
    dr = c.dram
    q_tm = dr("gla_q", [T, 512], BF16)
    k_tm = dr("gla_k", [T, 512], BF16)
    v_tm = dr("gla_v", [T, DI], BF16)
    z_tm = dr("gla_z", [T, DI], BF16)
    glT = dr("gla_glT", [32, T], F32)
    of = dr("gla_of", [T, DI], F32)

    with ExitStack() as es:
        uT = sb(es, nc, "uT", [128, 8, T], BF16)
        phase_norm(c, hin, W["gla_norm"], uT)
        phase_project(c, uT, W["gla_w_in"], [
            (0, 512, "tm", q_tm, BF16, DK ** -0.5),
            (512, 512, "tm", k_tm, BF16, 1.0),
            (1024, DI, "tm", v_tm, BF16, 1.0),
            (3072, DI, "tm", z_tm, BF16, 1.0, AF.Silu),
            (5120, 32, "fm", glT, F32, 1.0),
        ])

    with ExitStack() as es:
        fin = Finalizer(c, es, W["gla_w_out"], hin, hout)
        wg = sb(es, nc, "g_wg", [16, 2, 512], F32)
        bg = sb(es, nc, "g_bg", [128, 2, 512], F32)
        on = sb(es, nc, "g_on", [128, 512], F32)
        s.dma("sp", wg[:], V(W["gla_w_gate"].rearrange("d r k -> r d k"), "w_const"))
        s.dma("sp", V(bg.t[:].rearrange("p d k -> p (d k)"), bg.key),
              V(W["gla_b_gate"].rearrange("d k -> (d k)").partition_broadcast(128), "w_const"))
        s.dma("sp", on[:], V(W["gla_onorm"].partition_broadcast(128), "w_const"))
        qr = sbring(es, nc, "g_q", [128, 512], BF16, 2)
        kr = sbring(es, nc, "g_k", [128, 512], BF16, 2)
        vr = sbring(es, nc, "g_v", [128, DI], BF16, 2)
        glr = sbring(es, nc, "g_gl", [16, 128], F32, 2)
        Lpr = sbring(es, nc, "g_Lp", [128, 512], F32, 2)
        Eqr = sbring(es, nc, "g_Eq", [128, 512], F32, 2)
        Ekr = sbring(es, nc, "g_Ek", [128, 512], F32, 2)
        Ee = sbring(es, nc, "g_Ee", [128, 4], F32, 2)
        qsr = sbring(es, nc, "g_qs", [128, 512], BF16, 2)
        ksr = sbring(es, nc, "g_ks", [128, 512], BF16, 2)
        qkTr = sbring(es, nc, "g_qkT", [128, 8, 128], BF16, 2)
        PTr = sbring(es, nc, "g_PT", [128, 4, 128], BF16, 2)
        oaccr = sbring(es, nc, "g_oacc", [128, DI], F32, 2)
        S32 = sb(es, nc, "g_S32", [128, H, DV], F32)
        Sbf = sb(es, nc, "g_Sbf", [128, H, DV], BF16)
        oft = sb(es, nc, "g_of", [128, DI], F32)
        zt = sb(es, nc, "g_z", [128, DI], BF16)
        tmp = sb(es, nc, "g_tmp", [128, DI], F32)
        gst = sbring(es, nc, "g_st", [128, 4, 4], F32, 2)
        yb = sb(es, nc, "g_yb", [128, DI], BF16)
        allh = [str(h) for h in range(H)]

        def r3(buf, q):
            return V(buf.t[:].rearrange("p (h q) -> p h q", q=q), buf.key)

        def prep(b, d, ci, tout):
            tri = c.tri[:, d, :]
            i = b * NQ + ci
            r0 = i * 128
            qt, kt, vt, gt = qr.next(), kr.next(), vr.next(), glr.next()
            Lp, Eq, Ek, qs, ks = Lpr.next(), Eqr.next(), Ekr.next(), qsr.next(), ksr.next()
            qkT, PT, oacc = qkTr.next(), PTr.next(), oaccr.next()
            s.dma("sp", qt[:], q_tm[r0:r0 + 128, :])
            s.dma("sp", kt[:], k_tm[r0:r0 + 128, :])
            s.dma("sp", vt[:], v_tm[r0:r0 + 128, :])
            s.dma("sp", gt[:], glT[d * 16:(d + 1) * 16, r0:r0 + 128])
            pg = c.pr.next()
            s.mm(pg[:, :], gt[:, :], wg[:, d, :])
            s.tt("dve", Lp[:], pg[:, :], bg[:, d, :], ALU.add)
            s.act(Lp[:], Lp[:], AF.Exp, scale=-1.0)
            s.act(Lp[:], Lp[:], AF.Ln, bias=c.one[:, 0:1])
            yield
            pcum = c.pr.next()
            s.mm(pcum[:, :], tri, Lp[:])
            s.act(Eq[:], pcum[:, :], AF.Exp, scale=-1.0 / 16)
            s.act(Ek[:], pcum[:, :], AF.Exp, scale=1.0 / 16)
            yield
            ptot = c.pr.next()
            for h in range(H):
                s.mm(ptot[:, h:h + 1], Lp[:, h * 128:(h + 1) * 128], c.onesf[:, 0:1])
            ee = Ee.next()
            s.act(ee[:], ptot[:, 0:4], AF.Exp, scale=-1.0 / 16)
            s.tt("dve", qs[:], qt[:], Eq[:], ALU.mult)
            s.tt("dve", ks[:], kt[:], Ek[:], ALU.mult)
            yield
            pt = c.ptr.next()
            for h in range(H):
                s.tr(pt[:, h, :], qs[:, h * 128:(h + 1) * 128], c.identb[:])
                s.tr(pt[:, 4 + h, :], ks[:, h * 128:(h + 1) * 128], c.identb[:])
            s.cp("act", qkT[:], pt[:, :, :])
            yield
            pS = c.pr.next()
            for h in range(H):
                s.mm(pS[:, h * 128:(h + 1) * 128], qkT[:, 4 + h, :], qkT[:, h, :])
            s.tt("dve", PT[:], V(pS.t[:, :].rearrange("p (a l) -> p a l", a=4), pS.key),
                 bc(V(tri.ap.unsqueeze(1), tri.k), [128, 4, 128]), ALU.mult)
            tout.update(dict(i=i, r0=r0, vt=vt, qkT=qkT, PT=PT, ks=ks, ee=ee, oacc=oacc))

        def heavy(b, d, ci, t, bgs):
            i, r0, vt, qkT, PT, ks, ee, oacc = (t[k] for k in ("i", "r0", "vt", "qkT", "PT", "ks", "ee", "oacc"))
            for h in range(H):
                po = c.pr.next()
                s.mm(po[:, :], PT[:, h, :], vt[:, h * DV:(h + 1) * DV], start=True, stop=False)
                s.mm(po[:, :], qkT[:, h, :], Sbf.sub(str(h))[:, h, :], start=False, stop=True)
                s.cp("act", oacc[:, h * DV:(h + 1) * DV], po[:, :])
                pu = c.pr.next()
                s.mm(pu[:, :], ks[:, h * 128:(h + 1) * 128], vt[:, h * DV:(h + 1) * DV])
                sh = S32.sub(str(h))
                s.tt("dve", sh[:, h, :], sh[:, h, :], pu[:, :], ALU.add)
                s.act(sh[:, h, :], sh[:, h, :], AF.Copy, scale=ee[:, h:h + 1])
                s.cp("dve", Sbf.sub(str(h))[:, h, :], sh[:, h, :])
                drain(bgs, 3)
            drain_all(bgs)

            def epilogue():
                if d == 0:
                    s.dma(STQ, of[r0:r0 + 128, :], oacc[:])
                    return
                s.dma("sp", oft[:], of[r0:r0 + 128, :])
                s.dma("sp", zt[:], z_tm[r0:r0 + 128, :])
                s.tt("dve", oft[:], oft[:], oacc[:], ALU.add)
                yield
                gs = gst.next()
                s.act(tmp[:], oft[:], AF.Square)
                s.red("dve", gs[:, 0, :], r3(tmp, DV), ALU.add)
                s.ts("dve", gs[:, 1, :], gs[:, 0, :], 1.0 / DV, EPS, ALU.mult, ALU.add)
                s.act(gs[:, 2, :], gs[:, 1, :], AF.Ln)
                s.act(gs[:, 3, :], gs[:, 2, :], AF.Exp, scale=-0.5)
                yield
                for h in range(H):
                    s.stt("dve", oft[:, h * DV:(h + 1) * DV], oft[:, h * DV:(h + 1) * DV], gs[:, 3, h:h + 1], on[:],
                          ALU.mult, ALU.mult)
                    if h % 2 == 1:
                        yield
                s.tt("dve", yb[:], oft[:], zt[:], ALU.mult)
                yield
                yield from fin.run_gen(yb, i)

            return epilogue()


        seq = []
        for b in range(BL):
            for d in range(2):
                order = list(range(NQ)) if d == 0 else list(range(NQ - 1, -1, -1))
                for k, ci in enumerate(order):
                    seq.append((b, d, ci, k == 0))

        def reset_state():
            s.memset("dve", S32.subs(allh)[:], 0.0)
            s.memset("pool", Sbf.subs(allh)[:], 0.0)

        run_scan(s, seq, prep, heavy, reset_state)
    s.barrier()


def layer_mlstm(c, W, hin, hout):
    nc, s = c.nc, c.s
    H, DK, DV = 4, 256, 512
    dr = c.dram
    xmT = dr("ml_xmT", [DI, T], BF16)
    z_tm = dr("ml_z", [T, DI], BF16)
    og_tm = dr("ml_og", [T, DI], BF16)
    gt_tm = dr("ml_gates", [T, 16], F32)
    chT = dr("ml_chT", [DI, T], BF16)
    ch_tm = dr("ml_ch", [T, DI], BF16)
    qk_tm = dr("ml_qk", [T, H * 512], BF16)
    v_tm = dr("ml_v", [T, DI], BF16)
    hf = dr("ml_hf", [T, DI], F32)

    with ExitStack() as es:
        uT = sb(es, nc, "uT", [128, 8, T], BF16)
        phase_norm(c, hin, W["ml_norm"], uT)
        phase_project(c, uT, W["ml_w_in"], [
            (0, DI, "fm", xmT, BF16, 1.0),
            (DI, DI, "tm", z_tm, BF16, 1.0, AF.Silu),
            (2 * DI, DI, "tm", og_tm, BF16, 1.0, AF.Sigmoid),
            (3 * DI, 16, "tm", gt_tm, F32, 1.0),
        ])

    with ExitStack() as es:
        skc = sb(es, nc, "mc_skip", [128, DI], F32)
        s.dma("sp", skc[:], V(W["ml_skip"].rearrange("h d -> (h d)").partition_broadcast(128), "w_const"))
        conv_fm4(c, xmT, 16, 5, W["ml_conv_wT"], W["ml_conv_b"], True,
                 lambda cg: dict(tm=(ch_tm, cg * 512, skc), fm=(chT, cg * 512)))
    s.barrier()

    with ExitStack() as es:
        wq = sb(es, nc, "mq_wq", [128, H, 4, 256], BF16)
        wk = sb(es, nc, "mq_wk", [128, H, 4, 256], BF16)
        wv = sb(es, nc, "mq_wv", [128, H, 4, 512], BF16)
        with ExitStack() as es2:
            stf = sbring(es2, nc, "mq_stg", [128, 4096], F32, 2)
            st = stf.next()
            sv = V(st.t[:].rearrange("p (h k n) -> p h k n", h=4, k=4), st.key)
            s.dma("sp", sv, V(W["ml_w_q"].rearrange("h (k p) n -> p h k n", p=128), "w_const"))
            s.cp("dve", wq[:], sv)
            st = stf.next()
            sv = V(st.t[:].rearrange("p (h k n) -> p h k n", h=4, k=4), st.key)
            s.dma("sp", sv, V(W["ml_w_k"].rearrange("h (k p) n -> p h k n", p=128), "w_const"))
            s.cp("act", wk[:], sv)
            for hh in range(2):
                st = stf.next()
                sv = V(st.t[:].rearrange("p (h k n) -> p h k n", h=2, k=4), st.key)
                s.dma("sp", sv, V(W["ml_w_v"][hh * 2:hh * 2 + 2].rearrange("h (k p) n -> p h k n", p=128), "w_const"))
                s.cp("dve" if hh == 0 else "act", wv[:, hh * 2:hh * 2 + 2, :, :], sv)
            s.barrier()
        chr_ = sbring(es, nc, "mq_ch", [128, 16, 512], BF16, 2)
        xmr = sbring(es, nc, "mq_xm", [128, 16, 512], BF16, 2)
        qko = sbring(es, nc, "mq_qko", [128, H * 512], BF16, 2)
        vo = sbring(es, nc, "mq_vo", [128, DI], BF16, 2)
        for i in range(NT):
            if i % 4 == 0:
                cht, xmt = chr_.next(), xmr.next()
                for hf_ in range(2):
                    s.dma("sp", cht[:, hf_ * 8:(hf_ + 1) * 8, :],
                          V(chT.t[hf_ * 1024:(hf_ + 1) * 1024, i * 128:i * 128 + 512].rearrange("(j p) t -> p j t", p=128), chT.key))
                    s.dma("sp", xmt[:, hf_ * 8:(hf_ + 1) * 8, :],
                          V(xmT.t[hf_ * 1024:(hf_ + 1) * 1024, i * 128:i * 128 + 512].rearrange("(j p) t -> p j t", p=128), xmT.key))
            tsl = slice((i % 4) * 128, (i % 4 + 1) * 128)
            qo, vot = qko.next(), vo.next()
            for h in range(H):
                pq = c.pr.next()
                for kc in range(4):
                    s.mm(pq[:, 0:256], cht[:, h * 4 + kc, tsl], wq[:, h, kc, :], start=(kc == 0), stop=(kc == 3))
                for kc in range(4):
                    s.mm(pq[:, 256:512], cht[:, h * 4 + kc, tsl], wk[:, h, kc, :], start=(kc == 0), stop=(kc == 3))
                s.cp("act", qo[:, h * 512:(h + 1) * 512], pq[:, :])
                pv = c.pr.next()
                for kc in range(4):
                    s.mm(pv[:, :], xmt[:, h * 4 + kc, tsl], wv[:, h, kc, :], start=(kc == 0), stop=(kc == 3))
                s.cp("dve", vot[:, h * 512:(h + 1) * 512], pv[:, :])
            s.dma(STQ, qk_tm[i * 128:(i + 1) * 128, :], qo[:])
            s.dma(STQ, v_tm[i * 128:(i + 1) * 128, :], vot[:])
    s.barrier()

    with ExitStack() as es:
        c.pr = Ring(c.pbanks[0:5])
        psm = c.pbanks[5]
        fin = Finalizer(c, es, W["ml_w_out"], hin, hout)
        gb = sb(es, nc, "m_gb", [128, 16], F32)
        on = sb(es, nc, "m_on", [128, 512], F32)
        skp = sb(es, nc, "m_skip", [128, DI], F32)
        s.dma("sp", gb[:], V(W["ml_gate_b"].rearrange("a b c -> (a b c)").partition_broadcast(128), "w_const"))
        s.dma("sp", on[:], V(W["ml_onorm"].partition_broadcast(128), "w_const"))
        s.dma("sp", skp[:], V(W["ml_skip"].rearrange("h d -> (h d)").partition_broadcast(128), "w_const"))
        onesb = sb(es, nc, "m_onesb", [128, 2], BF16)
        s.memset("dve", onesb[:], 1.0)
        qkr = sbring(es, nc, "m_qk", [128, H, 512], BF16, 2)
        vr = sbring(es, nc, "m_v", [128, DI], BF16, 2)
        gr = sbring(es, nc, "m_g", [128, 16], F32, 2)
        sm = sbring(es, nc, "m_sm", [128, 8, 4], F32, 2)
        qsr = sbring(es, nc, "m_qs", [128, H, 256], BF16, 2)
        ksr = sbring(es, nc, "m_ks", [128, H, 256], BF16, 2)
        qTr = sbring(es, nc, "m_qT", [128, 8, 128], BF16, 2)
        kTr = sbring(es, nc, "m_kT", [128, 8, 128], BF16, 2)
        PTr = sbring(es, nc, "m_PT", [128, 4, 128], BF16, 2)
        haccr = sbring(es, nc, "m_hacc", [128, DI], F32, 2)
        C32 = sb(es, nc, "m_C32", [128, 8, DV], F32)
        Cbf = sb(es, nc, "m_Cbf", [128, 8, DV], BF16)
        n32 = sb(es, nc, "m_n32", [128, 8], F32)
        nbf = sb(es, nc, "m_nbf", [128, 8], BF16)
        hft = sb(es, nc, "m_hf", [128, DI], F32)
        ogt = sb(es, nc, "m_og", [128, DI], BF16)
        zt = sb(es, nc, "m_z", [128, DI], BF16)
        cht = sb(es, nc, "m_ch", [128, DI], BF16)
        tmp = sb(es, nc, "m_tmp", [128, DI], F32)
        gst = sbring(es, nc, "m_st", [128, 4, 4], F32, 2)
        yb = sb(es, nc, "m_yb", [128, DI], BF16)
        allj = [str(j) for j in range(8)]

        def r3(buf, q):
            return V(buf.t[:].rearrange("p (h q) -> p h q", q=q), buf.key)

        def prep(b, d, ci, tout):
            tri = c.tri[:, d, :]
            i = b * NQ + ci
            r0 = i * 128
            qkt, vt, gt = qkr.next(), vr.next(), gr.next()
            qs, ks, qT, kT, PT, hacc = qsr.next(), ksr.next(), qTr.next(), kTr.next(), PTr.next(), haccr.next()
            s.dma("sp", V(qkt.t[:].rearrange("p h n -> p (h n)"), qkt.key), qk_tm[r0:r0 + 128, :])
            s.dma("sp", vt[:], v_tm[r0:r0 + 128, :])
            s.dma("sp", gt[:], gt_tm[r0:r0 + 128, :])
            m = sm.next()
            s.tt("dve", gt[:], gt[:], gb[:], ALU.add)
            ig = gt[:, d * 8:d * 8 + 4]
            fr = gt[:, d * 8 + 4:d * 8 + 8]
            s.act(m[:, 0, :], fr, AF.Exp, scale=-1.0)
            s.act(m[:, 0, :], m[:, 0, :], AF.Ln, bias=c.one[:, 0:1])
            s.mm(psm[:, 0:4], tri, m[:, 0, :])
            s.mm(psm[:, 4:8], c.onesf[:], m[:, 0, :])
            s.act(m[:, 1, :], psm[:, 0:4], AF.Exp, scale=-1.0)
            s.tt("dve", m[:, 2, :], psm[:, 0:4], ig, ALU.add)
            s.act(m[:, 2, :], m[:, 2, :], AF.Exp, bias=c.one[:, 3:4])
            s.act(m[:, 3, :], psm[:, 4:8], AF.Exp, scale=-1.0)
            yield
            s.tt("dve", qs[:], V(qkt.t[:, :, 0:256], qkt.key),
                 bc(V(m.t[:, 1, :].unsqueeze(2), m.key), [128, H, 256]), ALU.mult)
            s.tt("dve", ks[:], V(qkt.t[:, :, 256:512], qkt.key),
                 bc(V(m.t[:, 2, :].unsqueeze(2), m.key), [128, H, 256]), ALU.mult)
            yield
            pt = c.ptr.next()
            for j in range(8):
                s.tr(pt[:, j, :], qs[:, j // 2, (j % 2) * 128:(j % 2 + 1) * 128], c.identb[:])
            s.cp("act", qT[:], pt[:, :, :])
            yield
            pt = c.ptr.next()
            for j in range(8):
                s.tr(pt[:, j, :], ks[:, j // 2, (j % 2) * 128:(j % 2 + 1) * 128], c.identb[:])
            s.cp("dve", kT[:], pt[:, :, :])
            yield
            pS = c.pr.next()
            for h in range(H):
                for kc in range(2):
                    s.mm(pS[:, h * 128:(h + 1) * 128], kT[:, h * 2 + kc, :], qT[:, h * 2 + kc, :],
                         start=(kc == 0), stop=(kc == 1))
            s.tt("dve", PT[:], V(pS.t[:, :].rearrange("p (a l) -> p a l", a=4), pS.key),
                 bc(V(tri.ap.unsqueeze(1), tri.k), [128, 4, 128]), ALU.mult)
            tout.update(dict(i=i, r0=r0, vt=vt, qT=qT, kT=kT, PT=PT, ks=ks, m=m, hacc=hacc))

        def heavy(b, d, ci, t, bgs):
            i, r0, vt, qT, kT, PT, ks, m, hacc = (t[k] for k in ("i", "r0", "vt", "qT", "kT", "PT", "ks", "m", "hacc"))
            for h in range(H):
                s.mm(psm[:, 8 + h:9 + h], PT[:, h, :], onesb[:, 0:1], start=True, stop=False)
                for kc in range(2):
                    s.mm(psm[:, 8 + h:9 + h], qT[:, h * 2 + kc, :], nbf[:, h * 2 + kc:h * 2 + kc + 1],
                         start=False, stop=(kc == 1))
            s.act(m[:, 4, :], psm[:, 8:12], AF.Abs)
            s.ts("dve", m[:, 4, :], m[:, 4, :], 1.0, None, ALU.max)
            s.recip("dve", m[:, 5, :], m[:, 4, :])
            for h in range(H):
                po = c.pr.next()
                s.mm(po[:, :], PT[:, h, :], vt[:, h * DV:(h + 1) * DV], start=True, stop=False)
                for kc in range(2):
                    s.mm(po[:, :], qT[:, h * 2 + kc, :], Cbf.sub(str(h * 2 + kc))[:, h * 2 + kc, :],
                         start=False, stop=(kc == 1))
                s.act(hacc[:, h * DV:(h + 1) * DV], po[:, :], AF.Copy, scale=m[:, 5, h:h + 1])
                for kc in range(2):
                    j = h * 2 + kc
                    pu = c.pr.next()
                    s.mm(pu[:, :], ks[:, h, kc * 128:(kc + 1) * 128], vt[:, h * DV:(h + 1) * DV])
                    s.mm(psm[:, 16 + j:17 + j], ks[:, h, kc * 128:(kc + 1) * 128], onesb[:, 0:1])
                    cj = C32.sub(str(j))
                    s.tt("dve", cj[:, j, :], cj[:, j, :], pu[:, :], ALU.add)
                    s.act(cj[:, j, :], cj[:, j, :], AF.Copy, scale=m[:, 3, h:h + 1])
                    s.cp("dve" if kc == 0 else "act", Cbf.sub(str(j))[:, j, :], cj[:, j, :])
                    drain(bgs, 2)
            s.tt("dve", n32[:], n32[:], psm[:, 16:24], ALU.add)
            s.tt("dve", V(n32.t[:].rearrange("p (h k) -> p h k", k=2), n32.key),
                 V(n32.t[:].rearrange("p (h k) -> p h k", k=2), n32.key),
                 bc(V(m.t[:, 3, :].unsqueeze(2), m.key), [128, H, 2]), ALU.mult)
            s.cp("dve", nbf[:], n32[:])
            drain_all(bgs)

            def epilogue():
                if d == 0:
                    s.dma(STQ, hf[r0:r0 + 128, :], hacc[:])
                    return
                s.dma("sp", hft[:], hf[r0:r0 + 128, :])
                s.dma("sp", ogt[:], og_tm[r0:r0 + 128, :])
                s.dma("sp", zt[:], z_tm[r0:r0 + 128, :])
                s.dma("sp", cht[:], ch_tm[r0:r0 + 128, :])
                s.tt("dve", hft[:], hft[:], hacc[:], ALU.add)
                yield
                s.tt("dve", hft[:], hft[:], ogt[:], ALU.mult)
                yield
                gs = gst.next()
                s.act(tmp[:], hft[:], AF.Square)
                s.red("dve", gs[:, 0, :], r3(tmp, DV), ALU.add)
                s.ts("dve", gs[:, 1, :], gs[:, 0, :], 1.0 / DV, EPS, ALU.mult, ALU.add)
                s.act(gs[:, 2, :], gs[:, 1, :], AF.Ln)
                s.act(gs[:, 3, :], gs[:, 2, :], AF.Exp, scale=-0.5)
                yield
                for h in range(H):
                    s.stt("dve", hft[:, h * DV:(h + 1) * DV], hft[:, h * DV:(h + 1) * DV], gs[:, 3, h:h + 1], on[:],
                          ALU.mult, ALU.mult)
                    if h % 2 == 1:
                        yield
                s.tt("dve", hft[:], hft[:], cht[:], ALU.add)
                yield
                s.tt("dve", yb[:], hft[:], zt[:], ALU.mult)
                yield
                yield from fin.run_gen(yb, i)

            return epilogue()


        seq = []
        for b in range(BL):
            for d in range(2):
                order = list(range(NQ)) if d == 0 else list(range(NQ - 1, -1, -1))
                for k, ci in enumerate(order):
                    seq.append((b, d, ci, k == 0))

        def reset_state():
            s.memset("dve", C32.subs(allj)[:], 0.0)
            s.memset("pool", Cbf.subs(allj)[:], 0.0)
            s.memset("dve", n32[:], 0.0)
            s.memset("pool", nbf[:], 0.0)

        run_scan(s, seq, prep, heavy, reset_state)
    s.barrier()
    c.pr = Ring(c.pbanks)


def load_mat(c, dst, src_ap):
    v = src_ap.rearrange("(st p) f -> p st f", p=128)
    for q in range(4):
        c.s.dma("sp", dst[:, q * 4:(q + 1) * 4, :], V(v[:, q * 4:(q + 1) * 4, :], "w_const"))


def layer_hyena(c, W, hin, hout):
    nc, s = c.nc, c.s
    C = c.C
    dr = c.dram
    vxT = dr("hy_vxT", [3 * DI, T], BF16)
    z_tm = dr("hy_z", [T, DI], BF16)
    sig = [dr("hy_v", [T, DI], BF16), dr("hy_x1", [T, DI], BF16), dr("hy_x2", [T, DI], BF16)]
    y1_tm = dr("hy_y1", [T, DI], BF16)
    y2_tm = dr("hy_y2", [T, DI], BF16)
    Kf = dr("hy_Kf", [2, 2, L, DI], BF16)
    Zd = dr("hy_Z", [BL, 2, L, DI], BF16)

    with ExitStack() as es:
        uT = sb(es, nc, "uT", [128, 8, T], BF16)
        phase_norm(c, hin, W["hy_norm"], uT)
        phase_project(c, uT, W["hy_w_in"], [
            (0, 3 * DI, "fm", vxT, BF16, 1.0),
            (3 * DI, DI, "tm", z_tm, BF16, 1.0, AF.Silu),
        ])

    conv_fm4(c, vxT, 48, 3, W["hy_conv_wT"], W["hy_conv_b"], False,
             lambda cg: dict(tm=(sig[cg // 4], (cg % 4) * 512, None)))
    s.barrier()

    def fwd_transform(Cm, Sn, o, ysrc):
        with ExitStack() as es:
            yr = sbring(es, nc, "hf_y", [128, 16, 512], BF16, 2)
            kr = sbring(es, nc, "hf_k", [128, 2, 512], BF16, 2)
            yre = sb(es, nc, "hf_yre", [128, 512], F32)
            yim = sb(es, nc, "hf_yim", [128, 512], F32)
            t1 = sb(es, nc, "hf_t1", [128, 512], F32)
            t2 = sb(es, nc, "hf_t2", [128, 512], F32)
            t3 = sb(es, nc, "hf_t3", [128, 512], F32)
            t4 = sb(es, nc, "hf_t4", [128, 512], F32)
            zr = sbring(es, nc, "hf_z", [128, 2, 512], BF16, 2)
            for b in range(BL):
                for cb in range(4):
                    yt = yr.next()
                    cs = slice(cb * 512, (cb + 1) * 512)
                    s.dma("sp", yt[:], V(ysrc.t[b * L:(b + 1) * L, cs].rearrange("(st p) c -> p st c", p=128), ysrc.key))
                    for ft in range(16):
                        fs = slice(ft * 128, (ft + 1) * 128)
                        kt = kr.next()
                        s.dma("sp", kt[:], V(Kf.t[o, :, fs, cs].rearrange("a p c -> p a c"), Kf.key))
                        pre, pim = c.pr.next(), c.pr.next()
                        for st in range(16):
                            s.mm(pre[:, :], Cm[:, st, fs], yt[:, st, :], start=(st == 0), stop=(st == 15))
                        for st in range(16):
                            s.mm(pim[:, :], Sn[:, st, fs], yt[:, st, :], start=(st == 0), stop=(st == 15))
                        s.cp("act", yre[:], pre[:, :])
                        s.cp("act", yim[:], pim[:, :])
                        s.tt("dve", t1[:], yre[:], kt[:, 0, :], ALU.mult)
                        s.tt("dve", t2[:], yim[:], kt[:, 1, :], ALU.mult)
                        s.tt("dve", t3[:], yre[:], kt[:, 1, :], ALU.mult)
                        s.tt("dve", t4[:], yim[:], kt[:, 0, :], ALU.mult)
                        zt = zr.next()
                        s.tt("dve", zt[:, 0, :], t1[:], t2[:], ALU.subtract)
                        s.tt("pool", zt[:, 1, :], t3[:], t4[:], ALU.add)
                        s.dma(STQ, V(Zd.t[b, :, fs, cs].rearrange("a p c -> p a c"), Zd.key), zt[:])
        s.barrier()

    def inv_transform(o, ysrc, xg_src, ydst):
        with ExitStack() as es:
            CI = sb(es, nc, "hi_CI", [128, 16, L], BF16)
            SI = sb(es, nc, "hi_SI", [128, 16, L], BF16)
            load_mat(c, CI, C["c_CI"])
            load_mat(c, SI, C["c_SI"])
            dbc = sb(es, nc, "hi_d", [128, DI], F32)
            s.dma("sp", dbc[:], V(W["hy_d"][o].partition_broadcast(128), "w_const"))
            zb = sb(es, nc, "hi_zb", [128, 2, 16, 512], BF16)
            ypr = sbring(es, nc, "hi_yp", [128, 512], BF16, 2)
            xgr = sbring(es, nc, "hi_xg", [128, 512], BF16, 2)
            zzr = sbring(es, nc, "hi_zz", [128, 512], BF16, 2)
            ta = sbring(es, nc, "hi_ta", [128, 512], F32, 2)
            tb_ = sbring(es, nc, "hi_tb", [128, 512], F32, 2)
            yo = sbring(es, nc, "hi_yo", [128, 512], BF16, 2)
            for b in range(BL):
                for cb in range(4):
                    cs = slice(cb * 512, (cb + 1) * 512)
                    for a in range(2):
                        for q in range(2):
                            s.dma("sp", zb[:, a, q * 8:(q + 1) * 8, :],
                                  V(Zd.t[b, a, q * 1024:(q + 1) * 1024, cs].rearrange("(ft p) c -> p ft c", p=128), Zd.key))
                    for tt in range(16):
                        ts_ = slice(tt * 128, (tt + 1) * 128)
                        rows = slice(b * L + tt * 128, b * L + (tt + 1) * 128)
                        po = c.pr.next()
                        for ft in range(16):
                            s.mm(po[:, :], CI[:, ft, ts_], zb[:, 0, ft, :], start=(ft == 0), stop=False)
                        for ft in range(16):
                            s.mm(po[:, :], SI[:, ft, ts_], zb[:, 1, ft, :], start=False, stop=(ft == 15))
                        yp, xg = ypr.next(), xgr.next()
                        s.dma("sp", yp[:], ysrc[rows, cs])
                        s.dma("sp", xg[:], xg_src[rows, cs])
                        t_a = ta.next()
                        s.tt("dve", t_a[:], yp[:], dbc[:, cs], ALU.mult)
                        s.tt("dve", t_a[:], t_a[:], po[:, :], ALU.add)
                        yot = yo.next()
                        if o == 0:
                            s.tt("dve", yot[:], t_a[:], xg[:], ALU.mult)
                        else:
                            zz, t_b = zzr.next(), tb_.next()
                            s.dma("sp", zz[:], z_tm[rows, cs])
                            s.tt("dve", t_a[:], t_a[:], xg[:], ALU.mult)
                            s.tt("dve", yot[:], t_a[:], zz[:], ALU.mult)
                        s.dma(STQ, ydst[rows, cs], yot[:])
        s.barrier()

    with ExitStack() as es:
        Cm = sb(es, nc, "hy_Cm", [128, 16, L], BF16)
        Sn = sb(es, nc, "hy_Sn", [128, 16, L], BF16)
        load_mat(c, Cm, C["c_Cm"])
        load_mat(c, Sn, C["c_Sn"])
        with ExitStack() as es2:
            hA = sb(es2, nc, "hm_hA", [64, L], F32)
            hB = sb(es2, nc, "hm_hB", [64, L], F32)
            with ExitStack() as es3:
                feats = sb(es3, nc, "hm_feats", [33, L], F32)
                w1 = sb(es3, nc, "hm_w1", [33, 64], F32)
                wh = sb(es3, nc, "hm_wh", [64, 2, 64], F32)
                prm = sb(es3, nc, "hm_prm", [64, 8], F32)
                tr_ = sb(es3, nc, "hm_t", [64, 512], F32)
                tki = sb(es3, nc, "hm_ki", [64, 512], mybir.dt.int32)
                tkf = sb(es3, nc, "hm_kf", [64, 512], F32)
                s.dma("sp", feats[:], V(C["c_featsT"], "w_const"))
                s.dma("sp", w1[:], V(W["hy_ffn_w_in"], "w_const"))
                s.dma("sp", wh[:], V(W["hy_ffn_w_hid"].rearrange("j a b -> a j b"), "w_const"))
                s.dma("sp", prm[:, 0:1], V(W["hy_ffn_b_in"].rearrange("(p o) -> p o", o=1), "w_const"))
                s.dma("sp", prm[:, 1:3], V(W["hy_ffn_b_hidT"], "w_const"))
                s.dma("sp", prm[:, 3:6], V(W["hy_ffn_freqT"], "w_const"))
                cur, nxt = hA, hB
                for layer in range(3):
                    for blk in range(4):
                        bs = slice(blk * 512, (blk + 1) * 512)
                        ps = c.pr.next()
                        if layer == 0:
                            s.mm(ps[0:64, :], w1[:, :], feats[:, bs])
                            dst = cur
                        else:
                            s.mm(ps[0:64, :], wh[:, layer - 1, :], cur[:, bs])
                            dst = nxt
                        s.ts("dve", tr_[:], ps[0:64, :], prm[:, layer:layer + 1], prm[:, 3 + layer:4 + layer], ALU.add, ALU.mult)
                        s.ts("dve", tr_[:], tr_[:], 1.0 / (2.0 * PI), 8.5, ALU.mult, ALU.add)
                        s.cp("dve", tki[:], tr_[:])
                        s.cp("dve", tkf[:], tki[:])
                        s.tt("dve", tr_[:], tr_[:], tkf[:], ALU.subtract)
                        s.ts("dve", tkf[:], tr_[:], 0.0, None, ALU.is_lt)
                        s.tt("dve", tr_[:], tr_[:], tkf[:], ALU.add)
                        s.act(dst[:, bs], tr_[:], AF.Sin, bias=c.one[0:64, 1:2], scale=2.0 * PI)
                    if layer > 0:
                        cur, nxt = nxt, cur
                h3 = cur
                s.barrier()
            dlt = sb(es2, nc, "hm_dlt", [128, DI], F32)
            ngt = sb(es2, nc, "hm_negt", [128, 16], F32)
            s.dma("sp", dlt[:], V(C["c_deltas"].partition_broadcast(128), "w_const"))
            s.dma("sp", ngt[:], V(C["c_negt"], "w_const"))
            wor = sbring(es2, nc, "hm_wo", [64, 2, 256], F32, 2)
            Ar = sb(es2, nc, "hm_A", [128, 16, 256], BF16)
            Br_ = sb(es2, nc, "hm_B", [128, 16, 256], BF16)
            dec = sbring(es2, nc, "hm_dec", [128, 256], F32, 2)
            hbs = sbring(es2, nc, "hm_hb", [128, 256], F32, 2)
            sa = sbring(es2, nc, "hm_sa", [128, 256], F32, 2)
            sbm = sbring(es2, nc, "hm_sb", [128, 256], F32, 2)
            ko = sbring(es2, nc, "hm_ko", [128, 512], BF16, 2)
            wov = W["hy_ffn_w_out"]
            for o in range(2):
                for cb in range(8):
                    cs = slice(cb * 256, (cb + 1) * 256)
                    wo = wor.next()
                    for dd in range(2):
                        c0 = dd * 2 * DI + o * DI + cb * 256
                        s.dma("sp", wo[:, dd, :], V(wov[:, c0:c0 + 256], "w_const"))
                    for tt in range(16):
                        ps = c.pr.next()
                        s.mm(ps[:, 0:256], h3[:, tt * 128:(tt + 1) * 128], wo[:, 0, :])
                        s.mm(ps[:, 256:512], h3[:, tt * 128:(tt + 1) * 128], wo[:, 1, :])
                        dc, hb_, a_, b_ = dec.next(), hbs.next(), sa.next(), sbm.next()
                        s.act(dc[:], dlt[:, cs], AF.Exp, scale=ngt[:, tt:tt + 1])
                        s.cp("dve", hb_[:], ps[:, 256:512])
                        if tt == 0:
                            s.memset("dve", hb_[0:1, :], 0.0)
                        s.tt("dve", a_[:], ps[:, 0:256], hb_[:], ALU.add)
                        s.tt("dve", b_[:], ps[:, 0:256], hb_[:], ALU.subtract)
                        s.tt("dve", Ar[:, tt, :], a_[:], dc[:], ALU.mult)
                        s.tt("pool", Br_[:, tt, :], b_[:], dc[:], ALU.mult)
                    for ft in range(16):
                        fs = slice(ft * 128, (ft + 1) * 128)
                        pk = c.pr.next()
                        for tt in range(16):
                            s.mm(pk[:, 0:256], Cm[:, tt, fs], Ar[:, tt, :], start=(tt == 0), stop=(tt == 15))
                        for tt in range(16):
                            s.mm(pk[:, 256:512], Sn[:, tt, fs], Br_[:, tt, :], start=(tt == 0), stop=(tt == 15))
                        kt = ko.next()
                        s.cp("act", kt[:], pk[:, :])
                        s.dma(STQ, V(Kf.t[o, :, fs, cs].rearrange("a p c -> p a c"), Kf.key),
                              V(kt.t[:].rearrange("p (a c) -> p a c", a=2), kt.key))
            s.barrier()
        fwd_transform(Cm, Sn, 0, sig[0])
    inv_transform(0, sig[0], sig[1], y1_tm)
    with ExitStack() as es:
        Cm = sb(es, nc, "hy_Cm2", [128, 16, L], BF16)
        Sn = sb(es, nc, "hy_Sn2", [128, 16, L], BF16)
        load_mat(c, Cm, C["c_Cm"])
        load_mat(c, Sn, C["c_Sn"])
        fwd_transform(Cm, Sn, 1, y1_tm)
    inv_transform(1, y1_tm, sig[2], y2_tm)

    with ExitStack() as es:
        fin = Finalizer(c, es, W["hy_w_out"], hin, hout)
        yr = sbring(es, nc, "ho_y", [128, DI], BF16, 2)
        for i in range(NT):
            yt = yr.next()
            s.dma("sp", yt[:], y2_tm[i * 128:(i + 1) * 128, :])
            fin.run(yt, i)
    s.barrier()


N_IMPL = 4
STQ = "act"
DEBUG_STOP = None


def host_constants():
    cst = {}
    cst["c_identb"] = np.eye(128, dtype=np.float32).astype(ml_dtypes.bfloat16)
    cst["c_identf"] = np.eye(128, dtype=np.float32)
    sidx = np.arange(128)[:, None]
    lidx = np.arange(128)[None, :]
    tri = np.stack([(sidx <= lidx), (sidx >= lidx)], axis=1).astype(np.float32)
    cst["c_tri"] = tri
    cst["c_negm"] = ((1.0 - tri) * -1.0e5).astype(np.float32)
    sI = np.arange(L, dtype=np.int64)[:, None]
    fI = np.arange(L, dtype=np.int64)[None, :]
    ph = ((2 * fI + 1) * sI) % (4 * L)
    th = (2.0 * np.pi / (4 * L)) * ph.astype(np.float64)
    cm = np.cos(th)
    sn = np.sin(th)
    bf = ml_dtypes.bfloat16
    cst["c_Cm"] = cm.astype(np.float32).astype(bf)
    cst["c_Sn"] = (-sn).astype(np.float32).astype(bf)
    cst["c_CI"] = np.ascontiguousarray((cm.T / L)).astype(np.float32).astype(bf)
    cst["c_SI"] = np.ascontiguousarray((-sn.T / L)).astype(np.float32).astype(bf)
    t = np.linspace(0.0, 1.0, L, dtype=np.float32)[:, None]
    pos = np.arange(L, dtype=np.float32)[:, None]
    bands = np.linspace(1e-4, 15.0, 16, dtype=np.float32)[None]
    ang = (np.float32(2.0 * math.pi / L) * pos * bands).astype(np.float32)
    feats = np.concatenate([t, np.cos(ang), -np.sin(ang)], axis=-1).astype(np.float32)
    cst["c_featsT"] = np.ascontiguousarray(feats.T)
    max_decay = math.log(1e-2) / 0.3
    min_decay = math.log(1e-2) / 1.5
    cst["c_deltas"] = np.abs(np.linspace(min_decay, max_decay, DI, dtype=np.float32)).astype(np.float32)
    cst["c_negt"] = np.ascontiguousarray(-(t[:, 0].reshape(16, 128).T)).astype(np.float32)
    return cst


def host_prepare(inputs):
    W = {}
    for k, v in inputs.items():
        if k in ("x", "final_norm"):
            continue
        W[k] = np.ascontiguousarray(v[0])
    W["final_norm"] = np.ascontiguousarray(inputs["final_norm"])
    W["ssd_conv_wT"] = np.ascontiguousarray(W.pop("ssd_conv_w").T)
    W["ml_conv_wT"] = np.ascontiguousarray(W.pop("ml_conv_w").T)
    W["hy_conv_wT"] = np.ascontiguousarray(W.pop("hy_conv_w").T)
    W["hy_ffn_b_hidT"] = np.ascontiguousarray(W.pop("hy_ffn_b_hid").T)
    W["hy_ffn_freqT"] = np.ascontiguousarray(W.pop("hy_ffn_freq").T)
    return W


def build_program(wshapes, cshapes, n_layers=4, final_norm=True):
    n_layers = min(n_layers, N_IMPL)
    nc = bass.Bass("TRN2", target_bir_lowering=False)
    c = Ctx()
    c.nc = nc
    c.s = Sched(nc)
    s = c.s
    x_in = Buf(nc.dram_tensor("x", [T, D], F32, kind="ExternalInput").ap(), "x_in")
    out = Buf(nc.dram_tensor("out", [T, D], F32, kind="ExternalOutput").ap(), "out")
    W = {k: nc.dram_tensor(k, list(shp), F32 if dt == np.float32 else BF16, kind="ExternalInput").ap()
         for k, (shp, dt) in wshapes.items()}
    C = {k: nc.dram_tensor(k, list(shp), F32 if dt == np.float32 else BF16, kind="ExternalInput").ap()
         for k, (shp, dt) in cshapes.items()}

    def dram(name, shape, dt):
        return Buf(nc.dram_tensor(name, shape, dt, kind="Internal").ap(), name)

    c.dram = dram
    c.C = C
    hA = dram("hA", [T, D], F32)
    hB = dram("hB", [T, D], F32)

    with ExitStack() as es:
        c.identb = sb(es, nc, "k_identb", [128, 128], BF16)
        c.identf = sb(es, nc, "k_identf", [128, 128], F32)
        c.tri = sb(es, nc, "k_tri", [128, 2, 128], F32)
        c.negm = sb(es, nc, "k_negm", [128, 2, 128], F32)
        c.onesf = sb(es, nc, "k_onesf", [128, 128], F32)
        c.nonesf = sb(es, nc, "k_nonesf", [128, 128], F32)
        c.one = sb(es, nc, "k_one", [128, 4], F32)
        s.dma("sp", c.identb[:], V(C["c_identb"], "w_const"))
        s.dma("sp", c.identf[:], V(C["c_identf"], "w_const"))
        s.dma("sp", c.tri[:], V(C["c_tri"], "w_const"))
        s.dma("sp", c.negm[:], V(C["c_negm"], "w_const"))
        s.memset("dve", c.onesf[:], 1.0)
        s.memset("dve", c.nonesf[:], -1.0)
        s.memset("dve", c.one[:, 0:1], 1.0)
        s.memset("dve", c.one[:, 1:2], -PI)
        s.memset("dve", c.one[:, 2:3], 0.0)
        s.memset("dve", c.one[:, 3:4], math.log(1.0 / 16.0))
        pbanks = [Buf(es.enter_context(nc.psum_tensor(f"ps{i}", [128, 512], F32)), f"ps{i}") for i in range(6)]
        tbanks = [Buf(es.enter_context(nc.psum_tensor(f"pt{i}", [128, 8, 128], BF16)), f"pt{i}") for i in range(2)]
        c.pbanks = pbanks
        c.pr = Ring(pbanks)
        c.ptr = Ring(tbanks)
        s.barrier()
        c.identb.key = c.identf.key = c.tri.key = c.negm.key = "konst"
        c.onesf.key = c.nonesf.key = c.one.key = "konst"

        layers = [layer_ssd, layer_gla, layer_hyena, layer_mlstm]
        hs = [x_in, hA, hB, hA, hB]
        hcur = x_in
        for li in range(n_layers):
            hnext = hA if (li % 2 == 0) else hB
            layers[li](c, W, hcur, hnext)
            hcur = hnext
        if final_norm:
            phase_final_norm(c, hcur, W["final_norm"], out)
        else:
            with ExitStack() as es2:
                cr = sbring(es2, nc, "cp_x", [128, D], F32, 2)
                for i in range(NT):
                    t = cr.next()
                    s.dma("sp", t[:], hcur[i * 128:(i + 1) * 128, :])
                    s.dma(STQ, out[i * 128:(i + 1) * 128, :], t[:])
            s.barrier()
    return nc


_CACHE = {}


def kernel(**inputs):
    x = np.ascontiguousarray(inputs["x"], dtype=np.float32)
    W = host_prepare(inputs)
    Cst = host_constants()
    wshapes = {k: (v.shape, v.dtype.type if v.dtype != ml_dtypes.bfloat16 else "bf16") for k, v in W.items()}
    cshapes = {k: (v.shape, v.dtype.type if v.dtype != ml_dtypes.bfloat16 else "bf16") for k, v in Cst.items()}
    nc = build_program(wshapes, cshapes)
    in_maps = []
    for i in range(NCORES):
        m = {"x": x[i * BL:(i + 1) * BL].reshape(T, D)}
        m.update(W)
        m.update(Cst)
        in_maps.append(m)
    res = run_bass_kernel_spmd(nc, in_maps, core_ids=list(range(NCORES)))
    outs = [r["out"].reshape(BL, L, D) for r in res.results]
    return np.concatenate(outs, axis=0).astype(np.float32)
```
